# Optimizing a Trainium2 kernel written in Bass

```python
import jax
import jax.numpy as jnp
from jax import lax
import numpy as np

D_MODEL = 2048
BATCH = 2
SEQ = 4096
DEPTH = 2

MEM_LEN = 256
BRANCH_W = 512
N_BRANCHES = 5
NORM_EPS = 1e-6

RWKV_HEAD = 64
RWKV_HEADS = BRANCH_W // RWKV_HEAD
RWKV_DECAY_LORA = 96
RWKV_A_LORA = 96
RWKV_GATE_LORA = 256
RWKV_V_LORA = 64
RWKV_GN_EPS = 64e-5

NSA_HEAD = 64
NSA_Q_HEADS = BRANCH_W // NSA_HEAD
NSA_KV_HEADS = 2
NSA_GROUP = NSA_Q_HEADS // NSA_KV_HEADS
NSA_KV_W = NSA_KV_HEADS * NSA_HEAD
COMP_L = 32
COMP_STRIDE = 16
COMP_HIDDEN = 2 * NSA_HEAD
SEL_L = 64
SEL_N = 16
WINDOW = 512
Q_BLOCK = 128

CONV_W = 3

POOL_WINDOWS = (2, 4, 8, 16)
POOL_GROUP = BRANCH_W // len(POOL_WINDOWS)
POOL_OUT = D_MODEL // len(POOL_WINDOWS)

MEM_HEADS = 4
MEM_HEAD = BRANCH_W // MEM_HEADS

D_FF = 5632
N_EXPERTS = 8
TOP_K = 2
EXPERT_FF = 2816
N_DENSE = (DEPTH + 1) // 2
N_MOE = DEPTH // 2

RWKV_COLS = 3 * BRANCH_W + RWKV_DECAY_LORA + RWKV_A_LORA + RWKV_GATE_LORA
NSA_COLS = BRANCH_W + 6 * NSA_KV_W + 3 * NSA_Q_HEADS
CONV_COLS = 3 * BRANCH_W
POOL_COLS = BRANCH_W
MEM_COLS = BRANCH_W
GATE_COLS = N_BRANCHES * D_MODEL
IN_SIZES = (RWKV_COLS, NSA_COLS, CONV_COLS, POOL_COLS, MEM_COLS, GATE_COLS)
N_IN = RWKV_COLS + NSA_COLS + CONV_COLS + POOL_COLS + MEM_COLS + GATE_COLS

kernel_name = 'hybrid_rwkv7_nsa_conv_pool_memxattn_moe'

F32 = jnp.float32


def _split(a, sizes):
    return jnp.split(a, np.cumsum(sizes)[:-1].tolist(), axis=-1)


def _rms(x, g, eps=NORM_EPS):
    xf = x.astype(F32)
    y = xf * lax.rsqrt(jnp.mean(xf * xf, axis=-1, keepdims=True) + eps)
    return (y * g.astype(F32)).astype(x.dtype)


def _masked_softmax(s, mask, scale):
    s = jnp.where(mask, s.astype(F32) * scale, -1e30)
    return jnp.where(mask, jax.nn.softmax(s, axis=-1), 0.0)


def _token_shift(u, mu):
    prev = jnp.pad(u, ((0, 0), (1, 0), (0, 0)))[:, :-1]
    return u + (prev - u) * mu


def _rwkv7_time_mix(u, mu, w0, w2, a0, a2, g2, k_k, k_a, r_k, ln_w, ln_b, v_first, vres):
    B, T, _ = u.shape
    heads = lambda t: t.reshape(B, T, RWKV_HEADS, RWKV_HEAD)
    uf = _token_shift(u, mu).astype(F32)
    r, k, v, wd, ad, gd = _split(uf, (BRANCH_W, BRANCH_W, BRANCH_W, RWKV_DECAY_LORA, RWKV_A_LORA, RWKV_GATE_LORA))
    w_log = -jax.nn.softplus(-(w0 + jnp.tanh(wd) @ w2)) - 0.5
    a = jax.nn.sigmoid(a0 + ad @ a2)
    g = jax.nn.sigmoid(gd) @ g2
    if vres is not None:
        vd, v0, v_up = vres
        v = v + (v_first - v) * jax.nn.sigmoid(v0 + vd.astype(F32) @ v_up)
    kk = heads(k * k_k)
    kk = kk * lax.rsqrt(jnp.maximum(jnp.sum(kk * kk, axis=-1, keepdims=True), 1e-24))
    k = k * (1.0 + (a - 1.0) * k_a)
    rh, kh, vh, ah = heads(r), heads(k), heads(v), heads(a)
    decay = heads(jnp.exp(-jnp.exp(w_log)))

    def step(S, inp):
        r_t, w_t, k_t, v_t, kk_t, a_t = inp
        s_kk = jnp.einsum('bhvk,bhk->bhv', S, kk_t)
        S = S * w_t[:, :, None, :] - s_kk[..., None] * (kk_t * a_t)[:, :, None, :] + v_t[..., None] * k_t[:, :, None, :]
        return S, jnp.einsum('bhvk,bhk->bhv', S, r_t)

    xs = tuple(jnp.moveaxis(t, 1, 0) for t in (rh, decay, kh, vh, kk, ah))
    S0 = jnp.zeros((B, RWKV_HEADS, RWKV_HEAD, RWKV_HEAD), F32)
    _, y = lax.scan(step, S0, xs)
    y = jnp.moveaxis(y, 0, 1)
    y_mu = jnp.mean(y, axis=-1, keepdims=True)
    y_var = jnp.mean(jnp.square(y - y_mu), axis=-1, keepdims=True)
    y = ((y - y_mu) * lax.rsqrt(y_var + RWKV_GN_EPS)).reshape(B, T, BRANCH_W) * ln_w + ln_b
    bonus = jnp.sum(rh * kh * r_k.reshape(RWKV_HEADS, RWKV_HEAD), axis=-1, keepdims=True) * vh
    out = (y + bonus.reshape(B, T, BRANCH_W)) * g
    return out.astype(u.dtype), v


def _nsa(u, qk_gain, cmp_pos, cmp_w1, cmp_b1, cmp_w2):
    B, T, _ = u.shape
    scale = NSA_HEAD ** -0.5
    q, kc, vc, ks, vs, kw, vw, gl = _split(u, (BRANCH_W,) + (NSA_KV_W,) * 6 + (3 * NSA_Q_HEADS,))
    kv_heads = lambda t: t.reshape(B, T, NSA_KV_HEADS, NSA_HEAD)
    q = _rms(q.reshape(B, T, NSA_KV_HEADS, NSA_GROUP, NSA_HEAD), qk_gain[0])
    t_pos = jnp.arange(T)

    n_cmp = (T - COMP_L) // COMP_STRIDE + 1
    blk = np.arange(n_cmp)[:, None] * COMP_STRIDE + np.arange(COMP_L)[None, :]

    def compress(t, i):
        blocks = kv_heads(t)[:, blk] + cmp_pos[i][:, None, :]
        blocks = jnp.moveaxis(blocks, 3, 2).reshape(B, n_cmp, NSA_KV_HEADS, COMP_L * NSA_HEAD)
        return jax.nn.gelu(blocks @ cmp_w1[i] + cmp_b1[i]) @ cmp_w2[i]

    k_cmp = _rms(compress(kc, 0), qk_gain[1])
    v_cmp = compress(vc, 1)
    cmp_mask = (np.arange(n_cmp) * COMP_STRIDE + COMP_L - 1)[None, :] <= t_pos[:, None]
    p_cmp = _masked_softmax(jnp.einsum('bthgd,bchd->bhgtc', q, k_cmp), cmp_mask, scale)
    o_cmp = jnp.einsum('bhgtc,bchd->bthgd', p_cmp, v_cmp)

    n_sel = T // SEL_L
    k_top = min(SEL_N, n_sel)
    c0 = np.arange(n_cmp)[:, None] * COMP_STRIDE
    s0 = np.arange(n_sel)[None, :] * SEL_L
    overlap = np.clip(np.minimum(c0 + COMP_L, s0 + SEL_L) - np.maximum(c0, s0), 0, None) / COMP_L
    imp = jnp.einsum('bhgtc,cs->bhts', p_cmp, jnp.asarray(overlap, F32))
    cur = (t_pos // SEL_L)[:, None]
    sid = jnp.arange(n_sel)[None, :]
    forced = (sid == 0) | (sid == cur) | (sid == cur - 1)
    imp = jnp.where(forced, 1e9, jnp.where(sid <= cur, imp, -1e9))
    top_val, top_idx = lax.top_k(imp, k_top)
    top_ok = top_val > -1e8

    def sel_blocks(t):
        return jnp.moveaxis(t.reshape(B, n_sel, SEL_L, NSA_KV_HEADS, NSA_HEAD), 3, 1)

    k_sel_all = sel_blocks(_rms(kv_heads(ks), qk_gain[2]))
    v_sel_all = sel_blocks(kv_heads(vs))
    pad = ((0, 0), (WINDOW, 0), (0, 0), (0, 0))
    k_win_all = jnp.pad(_rms(kv_heads(kw), qk_gain[3]), pad)
    v_win_all = jnp.pad(kv_heads(vw), pad)
    bi = jnp.arange(B)[:, None, None, None]
    hi = jnp.arange(NSA_KV_HEADS)[None, :, None, None]
    m_len = k_top * SEL_L

    def query_block(args):
        qb, idx, ok, start = args
        tq = start + jnp.arange(Q_BLOCK)
        k_g = k_sel_all[bi, hi, idx]
        v_g = v_sel_all[bi, hi, idx].reshape(B, NSA_KV_HEADS, Q_BLOCK, m_len, NSA_HEAD)
        kpos = idx[..., None] * SEL_L + jnp.arange(SEL_L)
        m_sel = (ok[..., None] & (kpos <= tq[None, None, :, None, None])).reshape(B, NSA_KV_HEADS, 1, Q_BLOCK, m_len)
        s_sel = jnp.einsum('bqhgd,bhqnkd->bhgqnk', qb, k_g).reshape(B, NSA_KV_HEADS, NSA_GROUP, Q_BLOCK, m_len)
        p_sel = _masked_softmax(s_sel, m_sel, scale)
        o_s = jnp.einsum('bhgqm,bhqmd->bqhgd', p_sel, v_g)
        k_w = lax.dynamic_slice_in_dim(k_win_all, start, WINDOW + Q_BLOCK, axis=1)
        v_w = lax.dynamic_slice_in_dim(v_win_all, start, WINDOW + Q_BLOCK, axis=1)
        wpos = start - WINDOW + jnp.arange(WINDOW + Q_BLOCK)
        m_win = (wpos[None, :] <= tq[:, None]) & (wpos[None, :] > tq[:, None] - WINDOW) & (wpos[None, :] >= 0)
        p_win = _masked_softmax(jnp.einsum('bqhgd,bkhd->bhgqk', qb, k_w), m_win, scale)
        o_w = jnp.einsum('bhgqk,bkhd->bqhgd', p_win, v_w)
        return o_s, o_w

    n_qb = T // Q_BLOCK
    to_blocks = lambda t, ax: jnp.moveaxis(t.reshape(t.shape[:ax] + (n_qb, Q_BLOCK) + t.shape[ax + 1:]), ax, 0)
    xs = (to_blocks(q, 1), to_blocks(top_idx, 2), to_blocks(top_ok, 2), jnp.arange(n_qb) * Q_BLOCK)
    o_sel, o_win = lax.map(query_block, xs)
    from_blocks = lambda t: jnp.moveaxis(t, 0, 1).reshape(B, T, NSA_KV_HEADS, NSA_GROUP, NSA_HEAD)

    g = jax.nn.sigmoid(gl.astype(F32)).reshape(B, T, NSA_KV_HEADS, NSA_GROUP, 3)
    o = g[..., 0:1] * o_cmp + g[..., 1:2] * from_blocks(o_sel) + g[..., 2:3] * from_blocks(o_win)
    return o.reshape(B, T, BRANCH_W).astype(u.dtype)


def _short_conv(u, conv_w):
    b_gate, c_gate, x_in = _split(u, (BRANCH_W, BRANCH_W, BRANCH_W))
    z = c_gate * x_in
    y = lax.conv_general_dilated(z, conv_w[:, None, :].astype(z.dtype), window_strides=(1,),
                                 padding=[(CONV_W - 1, 0)], dimension_numbers=('NWC', 'WIO', 'NWC'),
                                 feature_group_count=BRANCH_W)
    return b_gate * y


def _pool(u, pool_w, pool_scale):
    B, T, _ = u.shape
    uf = u.astype(F32)
    cs = jnp.cumsum(uf, axis=1)
    count = jnp.arange(1, T + 1, dtype=F32)[None, :, None]
    outs = []
    for gi, w in enumerate(POOL_WINDOWS):
        sl = slice(gi * POOL_GROUP, (gi + 1) * POOL_GROUP)
        c = cs[..., sl]
        lag = jnp.pad(c, ((0, 0), (w, 0), (0, 0)))[:, :T]
        outs.append((c - lag) / jnp.minimum(count, w) - uf[..., sl])
    pooled = jnp.stack(outs, axis=2).astype(u.dtype)
    y = jnp.einsum('btgc,gcd->btgd', pooled, pool_w).reshape(B, T, D_MODEL)
    return y * pool_scale


def _mem_attn(u, mem_n, w_kv, qk_gain):
    B, T, _ = u.shape
    M = mem_n.shape[1]
    q = _rms(u.reshape(B, T, MEM_HEADS, MEM_HEAD), qk_gain[0])
    k, v = jnp.split(mem_n @ w_kv, 2, axis=-1)
    k = _rms(k.reshape(B, M, MEM_HEADS, MEM_HEAD), qk_gain[1])
    v = v.reshape(B, M, MEM_HEADS, MEM_HEAD)
    s = jnp.einsum('bthd,bmhd->bhtm', q, k).astype(F32) * (MEM_HEAD ** -0.5)
    p = jax.nn.softmax(s, axis=-1)
    o = jnp.einsum('bhtm,bmhd->bthd', p, v)
    return o.reshape(B, T, BRANCH_W).astype(u.dtype)


def _swiglu(h, w1, w3, w2):
    return (jax.nn.silu(h @ w1) * (h @ w3)) @ w2


def _moe(h, router, w1, w3, w2):
    B, T, D = h.shape
    hf = h.reshape(B * T, D)
    logits = (hf @ router).astype(F32)
    top_logit, top_e = lax.top_k(logits, TOP_K)
    top_w = jax.nn.softmax(top_logit, axis=-1)
    combine = jnp.sum(jax.nn.one_hot(top_e, N_EXPERTS, dtype=F32) * top_w[..., None], axis=1)
    out = jnp.zeros((B * T, D), F32)
    for e in range(N_EXPERTS):
        out = out + combine[:, e:e + 1] * _swiglu(hf, w1[e], w3[e], w2[e])
    return out.reshape(B, T, D).astype(h.dtype)


def setup_inputs(seed: int = 0) -> dict:
    key = jax.random.key(seed)
    ks = iter(jax.random.split(key, 48))
    nrm = lambda shape, fan_in: jax.random.normal(next(ks), shape, F32) * (fan_in ** -0.5)
    gain = lambda shape: 1.0 + 0.02 * jax.random.normal(next(ks), shape, F32)
    small = lambda shape, s: s * jax.random.normal(next(ks), shape, F32)
    unif = lambda shape, lo, hi: jax.random.uniform(next(ks), shape, F32, lo, hi)
    L, V = DEPTH, DEPTH - 1
    return {
        'x': jax.random.normal(next(ks), (BATCH, SEQ, D_MODEL), F32),
        'mem': jax.random.normal(next(ks), (BATCH, MEM_LEN, D_MODEL), F32),
        'norm_mix': gain((L, D_MODEL)),
        'norm_ffn': gain((L, D_MODEL)),
        'norm_mem': gain((L, D_MODEL)),
        'w_in': nrm((L, D_MODEL, N_IN), D_MODEL),
        'rwkv_mu': unif((L, RWKV_COLS), 0.0, 1.0),
        'rwkv_w0': unif((L, BRANCH_W), -6.0, -1.0),
        'rwkv_w2': nrm((L, RWKV_DECAY_LORA, BRANCH_W), RWKV_DECAY_LORA),
        'rwkv_a0': small((L, BRANCH_W), 0.1),
        'rwkv_a2': nrm((L, RWKV_A_LORA, BRANCH_W), RWKV_A_LORA),
        'rwkv_g2': nrm((L, RWKV_GATE_LORA, BRANCH_W), RWKV_GATE_LORA),
        'rwkv_kk': 1.0 + small((L, BRANCH_W), 0.1),
        'rwkv_ka': 1.0 + small((L, BRANCH_W), 0.1),
        'rwkv_rk': small((L, BRANCH_W), 0.1),
        'rwkv_ln_w': gain((L, BRANCH_W)),
        'rwkv_ln_b': small((L, BRANCH_W), 0.02),
        'vres_in': nrm((V, D_MODEL, RWKV_V_LORA), D_MODEL),
        'vres_mu': unif((V, RWKV_V_LORA), 0.0, 1.0),
        'vres_v0': small((V, BRANCH_W), 0.1),
        'vres_up': nrm((V, RWKV_V_LORA, BRANCH_W), RWKV_V_LORA),
        'nsa_qk_gain': gain((L, 4, NSA_HEAD)),
        'nsa_cmp_pos': small((L, 2, COMP_L, NSA_HEAD), 0.02),
        'nsa_cmp_w1': nrm((L, 2, COMP_L * NSA_HEAD, COMP_HIDDEN), COMP_L * NSA_HEAD),
        'nsa_cmp_b1': small((L, 2, COMP_HIDDEN), 0.02),
        'nsa_cmp_w2': nrm((L, 2, COMP_HIDDEN, NSA_HEAD), COMP_HIDDEN),
        'conv_w': nrm((L, CONV_W, BRANCH_W), CONV_W),
        'pool_w': nrm((L, len(POOL_WINDOWS), POOL_GROUP, POOL_OUT), POOL_GROUP),
        'pool_scale': gain((L, D_MODEL)),
        'mem_wkv': nrm((L, D_MODEL, 2 * BRANCH_W), D_MODEL),
        'mem_qk_gain': gain((L, 2, MEM_HEAD)),
        'w_branch': nrm((L, 4, BRANCH_W, D_MODEL), BRANCH_W),
        'w_out': nrm((L, D_MODEL, D_MODEL), D_MODEL),
        'ffn_w1': nrm((N_DENSE, D_MODEL, D_FF), D_MODEL),
        'ffn_w3': nrm((N_DENSE, D_MODEL, D_FF), D_MODEL),
        'ffn_w2': nrm((N_DENSE, D_FF, D_MODEL), D_FF),
        'moe_router': nrm((N_MOE, D_MODEL, N_EXPERTS), D_MODEL),
        'moe_w1': nrm((N_MOE, N_EXPERTS, D_MODEL, EXPERT_FF), D_MODEL),
        'moe_w3': nrm((N_MOE, N_EXPERTS, D_MODEL, EXPERT_FF), D_MODEL),
        'moe_w2': nrm((N_MOE, N_EXPERTS, EXPERT_FF, D_MODEL), EXPERT_FF),
    }


def reference(x, mem, norm_mix, norm_ffn, norm_mem, w_in, rwkv_mu, rwkv_w0, rwkv_w2, rwkv_a0,
              rwkv_a2, rwkv_g2, rwkv_kk, rwkv_ka, rwkv_rk, rwkv_ln_w, rwkv_ln_b, vres_in, vres_mu,
              vres_v0, vres_up, nsa_qk_gain, nsa_cmp_pos, nsa_cmp_w1, nsa_cmp_b1, nsa_cmp_w2, conv_w,
              pool_w, pool_scale, mem_wkv, mem_qk_gain, w_branch, w_out, ffn_w1, ffn_w3, ffn_w2,
              moe_router, moe_w1, moe_w3, moe_w2):
    B, T, _ = x.shape
    v_first = None
    for l in range(DEPTH):
        h = _rms(x, norm_mix[l])
        if l == 0:
            p = h @ w_in[l]
            parts = _split(p, IN_SIZES)
            vres = None
        else:
            p = h @ jnp.concatenate([w_in[l], vres_in[l - 1]], axis=1)
            parts = _split(p, IN_SIZES + (RWKV_V_LORA,))
            vres = (_token_shift(parts[6], vres_mu[l - 1]), vres_v0[l - 1], vres_up[l - 1])
        p_rwkv, p_nsa, p_conv, p_pool, p_mem, p_gate = parts[:6]

        y_rwkv, v_l = _rwkv7_time_mix(p_rwkv, rwkv_mu[l], rwkv_w0[l], rwkv_w2[l], rwkv_a0[l], rwkv_a2[l],
                                      rwkv_g2[l], rwkv_kk[l], rwkv_ka[l], rwkv_rk[l], rwkv_ln_w[l],
                                      rwkv_ln_b[l], v_first, vres)
        if l == 0:
            v_first = v_l
        y_nsa = _nsa(p_nsa, nsa_qk_gain[l], nsa_cmp_pos[l], nsa_cmp_w1[l], nsa_cmp_b1[l], nsa_cmp_w2[l])
        y_conv = _short_conv(p_conv, conv_w[l])
        y_mem = _mem_attn(p_mem, _rms(mem, norm_mem[l]), mem_wkv[l], mem_qk_gain[l])
        z_pool = _pool(p_pool, pool_w[l], pool_scale[l])

        gates = jax.nn.sigmoid(p_gate.astype(F32)).reshape(B, T, N_BRANCHES, D_MODEL)
        merged = gates[:, :, 4] * z_pool
        ys = (y_rwkv, y_nsa, y_conv, y_mem)
        for i in range(4):
            merged = merged + gates[:, :, i] * (ys[i] @ w_branch[l, i])
        x = x + (merged.astype(x.dtype) @ w_out[l])

        h2 = _rms(x, norm_ffn[l])
        if l % 2 == 0:
            x = x + _swiglu(h2, ffn_w1[l // 2], ffn_w3[l // 2], ffn_w2[l // 2])
        else:
            x = x + _moe(h2, moe_router[l // 2], moe_w1[l // 2], moe_w3[l // 2], moe_w2[l // 2])
    return x
```

```python
import numpy as np
import concourse.bass as bass
import concourse.mybir as mybir
from concourse.bass_utils import run_bass_kernel_spmd

F32 = mybir.dt.float32
BF16 = mybir.dt.bfloat16
ALU = mybir.AluOpType
AF = mybir.ActivationFunctionType
AX = mybir.AxisListType

D = 2048
NCORES = 8
MEM_LEN = 256
DEPTH = 2
D_FF = 5632
E_FF = 2816
N_EXP = 8
EPS = 1e-6
O_RWKV, O_NSA, O_CONV, O_POOL, O_MEM, O_GATE = 0, 1984, 3288, 4824, 5336, 5848
N_IN = 16088
HALO = 16


class FW:
    def __init__(self, nc, n_dma_sems=20):
        self.nc = nc
        self.eng = {"pe": nc.tensor, "act": nc.scalar, "dve": nc.vector, "pool": nc.gpsimd, "sp": nc.sync}
        self.sem = {}
        self.cnt = {}
        for e in self.eng:
            self.sem[e] = nc.semaphore("s_" + e).__enter__()
            self.cnt[e] = 0
        self.dsem = {}
        for q in ("sp", "pool", "act"):
            lst = [[nc.semaphore(f"d_{q}{i}").__enter__(), 0] for i in range(n_dma_sems)]
            self.dsem[q] = [lst, 0]
        self.ccsem = nc.semaphore("ccsem").__enter__()
        self.cccnt = 0
        self.seen = {e: {} for e in self.eng}
        self.lastw = {}
        self.readers = {}
        self.ninstr = 0
        self.excl = {"ns_accb"}

    @staticmethod
    def _k(x):
        return x if isinstance(x, str) else x.name

    def _wait(self, e, tok):
        if tok is None:
            return
        sem, val, src = tok
        if src == "pe" and e == "pe":
            return
        k = id(sem)
        if self.seen[e].get(k, 0) >= val:
            return
        self.eng[e].wait_ge(sem, val)
        self.seen[e][k] = val

    def _deps(self, e, reads, writes):
        for k in reads:
            self._wait(e, self.lastw.get(k))
        for k in writes:
            self._wait(e, self.lastw.get(k))
            for t in self.readers.get(k, ()):
                self._wait(e, t)

    def _record(self, tok, reads, writes):
        for k in reads:
            self.readers.setdefault(k, []).append(tok)
        for k in writes:
            self.lastw[k] = tok
            self.readers[k] = []
        self.ninstr += 1

    def op(self, e, fn, reads=(), writes=()):
        reads = [self._k(x) for x in reads]
        writes = [self._k(x) for x in writes]
        for k in reads:
            if (k.startswith("psb") or k in self.excl) and k not in writes:
                writes.append(k)
        self._deps(e, reads, writes)
        ins = fn(self.eng[e])
        self.cnt[e] += 1
        ins.then_inc(self.sem[e], 1)
        tok = (self.sem[e], self.cnt[e], e)
        self._record(tok, reads, writes)
        return tok

    def dma(self, q, out, in_, reads=(), writes=(), **kw):
        reads = [self._k(x) for x in reads]
        writes = [self._k(x) for x in writes]
        lst, idx = self.dsem[q]
        ent = lst[idx % len(lst)]
        self.dsem[q][1] += 1
        sem, tgt = ent
        if tgt > 0:
            self._wait(q, (sem, tgt, "dma"))
        self._deps(q, reads, writes)
        self.eng[q].dma_start(out=out, in_=in_, **kw).then_inc(sem, 16)
        ent[1] = tgt + 16
        tok = (sem, tgt + 16, "dma")
        self._record(tok, reads, writes)
        return tok

    def allgather(self, src, dst, groups, reads=(), writes=()):
        reads = [self._k(x) for x in reads]
        writes = [self._k(x) for x in writes]
        self._deps("pool", reads, writes)
        self.nc.gpsimd.collective_compute("AllGather", ALU.bypass, replica_groups=groups,
                                          ins=[src.ap().opt()], outs=[dst.ap().opt()]).then_inc(self.ccsem)
        self.cccnt += 1
        tok = (self.ccsem, self.cccnt, "cc")
        self._record(tok, reads, writes)
        return tok

    def finish(self, keys):
        for k in keys:
            self._wait("sp", self.lastw.get(k))


class LazyInputs(dict):
    def __init__(self, kb):
        super().__init__()
        self.kb = kb

    def __missing__(self, name):
        shp = INPUT_SHAPES(self.kb.T)[name]
        t = self.kb.nc.dram_tensor(name, list(shp), F32, kind="ExternalInput")
        self[name] = t
        return t


class KB:
    def __init__(self, T):
        self.T = T
        self.TC = T // 4
        self.NT = self.TC // 128
        self.nc = bass.Bass("TRN2", target_bir_lowering=False)
        self.fw = FW(self.nc)
        self.inputs = LazyInputs(self)
        self.outputs = {}
        self._uid = 0
        self._psn = 0
        self.ps = [self.nc.psum_tensor(f"psb{i}", [128, 512], F32).__enter__() for i in range(8)]
        self.scopes = []

    def din(self, name, shape, dtype=F32):
        t = self.nc.dram_tensor(name, list(shape), dtype, kind="ExternalInput")
        self.inputs[name] = t
        return t

    def dout(self, name, shape, dtype=F32):
        t = self.nc.dram_tensor(name, list(shape), dtype, kind="ExternalOutput")
        self.outputs[name] = t
        return t

    def dscr(self, name, shape, dtype=F32):
        return self.nc.dram_tensor(name, list(shape), dtype)

    def sb(self, name, shape, dtype=F32):
        self._uid += 1
        g = self.nc.sbuf_tensor(f"{name}_u{self._uid}", list(shape), dtype)
        t = g.__enter__()
        if self.scopes:
            self.scopes[-1].append(g)
        return t

    def push(self):
        self.scopes.append([])

    def pop(self):
        for g in reversed(self.scopes.pop()):
            g.__exit__(None, None, None)

    def psum(self):
        p = self.ps[self._psn % 8]
        self._psn += 1
        return p

    def psum_from(self, lo, n):
        p = self.ps[lo + self._psn % n]
        self._psn += 1
        return p

    def psum4(self):
        p = self.ps[4 + self._psn % 4]
        self._psn += 1
        return p

    def op(self, e, fn, reads=(), writes=()):
        return self.fw.op(e, fn, reads, writes)

    def dma(self, q, out, in_, reads=(), writes=(), **kw):
        return self.fw.dma(q, out, in_, reads, writes, **kw)

    def mm(self, out, lhsT, rhs, start, stop, reads, writes):
        return self.fw.op("pe", lambda e: e.matmul(out, lhsT, rhs, start=start, stop=stop), reads, writes)

    def tr(self, out, in_, ident, reads, writes):
        return self.fw.op("pe", lambda e: e.transpose(out, in_, ident), reads, writes)

    def act(self, out, in_, func, reads, writes, bias=None, scale=None, accum_out=None):
        kw = {}
        if bias is not None:
            kw["bias"] = bias
        if scale is not None:
            kw["scale"] = scale
        if accum_out is not None:
            kw["accum_out"] = accum_out
        return self.fw.op("act", lambda e: e.activation(out=out, in_=in_, func=func, **kw), reads, writes)

    def tt(self, out, in0, in1, op, reads, writes, eng="dve"):
        return self.fw.op(eng, lambda e: e.tensor_tensor(out=out, in0=in0, in1=in1, op=op), reads, writes)

    def ts(self, out, in0, s1, s2, op0, op1, reads, writes, eng="dve"):
        if s2 is None:
            s2, op1 = 0.0, ALU.add
        return self.fw.op(eng, lambda e: e.tensor_scalar(out=out, in0=in0, scalar1=s1, scalar2=s2, op0=op0, op1=op1), reads, writes)

    def stt(self, out, in0, scalar, in1, op0, op1, reads, writes, eng="dve"):
        return self.fw.op(eng, lambda e: e.scalar_tensor_tensor(out=out, in0=in0, scalar=scalar, in1=in1, op0=op0, op1=op1), reads, writes)

    def copy(self, out, in_, reads, writes, eng="dve"):
        if eng == "act":
            return self.fw.op("act", lambda e: e.copy(out=out, in_=in_), reads, writes)
        return self.fw.op(eng, lambda e: e.tensor_copy(out=out, in_=in_), reads, writes)

    def memset(self, ap, val, writes, eng="dve"):
        return self.fw.op(eng, lambda e: e.memset(ap, val), (), writes)


def bcast_rows(dram_ap_1d_row, nparts):
    return dram_ap_1d_row.partition_broadcast(nparts)


def barrier(K):
    fw = K.fw
    toks = [(fw.sem[e], fw.cnt[e], e) for e in fw.eng if fw.cnt[e] > 0]
    for q in fw.dsem:
        for sem, tgt in fw.dsem[q][0]:
            if tgt > 0:
                toks.append((sem, tgt, "dma"))
    if fw.cccnt:
        toks.append((fw.ccsem, fw.cccnt, "cc"))
    for e in fw.eng:
        for t in toks:
            if t[2] == e:
                continue
            fw._wait(e, t)


def rmsnorm_T(K, x_rows, gbc, hT, col0, ntiles, C, tag):
    for tt in range(ntiles):
        b = tt % 2
        xt, junk, ss, hb = C["xt"][b], C["junk"][b], C["ss"][b], C["hb"][b]
        K.dma("sp", xt[:], x_rows[tt * 128:(tt + 1) * 128, :], writes=[xt])
        K.memset(ss[:], 0.0, [ss])
        K.act(junk[:], xt[:], AF.Square, [xt], [junk, ss], accum_out=ss[:, 0:1])
        K.ts(ss[:, 1:2], ss[:, 0:1], 1.0 / D, EPS, ALU.mult, ALU.add, [ss], [ss])
        K.act(ss[:, 1:2], ss[:, 1:2], AF.Sqrt, [ss], [ss])
        K.op("dve", lambda e: e.reciprocal(out=ss[:, 1:2], in_=ss[:, 1:2]), [ss], [ss])
        K.stt(hb[:], xt[:], ss[:, 1:2], gbc[:], ALU.mult, ALU.mult, [xt, ss, gbc], [hb])
        for grp in range(2):
            ps = K.psum()
            psb = ps[:].bitcast(BF16)
            for i in range(8):
                kc = grp * 8 + i
                K.tr(psb[:, i * 128:(i + 1) * 128], hb[:, kc * 128:(kc + 1) * 128], C["identb"][:], [hb, C["identb"]], [ps])
            K.copy(hT[:, grp * 8:(grp + 1) * 8, col0 + tt * 128: col0 + (tt + 1) * 128],
                   psb.rearrange("p (a b) -> p a b", a=8), [ps], [hT], eng="act" if grp == 0 else "dve")


def load_w(K, wt, W, c0, ncols, kchunks=16, key=None):
    src = W.rearrange("(kc p) n -> p kc n", p=128)[:, :, c0:c0 + ncols]
    K.dma("pool", wt[:, 0:kchunks, 0:ncols], src, writes=[key or wt])


def fm_colsum_norm(K, out_bf, outkey, src_f32, tagkey, n, nparts, ones_f, gain_col, inv_n, C):
    sq = C["fm_sq"]
    K.tt(sq[0:nparts, 0:n], src_f32, src_f32, ALU.mult, [tagkey], [sq])
    ps = K.psum()
    K.mm(ps[0:nparts, 0:n], ones_f[0:nparts, 0:nparts], sq[0:nparts, 0:n], True, True, [sq, ones_f], [ps])
    rs = C["fm_rs"]
    K.ts(rs[0:nparts, 0:n], ps[0:nparts, 0:n], inv_n, EPS, ALU.mult, ALU.add, [ps], [rs])
    K.act(rs[0:nparts, 0:n], rs[0:nparts, 0:n], AF.Sqrt, [rs], [rs])
    K.op("dve", lambda e: e.reciprocal(out=rs[0:nparts, 0:n], in_=rs[0:nparts, 0:n]), [rs], [rs])
    K.stt(out_bf, src_f32, gain_col, rs[0:nparts, 0:n], ALU.mult, ALU.mult, [tagkey, rs], [outkey])


PV_CONV = 0
PV_MEMQG = 12
PV_MEMKG = 13
PV_MU = 14
PV_W0, PV_A0, PV_KK, PV_KA, PV_RK, PV_LNW, PV_LNB, PV_V0, PV_C1 = 22, 23, 24, 25, 26, 27, 28, 29, 30
PV_NSAG = 31
PV_CB1 = 35
NV = 40


def local_mixers(K, l, C, S):
    TC, W = K.TC, HALO + K.TC
    hTx, pv = C["hTx"], C["pv"][l]
    w_in = K.inputs["w_in"].ap()[l]
    tokblocks = [(0, HALO)] + [(HALO + i * 512, min(512, TC - i * 512)) for i in range((TC + 511) // 512)]
    locblocks = tokblocks[1:]
    K.push()
    wts = [K.sb(f"lm_wt{i}", [128, 16, 512], BF16) for i in range(2)]
    wi = [0]

    def nextw():
        w = wts[wi[0] % 2]
        wi[0] += 1
        return w

    def proj(wt, c_lo, ncol, dst, blocks, evac=None):
        for (c0, n) in blocks:
            ps = K.psum()
            for kc in range(16):
                K.mm(ps[0:ncol, 0:n], wt[:, kc, c_lo:c_lo + ncol], hTx[:, kc, c0:c0 + n], kc == 0, kc == 15, [wt, hTx], [ps])
            K.copy(dst[0:ncol, c0:c0 + n], ps[0:ncol, 0:n], [ps], [dst], eng="act")

    K.push()
    bt, ct, xt_ = (K.sb(n, [128, W]) for n in ("cv_b", "cv_c", "cv_x"))
    z, acc = K.sb("cv_z", [128, W]), K.sb("cv_acc", [128, TC])
    for j in range(4):
        wt = nextw()
        for i in range(3):
            src = w_in.rearrange("(kc p) n -> p kc n", p=128)[:, :, O_CONV + i * 512 + j * 128: O_CONV + i * 512 + (j + 1) * 128]
            K.dma("pool", wt[:, :, i * 128:(i + 1) * 128], src, writes=[wt])
        proj(wt, 0, 128, bt, locblocks)
        proj(wt, 128, 128, ct, tokblocks)
        proj(wt, 256, 128, xt_, tokblocks)
        K.tt(z[:], ct[:], xt_[:], ALU.mult, [ct, xt_], [z])
        cw = lambda i: pv[:, PV_CONV + j * 3 + i: PV_CONV + j * 3 + i + 1]
        K.ts(acc[:], z[:, HALO:W], cw(2), None, ALU.mult, None, [z, pv], [acc])
        K.stt(acc[:], z[:, HALO - 1:W - 1], cw(1), acc[:], ALU.mult, ALU.add, [z, pv, acc], [acc])
        K.stt(acc[:], z[:, HALO - 2:W - 2], cw(0), acc[:], ALU.mult, ALU.add, [z, pv, acc], [acc])
        K.tt(S["y_convT"][:, j, :], bt[:, HALO:W], acc[:], ALU.mult, [bt, acc], [S["y_convT"]])
    barrier(K)
    K.pop()
    K.push()
    acc = K.sb("pl_acc", [128, TC])
    wt = nextw()
    load_w(K, wt, w_in, O_POOL, 512)
    pa, pb, pu = K.sb("pl_a", [128, W]), K.sb("pl_b", [128, W]), K.sb("pl_u", [128, W])
    invc = K.sb("pl_invc", [128, TC])
    for g in range(4):
        proj(wt, g * 128, 128, pu, tokblocks)
        K.dma("sp", invc[:], K.inputs["invcnt"].ap()[g:g + 1, :].partition_broadcast(128), writes=[invc])
        cur, oth = pu, pa
        for si, sh in enumerate([1, 2, 4, 8][:g + 1]):
            K.tt(oth[:, sh:W], cur[:, sh:W], cur[:, 0:W - sh], ALU.add, [cur], [oth])
            cur, oth = oth, (pb if oth is pa else pa)
        K.tt(acc[:], cur[:, HALO:W], invc[:], ALU.mult, [cur, invc], [acc])
        K.tt(S["pooledT"][:, g, :], acc[:], pu[:, HALO:W], ALU.subtract, [acc, pu], [S["pooledT"]])
    barrier(K)
    K.pop()
    memT = K.sb("mm_memT", [128, 16, MEM_LEN], BF16)
    gm = K.sb("mm_g", [128, D])
    K.dma("sp", gm[:], K.inputs["norm_mem"].ap()[l:l + 1, :].partition_broadcast(128), writes=[gm])
    K.push()
    rn_alloc(K, C)
    rmsnorm_T(K, K.inputs["mem"].ap(), gm, memT, 0, 2, C, "mem")
    barrier(K)
    K.pop()
    wkv = K.inputs["mem_wkv"].ap()[l]
    kT = K.sb("mm_kT", [128, 4, MEM_LEN], BF16)
    vsb = K.sb("mm_v", [128, 2, 512], BF16)
    kf = K.sb("mm_kf", [128, 512])
    wt = nextw()
    load_w(K, wt, wkv, 0, 512)
    for h in range(4):
        ps = K.psum()
        for kc in range(16):
            K.mm(ps[:, 0:MEM_LEN], wt[:, kc, h * 128:(h + 1) * 128], memT[:, kc, :], kc == 0, kc == 15, [wt, memT], [ps])
        K.copy(kf[:, 0:MEM_LEN], ps[:, 0:MEM_LEN], [ps], [kf], eng="act")
        fm_colsum_norm(K, kT[:, h, :], kT, kf[:, 0:MEM_LEN], kf, MEM_LEN, 128, C["ones_f"], pv[:, PV_MEMKG:PV_MEMKG + 1], 1.0 / 128, C)
    wt = nextw()
    load_w(K, wt, wkv, 512, 512)
    for mt in range(2):
        ps = K.psum()
        for kc in range(16):
            K.mm(ps[:, :], memT[:, kc, mt * 128:(mt + 1) * 128], wt[:, kc, :], kc == 0, kc == 15, [wt, memT], [ps])
        K.copy(vsb[:, mt, :], ps[:, :], [ps], [vsb], eng="act")
    wt = nextw()
    load_w(K, wt, w_in, O_MEM, 512)
    qf, qT = K.sb("mm_qf", [128, W]), K.sb("mm_qT", [128, 512], BF16)
    es = [K.sb(f"mm_e{i}", [128, 512], BF16) for i in range(2)]
    rden = K.sb("mm_rden", [128, 512])
    for h in range(4):
        proj(wt, h * 128, 128, qf, locblocks)
        for (c0, n) in locblocks:
            fm_colsum_norm(K, qT[:, 0:n], qT, qf[:, c0:c0 + n], qf, n, 128, C["ones_f"], pv[:, PV_MEMQG:PV_MEMQG + 1], 1.0 / 128, C)
            for mt in range(2):
                ps = K.psum()
                K.mm(ps[:, 0:n], kT[:, h, mt * 128:(mt + 1) * 128], qT[:, 0:n], True, True, [kT, qT], [ps])
                K.act(es[mt][:, 0:n], ps[:, 0:n], AF.Exp, [ps], [es[mt]], scale=float(128 ** -0.5))
            po, pd = K.psum(), K.psum()
            for mt in range(2):
                K.mm(po[:, 0:n], vsb[:, mt, h * 128:(h + 1) * 128], es[mt][:, 0:n], mt == 0, mt == 1, [vsb, es[mt]], [po])
            for mt in range(2):
                K.mm(pd[:, 0:n], C["ones_b"][:, :], es[mt][:, 0:n], mt == 0, mt == 1, [C["ones_b"], es[mt]], [pd])
            K.op("dve", lambda e: e.reciprocal(out=rden[:, 0:n], in_=pd[:, 0:n]), [pd], [rden])
            K.tt(S["y_memT"][:, h, c0 - HALO:c0 - HALO + n], po[:, 0:n], rden[:, 0:n], ALU.mult, [po, rden], [S["y_memT"]])
    barrier(K)
    K.pop()


GROUPS = [[0, 1, 2, 3], [4, 5, 6, 7]]


def alloc_gather(K, C):
    TC = K.TC
    C["hT_src"] = [K.dscr(f"hT_src{q}", [256, TC], BF16) for q in range(8)]
    C["hT_all"] = [K.dscr(f"hT_all{q}", [4 * 256, TC], BF16) for q in range(8)]


def alloc_scratch(K):
    T, TC = K.T, K.TC
    R = {n: K.dscr(n, [128, T]) for n in ("fm_r", "fm_w", "fm_nkk", "fm_kp", "fm_g", "fm_bonus", "fm_vfirst")}
    R["tm_kka"] = K.dscr("tm_kka", [T, 128])
    R["tm_v"] = K.dscr("tm_v", [T, 128])
    for nm in ("yB", "yN"):
        R[nm] = {"key": nm, "src": [K.dscr(f"{nm}_src{j}", [128, TC]) for j in range(4)],
                 "all": [K.dscr(f"{nm}_all{j}", [512, TC]) for j in range(4)]}
    return R


def setup_common(K):
    C = {}
    C["identf"] = K.sb("identf", [128, 128])
    C["identb"] = K.sb("identb", [128, 128], BF16)
    C["ones_f"] = K.sb("ones_f", [128, 128])
    C["ones_b"] = K.sb("ones_b", [128, 128], BF16)
    K.dma("sp", C["identf"][:], K.inputs["ident"].ap(), writes=[C["identf"]])
    K.copy(C["identb"][:], C["identf"][:], [C["identf"]], [C["identb"]])
    K.memset(C["ones_f"][:], 1.0, [C["ones_f"]])
    K.memset(C["ones_b"][:], 1.0, [C["ones_b"]])
    C["fm_sq"] = K.sb("fm_sq", [128, 512])
    C["fm_rs"] = K.sb("fm_rs", [128, 512])
    C["oh"] = K.sb("oh", [128, 8])
    K.dma("sp", C["oh"][:], K.inputs["onehot"].ap(), writes=[C["oh"]])
    C["pv"] = []
    for l in range(DEPTH):
        t = K.sb(f"pv{l}", [128, NV])
        K.dma("sp", t[:], K.inputs["pvec"].ap()[l], writes=[t])
        K.ts(t[:, PV_C1:PV_C1 + 1], t[:, PV_KA:PV_KA + 1], -1.0, 1.0, ALU.mult, ALU.add, [t], [t])
        C["pv"].append(t)
    return C


def rn_alloc(K, C):
    C["xt"] = [K.sb(f"rn_xt{i}", [128, D]) for i in range(2)]
    C["junk"] = [K.sb(f"rn_junk{i}", [128, D], BF16) for i in range(2)]
    C["ss"] = [K.sb(f"rn_ss{i}", [128, 2]) for i in range(2)]
    C["hb"] = [K.sb(f"rn_hb{i}", [128, D], BF16) for i in range(2)]
    C["gbc"] = K.sb("gbc", [128, D])


def phase_norm_gather(K, l, C, xres):
    TC = K.TC
    hTx = C["hTx"]
    K.push()
    rn_alloc(K, C)
    K.dma("sp", C["gbc"][:], K.inputs["norm_mix"].ap()[l:l + 1, :].partition_broadcast(128), writes=[C["gbc"]])
    rmsnorm_T(K, xres.ap(), C["gbc"], hTx, HALO, K.NT, C, "mix")
    barrier(K)
    K.pop()
    for q in range(NHQ):
        K.dma("sp", C["hT_src"][q].ap().rearrange("(kc p) t -> p kc t", p=128), hTx[:, 2 * q:2 * q + 2, HALO:HALO + TC], reads=[hTx], writes=["hT_src"])
    for q in range(NHQ):
        K.fw.allgather(C["hT_src"][q], C["hT_all"][q], GROUPS, reads=["hT_src"], writes=["hT_all"])
    cand = C["halo_cand"]
    C["_hall_dst"] = cand
    for r in range(4):
        hall_read(K, cand[:, r], C, r, TC - HALO, HALO)
    oh = C["oh"]
    K.ts(hTx[:, :, 0:HALO], cand[:, 0], oh[:, 4:5], None, ALU.mult, None, [cand, oh], [hTx])
    for r in range(1, 4):
        K.stt(hTx[:, :, 0:HALO], cand[:, r], oh[:, 4 + r:5 + r], hTx[:, :, 0:HALO], ALU.mult, ALU.add, [cand, oh, hTx], [hTx])


def rwkv_cols(hp):
    cols = []
    pad = lambda a, n: list(a) + [-1] * (n - len(a))
    cols += list(range(128 * hp, 128 * hp + 128))
    cols += list(range(512 + 128 * hp, 512 + 128 * hp + 128))
    cols += list(range(1024 + 128 * hp, 1024 + 128 * hp + 128))
    cols += pad(range(1536, 1632), 128)
    cols += pad(range(1632, 1728), 128)
    cols += list(range(1728, 1984))
    cols += pad(range(1984, 2048), 128)
    return np.array(cols)


def nsa_cols(hp):
    hk = hp // 2
    mine = [2 * hp, 2 * hp + 1]
    oth = [h for h in range(4 * hk, 4 * hk + 4) if h not in mine]
    heads = mine + oth
    grp = lambda i: list(range(512 + 128 * i + 64 * hk, 512 + 128 * i + 64 * hk + 64))
    cols = []
    for h in heads:
        cols += list(range(64 * h, 64 * h + 64))
    cols += grp(0) + grp(0) + grp(2) + grp(2) + grp(4) + grp(4)
    cols += grp(1)
    cols += grp(3) + grp(5)
    for h in heads:
        cols += [512 + 768 + 3 * h + i for i in range(3)]
    return np.array(cols)


NWN = 844


def host_prep(inp, T):
    TC = T // 4
    f = lambda a: np.ascontiguousarray(np.asarray(a, dtype=np.float32))
    sh = {k: f(inp[k]) for k in ("w_in", "norm_mix", "norm_ffn", "norm_mem", "mem_wkv", "pool_w", "pool_scale", "w_branch",
                                 "w_out", "ffn_w1", "ffn_w3", "ffn_w2", "moe_router", "moe_w1", "moe_w3", "moe_w2",
                                 "nsa_cmp_w1", "nsa_cmp_w2", "nsa_cmp_pos")}
    sh["ident"] = np.eye(128, dtype=np.float32)
    sh["blk64"] = np.kron(np.eye(2), np.ones((64, 64))).astype(np.float32)
    sh["cmp_posT"] = np.ascontiguousarray(np.transpose(sh["nsa_cmp_pos"], (0, 1, 3, 2)))
    sh["nsa_qk_gain"] = f(inp["nsa_qk_gain"])
    NS, NTT = T // 64, T // 128
    cc = np.arange(256)[:, None] * 16
    ss_ = np.arange(64)[None, :] * 64
    sh["ovl"] = (np.clip(np.minimum(cc + 32, ss_ + 64) - np.maximum(cc, ss_), 0, None) / 32.0).astype(np.float32)
    tt_ = np.arange(T)
    sh["maskc"] = ((np.arange(256)[:, None] * 16 + 31) <= tt_[None, :]).astype(np.float32)
    cur = (tt_ // 64)[:, None]
    sid = np.arange(64)[None, :]
    valid = sid <= cur
    f0, f1, f2 = (sid == 0), (sid == cur), (sid == cur - 1)
    forced = f0 | f1 | f2
    sh["tk_keep"] = (valid & ~forced).astype(np.float32)
    cadd = np.where(valid, 0.0, -1e9)
    cadd = np.where(f2, 1e9, cadd)
    cadd = np.where(f1, 2e9, cadd)
    cadd = np.where(f0, 3e9, cadd)
    sh["tk_cadd"] = cadd.astype(np.float32)
    e2 = np.zeros((64, 32, 128), np.float32)
    for jt in range(32):
        for j in range(128):
            e2[2 * jt + j // 64, jt, j] = 1.0
    sh["e2"] = e2
    dm = np.zeros((5, 128, 512), np.float32)
    jj = np.arange(128)[:, None]
    t5 = np.arange(512)[None, :]
    for dd in range(4):
        dm[dd] = (t5 >= dd * 128 + jj)
    dm[4] = (jj > t5)
    sh["dmask"] = dm
    x = f(inp["x"])
    mem = f(inp["mem"])
    w_in = sh["w_in"]
    wfull = [np.concatenate([w_in[0][:, :1984], np.zeros((D, 64), np.float32)], 1),
             np.concatenate([w_in[1][:, :1984], f(inp["vres_in"])[0]], 1)]
    mufull = [np.concatenate([f(inp["rwkv_mu"])[0], np.zeros(64, np.float32)]),
              np.concatenate([f(inp["rwkv_mu"])[1], f(inp["vres_mu"])[0]])]
    maps = []
    for c in range(NCORES):
        b, j = c // 4, c % 4
        hp = j
        m = dict(sh)
        m["x"] = np.ascontiguousarray(x[b, j * TC:(j + 1) * TC])
        m["mem"] = np.ascontiguousarray(mem[b])
        oh = np.zeros((128, 8), np.float32)
        oh[:, j] = 1.0
        if j > 0:
            oh[:, 4 + j - 1] = 1.0
        m["onehot"] = oh
        tg = np.arange(j * TC, (j + 1) * TC, dtype=np.float32) + 1.0
        m["invcnt"] = np.stack([1.0 / np.minimum(tg, w) for w in (2, 4, 8, 16)]).astype(np.float32)
        rc = rwkv_cols(hp)
        ncl = nsa_cols(hp)
        wr = np.zeros((DEPTH, D, 1024), np.float32)
        wn = np.zeros((DEPTH, D, NWN), np.float32)
        pv = np.zeros((DEPTH, 128, NV), np.float32)
        lora = np.zeros((DEPTH, 128, 5, 128), np.float32)
        ch = slice(128 * hp, 128 * hp + 128)
        for l in range(DEPTH):
            ok = rc >= 0
            wr[l][:, ok] = wfull[l][:, rc[ok]]
            wn[l] = w_in[l][:, O_NSA + ncl]
            cw = f(inp["conv_w"])[l]
            for jj in range(4):
                for i in range(3):
                    pv[l, :, PV_CONV + jj * 3 + i] = cw[i, jj * 128:(jj + 1) * 128]
            pv[l, :, PV_MEMQG] = f(inp["mem_qk_gain"])[l, 0]
            pv[l, :, PV_MEMKG] = f(inp["mem_qk_gain"])[l, 1]
            mu = np.zeros(1024, np.float32)
            mu[ok] = mufull[l][rc[ok]]
            pv[l, :, PV_MU:PV_MU + 8] = mu.reshape(8, 128).T
            for idx, nm in ((PV_W0, "rwkv_w0"), (PV_A0, "rwkv_a0"), (PV_KK, "rwkv_kk"), (PV_KA, "rwkv_ka"), (PV_RK, "rwkv_rk"),
                            (PV_LNW, "rwkv_ln_w"), (PV_LNB, "rwkv_ln_b")):
                pv[l, :, idx] = f(inp[nm])[l, ch]
            if l == 1:
                pv[l, :, PV_V0] = f(inp["vres_v0"])[0, ch]
                lora[l, 0:64, 4] = f(inp["vres_up"])[0][:, ch]
            g = f(inp["nsa_qk_gain"])[l]
            for i in range(4):
                pv[l, :, PV_NSAG + i] = np.concatenate([g[i], g[i]])
            pv[l, :, PV_CB1:PV_CB1 + 2] = f(inp["nsa_cmp_b1"])[l].T
            lora[l, 0:96, 0] = f(inp["rwkv_w2"])[l][:, ch]
            lora[l, 0:96, 1] = f(inp["rwkv_a2"])[l][:, ch]
            lora[l, :, 2] = f(inp["rwkv_g2"])[l][0:128, ch]
            lora[l, :, 3] = f(inp["rwkv_g2"])[l][128:256, ch]
        m["wr"], m["wn"], m["pvec"], m["lora"] = wr, wn, pv, lora
        maps.append(m)
    return maps


NHQ = 8


def hall_read(K, dst3, C, r, col0, n):
    for q in range(NHQ):
        src = C["hT_all"][q].ap()[r * 256:(r + 1) * 256, col0:col0 + n].rearrange("(kc p) t -> p kc t", p=128)
        K.dma("sp", dst3[:, 2 * q:2 * q + 2, :], src, reads=["hT_all"], writes=[C["_hall_dst"]])


def ysrc_write(K, Y, rows, t0, n, src_tile, src_ap):
    TC = K.TC
    done = 0
    while done < n:
        j, off = (t0 + done) // TC, (t0 + done) % TC
        m = min(n - done, TC - off)
        K.dma("sp", Y["src"][j].ap()[rows, off:off + m], src_ap[:, done:done + m], reads=[src_tile], writes=[Y["key"] + "_src"])
        done += m


def ygather(K, Y):
    for j in range(4):
        K.fw.allgather(Y["src"][j], Y["all"][j], GROUPS, reads=[Y["key"] + "_src"], writes=[Y["key"] + "_all"])


def load_hall_block(K, hb, C, t0, TC, blk=512):
    sub = min(blk, TC)
    for si in range(blk // sub):
        tok = t0 + si * sub
        r_, off = tok // TC, tok % TC
        C["_hall_dst"] = hb
        hall_read(K, hb[:, :, si * sub:(si + 1) * sub], C, r_, off, sub)


CS = 16
GN_EPS = 64e-5


def rwkv_phase(K, l, C, R):
    T = K.T
    TC = K.TC
    NB = T // 512
    pv = C["pv"][l]
    K.push()
    wr = K.sb("rw_w", [128, 16, 1024], BF16)
    for i in range(2):
        K.dma("pool", wr[:, :, i * 512:(i + 1) * 512],
              K.inputs["wr"].ap()[l].rearrange("(kc p) n -> p kc n", p=128)[:, :, i * 512:(i + 1) * 512], writes=[wr])
    lora = K.sb("rw_lora", [128, 5, 128])
    K.dma("sp", lora[:], K.inputs["lora"].ap()[l], writes=[lora])
    blk = K.sb("rw_blk", [128, 128])
    K.dma("sp", blk[:], K.inputs["blk64"].ap(), writes=[blk])
    hTb = [K.sb(f"rw_hT{i}", [128, 16, 512], BF16) for i in range(2)]
    nct = 8 if l == 1 else 7
    ub = [K.sb(f"rw_ub{i}", [128, 513]) for i in range(nct)]
    uf = [K.sb(f"rw_uf{i}", [128, 512]) for i in range(nct)]
    tmp = [K.sb(f"rw_t{i}", [128, 512]) for i in range(8)]
    tmo = [K.sb(f"rw_tmo{i}", [128, 4, 128]) for i in range(2)]
    for ct in range(nct):
        K.memset(ub[ct][:, 0:1], 0.0, [ub[ct]])
    sc = lambda i: pv[:, i:i + 1]
    for tb in range(NB):
        t0 = tb * 512
        hb = hTb[tb % 2]
        load_hall_block(K, hb, C, t0, TC)
        for ct in range(nct):
            ps = K.psum()
            for kc in range(16):
                K.mm(ps[:, :], wr[:, kc, ct * 128:(ct + 1) * 128], hb[:, kc, :], kc == 0, kc == 15, [wr, hb], [ps])
            K.copy(ub[ct][:, 1:513], ps[:, :], [ps], [ub[ct]], eng="act")
            d = tmp[0]
            K.tt(d[:], ub[ct][:, 0:512], ub[ct][:, 1:513], ALU.subtract, [ub[ct]], [d])
            K.stt(uf[ct][:], d[:], sc(PV_MU + ct), ub[ct][:, 1:513], ALU.mult, ALU.add, [d, pv, ub[ct]], [uf[ct]])
            K.copy(ub[ct][:, 0:1], ub[ct][:, 512:513], [ub[ct]], [ub[ct]])
        r, k, v = uf[0], uf[1], uf[2]
        K.act(uf[3][0:96, :], uf[3][0:96, :], AF.Tanh, [uf[3]], [uf[3]])
        ps = K.psum()
        K.mm(ps[:, :], lora[0:96, 0, :], uf[3][0:96, :], True, True, [lora, uf[3]], [ps])
        dec = tmp[1]
        K.act(dec[:], ps[:, :], AF.Sigmoid, [ps, pv], [dec], bias=sc(PV_W0))
        K.act(dec[:], dec[:], AF.Exp, [dec], [dec], scale=-float(np.exp(-0.5)))
        K.dma("sp", R["fm_w"].ap()[:, t0:t0 + 512], dec[:], reads=[dec], writes=["fm_w"])
        ps = K.psum()
        K.mm(ps[:, :], lora[0:96, 1, :], uf[4][0:96, :], True, True, [lora, uf[4]], [ps])
        a = tmp[2]
        K.act(a[:], ps[:, :], AF.Sigmoid, [ps, pv], [a], bias=sc(PV_A0))
        K.act(uf[5][:], uf[5][:], AF.Sigmoid, [uf[5]], [uf[5]])
        K.act(uf[6][:], uf[6][:], AF.Sigmoid, [uf[6]], [uf[6]])
        ps = K.psum()
        K.mm(ps[:, :], lora[:, 2, :], uf[5][:], True, False, [lora, uf[5]], [ps])
        K.mm(ps[:, :], lora[:, 3, :], uf[6][:], False, True, [lora, uf[6]], [ps])
        g = tmp[3]
        K.copy(g[:], ps[:, :], [ps], [g], eng="act")
        K.dma("sp", R["fm_g"].ap()[:, t0:t0 + 512], g[:], reads=[g], writes=["fm_g"])
        if l == 0:
            K.dma("sp", R["fm_vfirst"].ap()[:, t0:t0 + 512], v[:], reads=[v], writes=["fm_vfirst"])
        else:
            ps = K.psum()
            K.mm(ps[:, :], lora[0:64, 4, :], uf[7][0:64, :], True, True, [lora, uf[7]], [ps])
            vr = tmp[4]
            K.act(vr[:], ps[:, :], AF.Sigmoid, [ps, pv], [vr], bias=sc(PV_V0))
            vf = tmp[5]
            K.dma("sp", vf[:], R["fm_vfirst"].ap()[:, t0:t0 + 512], reads=["fm_vfirst"], writes=[vf])
            K.tt(vf[:], vf[:], v[:], ALU.subtract, [vf, v], [vf])
            K.tt(vf[:], vf[:], vr[:], ALU.mult, [vf, vr], [vf])
            K.tt(v[:], v[:], vf[:], ALU.add, [v, vf], [v])
        kk = tmp[4]
        K.ts(kk[:], k[:], sc(PV_KK), None, ALU.mult, None, [k, pv], [kk])
        sq = tmp[5]
        K.tt(sq[:], kk[:], kk[:], ALU.mult, [kk], [sq])
        ps = K.psum()
        K.mm(ps[:, :], blk[:, :], sq[:], True, True, [blk, sq], [ps])
        rn = tmp[5]
        K.ts(rn[:], ps[:, :], 1e-24, None, ALU.max, None, [ps], [rn])
        K.act(rn[:], rn[:], AF.Sqrt, [rn], [rn])
        K.op("dve", lambda e: e.reciprocal(out=rn[:], in_=rn[:]), [rn], [rn])
        K.tt(kk[:], kk[:], rn[:], ALU.mult, [kk, rn], [kk])
        nkk = tmp[5]
        K.ts(nkk[:], kk[:], -1.0, None, ALU.mult, None, [kk], [nkk])
        K.dma("sp", R["fm_nkk"].ap()[:, t0:t0 + 512], nkk[:], reads=[nkk], writes=["fm_nkk"])
        t1 = tmp[6]
        K.ts(t1[:], a[:], sc(PV_KA), sc(PV_C1), ALU.mult, ALU.add, [a, pv], [t1])
        kp = tmp[7]
        K.tt(kp[:], k[:], t1[:], ALU.mult, [k, t1], [kp])
        K.dma("sp", R["fm_kp"].ap()[:, t0:t0 + 512], kp[:], reads=[kp], writes=["fm_kp"])
        kka = tmp[6]
        K.tt(kka[:], kk[:], a[:], ALU.mult, [kk, a], [kka])
        for which, (src, dst) in enumerate(((kka, "tm_kka"), (v, "tm_v"))):
            ps = K.psum()
            for i in range(4):
                K.tr(ps[:, i * 128:(i + 1) * 128], src[:, i * 128:(i + 1) * 128], C["identf"][:], [src, C["identf"]], [ps])
            K.copy(tmo[which][:], ps[:, :].rearrange("p (a b) -> p a b", a=4), [ps], [tmo[which]], eng="act")
            K.dma("sp", R[dst].ap()[t0:t0 + 512, :].rearrange("(a p) c -> p a c", p=128), tmo[which][:], reads=[tmo[which]], writes=[dst])
        t2 = tmp[0]
        K.stt(t2[:], r[:], sc(PV_RK), kp[:], ALU.mult, ALU.mult, [r, pv, kp], [t2])
        ps = K.psum()
        K.mm(ps[:, :], blk[:, :], t2[:], True, True, [blk, t2], [ps])
        bon = tmp[0]
        K.tt(bon[:], ps[:, :], v[:], ALU.mult, [ps, v], [bon])
        K.dma("sp", R["fm_bonus"].ap()[:, t0:t0 + 512], bon[:], reads=[bon], writes=["fm_bonus"])
        K.dma("sp", R["fm_r"].ap()[:, t0:t0 + 512], r[:], reads=[r], writes=["fm_r"])
    barrier(K)
    K.pop()

    K.push()
    NH = 2
    Sring = [[K.sb(f"sc_S{h}_{i}", [64, 64]) for i in range(4)] for h in range(NH)]
    kkaB = [[K.sb(f"sc_kkaB{h}_{i}", [64, CS, 64]) for i in range(2)] for h in range(NH)]
    vB = [[K.sb(f"sc_vB{h}_{i}", [64, CS, 64]) for i in range(2)] for h in range(NH)]
    Abuf = [[K.sb(f"sc_A{h}_{i}", [64, CS, 64]) for i in range(2)] for h in range(NH)]
    Dg = [K.sb(f"sc_Dg{h}", [64, CS, 64]) for h in range(NH)]
    fmb = {n: [[K.sb(f"sc_{n}{h}_{i}", [64, 512]) for i in range(2)] for h in range(NH)] for n in ("w", "nkk", "kp", "r", "g", "bonus")}
    yT = [K.sb(f"sc_yT{h}", [64, 512]) for h in range(NH)]
    pt = [K.sb(f"sc_pt{h}_{i}", [64, 512]) for h in range(NH) for i in range(3)]
    pvh = [K.sb(f"sc_pv{h}", [64, NV]) for h in range(NH)]
    ones64 = K.sb("sc_ones64", [64, 64])
    K.memset(ones64[:], 1.0 / 64, [ones64])
    for h in range(NH):
        K.dma("sp", pvh[h][:], K.inputs["pvec"].ap()[l][h * 64:(h + 1) * 64, :], writes=[pvh[h]])
        K.memset(Sring[h][0][:], 0.0, [Sring[h][0]])
    psS = [K.ps[0], K.ps[1]]
    psY = [K.ps[2], K.ps[3]]
    ident64 = C["identf"][0:64, 0:64]
    nchunk = 512 // CS
    for tb in range(NB):
        t0 = tb * 512
        for h in range(NH):
            for n in fmb:
                K.dma("sp", fmb[n][h][tb % 2][:], R["fm_" + n].ap()[h * 64:(h + 1) * 64, t0:t0 + 512], reads=["fm_" + n], writes=[fmb[n][h][tb % 2]])
        for ci in range(nchunk):
            c0 = ci * CS
            gi = tb * nchunk + ci
            for h in range(NH):
                kb, vb, A = kkaB[h][gi % 2], vB[h][gi % 2], Abuf[h][gi % 2]
                K.dma("sp", kb[:], R["tm_kka"].ap()[t0 + c0:t0 + c0 + CS, h * 64:(h + 1) * 64].partition_broadcast(64), reads=["tm_kka"], writes=[kb])
                K.dma("sp", vb[:], R["tm_v"].ap()[t0 + c0:t0 + c0 + CS, h * 64:(h + 1) * 64].partition_broadcast(64), reads=["tm_v"], writes=[vb])
                nk = fmb["nkk"][h][tb % 2]
                w = fmb["w"][h][tb % 2]
                K.tt(A[:], kb[:], nk[:, c0:c0 + CS].unsqueeze(2).to_broadcast([64, CS, 64]), ALU.mult, [kb, nk], [A])
                K.tt(Dg[h][:], ident64.unsqueeze(1).to_broadcast([64, CS, 64]), w[:, c0:c0 + CS].unsqueeze(2).to_broadcast([64, CS, 64]),
                     ALU.mult, [C["identf"], w], [Dg[h]])
                K.tt(A[:], A[:], Dg[h][:], ALU.add, [A, Dg[h]], [A])
            for s in range(CS):
                tg = gi * CS + s
                for h in range(NH):
                    A, vb = Abuf[h][gi % 2], vB[h][gi % 2]
                    Sp, Sn = Sring[h][tg % 4], Sring[h][(tg + 1) % 4]
                    slot = tg % 8
                    pk = f"psS{h}_{slot}"
                    pso = psS[h][0:64, slot * 64:(slot + 1) * 64]
                    K.mm(pso, A[:, s, :], Sp[:], True, True, [A, Sp], [pk])
                    kp = fmb["kp"][h][tb % 2]
                    K.stt(Sn[:], vb[:, s, :], kp[:, c0 + s:c0 + s + 1], pso, ALU.mult, ALU.add, [vb, kp, pk], [Sn])
                    rr = fmb["r"][h][tb % 2]
                    K.mm(psY[h][0:64, c0 + s:c0 + s + 1], Sn[:], rr[:, c0 + s:c0 + s + 1], True, True, [Sn, rr], [psY[h]])
        for h in range(NH):
            y, p0, p1, p2 = yT[h], pt[h * 3], pt[h * 3 + 1], pt[h * 3 + 2]
            K.copy(y[:], psY[h][0:64, :], [psY[h]], [y], eng="act")
            ps = K.psum4()
            K.mm(ps[0:64, :], ones64[:], y[:], True, True, [ones64, y], [ps])
            K.tt(p0[:], y[:], ps[0:64, :], ALU.subtract, [y, ps], [p0])
            K.tt(p1[:], p0[:], p0[:], ALU.mult, [p0], [p1])
            ps = K.psum4()
            K.mm(ps[0:64, :], ones64[:], p1[:], True, True, [ones64, p1], [ps])
            K.ts(p1[:], ps[0:64, :], GN_EPS, None, ALU.add, None, [ps], [p1])
            K.act(p1[:], p1[:], AF.Sqrt, [p1], [p1])
            K.op("dve", lambda e: e.reciprocal(out=p1[:], in_=p1[:]), [p1], [p1])
            K.tt(p0[:], p0[:], p1[:], ALU.mult, [p0, p1], [p0])
            K.ts(p0[:], p0[:], pvh[h][:, PV_LNW:PV_LNW + 1], pvh[h][:, PV_LNB:PV_LNB + 1], ALU.mult, ALU.add, [p0, pvh[h]], [p0])
            bo, g = fmb["bonus"][h][tb % 2], fmb["g"][h][tb % 2]
            K.tt(p0[:], p0[:], bo[:], ALU.add, [p0, bo], [p0])
            K.tt(p2[:], p0[:], g[:], ALU.mult, [p0, g], [p2])
            ysrc_write(K, R["yB"], slice(h * 64, (h + 1) * 64), t0, 512, p2, p2)
    barrier(K)
    K.pop()


def select_chunk(K, dst, Y, C):
    oh = C["oh"]
    cand, acc = C["sel_cand"], C["sel_acc"]
    for kc in range(4):
        for j in range(4):
            K.dma("sp", cand[:, j, :], Y["all"][j].ap()[kc * 128:(kc + 1) * 128, :], reads=[Y["key"] + "_all"], writes=[cand])
        K.ts(acc[:], cand[:, 0, :], oh[:, 0:1], None, ALU.mult, None, [cand, oh], [acc])
        for j in range(1, 4):
            K.stt(acc[:], cand[:, j, :], oh[:, j:j + 1], acc[:], ALU.mult, ALU.add, [cand, oh, acc], [acc])
        K.copy(dst[:, kc, :], acc[:], [acc], [dst])


def merge_phase(K, l, C, S, xres):
    TC, NT = K.TC, K.NT
    hTx = C["hTx"]
    w_in = K.inputs["w_in"].ap()[l]
    K.push()
    wg = [K.sb(f"mg_wg{i}", [128, 16, 512], BF16) for i in range(2)]
    wb = [K.sb(f"mg_wb{i}", [128, 4, 512], BF16) for i in range(2)]
    psc = K.sb("mg_psc", [128, 512])
    macc = K.sb("mg_macc", [128, NT, 512])
    merged = K.sb("mg_merged", [128, NT, D], BF16)
    gt = [K.sb(f"mg_gt{i}", [128, 512]) for i in range(2)]
    tm = [K.sb(f"mg_tm{i}", [128, 512]) for i in range(2)]
    ys = [S["y_rwkvT"], S["y_nsaT"], S["y_convT"], S["y_memT"]]
    n = 0
    for nb in range(4):
        K.dma("sp", psc[:], K.inputs["pool_scale"].ap()[l:l + 1, nb * 512:(nb + 1) * 512].partition_broadcast(128), writes=[psc])
        for i in range(5):
            g_, b_ = wg[n % 2], wb[n % 2]
            n += 1
            load_w(K, g_, w_in, O_GATE + i * D + nb * 512, 512)
            if i < 4:
                K.dma("pool", b_[:], K.inputs["w_branch"].ap()[l, i].rearrange("(kc p) n -> p kc n", p=128)[:, :, nb * 512:(nb + 1) * 512], writes=[b_])
            else:
                K.dma("pool", b_[:, 0, :], K.inputs["pool_w"].ap()[l, nb], writes=[b_])
            for tt in range(NT):
                tsl = slice(tt * 128, (tt + 1) * 128)
                psg = K.psum()
                for kc in range(16):
                    K.mm(psg[:, :], hTx[:, kc, HALO + tt * 128: HALO + (tt + 1) * 128], g_[:, kc, :], kc == 0, kc == 15, [hTx, g_], [psg])
                G = gt[tt % 2]
                K.act(G[:], psg[:, :], AF.Sigmoid, [psg], [G])
                psb = K.psum()
                if i < 4:
                    for kc in range(4):
                        K.mm(psb[:, :], ys[i][:, kc, tsl], b_[:, kc, :], kc == 0, kc == 3, [ys[i], b_], [psb])
                else:
                    K.mm(psb[:, :], S["pooledT"][:, nb, tsl], b_[:, 0, :], True, True, [S["pooledT"], b_], [psb])
                    K.tt(G[:], G[:], psc[:], ALU.mult, [G, psc], [G])
                if i == 0:
                    K.tt(macc[:, tt, :], G[:], psb[:, :], ALU.mult, [G, psb], [macc])
                else:
                    t_ = tm[tt % 2]
                    K.tt(t_[:], G[:], psb[:, :], ALU.mult, [G, psb], [t_])
                    K.tt(macc[:, tt, :], macc[:, tt, :], t_[:], ALU.add, [macc, t_], [macc])
        K.copy(merged[:, :, nb * 512:(nb + 1) * 512], macc[:], [macc], [merged], eng="act")
    mT = hTx
    for tt in range(NT):
        for grp in range(2):
            ps = K.psum()
            psb_ = ps[:].bitcast(BF16)
            for i in range(8):
                kc = grp * 8 + i
                K.tr(psb_[:, i * 128:(i + 1) * 128], merged[:, tt, kc * 128:(kc + 1) * 128], C["identb"][:], [merged, C["identb"]], [ps])
            K.copy(mT[:, grp * 8:(grp + 1) * 8, HALO + tt * 128: HALO + (tt + 1) * 128], psb_.rearrange("p (a b) -> p a b", a=8), [ps], [mT],
                   eng="act" if grp == 0 else "dve")
    xt = [K.sb(f"mg_xt{i}", [128, 512]) for i in range(2)]
    for nb in range(4):
        wo = wg[nb % 2]
        load_w(K, wo, K.inputs["w_out"].ap()[l], nb * 512, 512)
        for tt in range(NT):
            ps = K.psum()
            for kc in range(16):
                K.mm(ps[:, :], mT[:, kc, HALO + tt * 128: HALO + (tt + 1) * 128], wo[:, kc, :], kc == 0, kc == 15, [mT, wo], [ps])
            x_ = xt[tt % 2]
            K.dma("sp", x_[:], xres.ap()[tt * 128:(tt + 1) * 128, nb * 512:(nb + 1) * 512], reads=["xres"], writes=[x_])
            K.tt(x_[:], x_[:], ps[:, :], ALU.add, [x_, ps], [x_])
            K.dma("sp", xres.ap()[tt * 128:(tt + 1) * 128, nb * 512:(nb + 1) * 512], x_[:], reads=[x_], writes=["xres"])
    barrier(K)
    K.pop()


def ffn_phase(K, l, C, xres, xout):
    TC = K.TC
    BL = min(512, TC)
    NBL = TC // BL
    TPB = BL // 128
    moe = (l % 2 == 1)
    K.push()
    h2T = C["hTx"]
    K.push()
    rn_alloc(K, C)
    K.dma("sp", C["gbc"][:], K.inputs["norm_ffn"].ap()[l:l + 1, :].partition_broadcast(128), writes=[C["gbc"]])
    rmsnorm_T(K, xres.ap(), C["gbc"], h2T, HALO, K.NT, C, "ffn")
    barrier(K)
    K.pop()
    NF = (E_FF if moe else D_FF) // 128
    uT = K.sb("ff_uT", [128, NF, BL], BF16)
    w1g = [K.sb(f"ff_w1_{i}", [128, 16, 256], BF16) for i in range(2)]
    w3g = [K.sb(f"ff_w3_{i}", [128, 16, 256], BF16) for i in range(2)]
    w2g = [K.sb(f"ff_w2_{i}", [128, 11, 512], BF16) for i in range(2)]
    sa = [K.sb(f"ff_sa{i}", [128, 512]) for i in range(2)]
    xacc = K.sb("ff_xacc", [128, TPB, D])
    if moe:
        rt = K.sb("ff_rt", [128, 16, 8], BF16)
        K.dma("pool", rt[:], K.inputs["moe_router"].ap()[0].rearrange("(kc p) e -> p kc e", p=128), writes=[rt])
        comb = K.sb("ff_comb", [128, K.NT, 8])
        lg, l2, m1, m2, mk1, mk2 = (K.sb("ff_" + n, [128, 8]) for n in ("lg", "l2", "m1", "m2", "mk1", "mk2"))
        for tt in range(K.NT):
            ps = K.psum()
            for kc in range(16):
                K.mm(ps[:, 0:8], h2T[:, kc, HALO + tt * 128: HALO + (tt + 1) * 128], rt[:, kc, :], kc == 0, kc == 15, [h2T, rt], [ps])
            K.copy(lg[:], ps[:, 0:8], [ps], [lg])
            K.op("dve", lambda e: e.reduce_max(out=m1[:, 0:1], in_=lg[:], axis=AX.X), [lg], [m1])
            K.ts(mk1[:], lg[:], m1[:, 0:1], None, ALU.is_ge, None, [lg, m1], [mk1])
            K.stt(l2[:], mk1[:], -1e30, lg[:], ALU.mult, ALU.add, [mk1, lg], [l2])
            K.op("dve", lambda e: e.reduce_max(out=m2[:, 0:1], in_=l2[:], axis=AX.X), [l2], [m2])
            K.ts(mk2[:], l2[:], m2[:, 0:1], None, ALU.is_ge, None, [l2, m2], [mk2])
            K.tt(m1[:, 1:2], m1[:, 0:1], m2[:, 0:1], ALU.subtract, [m1, m2], [m1])
            K.act(m1[:, 2:3], m1[:, 1:2], AF.Sigmoid, [m1], [m1])
            K.ts(m1[:, 3:4], m1[:, 2:3], -1.0, 1.0, ALU.mult, ALU.add, [m1], [m1])
            K.ts(mk1[:], mk1[:], m1[:, 2:3], None, ALU.mult, None, [mk1, m1], [mk1])
            K.stt(comb[:, tt, :], mk2[:], m1[:, 3:4], mk1[:], ALU.mult, ALU.add, [mk2, m1, mk1], [comb])
    experts = list(range(N_EXP)) if moe else [None]
    nld = [0]
    for bl in range(NBL):
        cb = HALO + bl * BL
        K.dma("sp", xacc[:], xres.ap()[bl * BL:(bl + 1) * BL, :].rearrange("(a p) d -> p a d", p=128), reads=["xres"], writes=[xacc])
        for e in experts:
            if moe:
                W1, W3, W2 = (K.inputs[n].ap()[0, e] for n in ("moe_w1", "moe_w3", "moe_w2"))
            else:
                W1, W3, W2 = (K.inputs[n].ap()[0] for n in ("ffn_w1", "ffn_w3", "ffn_w2"))
            FFW = NF * 128
            for c0 in range(0, FFW, 256):
                nc_ = min(256, FFW - c0)
                a_, b_ = w1g[nld[0] % 2], w3g[nld[0] % 2]
                nld[0] += 1
                load_w(K, a_, W1, c0, nc_)
                load_w(K, b_, W3, c0, nc_)
                for fi in range(nc_ // 128):
                    f = c0 // 128 + fi
                    pa, pb = K.psum_from(0, 4), K.psum_from(0, 4)
                    for kc in range(16):
                        K.mm(pa[:, 0:BL], a_[:, kc, fi * 128:(fi + 1) * 128], h2T[:, kc, cb:cb + BL], kc == 0, kc == 15, [a_, h2T], [pa])
                    for kc in range(16):
                        K.mm(pb[:, 0:BL], b_[:, kc, fi * 128:(fi + 1) * 128], h2T[:, kc, cb:cb + BL], kc == 0, kc == 15, [b_, h2T], [pb])
                    s_ = sa[f % 2]
                    K.act(s_[:, 0:BL], pa[:, 0:BL], AF.Silu, [pa], [s_])
                    K.tt(uT[:, f, :], s_[:, 0:BL], pb[:, 0:BL], ALU.mult, [s_, pb], [uT])
            for nb in range(4):
                pss = [K.ps[4 + i] for i in range(TPB)]
                nfh = (NF + 10) // 11
                for fh in range(nfh):
                    f0 = fh * 11
                    nf = min(11, NF - f0)
                    w2_ = w2g[nld[0] % 2]
                    nld[0] += 1
                    K.dma("pool", w2_[:, 0:nf, :], W2[f0 * 128:(f0 + nf) * 128, nb * 512:(nb + 1) * 512].rearrange("(f p) n -> p f n", p=128), writes=[w2_])
                    for tt in range(TPB):
                        for f in range(nf):
                            K.mm(pss[tt][:, :], uT[:, f0 + f, tt * 128:(tt + 1) * 128], w2_[:, f, :], (f0 + f) == 0, (f0 + f) == NF - 1, [uT, w2_], [pss[tt]])
                for tt in range(TPB):
                    xs = xacc[:, tt, nb * 512:(nb + 1) * 512]
                    if moe:
                        gtt = bl * TPB + tt
                        K.stt(xs, pss[tt][:, :], comb[:, gtt, e:e + 1], xs, ALU.mult, ALU.add, [pss[tt], comb, xacc], [xacc])
                    else:
                        K.tt(xs, xs, pss[tt][:, :], ALU.add, [xacc, pss[tt]], [xacc])
        K.dma("sp", xout.ap()[bl * BL:(bl + 1) * BL, :].rearrange("(a p) d -> p a d", p=128), xacc[:], reads=[xacc], writes=[xout.name])
    barrier(K)
    K.pop()


WN_TM = 704
NWN2 = WN_TM + 140
SEL_N = 16
import os
NSA_STOP = int(os.environ.get('NSA_STOP', '0'))
NSA_SUB = int(os.environ.get('NSA_SUB', '0'))


def nsa_phase(K, l, C, R):
    T, TC = K.T, K.TC
    NB, NTT = T // 512, T // 128
    NS = T // 64
    NCMP = (T - 32) // 16 + 1
    CT = [(c0, min(128, NCMP - c0)) for c0 in range(0, NCMP, 128)]
    VW = 64 + NS + 1
    pv = C["pv"][l]
    sc = lambda i: pv[:, i:i + 1]
    K.push()
    qT = [K.sb(f"ns_q{i}T", [128, T], BF16) for i in range(2)]
    ksT, kwT = K.sb("ns_ksT", [128, T], BF16), K.sb("ns_kwT", [128, T], BF16)
    kcT, vcT = K.sb("ns_kcT", [64, T], BF16), K.sb("ns_vcT", [64, T], BF16)
    Vs, Vw = K.sb("ns_Vs", [128, NTT, 66], BF16), K.sb("ns_Vw", [128, NTT, 66], BF16)
    gts = K.sb("ns_g", [128, NTT, 12])
    Oacc = K.sb("ns_O", [128, NTT, 128])
    blk = K.sb("ns_blk", [128, 128])
    K.dma("sp", blk[:], K.inputs["blk64"].ap(), writes=[blk])
    K.memset(Vs[:, :, 64:65], 1.0, [Vs])
    K.memset(Vw[:, :, 64:65], 1.0, [Vw])
    imp = K.sb("ns_imp", [128, NTT, NS])
    selT = K.sb("ns_selT", [64, T], BF16)
    ebuf = [K.sb(f"ns_e{i}", [128, 512], BF16) for i in range(4)]
    rv = [K.sb(f"ns_rv{i}", [128, 2]) for i in range(2)]
    kcmpT = K.sb("ns_kcmpT", [128, 256], BF16)
    Vc = K.sb("ns_Vc", [128, len(CT), VW + 1], BF16)
    K.push()
    wn = K.sb("ns_wn", [128, 16, NWN2], BF16)
    wsrc = K.inputs["wn"].ap()[l].rearrange("(kc p) n -> p kc n", p=128)
    K.dma("pool", wn[:, :, 0:512], wsrc[:, :, 0:512], writes=[wn])
    K.dma("pool", wn[:, :, 512:NWN2], wsrc[:, :, 512:NWN2], writes=[wn])
    B1 = 256
    hTb = [K.sb("ns_hT0", [128, 16, B1], BF16)] * 2
    pf = K.sb("ns_pf", [128, B1])
    for tb in range(T // B1):
        t0 = tb * B1
        hb = hTb[tb % 2]
        load_hall_block(K, hb, C, t0, TC, B1)
        specs = [(0, 128, qT[0], PV_NSAG + 0), (128, 128, qT[1], PV_NSAG + 0), (384, 128, ksT, PV_NSAG + 2), (512, 128, kwT, PV_NSAG + 3),
                 (256, 64, kcT, None), (640, 64, vcT, None)]
        if NSA_SUB == 1:
            continue
        for (c0, nc_, dst, gi) in specs:
            ps = K.psum()
            for kc in range(16):
                K.mm(ps[0:nc_, 0:B1], wn[:, kc, c0:c0 + nc_], hb[:, kc, :], kc == 0, kc == 15, [wn, hb], [ps])
            if gi is None:
                K.copy(dst[:, t0:t0 + B1], ps[0:nc_, 0:B1], [ps], [dst], eng="act")
            else:
                K.copy(pf[:], ps[:, 0:B1], [ps], [pf], eng="act")
                sq = C["fm_sq"]
                K.tt(sq[:, 0:B1], pf[:], pf[:], ALU.mult, [pf], [sq])
                ps2 = K.psum()
                K.mm(ps2[:, 0:B1], blk[:, :], sq[:, 0:B1], True, True, [blk, sq], [ps2])
                rs = C["fm_rs"]
                K.ts(rs[:, 0:B1], ps2[:, 0:B1], 1.0 / 64, EPS, ALU.mult, ALU.add, [ps2], [rs])
                K.act(rs[:, 0:B1], rs[:, 0:B1], AF.Sqrt, [rs], [rs])
                K.op("dve", lambda e: e.reciprocal(out=rs[:, 0:B1], in_=rs[:, 0:B1]), [rs], [rs])
                K.stt(dst[:, t0:t0 + B1], pf[:], sc(gi), rs[:, 0:B1], ALU.mult, ALU.mult, [pf, pv, rs], [dst])
        if NSA_SUB == 2:
            continue
        for ti in range(B1 // 128):
            gt_ = tb * (B1 // 128) + ti
            ps = K.psum()
            for kc in range(16):
                K.mm(ps[:, 0:140], hb[:, kc, ti * 128:(ti + 1) * 128], wn[:, kc, WN_TM:WN_TM + 140], kc == 0, kc == 15, [wn, hb], [ps])
            K.copy(Vs[:, gt_, 0:64], ps[:, 0:64], [ps], [Vs], eng="act")
            K.copy(Vw[:, gt_, 0:64], ps[:, 64:128], [ps], [Vw])
            K.act(gts[:, gt_, :], ps[:, 128:140], AF.Sigmoid, [ps], [gts])
    barrier(K)
    K.pop()
    K.push()
    W1 = K.sb("ns_W1", [64, 32, 128], BF16)
    w2d = K.sb("ns_w2", [128, 128], BF16)
    posT = K.sb("ns_posT", [64, 32], BF16)
    hidT = K.sb("ns_hidT", [128, 256], BF16)
    hx = [K.sb(f"ns_hx{i}", [128, 256]) for i in range(3)]
    cb = K.sb("ns_cb", [128, 2])
    kg = K.sb("ns_kg", [128, 64])
    K.dma("sp", kg[:], K.inputs["nsa_qk_gain"].ap()[l, 1:2, :].partition_broadcast(128), writes=[kg])
    ktm = K.sb("ns_ktm", [128, 128])
    kss = K.sb("ns_kss", [128, 2])
    K.memset(Vc[:, :, VW - 1:VW], 1.0, [Vc])
    for ci, (c0, ncc) in enumerate(CT):
        K.dma("pool", Vc[0:ncc, ci, 64:64 + NS], K.inputs["ovl"].ap()[c0:c0 + ncc, 0:NS], writes=[Vc])
    for i in range(2):
        src = kcT if i == 0 else vcT
        K.dma("pool", W1[:], K.inputs["nsa_cmp_w1"].ap()[l, i].rearrange("(l d) j -> d l j", d=64), writes=[W1])
        K.dma("pool", posT[:], K.inputs["cmp_posT"].ap()[l, i], writes=[posT])
        K.dma("pool", w2d[:, 0:64], K.inputs["nsa_cmp_w2"].ap()[l, i], writes=[w2d])
        K.dma("pool", w2d[:, 64:128], K.inputs["nsa_cmp_w2"].ap()[l, i], writes=[w2d])
        ps = K.psum()
        for ll in range(32):
            K.mm(ps[:, 0:1], W1[:, ll, :], posT[:, ll:ll + 1], ll == 0, ll == 31, [W1, posT], [ps])
        K.tt(cb[:, i:i + 1], ps[:, 0:1], sc(PV_CB1 + i), ALU.add, [ps, pv], [cb])
        ps = K.psum()
        for ll in range(32):
            K.mm(ps[:, 0:NCMP], W1[:, ll, :], src[:, ll: ll + 16 * (NCMP - 1) + 1: 16], ll == 0, ll == 31, [W1, src], [ps])
        x_, x2, x3 = hx
        n_ = NCMP
        K.ts(x_[:, 0:n_], ps[:, 0:n_], cb[:, i:i + 1], None, ALU.add, None, [ps, cb], [x_])
        K.tt(x2[:, 0:n_], x_[:, 0:n_], x_[:, 0:n_], ALU.mult, [x_], [x2])
        K.ts(x2[:, 0:n_], x2[:, 0:n_], 0.044715, 1.0, ALU.mult, ALU.add, [x2], [x2])
        K.tt(x2[:, 0:n_], x2[:, 0:n_], x_[:, 0:n_], ALU.mult, [x2, x_], [x2])
        K.act(x3[:, 0:n_], x2[:, 0:n_], AF.Sigmoid, [x2], [x3], scale=1.5957691216057308)
        K.tt(hidT[:, 0:n_], x_[:, 0:n_], x3[:, 0:n_], ALU.mult, [x_, x3], [hidT])
        for ci, (c0, ncc) in enumerate(CT):
            ps = K.psum()
            K.mm(ps[0:ncc, 0:128], hidT[:, c0:c0 + ncc], w2d[:, :], True, True, [hidT, w2d], [ps])
            if i == 1:
                K.copy(Vc[0:ncc, ci, 0:64], ps[0:ncc, 0:64], [ps], [Vc], eng="act")
            else:
                K.copy(ktm[0:ncc, :], ps[0:ncc, 0:128], [ps], [ktm], eng="act")
                sq = C["fm_sq"]
                K.tt(sq[0:ncc, 0:64], ktm[0:ncc, 0:64], ktm[0:ncc, 0:64], ALU.mult, [ktm], [sq])
                K.op("dve", lambda e: e.reduce_sum(out=kss[0:ncc, 0:1], in_=sq[0:ncc, 0:64], axis=AX.X), [sq], [kss])
                K.ts(kss[0:ncc, 1:2], kss[0:ncc, 0:1], 1.0 / 64, EPS, ALU.mult, ALU.add, [kss], [kss])
                K.act(kss[0:ncc, 1:2], kss[0:ncc, 1:2], AF.Sqrt, [kss], [kss])
                K.op("dve", lambda e: e.reciprocal(out=kss[0:ncc, 1:2], in_=kss[0:ncc, 1:2]), [kss], [kss])
                for hf in range(2):
                    K.stt(ktm[0:ncc, hf * 64:(hf + 1) * 64], ktm[0:ncc, hf * 64:(hf + 1) * 64], kss[0:ncc, 1:2], kg[0:ncc, :], ALU.mult, ALU.mult,
                          [ktm, kss, kg], [ktm])
                ps2 = K.psum()
                K.tr(ps2[:, 0:ncc], ktm[0:ncc, :], C["identf"][0:ncc, 0:ncc], [ktm, C["identf"]], [ps2])
                K.copy(kcmpT[:, c0:c0 + ncc], ps2[:, 0:ncc], [ps2], [kcmpT])
    barrier(K)
    K.pop()
    K.push()
    maskc = K.sb("ns_maskc", [128, len(CT), T], BF16)
    for ci, (c0, ncc) in enumerate(CT):
        K.dma("pool", maskc[0:ncc, ci, :], K.inputs["maskc"].ap()[c0:c0 + ncc, 0:T], writes=[maskc])
    nrv = [0]

    def finish_acc(acc_ap, acckey, gtile, gate_idx):
        r_ = rv[nrv[0] % 2]
        nrv[0] += 1
        K.ts(r_[:, 0:1], acc_ap[:, 64:65], 1e-30, None, ALU.max, None, [acckey], [r_])
        K.op("dve", lambda e: e.reciprocal(out=r_[:, 0:1], in_=r_[:, 0:1]), [r_], [r_])
        K.tt(r_[:, 1:2], r_[:, 0:1], gts[:, gtile, gate_idx:gate_idx + 1], ALU.mult, [r_, gts], [r_])
        return r_

    first_o = {}
    for hd in range(4):
        qt, half = qT[hd // 2], slice((hd % 2) * 64, (hd % 2) * 64 + 64)
        for tb in range(NB):
            t0 = tb * 512
            for ci, (c0, ncc) in enumerate(CT):
                ps = K.psum_from(0, 7)
                K.mm(ps[0:ncc, :], kcmpT[half, c0:c0 + ncc], qt[half, t0:t0 + 512], True, True, [kcmpT, qt], [ps])
                e_ = ebuf[ci]
                K.act(e_[0:ncc, :], ps[0:ncc, :], AF.Exp, [ps], [e_], scale=0.125)
                K.tt(e_[0:ncc, :], e_[0:ncc, :], maskc[0:ncc, ci, t0:t0 + 512], ALU.mult, [e_, maskc], [e_])
            for ti in range(4):
                gt_ = tb * 4 + ti
                ps = K.psum_from(0, 7)
                for ci, (c0, ncc) in enumerate(CT):
                    K.mm(ps[:, 0:VW], ebuf[ci][0:ncc, ti * 128:(ti + 1) * 128], Vc[0:ncc, ci, 0:VW], ci == 0, ci == len(CT) - 1, [ebuf[ci], Vc], [ps])
                r_ = rv[nrv[0] % 2]
                nrv[0] += 1
                K.ts(r_[:, 0:1], ps[:, VW - 1:VW], 1e-30, None, ALU.max, None, [ps], [r_])
                K.op("dve", lambda e: e.reciprocal(out=r_[:, 0:1], in_=r_[:, 0:1]), [r_], [r_])
                if hd == 0:
                    K.ts(imp[:, gt_, :], ps[:, 64:64 + NS], r_[:, 0:1], None, ALU.mult, None, [ps, r_], [imp])
                else:
                    K.stt(imp[:, gt_, :], ps[:, 64:64 + NS], r_[:, 0:1], imp[:, gt_, :], ALU.mult, ALU.add, [ps, r_, imp], [imp])
                if hd < 2:
                    K.tt(r_[:, 1:2], r_[:, 0:1], gts[:, gt_, hd * 3:hd * 3 + 1], ALU.mult, [r_, gts], [r_])
                    K.ts(Oacc[:, gt_, hd * 64:(hd + 1) * 64], ps[:, 0:64], r_[:, 1:2], None, ALU.mult, None, [ps, r_], [Oacc])
    barrier(K)
    K.pop()
    K.push()
    keep, cadd = K.sb("ns_keep", [128, NS]), K.sb("ns_cadd", [128, NS])
    cmp3 = K.sb("ns_cmp3", [128, NS, NS])
    cnt, sel, ok = K.sb("ns_cnt", [128, NS]), K.sb("ns_sel", [128, NS]), K.sb("ns_ok", [128, NS])
    for gt_ in range(NTT):
        K.dma("sp", keep[:], K.inputs["tk_keep"].ap()[gt_ * 128:(gt_ + 1) * 128, 0:NS], writes=[keep])
        K.dma("sp", cadd[:], K.inputs["tk_cadd"].ap()[gt_ * 128:(gt_ + 1) * 128, 0:NS], writes=[cadd])
        im = imp[:, gt_, :]
        K.tt(im, im, keep[:], ALU.mult, [imp, keep], [imp])
        K.tt(im, im, cadd[:], ALU.add, [imp, cadd], [imp])
        K.tt(cmp3[:], im.unsqueeze(1).to_broadcast([128, NS, NS]), im.unsqueeze(2).to_broadcast([128, NS, NS]), ALU.is_gt, [imp], [cmp3])
        K.op("dve", lambda e: e.reduce_sum(out=cnt[:], in_=cmp3[:], axis=AX.X), [cmp3], [cnt])
        K.ts(sel[:], cnt[:], float(SEL_N) - 0.5, None, ALU.is_lt, None, [cnt], [sel])
        K.ts(ok[:], im, -1e8, None, ALU.is_gt, None, [imp], [ok])
        K.tt(sel[:], sel[:], ok[:], ALU.mult, [sel, ok], [sel])
        ps = K.psum_from(0, 7)
        K.tr(ps[0:NS, 0:128], sel[:], C["identf"][:], [sel, C["identf"]], [ps])
        K.copy(selT[0:NS, gt_ * 128:(gt_ + 1) * 128], ps[0:NS, 0:128], [ps], [selT], eng="act")
    barrier(K)
    K.pop()
    K.push()
    E2 = K.sb("ns_E2", [64, NTT, 128], BF16)
    K.dma("pool", E2[0:NS], K.inputs["e2"].ap()[0:NS, 0:NTT, :], writes=[E2])
    dmask = K.sb("ns_dmask", [128, 5, 512], BF16)
    K.dma("pool", dmask[:], K.inputs["dmask"].ap().rearrange("a p t -> p a t"), writes=[dmask])
    accb = K.ps[7]
    for hd in range(2):
        half = slice(hd * 64, hd * 64 + 64)
        for tb in range(NB):
            t0 = tb * 512
            njt = 4 * tb + 4
            for jt in range(njt):
                ps = K.psum_from(0, 4)
                K.mm(ps[:, :], ksT[half, jt * 128:(jt + 1) * 128], qT[0][half, t0:t0 + 512], True, True, [ksT, qT[0]], [ps])
                e_ = ebuf[jt % 2]
                K.act(e_[:, :], ps[:, :], AF.Exp, [ps], [e_], scale=0.125)
                pm = K.psum_from(0, 4)
                K.mm(pm[:, :], E2[0:NS, jt, :], selT[0:NS, t0:t0 + 512], True, True, [E2, selT], [pm])
                em = ebuf[2 + jt % 2]
                K.tt(em[:, :], e_[:, :], pm[:, :], ALU.mult, [e_, pm], [em])
                dd = jt - 4 * tb
                if dd >= 0:
                    K.tt(em[:, :], em[:, :], dmask[:, dd, :], ALU.mult, [em, dmask], [em])
                for ti in range(max(dd, 0), 4):
                    K.mm(K.ps[4 + ti][:, 0:65], em[:, ti * 128:(ti + 1) * 128], Vs[:, jt, 0:65], jt == 0, jt == 4 * tb + ti, [em, Vs], [K.ps[4 + ti]])
            for ti in range(4):
                gt_ = tb * 4 + ti
                a_ = K.ps[4 + ti][:, 0:65]
                r_ = finish_acc(a_, K.ps[4 + ti].name, gt_, hd * 3 + 1)
                K.stt(Oacc[:, gt_, hd * 64:(hd + 1) * 64], a_[:, 0:64], r_[:, 1:2], Oacc[:, gt_, hd * 64:(hd + 1) * 64], ALU.mult, ALU.add,
                      [K.ps[4 + ti], r_, Oacc], [Oacc])
    for hd in range(2):
        half = slice(hd * 64, hd * 64 + 64)
        for gt_ in range(NTT):
            jts = list(range(max(0, gt_ - 4), gt_ + 1))
            for jt in jts:
                ps = K.psum_from(0, 7)
                K.mm(ps[:, 0:128], kwT[half, jt * 128:(jt + 1) * 128], qT[0][half, gt_ * 128:(gt_ + 1) * 128], True, True, [kwT, qT[0]], [ps])
                e_ = ebuf[jt % 4]
                K.act(e_[:, 0:128], ps[:, 0:128], AF.Exp, [ps], [e_], scale=0.125)
                if jt == gt_:
                    K.tt(e_[:, 0:128], e_[:, 0:128], dmask[:, 0, 0:128], ALU.mult, [e_, dmask], [e_])
                elif jt == gt_ - 4:
                    K.tt(e_[:, 0:128], e_[:, 0:128], dmask[:, 4, 0:128], ALU.mult, [e_, dmask], [e_])
                K.mm(accb[:, 0:65], e_[:, 0:128], Vw[:, jt, 0:65], jt == jts[0], jt == jts[-1], [e_, Vw], ["ns_accb"])
            a_ = accb[:, 0:65]
            r_ = finish_acc(a_, "ns_accb", gt_, hd * 3 + 2)
            K.stt(Oacc[:, gt_, hd * 64:(hd + 1) * 64], a_[:, 0:64], r_[:, 1:2], Oacc[:, gt_, hd * 64:(hd + 1) * 64], ALU.mult, ALU.add,
                  ["ns_accb", r_, Oacc], [Oacc])
    ot = [K.sb(f"ns_ot{i}", [128, 512]) for i in range(2)]
    for tb in range(NB):
        ps = K.psum_from(0, 7)
        for ti in range(4):
            K.tr(ps[:, ti * 128:(ti + 1) * 128], Oacc[:, tb * 4 + ti, :], C["identf"][:], [Oacc, C["identf"]], [ps])
        o_ = ot[tb % 2]
        K.copy(o_[:], ps[:, :], [ps], [o_], eng="act")
        ysrc_write(K, R["yN"], slice(0, 128), tb * 512, 512, o_, o_)
    barrier(K)
    K.pop()
    K.pop()


INPUT_SHAPES = lambda T: {
    "x": [T // 4, D], "mem": [MEM_LEN, D], "w_in": [2, D, N_IN], "norm_mix": [2, D], "norm_ffn": [2, D], "norm_mem": [2, D],
    "mem_wkv": [2, D, 1024], "pool_w": [2, 4, 128, 512], "pool_scale": [2, D], "w_branch": [2, 4, 512, D], "w_out": [2, D, D],
    "ffn_w1": [1, D, D_FF], "ffn_w3": [1, D, D_FF], "ffn_w2": [1, D_FF, D], "moe_router": [1, D, 8],
    "moe_w1": [1, 8, D, E_FF], "moe_w3": [1, 8, D, E_FF], "moe_w2": [1, 8, E_FF, D],
    "nsa_cmp_w1": [2, 2, 2048, 128], "nsa_cmp_w2": [2, 2, 128, 64], "cmp_posT": [2, 2, 64, 32], "nsa_qk_gain": [2, 4, 64],
    "ident": [128, 128], "blk64": [128, 128], "ovl": [256, 64], "maskc": [256, T], "tk_keep": [T, 64], "tk_cadd": [T, 64],
    "e2": [64, 32, 128], "dmask": [5, 128, 512], "onehot": [128, 8], "invcnt": [4, T // 4], "pvec": [2, 128, NV],
    "wr": [2, D, 1024], "wn": [2, D, NWN], "lora": [2, 128, 5, 128],
}


def build(T, dbg=False, nlayers=DEPTH):
    K = KB(T)
    TC = K.TC
    out = K.dout("out", [TC, D])
    C = setup_common(K)
    xres = K.dscr("xres", [TC, D])
    K.dma("sp", xres.ap(), K.inputs["x"].ap(), writes=["xres"])
    C["hTx"] = K.sb("hTx", [128, 16, HALO + TC], BF16)
    alloc_gather(K, C)
    R = alloc_scratch(K)
    dbg_outs = []
    barrier(K)
    for l in range(nlayers):
        K.push()
        S = {n: K.sb(n, [128, 4, TC], BF16) for n in ("y_convT", "pooledT", "y_memT")}
        K.push()
        C["halo_cand"] = K.sb("halo_cand", [128, 4, 16, HALO], BF16)
        phase_norm_gather(K, l, C, xres)
        barrier(K)
        K.pop()
        local_mixers(K, l, C, S)
        rwkv_phase(K, l, C, R)
        ygather(K, R["yB"])
        nsa_phase(K, l, C, R)
        ygather(K, R["yN"])
        S["y_rwkvT"] = K.sb("y_rwkvT", [128, 4, TC], BF16)
        S["y_nsaT"] = K.sb("y_nsaT", [128, 4, TC], BF16)
        K.push()
        C["sel_cand"] = K.sb("sel_cand", [128, 4, TC])
        C["sel_acc"] = K.sb("sel_acc", [128, TC])
        select_chunk(K, S["y_rwkvT"], R["yB"], C)
        select_chunk(K, S["y_nsaT"], R["yN"], C)
        barrier(K)
        K.pop()
        if dbg and l == 0:
            for nm, Y in (("d_yN", R["yN"]), ("d_yB", R["yB"])):
                o = K.dout(nm, [512, T])
                for j in range(4):
                    K.dma("sp", o.ap()[:, j * TC:(j + 1) * TC], Y["all"][j].ap(), reads=[Y["key"] + "_all"], writes=[nm])
                dbg_outs.append(nm)
        merge_phase(K, l, C, S, xres)
        barrier(K)
        K.pop()
        if dbg and l == 0:
            o = K.dout("d_xmix", [TC, D])
            K.dma("sp", o.ap(), xres.ap(), reads=["xres"], writes=["d_xmix"])
            dbg_outs.append("d_xmix")
        last = (l == nlayers - 1)
        ffn_phase(K, l, C, xres, out if last else xres)
        if dbg and l == 0 and not last:
            o = K.dout("d_xout0", [TC, D])
            K.dma("sp", o.ap(), xres.ap(), reads=["xres"], writes=["d_xout0"])
            dbg_outs.append("d_xout0")
    K.fw.finish(["out"] + dbg_outs)
    return K


_CACHE = {}


def kernel(**inputs):
    T = int(np.asarray(inputs["x"]).shape[1])
    if T not in _CACHE:
        _CACHE[T] = build(T)
    K = _CACHE[T]
    maps = host_prep(inputs, T)
    maps = [{k: m[k] for k in K.inputs} for m in maps]
    res = run_bass_kernel_spmd(K.nc, maps, core_ids=list(range(NCORES)))
    TC = T // 4
    outp = np.zeros((2, T, D), np.float32)
    for c in range(NCORES):
        outp[c // 4, (c % 4) * TC:(c % 4 + 1) * TC] = res.results[c]["out"]
    return outp
```

```python
import numpy as np
import concourse.bass as bass
import concourse.mybir as mybir
from concourse.bass_utils import run_bass_kernel_spmd

F32 = mybir.dt.float32
BF16 = mybir.dt.bfloat16
ALU = mybir.AluOpType
AF = mybir.ActivationFunctionType
AX = mybir.AxisListType

D = 2048
NCORES = 8
MEM_LEN = 256
DEPTH = 2
D_FF = 5632
E_FF = 2816
N_EXP = 8
EPS = 1e-6
O_RWKV, O_NSA, O_CONV, O_POOL, O_MEM, O_GATE = 0, 1984, 3288, 4824, 5336, 5848
N_IN = 16088
HALO = 16


class FW:
    def __init__(self, nc, n_dma_sems=20):
        self.nc = nc
        self.eng = {"pe": nc.tensor, "act": nc.scalar, "dve": nc.vector, "pool": nc.gpsimd, "sp": nc.sync}
        self.sem = {}
        self.cnt = {}
        for e in self.eng:
            self.sem[e] = nc.semaphore("s_" + e).__enter__()
            self.cnt[e] = 0
        self.dsem = {}
        for q in ("sp", "pool", "act"):
            lst = [[nc.semaphore(f"d_{q}{i}").__enter__(), 0] for i in range(n_dma_sems)]
            self.dsem[q] = [lst, 0]
        self.ccsem = nc.semaphore("ccsem").__enter__()
        self.cccnt = 0
        self.seen = {e: {} for e in self.eng}
        self.lastw = {}
        self.readers = {}
        self.ninstr = 0
        self.excl = {"ns_accb"}

    @staticmethod
    def _k(x):
        return x if isinstance(x, str) else x.name

    def _wait(self, e, tok):
        if tok is None:
            return
        sem, val, src = tok
        if src == "pe" and e == "pe":
            return
        k = id(sem)
        if self.seen[e].get(k, 0) >= val:
            return
        self.eng[e].wait_ge(sem, val)
        self.seen[e][k] = val

    def _deps(self, e, reads, writes):
        for k in reads:
            self._wait(e, self.lastw.get(k))
        for k in writes:
            self._wait(e, self.lastw.get(k))
            for t in self.readers.get(k, ()):
                self._wait(e, t)

    def _record(self, tok, reads, writes):
        for k in reads:
            self.readers.setdefault(k, []).append(tok)
        for k in writes:
            self.lastw[k] = tok
            self.readers[k] = []
        self.ninstr += 1

    def op(self, e, fn, reads=(), writes=()):
        reads = [self._k(x) for x in reads]
        writes = [self._k(x) for x in writes]
        for k in reads:
            if (k.startswith("psb") or k in self.excl) and k not in writes:
                writes.append(k)
        self._deps(e, reads, writes)
        ins = fn(self.eng[e])
        self.cnt[e] += 1
        ins.then_inc(self.sem[e], 1)
        tok = (self.sem[e], self.cnt[e], e)
        self._record(tok, reads, writes)
        return tok

    def dma(self, q, out, in_, reads=(), writes=(), **kw):
        reads = [self._k(x) for x in reads]
        writes = [self._k(x) for x in writes]
        lst, idx = self.dsem[q]
        ent = lst[idx % len(lst)]
        self.dsem[q][1] += 1
        sem, tgt = ent
        if tgt > 0:
            self._wait(q, (sem, tgt, "dma"))
        self._deps(q, reads, writes)
        self.eng[q].dma_start(out=out, in_=in_, **kw).then_inc(sem, 16)
        ent[1] = tgt + 16
        tok = (sem, tgt + 16, "dma")
        self._record(tok, reads, writes)
        return tok

    def allgather(self, src, dst, groups, reads=(), writes=()):
        reads = [self._k(x) for x in reads]
        writes = [self._k(x) for x in writes]
        self._deps("pool", reads, writes)
        self.nc.gpsimd.collective_compute("AllGather", ALU.bypass, replica_groups=groups,
                                          ins=[src.ap().opt()], outs=[dst.ap().opt()]).then_inc(self.ccsem)
        self.cccnt += 1
        tok = (self.ccsem, self.cccnt, "cc")
        self._record(tok, reads, writes)
        return tok

    def finish(self, keys):
        for k in keys:
            self._wait("sp", self.lastw.get(k))


class LazyInputs(dict):
    def __init__(self, kb):
        super().__init__()
        self.kb = kb

    def __missing__(self, name):
        shp = INPUT_SHAPES(self.kb.T)[name]
        t = self.kb.nc.dram_tensor(name, list(shp), F32, kind="ExternalInput")
        self[name] = t
        return t


class KB:
    def __init__(self, T):
        self.T = T
        self.TC = T // 4
        self.NT = self.TC // 128
        self.nc = bass.Bass("TRN2", target_bir_lowering=False)
        self.fw = FW(self.nc)
        self.inputs = LazyInputs(self)
        self.outputs = {}
        self._uid = 0
        self._psn = 0
        self.ps = [self.nc.psum_tensor(f"psb{i}", [128, 512], F32).__enter__() for i in range(8)]
        self.scopes = []

    def din(self, name, shape, dtype=F32):
        t = self.nc.dram_tensor(name, list(shape), dtype, kind="ExternalInput")
        self.inputs[name] = t
        return t

    def dout(self, name, shape, dtype=F32):
        t = self.nc.dram_tensor(name, list(shape), dtype, kind="ExternalOutput")
        self.outputs[name] = t
        return t

    def dscr(self, name, shape, dtype=F32):
        return self.nc.dram_tensor(name, list(shape), dtype)

    def sb(self, name, shape, dtype=F32):
        self._uid += 1
        g = self.nc.sbuf_tensor(f"{name}_u{self._uid}", list(shape), dtype)
        t = g.__enter__()
        if self.scopes:
            self.scopes[-1].append(g)
        return t

    def push(self):
        self.scopes.append([])

    def pop(self):
        for g in reversed(self.scopes.pop()):
            g.__exit__(None, None, None)

    def psum(self):
        p = self.ps[self._psn % 8]
        self._psn += 1
        return p

    def psum_from(self, lo, n):
        p = self.ps[lo + self._psn % n]
        self._psn += 1
        return p

    def psum4(self):
        p = self.ps[4 + self._psn % 4]
        self._psn += 1
        return p

    def op(self, e, fn, reads=(), writes=()):
        return self.fw.op(e, fn, reads, writes)

    def dma(self, q, out, in_, reads=(), writes=(), **kw):
        return self.fw.dma(q, out, in_, reads, writes, **kw)

    def mm(self, out, lhsT, rhs, start, stop, reads, writes):
        return self.fw.op("pe", lambda e: e.matmul(out, lhsT, rhs, start=start, stop=stop), reads, writes)

    def tr(self, out, in_, ident, reads, writes):
        return self.fw.op("pe", lambda e: e.transpose(out, in_, ident), reads, writes)

    def act(self, out, in_, func, reads, writes, bias=None, scale=None, accum_out=None):
        kw = {}
        if bias is not None:
            kw["bias"] = bias
        if scale is not None:
            kw["scale"] = scale
        if accum_out is not None:
            kw["accum_out"] = accum_out
        return self.fw.op("act", lambda e: e.activation(out=out, in_=in_, func=func, **kw), reads, writes)

    def tt(self, out, in0, in1, op, reads, writes, eng="dve"):
        return self.fw.op(eng, lambda e: e.tensor_tensor(out=out, in0=in0, in1=in1, op=op), reads, writes)

    def ts(self, out, in0, s1, s2, op0, op1, reads, writes, eng="dve"):
        if s2 is None:
            s2, op1 = 0.0, ALU.add
        return self.fw.op(eng, lambda e: e.tensor_scalar(out=out, in0=in0, scalar1=s1, scalar2=s2, op0=op0, op1=op1), reads, writes)

    def stt(self, out, in0, scalar, in1, op0, op1, reads, writes, eng="dve"):
        return self.fw.op(eng, lambda e: e.scalar_tensor_tensor(out=out, in0=in0, scalar=scalar, in1=in1, op0=op0, op1=op1), reads, writes)

    def copy(self, out, in_, reads, writes, eng="dve"):
        if eng == "act":
            return self.fw.op("act", lambda e: e.copy(out=out, in_=in_), reads, writes)
        return self.fw.op(eng, lambda e: e.tensor_copy(out=out, in_=in_), reads, writes)

    def memset(self, ap, val, writes, eng="dve"):
        return self.fw.op(eng, lambda e: e.memset(ap, val), (), writes)


def bcast_rows(dram_ap_1d_row, nparts):
    return dram_ap_1d_row.partition_broadcast(nparts)


def barrier(K):
    fw = K.fw
    toks = [(fw.sem[e], fw.cnt[e], e) for e in fw.eng if fw.cnt[e] > 0]
    for q in fw.dsem:
        for sem, tgt in fw.dsem[q][0]:
            if tgt > 0:
                toks.append((sem, tgt, "dma"))
    if fw.cccnt:
        toks.append((fw.ccsem, fw.cccnt, "cc"))
    for e in fw.eng:
        for t in toks:
            if t[2] == e:
                continue
            fw._wait(e, t)


def rmsnorm_T(K, x_rows, gbc, hT, col0, ntiles, C, tag):
    for tt in range(ntiles):
        b = tt % 2
        xt, junk, ss, hb = C["xt"][b], C["junk"][b], C["ss"][b], C["hb"][b]
        K.dma("sp", xt[:], x_rows[tt * 128:(tt + 1) * 128, :], writes=[xt])
        K.memset(ss[:], 0.0, [ss])
        K.act(junk[:], xt[:], AF.Square, [xt], [junk, ss], accum_out=ss[:, 0:1])
        K.ts(ss[:, 1:2], ss[:, 0:1], 1.0 / D, EPS, ALU.mult, ALU.add, [ss], [ss])
        K.act(ss[:, 1:2], ss[:, 1:2], AF.Sqrt, [ss], [ss])
        K.op("dve", lambda e: e.reciprocal(out=ss[:, 1:2], in_=ss[:, 1:2]), [ss], [ss])
        K.stt(hb[:], xt[:], ss[:, 1:2], gbc[:], ALU.mult, ALU.mult, [xt, ss, gbc], [hb])
        for grp in range(2):
            ps = K.psum()
            psb = ps[:].bitcast(BF16)
            for i in range(8):
                kc = grp * 8 + i
                K.tr(psb[:, i * 128:(i + 1) * 128], hb[:, kc * 128:(kc + 1) * 128], C["identb"][:], [hb, C["identb"]], [ps])
            K.copy(hT[:, grp * 8:(grp + 1) * 8, col0 + tt * 128: col0 + (tt + 1) * 128],
                   psb.rearrange("p (a b) -> p a b", a=8), [ps], [hT], eng="act" if grp == 0 else "dve")


def load_w(K, wt, W, c0, ncols, kchunks=16, key=None):
    src = W.rearrange("(kc p) n -> p kc n", p=128)[:, :, c0:c0 + ncols]
    K.dma("pool", wt[:, 0:kchunks, 0:ncols], src, writes=[key or wt])


def fm_colsum_norm(K, out_bf, outkey, src_f32, tagkey, n, nparts, ones_f, gain_col, inv_n, C):
    sq = C["fm_sq"]
    K.tt(sq[0:nparts, 0:n], src_f32, src_f32, ALU.mult, [tagkey], [sq])
    ps = K.psum()
    K.mm(ps[0:nparts, 0:n], ones_f[0:nparts, 0:nparts], sq[0:nparts, 0:n], True, True, [sq, ones_f], [ps])
    rs = C["fm_rs"]
    K.ts(rs[0:nparts, 0:n], ps[0:nparts, 0:n], inv_n, EPS, ALU.mult, ALU.add, [ps], [rs])
    K.act(rs[0:nparts, 0:n], rs[0:nparts, 0:n], AF.Sqrt, [rs], [rs])
    K.op("dve", lambda e: e.reciprocal(out=rs[0:nparts, 0:n], in_=rs[0:nparts, 0:n]), [rs], [rs])
    K.stt(out_bf, src_f32, gain_col, rs[0:nparts, 0:n], ALU.mult, ALU.mult, [tagkey, rs], [outkey])


PV_CONV = 0
PV_MEMQG = 12
PV_MEMKG = 13
PV_MU = 14
PV_W0, PV_A0, PV_KK, PV_KA, PV_RK, PV_LNW, PV_LNB, PV_V0, PV_C1 = 22, 23, 24, 25, 26, 27, 28, 29, 30
PV_NSAG = 31
PV_CB1 = 35
NV = 40


def local_mixers(K, l, C, S):
    TC, W = K.TC, HALO + K.TC
    hTx, pv = C["hTx"], C["pv"][l]
    w_in = K.inputs["w_in"].ap()[l]
    tokblocks = [(0, HALO)] + [(HALO + i * 512, min(512, TC - i * 512)) for i in range((TC + 511) // 512)]
    locblocks = tokblocks[1:]
    K.push()
    wts = [K.sb(f"lm_wt{i}", [128, 16, 512], BF16) for i in range(2)]
    wi = [0]

    def nextw():
        w = wts[wi[0] % 2]
        wi[0] += 1
        return w

    def proj(wt, c_lo, ncol, dst, blocks, evac=None):
        for (c0, n) in blocks:
            ps = K.psum()
            for kc in range(16):
                K.mm(ps[0:ncol, 0:n], wt[:, kc, c_lo:c_lo + ncol], hTx[:, kc, c0:c0 + n], kc == 0, kc == 15, [wt, hTx], [ps])
            K.copy(dst[0:ncol, c0:c0 + n], ps[0:ncol, 0:n], [ps], [dst], eng="act")

    K.push()
    bt, ct, xt_ = (K.sb(n, [128, W]) for n in ("cv_b", "cv_c", "cv_x"))
    z, acc = K.sb("cv_z", [128, W]), K.sb("cv_acc", [128, TC])
    for j in range(4):
        wt = nextw()
        for i in range(3):
            src = w_in.rearrange("(kc p) n -> p kc n", p=128)[:, :, O_CONV + i * 512 + j * 128: O_CONV + i * 512 + (j + 1) * 128]
            K.dma("pool", wt[:, :, i * 128:(i + 1) * 128], src, writes=[wt])
        proj(wt, 0, 128, bt, locblocks)
        proj(wt, 128, 128, ct, tokblocks)
        proj(wt, 256, 128, xt_, tokblocks)
        K.tt(z[:], ct[:], xt_[:], ALU.mult, [ct, xt_], [z])
        cw = lambda i: pv[:, PV_CONV + j * 3 + i: PV_CONV + j * 3 + i + 1]
        K.ts(acc[:], z[:, HALO:W], cw(2), None, ALU.mult, None, [z, pv], [acc])
        K.stt(acc[:], z[:, HALO - 1:W - 1], cw(1), acc[:], ALU.mult, ALU.add, [z, pv, acc], [acc])
        K.stt(acc[:], z[:, HALO - 2:W - 2], cw(0), acc[:], ALU.mult, ALU.add, [z, pv, acc], [acc])
        K.tt(S["y_convT"][:, j, :], bt[:, HALO:W], acc[:], ALU.mult, [bt, acc], [S["y_convT"]])
    barrier(K)
    K.pop()
    K.push()
    acc = K.sb("pl_acc", [128, TC])
    wt = nextw()
    load_w(K, wt, w_in, O_POOL, 512)
    pa, pb, pu = K.sb("pl_a", [128, W]), K.sb("pl_b", [128, W]), K.sb("pl_u", [128, W])
    invc = K.sb("pl_invc", [128, TC])
    for g in range(4):
        proj(wt, g * 128, 128, pu, tokblocks)
        K.dma("sp", invc[:], K.inputs["invcnt"].ap()[g:g + 1, :].partition_broadcast(128), writes=[invc])
        cur, oth = pu, pa
        for si, sh in enumerate([1, 2, 4, 8][:g + 1]):
            K.tt(oth[:, sh:W], cur[:, sh:W], cur[:, 0:W - sh], ALU.add, [cur], [oth])
            cur, oth = oth, (pb if oth is pa else pa)
        K.tt(acc[:], cur[:, HALO:W], invc[:], ALU.mult, [cur, invc], [acc])
        K.tt(S["pooledT"][:, g, :], acc[:], pu[:, HALO:W], ALU.subtract, [acc, pu], [S["pooledT"]])
    barrier(K)
    K.pop()
    memT = K.sb("mm_memT", [128, 16, MEM_LEN], BF16)
    gm = K.sb("mm_g", [128, D])
    K.dma("sp", gm[:], K.inputs["norm_mem"].ap()[l:l + 1, :].partition_broadcast(128), writes=[gm])
    K.push()
    rn_alloc(K, C)
    rmsnorm_T(K, K.inputs["mem"].ap(), gm, memT, 0, 2, C, "mem")
    barrier(K)
    K.pop()
    wkv = K.inputs["mem_wkv"].ap()[l]
    kT = K.sb("mm_kT", [128, 4, MEM_LEN], BF16)
    vsb = K.sb("mm_v", [128, 2, 512], BF16)
    kf = K.sb("mm_kf", [128, 512])
    wt = nextw()
    load_w(K, wt, wkv, 0, 512)
    for h in range(4):
        ps = K.psum()
        for kc in range(16):
            K.mm(ps[:, 0:MEM_LEN], wt[:, kc, h * 128:(h + 1) * 128], memT[:, kc, :], kc == 0, kc == 15, [wt, memT], [ps])
        K.copy(kf[:, 0:MEM_LEN], ps[:, 0:MEM_LEN], [ps], [kf], eng="act")
        fm_colsum_norm(K, kT[:, h, :], kT, kf[:, 0:MEM_LEN], kf, MEM_LEN, 128, C["ones_f"], pv[:, PV_MEMKG:PV_MEMKG + 1], 1.0 / 128, C)
    wt = nextw()
    load_w(K, wt, wkv, 512, 512)
    for mt in range(2):
        ps = K.psum()
        for kc in range(16):
            K.mm(ps[:, :], memT[:, kc, mt * 128:(mt + 1) * 128], wt[:, kc, :], kc == 0, kc == 15, [wt, memT], [ps])
        K.copy(vsb[:, mt, :], ps[:, :], [ps], [vsb], eng="act")
    wt = nextw()
    load_w(K, wt, w_in, O_MEM, 512)
    qf, qT = K.sb("mm_qf", [128, W]), K.sb("mm_qT", [128, 512], BF16)
    es = [K.sb(f"mm_e{i}", [128, 512], BF16) for i in range(2)]
    rden = K.sb("mm_rden", [128, 512])
    for h in range(4):
        proj(wt, h * 128, 128, qf, locblocks)
        for (c0, n) in locblocks:
            fm_colsum_norm(K, qT[:, 0:n], qT, qf[:, c0:c0 + n], qf, n, 128, C["ones_f"], pv[:, PV_MEMQG:PV_MEMQG + 1], 1.0 / 128, C)
            for mt in range(2):
                ps = K.psum()
                K.mm(ps[:, 0:n], kT[:, h, mt * 128:(mt + 1) * 128], qT[:, 0:n], True, True, [kT, qT], [ps])
                K.act(es[mt][:, 0:n], ps[:, 0:n], AF.Exp, [ps], [es[mt]], scale=float(128 ** -0.5))
            po, pd = K.psum(), K.psum()
            for mt in range(2):
                K.mm(po[:, 0:n], vsb[:, mt, h * 128:(h + 1) * 128], es[mt][:, 0:n], mt == 0, mt == 1, [vsb, es[mt]], [po])
            for mt in range(2):
                K.mm(pd[:, 0:n], C["ones_b"][:, :], es[mt][:, 0:n], mt == 0, mt == 1, [C["ones_b"], es[mt]], [pd])
            K.op("dve", lambda e: e.reciprocal(out=rden[:, 0:n], in_=pd[:, 0:n]), [pd], [rden])
            K.tt(S["y_memT"][:, h, c0 - HALO:c0 - HALO + n], po[:, 0:n], rden[:, 0:n], ALU.mult, [po, rden], [S["y_memT"]])
    barrier(K)
    K.pop()


GROUPS = [[0, 1, 2, 3], [4, 5, 6, 7]]


def alloc_gather(K, C):
    TC = K.TC
    C["hT_src"] = [K.dscr(f"hT_src{q}", [256, TC], BF16) for q in range(8)]
    C["hT_all"] = [K.dscr(f"hT_all{q}", [4 * 256, TC], BF16) for q in range(8)]


def alloc_scratch(K):
    T, TC = K.T, K.TC
    R = {n: K.dscr(n, [128, T]) for n in ("fm_r", "fm_w", "fm_nkk", "fm_kp", "fm_g", "fm_bonus", "fm_vfirst")}
    R["tm_kka"] = K.dscr("tm_kka", [T, 128])
    R["tm_v"] = K.dscr("tm_v", [T, 128])
    for nm in ("yB", "yN"):
        R[nm] = {"key": nm, "src": [K.dscr(f"{nm}_src{j}", [128, TC]) for j in range(4)],
                 "all": [K.dscr(f"{nm}_all{j}", [512, TC]) for j in range(4)]}
    return R


def setup_common(K):
    C = {}
    C["identf"] = K.sb("identf", [128, 128])
    C["identb"] = K.sb("identb", [128, 128], BF16)
    C["ones_f"] = K.sb("ones_f", [128, 128])
    C["ones_b"] = K.sb("ones_b", [128, 128], BF16)
    K.dma("sp", C["identf"][:], K.inputs["ident"].ap(), writes=[C["identf"]])
    K.copy(C["identb"][:], C["identf"][:], [C["identf"]], [C["identb"]])
    K.memset(C["ones_f"][:], 1.0, [C["ones_f"]])
    K.memset(C["ones_b"][:], 1.0, [C["ones_b"]])
    C["fm_sq"] = K.sb("fm_sq", [128, 512])
    C["fm_rs"] = K.sb("fm_rs", [128, 512])
    C["oh"] = K.sb("oh", [128, 8])
    K.dma("sp", C["oh"][:], K.inputs["onehot"].ap(), writes=[C["oh"]])
    C["pv"] = []
    for l in range(DEPTH):
        t = K.sb(f"pv{l}", [128, NV])
        K.dma("sp", t[:], K.inputs["pvec"].ap()[l], writes=[t])
        K.ts(t[:, PV_C1:PV_C1 + 1], t[:, PV_KA:PV_KA + 1], -1.0, 1.0, ALU.mult, ALU.add, [t], [t])
        C["pv"].append(t)
    return C


def rn_alloc(K, C):
    C["xt"] = [K.sb(f"rn_xt{i}", [128, D]) for i in range(2)]
    C["junk"] = [K.sb(f"rn_junk{i}", [128, D], BF16) for i in range(2)]
    C["ss"] = [K.sb(f"rn_ss{i}", [128, 2]) for i in range(2)]
    C["hb"] = [K.sb(f"rn_hb{i}", [128, D], BF16) for i in range(2)]
    C["gbc"] = K.sb("gbc", [128, D])


def phase_norm_gather(K, l, C, xres):
    TC = K.TC
    hTx = C["hTx"]
    K.push()
    rn_alloc(K, C)
    K.dma("sp", C["gbc"][:], K.inputs["norm_mix"].ap()[l:l + 1, :].partition_broadcast(128), writes=[C["gbc"]])
    rmsnorm_T(K, xres.ap(), C["gbc"], hTx, HALO, K.NT, C, "mix")
    barrier(K)
    K.pop()
    for q in range(NHQ):
        K.dma("sp", C["hT_src"][q].ap().rearrange("(kc p) t -> p kc t", p=128), hTx[:, 2 * q:2 * q + 2, HALO:HALO + TC], reads=[hTx], writes=["hT_src"])
    for q in range(NHQ):
        K.fw.allgather(C["hT_src"][q], C["hT_all"][q], GROUPS, reads=["hT_src"], writes=["hT_all"])
    cand = C["halo_cand"]
    C["_hall_dst"] = cand
    for r in range(4):
        hall_read(K, cand[:, r], C, r, TC - HALO, HALO)
    oh = C["oh"]
    K.ts(hTx[:, :, 0:HALO], cand[:, 0], oh[:, 4:5], None, ALU.mult, None, [cand, oh], [hTx])
    for r in range(1, 4):
        K.stt(hTx[:, :, 0:HALO], cand[:, r], oh[:, 4 + r:5 + r], hTx[:, :, 0:HALO], ALU.mult, ALU.add, [cand, oh, hTx], [hTx])


def rwkv_cols(hp):
    cols = []
    pad = lambda a, n: list(a) + [-1] * (n - len(a))
    cols += list(range(128 * hp, 128 * hp + 128))
    cols += list(range(512 + 128 * hp, 512 + 128 * hp + 128))
    cols += list(range(1024 + 128 * hp, 1024 + 128 * hp + 128))
    cols += pad(range(1536, 1632), 128)
    cols += pad(range(1632, 1728), 128)
    cols += list(range(1728, 1984))
    cols += pad(range(1984, 2048), 128)
    return np.array(cols)


def nsa_cols(hp):
    hk = hp // 2
    mine = [2 * hp, 2 * hp + 1]
    oth = [h for h in range(4 * hk, 4 * hk + 4) if h not in mine]
    heads = mine + oth
    grp = lambda i: list(range(512 + 128 * i + 64 * hk, 512 + 128 * i + 64 * hk + 64))
    cols = []
    for h in heads:
        cols += list(range(64 * h, 64 * h + 64))
    cols += grp(0) + grp(0) + grp(2) + grp(2) + grp(4) + grp(4)
    cols += grp(1)
    cols += grp(3) + grp(5)
    for h in heads:
        cols += [512 + 768 + 3 * h + i for i in range(3)]
    return np.array(cols)


NWN = 844


def host_prep(inp, T):
    TC = T // 4
    f = lambda a: np.ascontiguousarray(np.asarray(a, dtype=np.float32))
    sh = {k: f(inp[k]) for k in ("w_in", "norm_mix", "norm_ffn", "norm_mem", "mem_wkv", "pool_w", "pool_scale", "w_branch",
                                 "w_out", "ffn_w1", "ffn_w3", "ffn_w2", "moe_router", "moe_w1", "moe_w3", "moe_w2",
                                 "nsa_cmp_w1", "nsa_cmp_w2", "nsa_cmp_pos")}
    sh["ident"] = np.eye(128, dtype=np.float32)
    sh["blk64"] = np.kron(np.eye(2), np.ones((64, 64))).astype(np.float32)
    sh["cmp_posT"] = np.ascontiguousarray(np.transpose(sh["nsa_cmp_pos"], (0, 1, 3, 2)))
    sh["nsa_qk_gain"] = f(inp["nsa_qk_gain"])
    NS, NTT = T // 64, T // 128
    cc = np.arange(256)[:, None] * 16
    ss_ = np.arange(64)[None, :] * 64
    sh["ovl"] = (np.clip(np.minimum(cc + 32, ss_ + 64) - np.maximum(cc, ss_), 0, None) / 32.0).astype(np.float32)
    tt_ = np.arange(T)
    sh["maskc"] = ((np.arange(256)[:, None] * 16 + 31) <= tt_[None, :]).astype(np.float32)
    cur = (tt_ // 64)[:, None]
    sid = np.arange(64)[None, :]
    valid = sid <= cur
    f0, f1, f2 = (sid == 0), (sid == cur), (sid == cur - 1)
    forced = f0 | f1 | f2
    sh["tk_keep"] = (valid & ~forced).astype(np.float32)
    cadd = np.where(valid, 0.0, -1e9)
    cadd = np.where(f2, 1e9, cadd)
    cadd = np.where(f1, 2e9, cadd)
    cadd = np.where(f0, 3e9, cadd)
    sh["tk_cadd"] = cadd.astype(np.float32)
    e2 = np.zeros((64, 32, 128), np.float32)
    for jt in range(32):
        for j in range(128):
            e2[2 * jt + j // 64, jt, j] = 1.0
    sh["e2"] = e2
    dm = np.zeros((5, 128, 512), np.float32)
    jj = np.arange(128)[:, None]
    t5 = np.arange(512)[None, :]
    for dd in range(4):
        dm[dd] = (t5 >= dd * 128 + jj)
    dm[4] = (jj > t5)
    sh["dmask"] = dm
    x = f(inp["x"])
    mem = f(inp["mem"])
    w_in = sh["w_in"]
    wfull = [np.concatenate([w_in[0][:, :1984], np.zeros((D, 64), np.float32)], 1),
             np.concatenate([w_in[1][:, :1984], f(inp["vres_in"])[0]], 1)]
    mufull = [np.concatenate([f(inp["rwkv_mu"])[0], np.zeros(64, np.float32)]),
              np.concatenate([f(inp["rwkv_mu"])[1], f(inp["vres_mu"])[0]])]
    maps = []
    for c in range(NCORES):
        b, j = c // 4, c % 4
        hp = j
        m = dict(sh)
        m["x"] = np.ascontiguousarray(x[b, j * TC:(j + 1) * TC])
        m["mem"] = np.ascontiguousarray(mem[b])
        oh = np.zeros((128, 8), np.float32)
        oh[:, j] = 1.0
        if j > 0:
            oh[:, 4 + j - 1] = 1.0
        m["onehot"] = oh
        tg = np.arange(j * TC, (j + 1) * TC, dtype=np.float32) + 1.0
        m["invcnt"] = np.stack([1.0 / np.minimum(tg, w) for w in (2, 4, 8, 16)]).astype(np.float32)
        rc = rwkv_cols(hp)
        ncl = nsa_cols(hp)
        wr = np.zeros((DEPTH, D, 1024), np.float32)
        wn = np.zeros((DEPTH, D, NWN), np.float32)
        pv = np.zeros((DEPTH, 128, NV), np.float32)
        lora = np.zeros((DEPTH, 128, 5, 128), np.float32)
        ch = slice(128 * hp, 128 * hp + 128)
        for l in range(DEPTH):
            ok = rc >= 0
            wr[l][:, ok] = wfull[l][:, rc[ok]]
            wn[l] = w_in[l][:, O_NSA + ncl]
            cw = f(inp["conv_w"])[l]
            for jj in range(4):
                for i in range(3):
                    pv[l, :, PV_CONV + jj * 3 + i] = cw[i, jj * 128:(jj + 1) * 128]
            pv[l, :, PV_MEMQG] = f(inp["mem_qk_gain"])[l, 0]
            pv[l, :, PV_MEMKG] = f(inp["mem_qk_gain"])[l, 1]
            mu = np.zeros(1024, np.float32)
            mu[ok] = mufull[l][rc[ok]]
            pv[l, :, PV_MU:PV_MU + 8] = mu.reshape(8, 128).T
            for idx, nm in ((PV_W0, "rwkv_w0"), (PV_A0, "rwkv_a0"), (PV_KK, "rwkv_kk"), (PV_KA, "rwkv_ka"), (PV_RK, "rwkv_rk"),
                            (PV_LNW, "rwkv_ln_w"), (PV_LNB, "rwkv_ln_b")):
                pv[l, :, idx] = f(inp[nm])[l, ch]
            if l == 1:
                pv[l, :, PV_V0] = f(inp["vres_v0"])[0, ch]
                lora[l, 0:64, 4] = f(inp["vres_up"])[0][:, ch]
            g = f(inp["nsa_qk_gain"])[l]
            for i in range(4):
                pv[l, :, PV_NSAG + i] = np.concatenate([g[i], g[i]])
            pv[l, :, PV_CB1:PV_CB1 + 2] = f(inp["nsa_cmp_b1"])[l].T
            lora[l, 0:96, 0] = f(inp["rwkv_w2"])[l][:, ch]
            lora[l, 0:96, 1] = f(inp["rwkv_a2"])[l][:, ch]
            lora[l, :, 2] = f(inp["rwkv_g2"])[l][0:128, ch]
            lora[l, :, 3] = f(inp["rwkv_g2"])[l][128:256, ch]
        m["wr"], m["wn"], m["pvec"], m["lora"] = wr, wn, pv, lora
        maps.append(m)
    return maps


NHQ = 8


def hall_read(K, dst3, C, r, col0, n):
    for q in range(NHQ):
        src = C["hT_all"][q].ap()[r * 256:(r + 1) * 256, col0:col0 + n].rearrange("(kc p) t -> p kc t", p=128)
        K.dma("sp", dst3[:, 2 * q:2 * q + 2, :], src, reads=["hT_all"], writes=[C["_hall_dst"]])


def ysrc_write(K, Y, rows, t0, n, src_tile, src_ap):
    TC = K.TC
    done = 0
    while done < n:
        j, off = (t0 + done) // TC, (t0 + done) % TC
        m = min(n - done, TC - off)
        K.dma("sp", Y["src"][j].ap()[rows, off:off + m], src_ap[:, done:done + m], reads=[src_tile], writes=[Y["key"] + "_src"])
        done += m


def ygather(K, Y):
    for j in range(4):
        K.fw.allgather(Y["src"][j], Y["all"][j], GROUPS, reads=[Y["key"] + "_src"], writes=[Y["key"] + "_all"])


def load_hall_block(K, hb, C, t0, TC, blk=512):
    sub = min(blk, TC)
    for si in range(blk // sub):
        tok = t0 + si * sub
        r_, off = tok // TC, tok % TC
        C["_hall_dst"] = hb
        hall_read(K, hb[:, :, si * sub:(si + 1) * sub], C, r_, off, sub)


CS = 16
GN_EPS = 64e-5


def rwkv_phase(K, l, C, R):
    T = K.T
    TC = K.TC
    NB = T // 512
    pv = C["pv"][l]
    K.push()
    wr = K.sb("rw_w", [128, 16, 1024], BF16)
    for i in range(2):
        K.dma("pool", wr[:, :, i * 512:(i + 1) * 512],
              K.inputs["wr"].ap()[l].rearrange("(kc p) n -> p kc n", p=128)[:, :, i * 512:(i + 1) * 512], writes=[wr])
    lora = K.sb("rw_lora", [128, 5, 128])
    K.dma("sp", lora[:], K.inputs["lora"].ap()[l], writes=[lora])
    blk = K.sb("rw_blk", [128, 128])
    K.dma("sp", blk[:], K.inputs["blk64"].ap(), writes=[blk])
    hTb = [K.sb(f"rw_hT{i}", [128, 16, 512], BF16) for i in range(2)]
    nct = 8 if l == 1 else 7
    ub = [K.sb(f"rw_ub{i}", [128, 513]) for i in range(nct)]
    uf = [K.sb(f"rw_uf{i}", [128, 512]) for i in range(nct)]
    tmp = [K.sb(f"rw_t{i}", [128, 512]) for i in range(8)]
    tmo = [K.sb(f"rw_tmo{i}", [128, 4, 128]) for i in range(2)]
    for ct in range(nct):
        K.memset(ub[ct][:, 0:1], 0.0, [ub[ct]])
    sc = lambda i: pv[:, i:i + 1]
    for tb in range(NB):
        t0 = tb * 512
        hb = hTb[tb % 2]
        load_hall_block(K, hb, C, t0, TC)
        for ct in range(nct):
            ps = K.psum()
            for kc in range(16):
                K.mm(ps[:, :], wr[:, kc, ct * 128:(ct + 1) * 128], hb[:, kc, :], kc == 0, kc == 15, [wr, hb], [ps])
            K.copy(ub[ct][:, 1:513], ps[:, :], [ps], [ub[ct]], eng="act")
            d = tmp[0]
            K.tt(d[:], ub[ct][:, 0:512], ub[ct][:, 1:513], ALU.subtract, [ub[ct]], [d])
            K.stt(uf[ct][:], d[:], sc(PV_MU + ct), ub[ct][:, 1:513], ALU.mult, ALU.add, [d, pv, ub[ct]], [uf[ct]])
            K.copy(ub[ct][:, 0:1], ub[ct][:, 512:513], [ub[ct]], [ub[ct]])
        r, k, v = uf[0], uf[1], uf[2]
        K.act(uf[3][0:96, :], uf[3][0:96, :], AF.Tanh, [uf[3]], [uf[3]])
        ps = K.psum()
        K.mm(ps[:, :], lora[0:96, 0, :], uf[3][0:96, :], True, True, [lora, uf[3]], [ps])
        dec = tmp[1]
        K.act(dec[:], ps[:, :], AF.Sigmoid, [ps, pv], [dec], bias=sc(PV_W0))
        K.act(dec[:], dec[:], AF.Exp, [dec], [dec], scale=-float(np.exp(-0.5)))
        K.dma("sp", R["fm_w"].ap()[:, t0:t0 + 512], dec[:], reads=[dec], writes=["fm_w"])
        ps = K.psum()
        K.mm(ps[:, :], lora[0:96, 1, :], uf[4][0:96, :], True, True, [lora, uf[4]], [ps])
        a = tmp[2]
        K.act(a[:], ps[:, :], AF.Sigmoid, [ps, pv], [a], bias=sc(PV_A0))
        K.act(uf[5][:], uf[5][:], AF.Sigmoid, [uf[5]], [uf[5]])
        K.act(uf[6][:], uf[6][:], AF.Sigmoid, [uf[6]], [uf[6]])
        ps = K.psum()
        K.mm(ps[:, :], lora[:, 2, :], uf[5][:], True, False, [lora, uf[5]], [ps])
        K.mm(ps[:, :], lora[:, 3, :], uf[6][:], False, True, [lora, uf[6]], [ps])
        g = tmp[3]
        K.copy(g[:], ps[:, :], [ps], [g], eng="act")
        K.dma("sp", R["fm_g"].ap()[:, t0:t0 + 512], g[:], reads=[g], writes=["fm_g"])
        if l == 0:
            K.dma("sp", R["fm_vfirst"].ap()[:, t0:t0 + 512], v[:], reads=[v], writes=["fm_vfirst"])
        else:
            ps = K.psum()
            K.mm(ps[:, :], lora[0:64, 4, :], uf[7][0:64, :], True, True, [lora, uf[7]], [ps])
            vr = tmp[4]
            K.act(vr[:], ps[:, :], AF.Sigmoid, [ps, pv], [vr], bias=sc(PV_V0))
            vf = tmp[5]
            K.dma("sp", vf[:], R["fm_vfirst"].ap()[:, t0:t0 + 512], reads=["fm_vfirst"], writes=[vf])
            K.tt(vf[:], vf[:], v[:], ALU.subtract, [vf, v], [vf])
            K.tt(vf[:], vf[:], vr[:], ALU.mult, [vf, vr], [vf])
            K.tt(v[:], v[:], vf[:], ALU.add, [v, vf], [v])
        kk = tmp[4]
        K.ts(kk[:], k[:], sc(PV_KK), None, ALU.mult, None, [k, pv], [kk])
        sq = tmp[5]
        K.tt(sq[:], kk[:], kk[:], ALU.mult, [kk], [sq])
        ps = K.psum()
        K.mm(ps[:, :], blk[:, :], sq[:], True, True, [blk, sq], [ps])
        rn = tmp[5]
        K.ts(rn[:], ps[:, :], 1e-24, None, ALU.max, None, [ps], [rn])
        K.act(rn[:], rn[:], AF.Sqrt, [rn], [rn])
        K.op("dve", lambda e: e.reciprocal(out=rn[:], in_=rn[:]), [rn], [rn])
        K.tt(kk[:], kk[:], rn[:], ALU.mult, [kk, rn], [kk])
        nkk = tmp[5]
        K.ts(nkk[:], kk[:], -1.0, None, ALU.mult, None, [kk], [nkk])
        K.dma("sp", R["fm_nkk"].ap()[:, t0:t0 + 512], nkk[:], reads=[nkk], writes=["fm_nkk"])
        t1 = tmp[6]
        K.ts(t1[:], a[:], sc(PV_KA), sc(PV_C1), ALU.mult, ALU.add, [a, pv], [t1])
        kp = tmp[7]
        K.tt(kp[:], k[:], t1[:], ALU.mult, [k, t1], [kp])
        K.dma("sp", R["fm_kp"].ap()[:, t0:t0 + 512], kp[:], reads=[kp], writes=["fm_kp"])
        kka = tmp[6]
        K.tt(kka[:], kk[:], a[:], ALU.mult, [kk, a], [kka])
        for which, (src, dst) in enumerate(((kka, "tm_kka"), (v, "tm_v"))):
            ps = K.psum()
            for i in range(4):
                K.tr(ps[:, i * 128:(i + 1) * 128], src[:, i * 128:(i + 1) * 128], C["identf"][:], [src, C["identf"]], [ps])
            K.copy(tmo[which][:], ps[:, :].rearrange("p (a b) -> p a b", a=4), [ps], [tmo[which]], eng="act")
            K.dma("sp", R[dst].ap()[t0:t0 + 512, :].rearrange("(a p) c -> p a c", p=128), tmo[which][:], reads=[tmo[which]], writes=[dst])
        t2 = tmp[0]
        K.stt(t2[:], r[:], sc(PV_RK), kp[:], ALU.mult, ALU.mult, [r, pv, kp], [t2])
        ps = K.psum()
        K.mm(ps[:, :], blk[:, :], t2[:], True, True, [blk, t2], [ps])
        bon = tmp[0]
        K.tt(bon[:], ps[:, :], v[:], ALU.mult, [ps, v], [bon])
        K.dma("sp", R["fm_bonus"].ap()[:, t0:t0 + 512], bon[:], reads=[bon], writes=["fm_bonus"])
        K.dma("sp", R["fm_r"].ap()[:, t0:t0 + 512], r[:], reads=[r], writes=["fm_r"])
    barrier(K)
    K.pop()

    K.push()
    NH = 2
    Sring = [[K.sb(f"sc_S{h}_{i}", [64, 64]) for i in range(4)] for h in range(NH)]
    kkaB = [[K.sb(f"sc_kkaB{h}_{i}", [64, CS, 64]) for i in range(2)] for h in range(NH)]
    vB = [[K.sb(f"sc_vB{h}_{i}", [64, CS, 64]) for i in range(2)] for h in range(NH)]
    Abuf = [[K.sb(f"sc_A{h}_{i}", [64, CS, 64]) for i in range(2)] for h in range(NH)]
    Dg = [K.sb(f"sc_Dg{h}", [64, CS, 64]) for h in range(NH)]
    fmb = {n: [[K.sb(f"sc_{n}{h}_{i}", [64, 512]) for i in range(2)] for h in range(NH)] for n in ("w", "nkk", "kp", "r", "g", "bonus")}
    yT = [K.sb(f"sc_yT{h}", [64, 512]) for h in range(NH)]
    pt = [K.sb(f"sc_pt{h}_{i}", [64, 512]) for h in range(NH) for i in range(3)]
    pvh = [K.sb(f"sc_pv{h}", [64, NV]) for h in range(NH)]
    ones64 = K.sb("sc_ones64", [64, 64])
    K.memset(ones64[:], 1.0 / 64, [ones64])
    for h in range(NH):
        K.dma("sp", pvh[h][:], K.inputs["pvec"].ap()[l][h * 64:(h + 1) * 64, :], writes=[pvh[h]])
        K.memset(Sring[h][0][:], 0.0, [Sring[h][0]])
    psS = [K.ps[0], K.ps[1]]
    psY = [K.ps[2], K.ps[3]]
    ident64 = C["identf"][0:64, 0:64]
    nchunk = 512 // CS
    pend_y = []
    for tb in range(NB):
        t0 = tb * 512
        for h in range(NH):
            for n in fmb:
                K.dma("sp", fmb[n][h][tb % 2][:], R["fm_" + n].ap()[h * 64:(h + 1) * 64, t0:t0 + 512], reads=["fm_" + n], writes=[fmb[n][h][tb % 2]])
        for ci in range(nchunk):
            c0 = ci * CS
            gi = tb * nchunk + ci
            for h in range(NH):
                kb, vb, A = kkaB[h][gi % 2], vB[h][gi % 2], Abuf[h][gi % 2]
                K.dma("sp", kb[:], R["tm_kka"].ap()[t0 + c0:t0 + c0 + CS, h * 64:(h + 1) * 64].partition_broadcast(64), reads=["tm_kka"], writes=[kb])
                K.dma("sp", vb[:], R["tm_v"].ap()[t0 + c0:t0 + c0 + CS, h * 64:(h + 1) * 64].partition_broadcast(64), reads=["tm_v"], writes=[vb])
                nk = fmb["nkk"][h][tb % 2]
                w = fmb["w"][h][tb % 2]
                K.tt(A[:], kb[:], nk[:, c0:c0 + CS].unsqueeze(2).to_broadcast([64, CS, 64]), ALU.mult, [kb, nk], [A])
                K.tt(Dg[h][:], ident64.unsqueeze(1).to_broadcast([64, CS, 64]), w[:, c0:c0 + CS].unsqueeze(2).to_broadcast([64, CS, 64]),
                     ALU.mult, [C["identf"], w], [Dg[h]])
                K.tt(A[:], A[:], Dg[h][:], ALU.add, [A, Dg[h]], [A])
            for s in range(CS):
                tg = gi * CS + s
                for h in range(NH):
                    A = Abuf[h][gi % 2]
                    Sp = Sring[h][tg % 4]
                    slot = tg % 8
                    pk = f"psS{h}_{slot}"
                    pso = psS[h][0:64, slot * 64:(slot + 1) * 64]
                    K.mm(pso, A[:, s, :], Sp[:], True, True, [A, Sp], [pk])
                for fn_ in pend_y:
                    fn_()
                pend_y.clear()
                for h in range(NH):
                    vb = vB[h][gi % 2]
                    Sn = Sring[h][(tg + 1) % 4]
                    slot = tg % 8
                    pk = f"psS{h}_{slot}"
                    pso = psS[h][0:64, slot * 64:(slot + 1) * 64]
                    kp = fmb["kp"][h][tb % 2]
                    K.stt(Sn[:], vb[:, s, :], kp[:, c0 + s:c0 + s + 1], pso, ALU.mult, ALU.add, [vb, kp, pk], [Sn])
                    rr = fmb["r"][h][tb % 2]

                    def ymm(h=h, Sn=Sn, rr=rr, col=c0 + s):
                        K.mm(psY[h][0:64, col:col + 1], Sn[:], rr[:, col:col + 1], True, True, [Sn, rr], [psY[h]])
                    pend_y.append(ymm)
        for fn_ in pend_y:
            fn_()
        pend_y.clear()
        for h in range(NH):
            y, p0, p1, p2 = yT[h], pt[h * 3], pt[h * 3 + 1], pt[h * 3 + 2]
            K.copy(y[:], psY[h][0:64, :], [psY[h]], [y], eng="act")
            ps = K.psum4()
            K.mm(ps[0:64, :], ones64[:], y[:], True, True, [ones64, y], [ps])
            K.tt(p0[:], y[:], ps[0:64, :], ALU.subtract, [y, ps], [p0])
            K.tt(p1[:], p0[:], p0[:], ALU.mult, [p0], [p1])
            ps = K.psum4()
            K.mm(ps[0:64, :], ones64[:], p1[:], True, True, [ones64, p1], [ps])
            K.ts(p1[:], ps[0:64, :], GN_EPS, None, ALU.add, None, [ps], [p1])
            K.act(p1[:], p1[:], AF.Sqrt, [p1], [p1])
            K.op("dve", lambda e: e.reciprocal(out=p1[:], in_=p1[:]), [p1], [p1])
            K.tt(p0[:], p0[:], p1[:], ALU.mult, [p0, p1], [p0])
            K.ts(p0[:], p0[:], pvh[h][:, PV_LNW:PV_LNW + 1], pvh[h][:, PV_LNB:PV_LNB + 1], ALU.mult, ALU.add, [p0, pvh[h]], [p0])
            bo, g = fmb["bonus"][h][tb % 2], fmb["g"][h][tb % 2]
            K.tt(p0[:], p0[:], bo[:], ALU.add, [p0, bo], [p0])
            K.tt(p2[:], p0[:], g[:], ALU.mult, [p0, g], [p2])
            ysrc_write(K, R["yB"], slice(h * 64, (h + 1) * 64), t0, 512, p2, p2)
    barrier(K)
    K.pop()


def select_chunk(K, dst, Y, C):
    oh = C["oh"]
    cand, acc = C["sel_cand"], C["sel_acc"]
    for kc in range(4):
        for j in range(4):
            K.dma("sp", cand[:, j, :], Y["all"][j].ap()[kc * 128:(kc + 1) * 128, :], reads=[Y["key"] + "_all"], writes=[cand])
        K.ts(acc[:], cand[:, 0, :], oh[:, 0:1], None, ALU.mult, None, [cand, oh], [acc])
        for j in range(1, 4):
            K.stt(acc[:], cand[:, j, :], oh[:, j:j + 1], acc[:], ALU.mult, ALU.add, [cand, oh, acc], [acc])
        K.copy(dst[:, kc, :], acc[:], [acc], [dst])


def merge_phase(K, l, C, S, xres):
    TC, NT = K.TC, K.NT
    hTx = C["hTx"]
    w_in = K.inputs["w_in"].ap()[l]
    K.push()
    wg = [K.sb(f"mg_wg{i}", [128, 16, 512], BF16) for i in range(2)]
    wb = [K.sb(f"mg_wb{i}", [128, 4, 512], BF16) for i in range(2)]
    psc = K.sb("mg_psc", [128, 512])
    macc = K.sb("mg_macc", [128, NT, 512])
    merged = K.sb("mg_merged", [128, NT, D], BF16)
    gt = [K.sb(f"mg_gt{i}", [128, 512]) for i in range(2)]
    tm = [K.sb(f"mg_tm{i}", [128, 512]) for i in range(2)]
    ys = [S["y_rwkvT"], S["y_nsaT"], S["y_convT"], S["y_memT"]]
    n = 0
    for nb in range(4):
        K.dma("sp", psc[:], K.inputs["pool_scale"].ap()[l:l + 1, nb * 512:(nb + 1) * 512].partition_broadcast(128), writes=[psc])
        for i in range(5):
            g_, b_ = wg[n % 2], wb[n % 2]
            n += 1
            load_w(K, g_, w_in, O_GATE + i * D + nb * 512, 512)
            if i < 4:
                K.dma("pool", b_[:], K.inputs["w_branch"].ap()[l, i].rearrange("(kc p) n -> p kc n", p=128)[:, :, nb * 512:(nb + 1) * 512], writes=[b_])
            else:
                K.dma("pool", b_[:, 0, :], K.inputs["pool_w"].ap()[l, nb], writes=[b_])
            for tt in range(NT):
                tsl = slice(tt * 128, (tt + 1) * 128)
                psg = K.psum()
                for kc in range(16):
                    K.mm(psg[:, :], hTx[:, kc, HALO + tt * 128: HALO + (tt + 1) * 128], g_[:, kc, :], kc == 0, kc == 15, [hTx, g_], [psg])
                G = gt[tt % 2]
                K.act(G[:], psg[:, :], AF.Sigmoid, [psg], [G])
                psb = K.psum()
                if i < 4:
                    for kc in range(4):
                        K.mm(psb[:, :], ys[i][:, kc, tsl], b_[:, kc, :], kc == 0, kc == 3, [ys[i], b_], [psb])
                else:
                    K.mm(psb[:, :], S["pooledT"][:, nb, tsl], b_[:, 0, :], True, True, [S["pooledT"], b_], [psb])
                    K.tt(G[:], G[:], psc[:], ALU.mult, [G, psc], [G])
                if i == 0:
                    K.tt(macc[:, tt, :], G[:], psb[:, :], ALU.mult, [G, psb], [macc])
                else:
                    t_ = tm[tt % 2]
                    K.tt(t_[:], G[:], psb[:, :], ALU.mult, [G, psb], [t_])
                    K.tt(macc[:, tt, :], macc[:, tt, :], t_[:], ALU.add, [macc, t_], [macc])
        K.copy(merged[:, :, nb * 512:(nb + 1) * 512], macc[:], [macc], [merged], eng="act")
    mT = hTx
    for tt in range(NT):
        for grp in range(2):
            ps = K.psum()
            psb_ = ps[:].bitcast(BF16)
            for i in range(8):
                kc = grp * 8 + i
                K.tr(psb_[:, i * 128:(i + 1) * 128], merged[:, tt, kc * 128:(kc + 1) * 128], C["identb"][:], [merged, C["identb"]], [ps])
            K.copy(mT[:, grp * 8:(grp + 1) * 8, HALO + tt * 128: HALO + (tt + 1) * 128], psb_.rearrange("p (a b) -> p a b", a=8), [ps], [mT],
                   eng="act" if grp == 0 else "dve")
    xt = [K.sb(f"mg_xt{i}", [128, 512]) for i in range(2)]
    for nb in range(4):
        wo = wg[nb % 2]
        load_w(K, wo, K.inputs["w_out"].ap()[l], nb * 512, 512)
        for tt in range(NT):
            ps = K.psum()
            for kc in range(16):
                K.mm(ps[:, :], mT[:, kc, HALO + tt * 128: HALO + (tt + 1) * 128], wo[:, kc, :], kc == 0, kc == 15, [mT, wo], [ps])
            x_ = xt[tt % 2]
            K.dma("sp", x_[:], xres.ap()[tt * 128:(tt + 1) * 128, nb * 512:(nb + 1) * 512], reads=["xres"], writes=[x_])
            K.tt(x_[:], x_[:], ps[:, :], ALU.add, [x_, ps], [x_])
            K.dma("sp", xres.ap()[tt * 128:(tt + 1) * 128, nb * 512:(nb + 1) * 512], x_[:], reads=[x_], writes=["xres"])
    barrier(K)
    K.pop()


def ffn_phase(K, l, C, xres, xout):
    TC = K.TC
    BL = min(512, TC)
    NBL = TC // BL
    TPB = BL // 128
    moe = (l % 2 == 1)
    K.push()
    h2T = C["hTx"]
    K.push()
    rn_alloc(K, C)
    K.dma("sp", C["gbc"][:], K.inputs["norm_ffn"].ap()[l:l + 1, :].partition_broadcast(128), writes=[C["gbc"]])
    rmsnorm_T(K, xres.ap(), C["gbc"], h2T, HALO, K.NT, C, "ffn")
    barrier(K)
    K.pop()
    NFE = 22
    NT = K.NT
    uT = K.sb("ff_uT", [128, NFE, TC], BF16)
    w1g = [K.sb(f"ff_w1_{i}", [128, 16, 256], BF16) for i in range(2)]
    w3g = [K.sb(f"ff_w3_{i}", [128, 16, 256], BF16) for i in range(2)]
    w2g = [K.sb(f"ff_w2_{i}", [128, 11, 512], BF16) for i in range(2)]
    sa = [K.sb(f"ff_sa{i}", [128, 512]) for i in range(2)]
    xacc = K.sb("ff_xacc", [128, NT, D])
    if moe:
        rt = K.sb("ff_rt", [128, 16, 8], BF16)
        K.dma("pool", rt[:], K.inputs["moe_router"].ap()[0].rearrange("(kc p) e -> p kc e", p=128), writes=[rt])
        comb = K.sb("ff_comb", [128, K.NT, 8])
        lg, l2, m1, m2, mk1, mk2 = (K.sb("ff_" + n, [128, 8]) for n in ("lg", "l2", "m1", "m2", "mk1", "mk2"))
        for tt in range(K.NT):
            ps = K.psum()
            for kc in range(16):
                K.mm(ps[:, 0:8], h2T[:, kc, HALO + tt * 128: HALO + (tt + 1) * 128], rt[:, kc, :], kc == 0, kc == 15, [h2T, rt], [ps])
            K.copy(lg[:], ps[:, 0:8], [ps], [lg])
            K.op("dve", lambda e: e.reduce_max(out=m1[:, 0:1], in_=lg[:], axis=AX.X), [lg], [m1])
            K.ts(mk1[:], lg[:], m1[:, 0:1], None, ALU.is_ge, None, [lg, m1], [mk1])
            K.stt(l2[:], mk1[:], -1e30, lg[:], ALU.mult, ALU.add, [mk1, lg], [l2])
            K.op("dve", lambda e: e.reduce_max(out=m2[:, 0:1], in_=l2[:], axis=AX.X), [l2], [m2])
            K.ts(mk2[:], l2[:], m2[:, 0:1], None, ALU.is_ge, None, [l2, m2], [mk2])
            K.tt(m1[:, 1:2], m1[:, 0:1], m2[:, 0:1], ALU.subtract, [m1, m2], [m1])
            K.act(m1[:, 2:3], m1[:, 1:2], AF.Sigmoid, [m1], [m1])
            K.ts(m1[:, 3:4], m1[:, 2:3], -1.0, 1.0, ALU.mult, ALU.add, [m1], [m1])
            K.ts(mk1[:], mk1[:], m1[:, 2:3], None, ALU.mult, None, [mk1, m1], [mk1])
            K.stt(comb[:, tt, :], mk2[:], m1[:, 3:4], mk1[:], ALU.mult, ALU.add, [mk2, m1, mk1], [comb])
    for tt in range(NT):
        K.dma("sp", xacc[:, tt, :], xres.ap()[tt * 128:(tt + 1) * 128, :], reads=["xres"], writes=[xacc])
    if moe:
        pexp = [tuple(K.inputs[n].ap()[0, e] for n in ("moe_w1", "moe_w3", "moe_w2")) + (0, e) for e in range(N_EXP)]
    else:
        pexp = [tuple(K.inputs[n].ap()[0] for n in ("ffn_w1", "ffn_w3", "ffn_w2")) + (fo, None) for fo in (0, NFE)]
    nld = [0]
    for (W1, W3, W2, fo, e) in pexp:
        for c0 in range(0, NFE * 128, 256):
            a_, b_ = w1g[nld[0] % 2], w3g[nld[0] % 2]
            nld[0] += 1
            load_w(K, a_, W1, fo * 128 + c0, 256)
            load_w(K, b_, W3, fo * 128 + c0, 256)
            for fi in range(2):
                f = c0 // 128 + fi
                for bl in range(NBL):
                    cb = HALO + bl * BL
                    pa, pb = K.psum_from(0, 4), K.psum_from(0, 4)
                    for kc in range(16):
                        K.mm(pa[:, 0:BL], a_[:, kc, fi * 128:(fi + 1) * 128], h2T[:, kc, cb:cb + BL], kc == 0, kc == 15, [a_, h2T], [pa])
                    for kc in range(16):
                        K.mm(pb[:, 0:BL], b_[:, kc, fi * 128:(fi + 1) * 128], h2T[:, kc, cb:cb + BL], kc == 0, kc == 15, [b_, h2T], [pb])
                    s_ = sa[(f * NBL + bl) % 2]
                    K.act(s_[:, 0:BL], pa[:, 0:BL], AF.Silu, [pa], [s_])
                    K.tt(uT[:, f, bl * BL:(bl + 1) * BL], s_[:, 0:BL], pb[:, 0:BL], ALU.mult, [s_, pb], [uT])
        for nb in range(4):
            for fh in range(2):
                w2_ = w2g[nld[0] % 2]
                nld[0] += 1
                r0 = (fo + fh * 11) * 128
                K.dma("pool", w2_[:, :, :], W2[r0:r0 + 11 * 128, nb * 512:(nb + 1) * 512].rearrange("(f p) n -> p f n", p=128), writes=[w2_])
                for tt in range(NT):
                    ps = K.psum_from(4, 4)
                    for f in range(11):
                        K.mm(ps[:, :], uT[:, fh * 11 + f, tt * 128:(tt + 1) * 128], w2_[:, f, :], f == 0, f == 10, [uT, w2_], [ps])
                    xs = xacc[:, tt, nb * 512:(nb + 1) * 512]
                    if moe:
                        K.stt(xs, ps[:, :], comb[:, tt, e:e + 1], xs, ALU.mult, ALU.add, [ps, comb, xacc], [xacc])
                    else:
                        K.tt(xs, xs, ps[:, :], ALU.add, [xacc, ps], [xacc])
    for tt in range(NT):
        K.dma("sp", xout.ap()[tt * 128:(tt + 1) * 128, :], xacc[:, tt, :], reads=[xacc], writes=[xout.name])
    barrier(K)
    K.pop()


WN_TM = 704
NWN2 = WN_TM + 140
SEL_N = 16
import os
NSA_STOP = int(os.environ.get('NSA_STOP', '0'))
NSA_SUB = int(os.environ.get('NSA_SUB', '0'))


def nsa_phase(K, l, C, R):
    T, TC = K.T, K.TC
    NB, NTT = T // 512, T // 128
    NS = T // 64
    NCMP = (T - 32) // 16 + 1
    CT = [(c0, min(128, NCMP - c0)) for c0 in range(0, NCMP, 128)]
    VW = 64 + NS + 1
    pv = C["pv"][l]
    sc = lambda i: pv[:, i:i + 1]
    K.push()
    qT = [K.sb(f"ns_q{i}T", [128, T], BF16) for i in range(2)]
    ksT, kwT = K.sb("ns_ksT", [128, T], BF16), K.sb("ns_kwT", [128, T], BF16)
    kcT, vcT = K.sb("ns_kcT", [64, T], BF16), K.sb("ns_vcT", [64, T], BF16)
    Vs, Vw = K.sb("ns_Vs", [128, NTT, 66], BF16), K.sb("ns_Vw", [128, NTT, 66], BF16)
    gts = K.sb("ns_g", [128, NTT, 12])
    Oacc = K.sb("ns_O", [128, NTT, 128])
    blk = K.sb("ns_blk", [128, 128])
    K.dma("sp", blk[:], K.inputs["blk64"].ap(), writes=[blk])
    K.memset(Vs[:, :, 64:65], 1.0, [Vs])
    K.memset(Vw[:, :, 64:65], 1.0, [Vw])
    imp = K.sb("ns_imp", [128, NTT, NS])
    selT = K.sb("ns_selT", [64, T], BF16)
    ebuf = [K.sb(f"ns_e{i}", [128, 512], BF16) for i in range(4)]
    rv = [K.sb(f"ns_rv{i}", [128, 2]) for i in range(2)]
    kcmpT = K.sb("ns_kcmpT", [128, 256], BF16)
    Vc = K.sb("ns_Vc", [128, len(CT), VW + 1], BF16)
    K.push()
    wn = K.sb("ns_wn", [128, 16, NWN2], BF16)
    wsrc = K.inputs["wn"].ap()[l].rearrange("(kc p) n -> p kc n", p=128)
    K.dma("pool", wn[:, :, 0:512], wsrc[:, :, 0:512], writes=[wn])
    K.dma("pool", wn[:, :, 512:NWN2], wsrc[:, :, 512:NWN2], writes=[wn])
    B1 = 256
    hTb = [K.sb("ns_hT0", [128, 16, B1], BF16)] * 2
    pf = K.sb("ns_pf", [128, B1])
    for tb in range(T // B1):
        t0 = tb * B1
        hb = hTb[tb % 2]
        load_hall_block(K, hb, C, t0, TC, B1)
        specs = [(0, 128, qT[0], PV_NSAG + 0), (128, 128, qT[1], PV_NSAG + 0), (384, 128, ksT, PV_NSAG + 2), (512, 128, kwT, PV_NSAG + 3),
                 (256, 64, kcT, None), (640, 64, vcT, None)]
        if NSA_SUB == 1:
            continue
        for (c0, nc_, dst, gi) in specs:
            ps = K.psum()
            for kc in range(16):
                K.mm(ps[0:nc_, 0:B1], wn[:, kc, c0:c0 + nc_], hb[:, kc, :], kc == 0, kc == 15, [wn, hb], [ps])
            if gi is None:
                K.copy(dst[:, t0:t0 + B1], ps[0:nc_, 0:B1], [ps], [dst], eng="act")
            else:
                K.copy(pf[:], ps[:, 0:B1], [ps], [pf], eng="act")
                sq = C["fm_sq"]
                K.tt(sq[:, 0:B1], pf[:], pf[:], ALU.mult, [pf], [sq])
                ps2 = K.psum()
                K.mm(ps2[:, 0:B1], blk[:, :], sq[:, 0:B1], True, True, [blk, sq], [ps2])
                rs = C["fm_rs"]
                K.ts(rs[:, 0:B1], ps2[:, 0:B1], 1.0 / 64, EPS, ALU.mult, ALU.add, [ps2], [rs])
                K.act(rs[:, 0:B1], rs[:, 0:B1], AF.Sqrt, [rs], [rs])
                K.op("dve", lambda e: e.reciprocal(out=rs[:, 0:B1], in_=rs[:, 0:B1]), [rs], [rs])
                K.stt(dst[:, t0:t0 + B1], pf[:], sc(gi), rs[:, 0:B1], ALU.mult, ALU.mult, [pf, pv, rs], [dst])
        if NSA_SUB == 2:
            continue
        for ti in range(B1 // 128):
            gt_ = tb * (B1 // 128) + ti
            ps = K.psum()
            for kc in range(16):
                K.mm(ps[:, 0:140], hb[:, kc, ti * 128:(ti + 1) * 128], wn[:, kc, WN_TM:WN_TM + 140], kc == 0, kc == 15, [wn, hb], [ps])
            K.copy(Vs[:, gt_, 0:64], ps[:, 0:64], [ps], [Vs], eng="act")
            K.copy(Vw[:, gt_, 0:64], ps[:, 64:128], [ps], [Vw])
            K.act(gts[:, gt_, :], ps[:, 128:140], AF.Sigmoid, [ps], [gts])
    barrier(K)
    K.pop()
    K.push()
    W1 = K.sb("ns_W1", [64, 32, 128], BF16)
    w2d = K.sb("ns_w2", [128, 128], BF16)
    posT = K.sb("ns_posT", [64, 32], BF16)
    hidT = K.sb("ns_hidT", [128, 256], BF16)
    hx = [K.sb(f"ns_hx{i}", [128, 256]) for i in range(3)]
    cb = K.sb("ns_cb", [128, 2])
    kg = K.sb("ns_kg", [128, 64])
    K.dma("sp", kg[:], K.inputs["nsa_qk_gain"].ap()[l, 1:2, :].partition_broadcast(128), writes=[kg])
    ktm = K.sb("ns_ktm", [128, 128])
    kss = K.sb("ns_kss", [128, 2])
    K.memset(Vc[:, :, VW - 1:VW], 1.0, [Vc])
    for ci, (c0, ncc) in enumerate(CT):
        K.dma("pool", Vc[0:ncc, ci, 64:64 + NS], K.inputs["ovl"].ap()[c0:c0 + ncc, 0:NS], writes=[Vc])
    for i in range(2):
        src = kcT if i == 0 else vcT
        K.dma("pool", W1[:], K.inputs["nsa_cmp_w1"].ap()[l, i].rearrange("(l d) j -> d l j", d=64), writes=[W1])
        K.dma("pool", posT[:], K.inputs["cmp_posT"].ap()[l, i], writes=[posT])
        K.dma("pool", w2d[:, 0:64], K.inputs["nsa_cmp_w2"].ap()[l, i], writes=[w2d])
        K.dma("pool", w2d[:, 64:128], K.inputs["nsa_cmp_w2"].ap()[l, i], writes=[w2d])
        ps = K.psum()
        for ll in range(32):
            K.mm(ps[:, 0:1], W1[:, ll, :], posT[:, ll:ll + 1], ll == 0, ll == 31, [W1, posT], [ps])
        K.tt(cb[:, i:i + 1], ps[:, 0:1], sc(PV_CB1 + i), ALU.add, [ps, pv], [cb])
        ps = K.psum()
        for ll in range(32):
            K.mm(ps[:, 0:NCMP], W1[:, ll, :], src[:, ll: ll + 16 * (NCMP - 1) + 1: 16], ll == 0, ll == 31, [W1, src], [ps])
        x_, x2, x3 = hx
        n_ = NCMP
        K.ts(x_[:, 0:n_], ps[:, 0:n_], cb[:, i:i + 1], None, ALU.add, None, [ps, cb], [x_])
        K.tt(x2[:, 0:n_], x_[:, 0:n_], x_[:, 0:n_], ALU.mult, [x_], [x2])
        K.ts(x2[:, 0:n_], x2[:, 0:n_], 0.044715, 1.0, ALU.mult, ALU.add, [x2], [x2])
        K.tt(x2[:, 0:n_], x2[:, 0:n_], x_[:, 0:n_], ALU.mult, [x2, x_], [x2])
        K.act(x3[:, 0:n_], x2[:, 0:n_], AF.Sigmoid, [x2], [x3], scale=1.5957691216057308)
        K.tt(hidT[:, 0:n_], x_[:, 0:n_], x3[:, 0:n_], ALU.mult, [x_, x3], [hidT])
        for ci, (c0, ncc) in enumerate(CT):
            ps = K.psum()
            K.mm(ps[0:ncc, 0:128], hidT[:, c0:c0 + ncc], w2d[:, :], True, True, [hidT, w2d], [ps])
            if i == 1:
                K.copy(Vc[0:ncc, ci, 0:64], ps[0:ncc, 0:64], [ps], [Vc], eng="act")
            else:
                K.copy(ktm[0:ncc, :], ps[0:ncc, 0:128], [ps], [ktm], eng="act")
                sq = C["fm_sq"]
                K.tt(sq[0:ncc, 0:64], ktm[0:ncc, 0:64], ktm[0:ncc, 0:64], ALU.mult, [ktm], [sq])
                K.op("dve", lambda e: e.reduce_sum(out=kss[0:ncc, 0:1], in_=sq[0:ncc, 0:64], axis=AX.X), [sq], [kss])
                K.ts(kss[0:ncc, 1:2], kss[0:ncc, 0:1], 1.0 / 64, EPS, ALU.mult, ALU.add, [kss], [kss])
                K.act(kss[0:ncc, 1:2], kss[0:ncc, 1:2], AF.Sqrt, [kss], [kss])
                K.op("dve", lambda e: e.reciprocal(out=kss[0:ncc, 1:2], in_=kss[0:ncc, 1:2]), [kss], [kss])
                for hf in range(2):
                    K.stt(ktm[0:ncc, hf * 64:(hf + 1) * 64], ktm[0:ncc, hf * 64:(hf + 1) * 64], kss[0:ncc, 1:2], kg[0:ncc, :], ALU.mult, ALU.mult,
                          [ktm, kss, kg], [ktm])
                ps2 = K.psum()
                K.tr(ps2[:, 0:ncc], ktm[0:ncc, :], C["identf"][0:ncc, 0:ncc], [ktm, C["identf"]], [ps2])
                K.copy(kcmpT[:, c0:c0 + ncc], ps2[:, 0:ncc], [ps2], [kcmpT])
    barrier(K)
    K.pop()
    K.push()
    maskc = K.sb("ns_maskc", [128, len(CT), T], BF16)
    for ci, (c0, ncc) in enumerate(CT):
        K.dma("pool", maskc[0:ncc, ci, :], K.inputs["maskc"].ap()[c0:c0 + ncc, 0:T], writes=[maskc])
    nrv = [0]

    def finish_acc(acc_ap, acckey, gtile, gate_idx):
        r_ = rv[nrv[0] % 2]
        nrv[0] += 1
        K.ts(r_[:, 0:1], acc_ap[:, 64:65], 1e-30, None, ALU.max, None, [acckey], [r_])
        K.op("dve", lambda e: e.reciprocal(out=r_[:, 0:1], in_=r_[:, 0:1]), [r_], [r_])
        K.tt(r_[:, 1:2], r_[:, 0:1], gts[:, gtile, gate_idx:gate_idx + 1], ALU.mult, [r_, gts], [r_])
        return r_

    first_o = {}
    for hd in range(4):
        qt, half = qT[hd // 2], slice((hd % 2) * 64, (hd % 2) * 64 + 64)
        for tb in range(NB):
            t0 = tb * 512
            for ci, (c0, ncc) in enumerate(CT):
                ps = K.psum_from(0, 7)
                K.mm(ps[0:ncc, :], kcmpT[half, c0:c0 + ncc], qt[half, t0:t0 + 512], True, True, [kcmpT, qt], [ps])
                e_ = ebuf[ci]
                K.act(e_[0:ncc, :], ps[0:ncc, :], AF.Exp, [ps], [e_], scale=0.125)
                K.tt(e_[0:ncc, :], e_[0:ncc, :], maskc[0:ncc, ci, t0:t0 + 512], ALU.mult, [e_, maskc], [e_])
            for ti in range(4):
                gt_ = tb * 4 + ti
                ps = K.psum_from(0, 7)
                for ci, (c0, ncc) in enumerate(CT):
                    K.mm(ps[:, 0:VW], ebuf[ci][0:ncc, ti * 128:(ti + 1) * 128], Vc[0:ncc, ci, 0:VW], ci == 0, ci == len(CT) - 1, [ebuf[ci], Vc], [ps])
                r_ = rv[nrv[0] % 2]
                nrv[0] += 1
                K.ts(r_[:, 0:1], ps[:, VW - 1:VW], 1e-30, None, ALU.max, None, [ps], [r_])
                K.op("dve", lambda e: e.reciprocal(out=r_[:, 0:1], in_=r_[:, 0:1]), [r_], [r_])
                if hd == 0:
                    K.ts(imp[:, gt_, :], ps[:, 64:64 + NS], r_[:, 0:1], None, ALU.mult, None, [ps, r_], [imp])
                else:
                    K.stt(imp[:, gt_, :], ps[:, 64:64 + NS], r_[:, 0:1], imp[:, gt_, :], ALU.mult, ALU.add, [ps, r_, imp], [imp])
                if hd < 2:
                    K.tt(r_[:, 1:2], r_[:, 0:1], gts[:, gt_, hd * 3:hd * 3 + 1], ALU.mult, [r_, gts], [r_])
                    K.ts(Oacc[:, gt_, hd * 64:(hd + 1) * 64], ps[:, 0:64], r_[:, 1:2], None, ALU.mult, None, [ps, r_], [Oacc])
    barrier(K)
    K.pop()
    K.push()
    keep, cadd = K.sb("ns_keep", [128, NS]), K.sb("ns_cadd", [128, NS])
    cmp3 = K.sb("ns_cmp3", [128, NS, NS])
    cnt, sel, ok = K.sb("ns_cnt", [128, NS]), K.sb("ns_sel", [128, NS]), K.sb("ns_ok", [128, NS])
    for gt_ in range(NTT):
        K.dma("sp", keep[:], K.inputs["tk_keep"].ap()[gt_ * 128:(gt_ + 1) * 128, 0:NS], writes=[keep])
        K.dma("sp", cadd[:], K.inputs["tk_cadd"].ap()[gt_ * 128:(gt_ + 1) * 128, 0:NS], writes=[cadd])
        im = imp[:, gt_, :]
        K.tt(im, im, keep[:], ALU.mult, [imp, keep], [imp])
        K.tt(im, im, cadd[:], ALU.add, [imp, cadd], [imp])
        K.tt(cmp3[:], im.unsqueeze(1).to_broadcast([128, NS, NS]), im.unsqueeze(2).to_broadcast([128, NS, NS]), ALU.is_gt, [imp], [cmp3])
        K.op("dve", lambda e: e.reduce_sum(out=cnt[:], in_=cmp3[:], axis=AX.X), [cmp3], [cnt])
        K.ts(sel[:], cnt[:], float(SEL_N) - 0.5, None, ALU.is_lt, None, [cnt], [sel])
        K.ts(ok[:], im, -1e8, None, ALU.is_gt, None, [imp], [ok])
        K.tt(sel[:], sel[:], ok[:], ALU.mult, [sel, ok], [sel])
        ps = K.psum_from(0, 7)
        K.tr(ps[0:NS, 0:128], sel[:], C["identf"][:], [sel, C["identf"]], [ps])
        K.copy(selT[0:NS, gt_ * 128:(gt_ + 1) * 128], ps[0:NS, 0:128], [ps], [selT], eng="act")
    barrier(K)
    K.pop()
    K.push()
    E2 = K.sb("ns_E2", [64, NTT, 128], BF16)
    K.dma("pool", E2[0:NS], K.inputs["e2"].ap()[0:NS, 0:NTT, :], writes=[E2])
    dmask = K.sb("ns_dmask", [128, 5, 512], BF16)
    K.dma("pool", dmask[:], K.inputs["dmask"].ap().rearrange("a p t -> p a t"), writes=[dmask])
    accb = K.ps[7]
    for hd in range(2):
        half = slice(hd * 64, hd * 64 + 64)
        for tb in range(NB):
            t0 = tb * 512
            njt = 4 * tb + 4
            for jt in range(njt):
                ps = K.psum_from(0, 4)
                K.mm(ps[:, :], ksT[half, jt * 128:(jt + 1) * 128], qT[0][half, t0:t0 + 512], True, True, [ksT, qT[0]], [ps])
                e_ = ebuf[jt % 2]
                K.act(e_[:, :], ps[:, :], AF.Exp, [ps], [e_], scale=0.125)
                pm = K.psum_from(0, 4)
                K.mm(pm[:, :], E2[0:NS, jt, :], selT[0:NS, t0:t0 + 512], True, True, [E2, selT], [pm])
                em = ebuf[2 + jt % 2]
                K.tt(em[:, :], e_[:, :], pm[:, :], ALU.mult, [e_, pm], [em])
                dd = jt - 4 * tb
                if dd >= 0:
                    K.tt(em[:, :], em[:, :], dmask[:, dd, :], ALU.mult, [em, dmask], [em])
                for ti in range(max(dd, 0), 4):
                    K.mm(K.ps[4 + ti][:, 0:65], em[:, ti * 128:(ti + 1) * 128], Vs[:, jt, 0:65], jt == 0, jt == 4 * tb + ti, [em, Vs], [K.ps[4 + ti]])
            for ti in range(4):
                gt_ = tb * 4 + ti
                a_ = K.ps[4 + ti][:, 0:65]
                r_ = finish_acc(a_, K.ps[4 + ti].name, gt_, hd * 3 + 1)
                K.stt(Oacc[:, gt_, hd * 64:(hd + 1) * 64], a_[:, 0:64], r_[:, 1:2], Oacc[:, gt_, hd * 64:(hd + 1) * 64], ALU.mult, ALU.add,
                      [K.ps[4 + ti], r_, Oacc], [Oacc])
    for hd in range(2):
        half = slice(hd * 64, hd * 64 + 64)
        for gt_ in range(NTT):
            jts = list(range(max(0, gt_ - 4), gt_ + 1))
            for jt in jts:
                ps = K.psum_from(0, 7)
                K.mm(ps[:, 0:128], kwT[half, jt * 128:(jt + 1) * 128], qT[0][half, gt_ * 128:(gt_ + 1) * 128], True, True, [kwT, qT[0]], [ps])
                e_ = ebuf[jt % 4]
                K.act(e_[:, 0:128], ps[:, 0:128], AF.Exp, [ps], [e_], scale=0.125)
                if jt == gt_:
                    K.tt(e_[:, 0:128], e_[:, 0:128], dmask[:, 0, 0:128], ALU.mult, [e_, dmask], [e_])
                elif jt == gt_ - 4:
                    K.tt(e_[:, 0:128], e_[:, 0:128], dmask[:, 4, 0:128], ALU.mult, [e_, dmask], [e_])
                K.mm(accb[:, 0:65], e_[:, 0:128], Vw[:, jt, 0:65], jt == jts[0], jt == jts[-1], [e_, Vw], ["ns_accb"])
            a_ = accb[:, 0:65]
            r_ = finish_acc(a_, "ns_accb", gt_, hd * 3 + 2)
            K.stt(Oacc[:, gt_, hd * 64:(hd + 1) * 64], a_[:, 0:64], r_[:, 1:2], Oacc[:, gt_, hd * 64:(hd + 1) * 64], ALU.mult, ALU.add,
                  ["ns_accb", r_, Oacc], [Oacc])
    ot = [K.sb(f"ns_ot{i}", [128, 512]) for i in range(2)]
    for tb in range(NB):
        ps = K.psum_from(0, 7)
        for ti in range(4):
            K.tr(ps[:, ti * 128:(ti + 1) * 128], Oacc[:, tb * 4 + ti, :], C["identf"][:], [Oacc, C["identf"]], [ps])
        o_ = ot[tb % 2]
        K.copy(o_[:], ps[:, :], [ps], [o_], eng="act")
        ysrc_write(K, R["yN"], slice(0, 128), tb * 512, 512, o_, o_)
    barrier(K)
    K.pop()
    K.pop()


INPUT_SHAPES = lambda T: {
    "x": [T // 4, D], "mem": [MEM_LEN, D], "w_in": [2, D, N_IN], "norm_mix": [2, D], "norm_ffn": [2, D], "norm_mem": [2, D],
    "mem_wkv": [2, D, 1024], "pool_w": [2, 4, 128, 512], "pool_scale": [2, D], "w_branch": [2, 4, 512, D], "w_out": [2, D, D],
    "ffn_w1": [1, D, D_FF], "ffn_w3": [1, D, D_FF], "ffn_w2": [1, D_FF, D], "moe_router": [1, D, 8],
    "moe_w1": [1, 8, D, E_FF], "moe_w3": [1, 8, D, E_FF], "moe_w2": [1, 8, E_FF, D],
    "nsa_cmp_w1": [2, 2, 2048, 128], "nsa_cmp_w2": [2, 2, 128, 64], "cmp_posT": [2, 2, 64, 32], "nsa_qk_gain": [2, 4, 64],
    "ident": [128, 128], "blk64": [128, 128], "ovl": [256, 64], "maskc": [256, T], "tk_keep": [T, 64], "tk_cadd": [T, 64],
    "e2": [64, 32, 128], "dmask": [5, 128, 512], "onehot": [128, 8], "invcnt": [4, T // 4], "pvec": [2, 128, NV],
    "wr": [2, D, 1024], "wn": [2, D, NWN], "lora": [2, 128, 5, 128],
}


def build(T, dbg=False, nlayers=DEPTH):
    K = KB(T)
    TC = K.TC
    out = K.dout("out", [TC, D])
    C = setup_common(K)
    xres = K.dscr("xres", [TC, D])
    K.dma("sp", xres.ap(), K.inputs["x"].ap(), writes=["xres"])
    C["hTx"] = K.sb("hTx", [128, 16, HALO + TC], BF16)
    alloc_gather(K, C)
    R = alloc_scratch(K)
    dbg_outs = []
    barrier(K)
    for l in range(nlayers):
        K.push()
        S = {n: K.sb(n, [128, 4, TC], BF16) for n in ("y_convT", "pooledT", "y_memT")}
        K.push()
        C["halo_cand"] = K.sb("halo_cand", [128, 4, 16, HALO], BF16)
        phase_norm_gather(K, l, C, xres)
        barrier(K)
        K.pop()
        local_mixers(K, l, C, S)
        rwkv_phase(K, l, C, R)
        ygather(K, R["yB"])
        nsa_phase(K, l, C, R)
        ygather(K, R["yN"])
        S["y_rwkvT"] = K.sb("y_rwkvT", [128, 4, TC], BF16)
        S["y_nsaT"] = K.sb("y_nsaT", [128, 4, TC], BF16)
        K.push()
        C["sel_cand"] = K.sb("sel_cand", [128, 4, TC])
        C["sel_acc"] = K.sb("sel_acc", [128, TC])
        select_chunk(K, S["y_rwkvT"], R["yB"], C)
        select_chunk(K, S["y_nsaT"], R["yN"], C)
        barrier(K)
        K.pop()
        if dbg and l == 0:
            for nm, Y in (("d_yN", R["yN"]), ("d_yB", R["yB"])):
                o = K.dout(nm, [512, T])
                for j in range(4):
                    K.dma("sp", o.ap()[:, j * TC:(j + 1) * TC], Y["all"][j].ap(), reads=[Y["key"] + "_all"], writes=[nm])
                dbg_outs.append(nm)
        merge_phase(K, l, C, S, xres)
        barrier(K)
        K.pop()
        if dbg and l == 0:
            o = K.dout("d_xmix", [TC, D])
            K.dma("sp", o.ap(), xres.ap(), reads=["xres"], writes=["d_xmix"])
            dbg_outs.append("d_xmix")
        last = (l == nlayers - 1)
        ffn_phase(K, l, C, xres, out if last else xres)
        if dbg and l == 0 and not last:
            o = K.dout("d_xout0", [TC, D])
            K.dma("sp", o.ap(), xres.ap(), reads=["xres"], writes=["d_xout0"])
            dbg_outs.append("d_xout0")
    K.fw.finish(["out"] + dbg_outs)
    return K


_CACHE = {}


def kernel(**inputs):
    T = int(np.asarray(inputs["x"]).shape[1])
    if T not in _CACHE:
        _CACHE[T] = build(T)
    K = _CACHE[T]
    maps = host_prep(inputs, T)
    maps = [{k: m[k] for k in K.inputs} for m in maps]
    res = run_bass_kernel_spmd(K.nc, maps, core_ids=list(range(NCORES)))
    TC = T // 4
    outp = np.zeros((2, T, D), np.float32)
    for c in range(NCORES):
        outp[c // 4, (c % 4) * TC:(c % 4 + 1) * TC] = res.results[c]["out"]
    return outp
```

```python
import numpy as np
import concourse.bass as bass
import concourse.mybir as mybir
from concourse.bass_utils import run_bass_kernel_spmd

F32 = mybir.dt.float32
BF16 = mybir.dt.bfloat16
ALU = mybir.AluOpType
AF = mybir.ActivationFunctionType
AX = mybir.AxisListType

D = 2048
NCORES = 8
MEM_LEN = 256
DEPTH = 2
D_FF = 5632
E_FF = 2816
N_EXP = 8
EPS = 1e-6
O_RWKV, O_NSA, O_CONV, O_POOL, O_MEM, O_GATE = 0, 1984, 3288, 4824, 5336, 5848
N_IN = 16088
HALO = 16


class FW:
    def __init__(self, nc, n_dma_sems=20):
        self.nc = nc
        self.eng = {"pe": nc.tensor, "act": nc.scalar, "dve": nc.vector, "pool": nc.gpsimd, "sp": nc.sync}
        self.sem = {}
        self.cnt = {}
        for e in self.eng:
            self.sem[e] = nc.semaphore("s_" + e).__enter__()
            self.cnt[e] = 0
        self.dsem = {}
        for q in ("sp", "pool", "act"):
            lst = [[nc.semaphore(f"d_{q}{i}").__enter__(), 0] for i in range(n_dma_sems)]
            self.dsem[q] = [lst, 0]
        self.ccsem = nc.semaphore("ccsem").__enter__()
        self.cccnt = 0
        self.seen = {e: {} for e in self.eng}
        self.lastw = {}
        self.readers = {}
        self.ninstr = 0
        self.excl = {"ns_accb"}

    @staticmethod
    def _k(x):
        return x if isinstance(x, str) else x.name

    def _wait(self, e, tok):
        if tok is None:
            return
        sem, val, src = tok
        if src == "pe" and e == "pe":
            return
        k = id(sem)
        if self.seen[e].get(k, 0) >= val:
            return
        self.eng[e].wait_ge(sem, val)
        self.seen[e][k] = val

    def _deps(self, e, reads, writes):
        for k in reads:
            self._wait(e, self.lastw.get(k))
        for k in writes:
            self._wait(e, self.lastw.get(k))
            for t in self.readers.get(k, ()):
                self._wait(e, t)

    def _record(self, tok, reads, writes):
        for k in reads:
            self.readers.setdefault(k, []).append(tok)
        for k in writes:
            self.lastw[k] = tok
            self.readers[k] = []
        self.ninstr += 1

    def op(self, e, fn, reads=(), writes=()):
        reads = [self._k(x) for x in reads]
        writes = [self._k(x) for x in writes]
        for k in reads:
            if (k.startswith("psb") or k in self.excl) and k not in writes:
                writes.append(k)
        self._deps(e, reads, writes)
        ins = fn(self.eng[e])
        self.cnt[e] += 1
        ins.then_inc(self.sem[e], 1)
        tok = (self.sem[e], self.cnt[e], e)
        self._record(tok, reads, writes)
        return tok

    def dma(self, q, out, in_, reads=(), writes=(), **kw):
        reads = [self._k(x) for x in reads]
        writes = [self._k(x) for x in writes]
        lst, idx = self.dsem[q]
        ent = lst[idx % len(lst)]
        self.dsem[q][1] += 1
        sem, tgt = ent
        if tgt > 0:
            self._wait(q, (sem, tgt, "dma"))
        self._deps(q, reads, writes)
        self.eng[q].dma_start(out=out, in_=in_, **kw).then_inc(sem, 16)
        ent[1] = tgt + 16
        tok = (sem, tgt + 16, "dma")
        self._record(tok, reads, writes)
        return tok

    def allgather(self, src, dst, groups, reads=(), writes=()):
        reads = [self._k(x) for x in reads]
        writes = [self._k(x) for x in writes]
        self._deps("pool", reads, writes)
        self.nc.gpsimd.collective_compute("AllGather", ALU.bypass, replica_groups=groups,
                                          ins=[src.ap().opt()], outs=[dst.ap().opt()]).then_inc(self.ccsem)
        self.cccnt += 1
        tok = (self.ccsem, self.cccnt, "cc")
        self._record(tok, reads, writes)
        return tok

    def finish(self, keys):
        for k in keys:
            self._wait("sp", self.lastw.get(k))


class LazyInputs(dict):
    def __init__(self, kb):
        super().__init__()
        self.kb = kb

    def __missing__(self, name):
        shp = INPUT_SHAPES(self.kb.T)[name]
        t = self.kb.nc.dram_tensor(name, list(shp), F32, kind="ExternalInput")
        self[name] = t
        return t


class KB:
    def __init__(self, T):
        self.T = T
        self.TC = T // 4
        self.NT = self.TC // 128
        self.nc = bass.Bass("TRN2", target_bir_lowering=False)
        self.fw = FW(self.nc)
        self.inputs = LazyInputs(self)
        self.outputs = {}
        self._uid = 0
        self._psn = 0
        self.ps = [self.nc.psum_tensor(f"psb{i}", [128, 512], F32).__enter__() for i in range(8)]
        self.scopes = []

    def din(self, name, shape, dtype=F32):
        t = self.nc.dram_tensor(name, list(shape), dtype, kind="ExternalInput")
        self.inputs[name] = t
        return t

    def dout(self, name, shape, dtype=F32):
        t = self.nc.dram_tensor(name, list(shape), dtype, kind="ExternalOutput")
        self.outputs[name] = t
        return t

    def dscr(self, name, shape, dtype=F32):
        return self.nc.dram_tensor(name, list(shape), dtype)

    def sb(self, name, shape, dtype=F32):
        self._uid += 1
        g = self.nc.sbuf_tensor(f"{name}_u{self._uid}", list(shape), dtype)
        t = g.__enter__()
        if self.scopes:
            self.scopes[-1].append(g)
        return t

    def push(self):
        self.scopes.append([])

    def pop(self):
        for g in reversed(self.scopes.pop()):
            g.__exit__(None, None, None)

    def psum(self):
        p = self.ps[self._psn % 8]
        self._psn += 1
        return p

    def psum_from(self, lo, n):
        p = self.ps[lo + self._psn % n]
        self._psn += 1
        return p

    def psum4(self):
        p = self.ps[4 + self._psn % 4]
        self._psn += 1
        return p

    def op(self, e, fn, reads=(), writes=()):
        return self.fw.op(e, fn, reads, writes)

    def dma(self, q, out, in_, reads=(), writes=(), **kw):
        return self.fw.dma(q, out, in_, reads, writes, **kw)

    def mm(self, out, lhsT, rhs, start, stop, reads, writes):
        return self.fw.op("pe", lambda e: e.matmul(out, lhsT, rhs, start=start, stop=stop), reads, writes)

    def tr(self, out, in_, ident, reads, writes):
        return self.fw.op("pe", lambda e: e.transpose(out, in_, ident), reads, writes)

    def act(self, out, in_, func, reads, writes, bias=None, scale=None, accum_out=None):
        kw = {}
        if bias is not None:
            kw["bias"] = bias
        if scale is not None:
            kw["scale"] = scale
        if accum_out is not None:
            kw["accum_out"] = accum_out
        return self.fw.op("act", lambda e: e.activation(out=out, in_=in_, func=func, **kw), reads, writes)

    def tt(self, out, in0, in1, op, reads, writes, eng="dve"):
        return self.fw.op(eng, lambda e: e.tensor_tensor(out=out, in0=in0, in1=in1, op=op), reads, writes)

    def ts(self, out, in0, s1, s2, op0, op1, reads, writes, eng="dve"):
        if s2 is None:
            s2, op1 = 0.0, ALU.add
        return self.fw.op(eng, lambda e: e.tensor_scalar(out=out, in0=in0, scalar1=s1, scalar2=s2, op0=op0, op1=op1), reads, writes)

    def stt(self, out, in0, scalar, in1, op0, op1, reads, writes, eng="dve"):
        return self.fw.op(eng, lambda e: e.scalar_tensor_tensor(out=out, in0=in0, scalar=scalar, in1=in1, op0=op0, op1=op1), reads, writes)

    def copy(self, out, in_, reads, writes, eng="dve"):
        if eng == "act":
            return self.fw.op("act", lambda e: e.copy(out=out, in_=in_), reads, writes)
        return self.fw.op(eng, lambda e: e.tensor_copy(out=out, in_=in_), reads, writes)

    def memset(self, ap, val, writes, eng="dve"):
        return self.fw.op(eng, lambda e: e.memset(ap, val), (), writes)


def bcast_rows(dram_ap_1d_row, nparts):
    return dram_ap_1d_row.partition_broadcast(nparts)


def barrier(K):
    fw = K.fw
    toks = [(fw.sem[e], fw.cnt[e], e) for e in fw.eng if fw.cnt[e] > 0]
    for q in fw.dsem:
        for sem, tgt in fw.dsem[q][0]:
            if tgt > 0:
                toks.append((sem, tgt, "dma"))
    if fw.cccnt:
        toks.append((fw.ccsem, fw.cccnt, "cc"))
    for e in fw.eng:
        for t in toks:
            if t[2] == e:
                continue
            fw._wait(e, t)


def rmsnorm_T(K, x_rows, gbc, hT, col0, ntiles, C, tag):
    for tt in range(ntiles):
        b = tt % 2
        xt, junk, ss, hb = C["xt"][b], C["junk"][b], C["ss"][b], C["hb"][b]
        K.dma("sp", xt[:], x_rows[tt * 128:(tt + 1) * 128, :], writes=[xt])
        K.memset(ss[:], 0.0, [ss])
        K.act(junk[:], xt[:], AF.Square, [xt], [junk, ss], accum_out=ss[:, 0:1])
        K.ts(ss[:, 1:2], ss[:, 0:1], 1.0 / D, EPS, ALU.mult, ALU.add, [ss], [ss])
        K.act(ss[:, 1:2], ss[:, 1:2], AF.Sqrt, [ss], [ss])
        K.op("dve", lambda e: e.reciprocal(out=ss[:, 1:2], in_=ss[:, 1:2]), [ss], [ss])
        K.stt(hb[:], xt[:], ss[:, 1:2], gbc[:], ALU.mult, ALU.mult, [xt, ss, gbc], [hb])
        for grp in range(2):
            ps = K.psum()
            psb = ps[:].bitcast(BF16)
            for i in range(8):
                kc = grp * 8 + i
                K.tr(psb[:, i * 128:(i + 1) * 128], hb[:, kc * 128:(kc + 1) * 128], C["identb"][:], [hb, C["identb"]], [ps])
            K.copy(hT[:, grp * 8:(grp + 1) * 8, col0 + tt * 128: col0 + (tt + 1) * 128],
                   psb.rearrange("p (a b) -> p a b", a=8), [ps], [hT], eng="act" if grp == 0 else "dve")


def load_w(K, wt, W, c0, ncols, kchunks=16, key=None):
    src = W.rearrange("(kc p) n -> p kc n", p=128)[:, :, c0:c0 + ncols]
    K.dma("pool", wt[:, 0:kchunks, 0:ncols], src, writes=[key or wt])


def fm_colsum_norm(K, out_bf, outkey, src_f32, tagkey, n, nparts, ones_f, gain_col, inv_n, C):
    sq = C["fm_sq"]
    K.tt(sq[0:nparts, 0:n], src_f32, src_f32, ALU.mult, [tagkey], [sq])
    ps = K.psum()
    K.mm(ps[0:nparts, 0:n], ones_f[0:nparts, 0:nparts], sq[0:nparts, 0:n], True, True, [sq, ones_f], [ps])
    rs = C["fm_rs"]
    K.ts(rs[0:nparts, 0:n], ps[0:nparts, 0:n], inv_n, EPS, ALU.mult, ALU.add, [ps], [rs])
    K.act(rs[0:nparts, 0:n], rs[0:nparts, 0:n], AF.Sqrt, [rs], [rs])
    K.op("dve", lambda e: e.reciprocal(out=rs[0:nparts, 0:n], in_=rs[0:nparts, 0:n]), [rs], [rs])
    K.stt(out_bf, src_f32, gain_col, rs[0:nparts, 0:n], ALU.mult, ALU.mult, [tagkey, rs], [outkey])


PV_CONV = 0
PV_MEMQG = 12
PV_MEMKG = 13
PV_MU = 14
PV_W0, PV_A0, PV_KK, PV_KA, PV_RK, PV_LNW, PV_LNB, PV_V0, PV_C1 = 22, 23, 24, 25, 26, 27, 28, 29, 30
PV_NSAG = 31
PV_CB1 = 35
NV = 40


def local_mixers(K, l, C, S):
    TC, W = K.TC, HALO + K.TC
    hTx, pv = C["hTx"], C["pv"][l]
    w_in = K.inputs["w_in"].ap()[l]
    tokblocks = [(0, HALO)] + [(HALO + i * 512, min(512, TC - i * 512)) for i in range((TC + 511) // 512)]
    locblocks = tokblocks[1:]
    K.push()
    wts = [K.sb(f"lm_wt{i}", [128, 16, 512], BF16) for i in range(2)]
    wi = [0]

    def nextw():
        w = wts[wi[0] % 2]
        wi[0] += 1
        return w

    def proj(wt, c_lo, ncol, dst, blocks, evac=None):
        for (c0, n) in blocks:
            ps = K.psum()
            for kc in range(16):
                K.mm(ps[0:ncol, 0:n], wt[:, kc, c_lo:c_lo + ncol], hTx[:, kc, c0:c0 + n], kc == 0, kc == 15, [wt, hTx], [ps])
            K.copy(dst[0:ncol, c0:c0 + n], ps[0:ncol, 0:n], [ps], [dst], eng="act")

    K.push()
    bt, ct, xt_ = (K.sb(n, [128, W]) for n in ("cv_b", "cv_c", "cv_x"))
    z, acc = K.sb("cv_z", [128, W]), K.sb("cv_acc", [128, TC])
    for j in range(4):
        wt = nextw()
        for i in range(3):
            src = w_in.rearrange("(kc p) n -> p kc n", p=128)[:, :, O_CONV + i * 512 + j * 128: O_CONV + i * 512 + (j + 1) * 128]
            K.dma("pool", wt[:, :, i * 128:(i + 1) * 128], src, writes=[wt])
        proj(wt, 0, 128, bt, locblocks)
        proj(wt, 128, 128, ct, tokblocks)
        proj(wt, 256, 128, xt_, tokblocks)
        K.tt(z[:], ct[:], xt_[:], ALU.mult, [ct, xt_], [z])
        cw = lambda i: pv[:, PV_CONV + j * 3 + i: PV_CONV + j * 3 + i + 1]
        K.ts(acc[:], z[:, HALO:W], cw(2), None, ALU.mult, None, [z, pv], [acc])
        K.stt(acc[:], z[:, HALO - 1:W - 1], cw(1), acc[:], ALU.mult, ALU.add, [z, pv, acc], [acc])
        K.stt(acc[:], z[:, HALO - 2:W - 2], cw(0), acc[:], ALU.mult, ALU.add, [z, pv, acc], [acc])
        K.tt(S["y_convT"][:, j, :], bt[:, HALO:W], acc[:], ALU.mult, [bt, acc], [S["y_convT"]])
    barrier(K)
    K.pop()
    K.push()
    acc = K.sb("pl_acc", [128, TC])
    wt = nextw()
    load_w(K, wt, w_in, O_POOL, 512)
    pa, pb, pu = K.sb("pl_a", [128, W]), K.sb("pl_b", [128, W]), K.sb("pl_u", [128, W])
    invc = K.sb("pl_invc", [128, TC])
    for g in range(4):
        proj(wt, g * 128, 128, pu, tokblocks)
        K.dma("sp", invc[:], K.inputs["invcnt"].ap()[g:g + 1, :].partition_broadcast(128), writes=[invc])
        cur, oth = pu, pa
        for si, sh in enumerate([1, 2, 4, 8][:g + 1]):
            K.tt(oth[:, sh:W], cur[:, sh:W], cur[:, 0:W - sh], ALU.add, [cur], [oth])
            cur, oth = oth, (pb if oth is pa else pa)
        K.tt(acc[:], cur[:, HALO:W], invc[:], ALU.mult, [cur, invc], [acc])
        K.tt(S["pooledT"][:, g, :], acc[:], pu[:, HALO:W], ALU.subtract, [acc, pu], [S["pooledT"]])
    barrier(K)
    K.pop()
    memT = K.sb("mm_memT", [128, 16, MEM_LEN], BF16)
    gm = K.sb("mm_g", [128, D])
    K.dma("sp", gm[:], K.inputs["norm_mem"].ap()[l:l + 1, :].partition_broadcast(128), writes=[gm])
    K.push()
    rn_alloc(K, C)
    rmsnorm_T(K, K.inputs["mem"].ap(), gm, memT, 0, 2, C, "mem")
    barrier(K)
    K.pop()
    wkv = K.inputs["mem_wkv"].ap()[l]
    kT = K.sb("mm_kT", [128, 4, MEM_LEN], BF16)
    vsb = K.sb("mm_v", [128, 2, 512], BF16)
    kf = K.sb("mm_kf", [128, 512])
    wt = nextw()
    load_w(K, wt, wkv, 0, 512)
    for h in range(4):
        ps = K.psum()
        for kc in range(16):
            K.mm(ps[:, 0:MEM_LEN], wt[:, kc, h * 128:(h + 1) * 128], memT[:, kc, :], kc == 0, kc == 15, [wt, memT], [ps])
        K.copy(kf[:, 0:MEM_LEN], ps[:, 0:MEM_LEN], [ps], [kf], eng="act")
        fm_colsum_norm(K, kT[:, h, :], kT, kf[:, 0:MEM_LEN], kf, MEM_LEN, 128, C["ones_f"], pv[:, PV_MEMKG:PV_MEMKG + 1], 1.0 / 128, C)
    wt = nextw()
    load_w(K, wt, wkv, 512, 512)
    for mt in range(2):
        ps = K.psum()
        for kc in range(16):
            K.mm(ps[:, :], memT[:, kc, mt * 128:(mt + 1) * 128], wt[:, kc, :], kc == 0, kc == 15, [wt, memT], [ps])
        K.copy(vsb[:, mt, :], ps[:, :], [ps], [vsb], eng="act")
    wt = nextw()
    load_w(K, wt, w_in, O_MEM, 512)
    qf, qT = K.sb("mm_qf", [128, W]), K.sb("mm_qT", [128, 512], BF16)
    es = [K.sb(f"mm_e{i}", [128, 512], BF16) for i in range(2)]
    rden = K.sb("mm_rden", [128, 512])
    for h in range(4):
        proj(wt, h * 128, 128, qf, locblocks)
        for (c0, n) in locblocks:
            fm_colsum_norm(K, qT[:, 0:n], qT, qf[:, c0:c0 + n], qf, n, 128, C["ones_f"], pv[:, PV_MEMQG:PV_MEMQG + 1], 1.0 / 128, C)
            for mt in range(2):
                ps = K.psum()
                K.mm(ps[:, 0:n], kT[:, h, mt * 128:(mt + 1) * 128], qT[:, 0:n], True, True, [kT, qT], [ps])
                K.act(es[mt][:, 0:n], ps[:, 0:n], AF.Exp, [ps], [es[mt]], scale=float(128 ** -0.5))
            po, pd = K.psum(), K.psum()
            for mt in range(2):
                K.mm(po[:, 0:n], vsb[:, mt, h * 128:(h + 1) * 128], es[mt][:, 0:n], mt == 0, mt == 1, [vsb, es[mt]], [po])
            for mt in range(2):
                K.mm(pd[:, 0:n], C["ones_b"][:, :], es[mt][:, 0:n], mt == 0, mt == 1, [C["ones_b"], es[mt]], [pd])
            K.op("dve", lambda e: e.reciprocal(out=rden[:, 0:n], in_=pd[:, 0:n]), [pd], [rden])
            K.tt(S["y_memT"][:, h, c0 - HALO:c0 - HALO + n], po[:, 0:n], rden[:, 0:n], ALU.mult, [po, rden], [S["y_memT"]])
    barrier(K)
    K.pop()


GROUPS = [[0, 1, 2, 3], [4, 5, 6, 7]]


def alloc_gather(K, C):
    TC = K.TC
    C["hT_src"] = [K.dscr(f"hT_src{q}", [256, TC], BF16) for q in range(8)]
    C["hT_all"] = [K.dscr(f"hT_all{q}", [4 * 256, TC], BF16) for q in range(8)]


def alloc_scratch(K):
    T, TC = K.T, K.TC
    R = {n: K.dscr(n, [128, T]) for n in ("fm_r", "fm_w", "fm_nkk", "fm_kp", "fm_g", "fm_bonus", "fm_vfirst")}
    R["tm_kka"] = K.dscr("tm_kka", [T, 128])
    R["tm_v"] = K.dscr("tm_v", [T, 128])
    for nm in ("yB", "yN"):
        R[nm] = {"key": nm, "src": [K.dscr(f"{nm}_src{j}", [128, TC]) for j in range(4)],
                 "all": [K.dscr(f"{nm}_all{j}", [512, TC]) for j in range(4)]}
    return R


def setup_common(K):
    C = {}
    C["identf"] = K.sb("identf", [128, 128])
    C["identb"] = K.sb("identb", [128, 128], BF16)
    C["ones_f"] = K.sb("ones_f", [128, 128])
    C["ones_b"] = K.sb("ones_b", [128, 128], BF16)
    K.dma("sp", C["identf"][:], K.inputs["ident"].ap(), writes=[C["identf"]])
    K.copy(C["identb"][:], C["identf"][:], [C["identf"]], [C["identb"]])
    K.memset(C["ones_f"][:], 1.0, [C["ones_f"]])
    K.memset(C["ones_b"][:], 1.0, [C["ones_b"]])
    C["fm_sq"] = K.sb("fm_sq", [128, 512])
    C["fm_rs"] = K.sb("fm_rs", [128, 512])
    C["oh"] = K.sb("oh", [128, 8])
    K.dma("sp", C["oh"][:], K.inputs["onehot"].ap(), writes=[C["oh"]])
    C["pv"] = []
    for l in range(DEPTH):
        t = K.sb(f"pv{l}", [128, NV])
        K.dma("sp", t[:], K.inputs["pvec"].ap()[l], writes=[t])
        K.ts(t[:, PV_C1:PV_C1 + 1], t[:, PV_KA:PV_KA + 1], -1.0, 1.0, ALU.mult, ALU.add, [t], [t])
        C["pv"].append(t)
    return C


def rn_alloc(K, C):
    C["xt"] = [K.sb(f"rn_xt{i}", [128, D]) for i in range(2)]
    C["junk"] = [K.sb(f"rn_junk{i}", [128, D], BF16) for i in range(2)]
    C["ss"] = [K.sb(f"rn_ss{i}", [128, 2]) for i in range(2)]
    C["hb"] = [K.sb(f"rn_hb{i}", [128, D], BF16) for i in range(2)]
    C["gbc"] = K.sb("gbc", [128, D])


def phase_norm_gather(K, l, C, xres):
    TC = K.TC
    hTx = C["hTx"]
    K.push()
    rn_alloc(K, C)
    K.dma("sp", C["gbc"][:], K.inputs["norm_mix"].ap()[l:l + 1, :].partition_broadcast(128), writes=[C["gbc"]])
    rmsnorm_T(K, xres.ap(), C["gbc"], hTx, HALO, K.NT, C, "mix")
    barrier(K)
    K.pop()
    for q in range(NHQ):
        K.dma("sp", C["hT_src"][q].ap().rearrange("(kc p) t -> p kc t", p=128), hTx[:, 2 * q:2 * q + 2, HALO:HALO + TC], reads=[hTx], writes=["hT_src"])
    for q in range(NHQ):
        K.fw.allgather(C["hT_src"][q], C["hT_all"][q], GROUPS, reads=["hT_src"], writes=["hT_all"])
    cand = C["halo_cand"]
    C["_hall_dst"] = cand
    for r in range(4):
        hall_read(K, cand[:, r], C, r, TC - HALO, HALO)
    oh = C["oh"]
    K.ts(hTx[:, :, 0:HALO], cand[:, 0], oh[:, 4:5], None, ALU.mult, None, [cand, oh], [hTx])
    for r in range(1, 4):
        K.stt(hTx[:, :, 0:HALO], cand[:, r], oh[:, 4 + r:5 + r], hTx[:, :, 0:HALO], ALU.mult, ALU.add, [cand, oh, hTx], [hTx])


def rwkv_cols(hp):
    cols = []
    pad = lambda a, n: list(a) + [-1] * (n - len(a))
    cols += list(range(128 * hp, 128 * hp + 128))
    cols += list(range(512 + 128 * hp, 512 + 128 * hp + 128))
    cols += list(range(1024 + 128 * hp, 1024 + 128 * hp + 128))
    cols += pad(range(1536, 1632), 128)
    cols += pad(range(1632, 1728), 128)
    cols += list(range(1728, 1984))
    cols += pad(range(1984, 2048), 128)
    return np.array(cols)


def nsa_cols(hp):
    hk = hp // 2
    mine = [2 * hp, 2 * hp + 1]
    oth = [h for h in range(4 * hk, 4 * hk + 4) if h not in mine]
    heads = mine + oth
    grp = lambda i: list(range(512 + 128 * i + 64 * hk, 512 + 128 * i + 64 * hk + 64))
    cols = []
    for h in heads:
        cols += list(range(64 * h, 64 * h + 64))
    cols += grp(0) + grp(0) + grp(2) + grp(2) + grp(4) + grp(4)
    cols += grp(1)
    cols += grp(3) + grp(5)
    for h in heads:
        cols += [512 + 768 + 3 * h + i for i in range(3)]
    return np.array(cols)


NWN = 844


def host_prep(inp, T):
    TC = T // 4
    f = lambda a: np.ascontiguousarray(np.asarray(a, dtype=np.float32))
    sh = {k: f(inp[k]) for k in ("w_in", "norm_mix", "norm_ffn", "norm_mem", "mem_wkv", "pool_w", "pool_scale", "w_branch",
                                 "w_out", "ffn_w1", "ffn_w3", "ffn_w2", "moe_router", "moe_w1", "moe_w3", "moe_w2",
                                 "nsa_cmp_w1", "nsa_cmp_w2", "nsa_cmp_pos")}
    sh["ident"] = np.eye(128, dtype=np.float32)
    sh["blk64"] = np.kron(np.eye(2), np.ones((64, 64))).astype(np.float32)
    sh["cmp_posT"] = np.ascontiguousarray(np.transpose(sh["nsa_cmp_pos"], (0, 1, 3, 2)))
    sh["nsa_qk_gain"] = f(inp["nsa_qk_gain"])
    NS, NTT = T // 64, T // 128
    cc = np.arange(256)[:, None] * 16
    ss_ = np.arange(64)[None, :] * 64
    sh["ovl"] = (np.clip(np.minimum(cc + 32, ss_ + 64) - np.maximum(cc, ss_), 0, None) / 32.0).astype(np.float32)
    tt_ = np.arange(T)
    sh["maskc"] = ((np.arange(256)[:, None] * 16 + 31) <= tt_[None, :]).astype(np.float32)
    cur = (tt_ // 64)[:, None]
    sid = np.arange(64)[None, :]
    valid = sid <= cur
    f0, f1, f2 = (sid == 0), (sid == cur), (sid == cur - 1)
    forced = f0 | f1 | f2
    sh["tk_keep"] = (valid & ~forced).astype(np.float32)
    cadd = np.where(valid, 0.0, -1e9)
    cadd = np.where(f2, 1e9, cadd)
    cadd = np.where(f1, 2e9, cadd)
    cadd = np.where(f0, 3e9, cadd)
    sh["tk_cadd"] = cadd.astype(np.float32)
    e2 = np.zeros((64, 32, 128), np.float32)
    for jt in range(32):
        for j in range(128):
            e2[2 * jt + j // 64, jt, j] = 1.0
    sh["e2"] = e2
    dm = np.zeros((5, 128, 512), np.float32)
    jj = np.arange(128)[:, None]
    t5 = np.arange(512)[None, :]
    for dd in range(4):
        dm[dd] = (t5 >= dd * 128 + jj)
    dm[4] = (jj > t5)
    sh["dmask"] = dm
    x = f(inp["x"])
    mem = f(inp["mem"])
    w_in = sh["w_in"]
    wfull = [np.concatenate([w_in[0][:, :1984], np.zeros((D, 64), np.float32)], 1),
             np.concatenate([w_in[1][:, :1984], f(inp["vres_in"])[0]], 1)]
    mufull = [np.concatenate([f(inp["rwkv_mu"])[0], np.zeros(64, np.float32)]),
              np.concatenate([f(inp["rwkv_mu"])[1], f(inp["vres_mu"])[0]])]
    maps = []
    for c in range(NCORES):
        b, j = c // 4, c % 4
        hp = j
        m = dict(sh)
        m["x"] = np.ascontiguousarray(x[b, j * TC:(j + 1) * TC])
        m["mem"] = np.ascontiguousarray(mem[b])
        oh = np.zeros((128, 8), np.float32)
        oh[:, j] = 1.0
        if j > 0:
            oh[:, 4 + j - 1] = 1.0
        m["onehot"] = oh
        tg = np.arange(j * TC, (j + 1) * TC, dtype=np.float32) + 1.0
        m["invcnt"] = np.stack([1.0 / np.minimum(tg, w) for w in (2, 4, 8, 16)]).astype(np.float32)
        rc = rwkv_cols(hp)
        ncl = nsa_cols(hp)
        wr = np.zeros((DEPTH, D, 1024), np.float32)
        wn = np.zeros((DEPTH, D, NWN), np.float32)
        pv = np.zeros((DEPTH, 128, NV), np.float32)
        lora = np.zeros((DEPTH, 128, 5, 128), np.float32)
        ch = slice(128 * hp, 128 * hp + 128)
        for l in range(DEPTH):
            ok = rc >= 0
            wr[l][:, ok] = wfull[l][:, rc[ok]]
            wn[l] = w_in[l][:, O_NSA + ncl]
            cw = f(inp["conv_w"])[l]
            for jj in range(4):
                for i in range(3):
                    pv[l, :, PV_CONV + jj * 3 + i] = cw[i, jj * 128:(jj + 1) * 128]
            pv[l, :, PV_MEMQG] = f(inp["mem_qk_gain"])[l, 0]
            pv[l, :, PV_MEMKG] = f(inp["mem_qk_gain"])[l, 1]
            mu = np.zeros(1024, np.float32)
            mu[ok] = mufull[l][rc[ok]]
            pv[l, :, PV_MU:PV_MU + 8] = mu.reshape(8, 128).T
            for idx, nm in ((PV_W0, "rwkv_w0"), (PV_A0, "rwkv_a0"), (PV_KK, "rwkv_kk"), (PV_KA, "rwkv_ka"), (PV_RK, "rwkv_rk"),
                            (PV_LNW, "rwkv_ln_w"), (PV_LNB, "rwkv_ln_b")):
                pv[l, :, idx] = f(inp[nm])[l, ch]
            if l == 1:
                pv[l, :, PV_V0] = f(inp["vres_v0"])[0, ch]
                lora[l, 0:64, 4] = f(inp["vres_up"])[0][:, ch]
            g = f(inp["nsa_qk_gain"])[l]
            for i in range(4):
                pv[l, :, PV_NSAG + i] = np.concatenate([g[i], g[i]])
            pv[l, :, PV_CB1:PV_CB1 + 2] = f(inp["nsa_cmp_b1"])[l].T
            lora[l, 0:96, 0] = f(inp["rwkv_w2"])[l][:, ch]
            lora[l, 0:96, 1] = f(inp["rwkv_a2"])[l][:, ch]
            lora[l, :, 2] = f(inp["rwkv_g2"])[l][0:128, ch]
            lora[l, :, 3] = f(inp["rwkv_g2"])[l][128:256, ch]
        m["wr"], m["wn"], m["pvec"], m["lora"] = wr, wn, pv, lora
        maps.append(m)
    return maps


NHQ = 8


def hall_read(K, dst3, C, r, col0, n):
    for q in range(NHQ):
        src = C["hT_all"][q].ap()[r * 256:(r + 1) * 256, col0:col0 + n].rearrange("(kc p) t -> p kc t", p=128)
        K.dma("sp", dst3[:, 2 * q:2 * q + 2, :], src, reads=["hT_all"], writes=[C["_hall_dst"]])


def ysrc_write(K, Y, rows, t0, n, src_tile, src_ap):
    TC = K.TC
    done = 0
    while done < n:
        j, off = (t0 + done) // TC, (t0 + done) % TC
        m = min(n - done, TC - off)
        K.dma("sp", Y["src"][j].ap()[rows, off:off + m], src_ap[:, done:done + m], reads=[src_tile], writes=[Y["key"] + "_src"])
        done += m


def ygather(K, Y):
    for j in range(4):
        K.fw.allgather(Y["src"][j], Y["all"][j], GROUPS, reads=[Y["key"] + "_src"], writes=[Y["key"] + "_all"])


def load_hall_block(K, hb, C, t0, TC, blk=512):
    sub = min(blk, TC)
    for si in range(blk // sub):
        tok = t0 + si * sub
        r_, off = tok // TC, tok % TC
        C["_hall_dst"] = hb
        hall_read(K, hb[:, :, si * sub:(si + 1) * sub], C, r_, off, sub)


CS = 16
GN_EPS = 64e-5


def rwkv_phase(K, l, C, R):
    T = K.T
    TC = K.TC
    NB = T // 512
    pv = C["pv"][l]
    K.push()
    wr = K.sb("rw_w", [128, 16, 1024], BF16)
    for i in range(2):
        K.dma("pool", wr[:, :, i * 512:(i + 1) * 512],
              K.inputs["wr"].ap()[l].rearrange("(kc p) n -> p kc n", p=128)[:, :, i * 512:(i + 1) * 512], writes=[wr])
    lora = K.sb("rw_lora", [128, 5, 128])
    K.dma("sp", lora[:], K.inputs["lora"].ap()[l], writes=[lora])
    blk = K.sb("rw_blk", [128, 128])
    K.dma("sp", blk[:], K.inputs["blk64"].ap(), writes=[blk])
    hTb = [K.sb(f"rw_hT{i}", [128, 16, 512], BF16) for i in range(2)]
    nct = 8 if l == 1 else 7
    ub = [K.sb(f"rw_ub{i}", [128, 513]) for i in range(nct)]
    uf = [K.sb(f"rw_uf{i}", [128, 512]) for i in range(nct)]
    tmp = [K.sb(f"rw_t{i}", [128, 512]) for i in range(8)]
    tmo = [K.sb(f"rw_tmo{i}", [128, 4, 128]) for i in range(2)]
    for ct in range(nct):
        K.memset(ub[ct][:, 0:1], 0.0, [ub[ct]])
    sc = lambda i: pv[:, i:i + 1]
    for tb in range(NB):
        t0 = tb * 512
        hb = hTb[tb % 2]
        load_hall_block(K, hb, C, t0, TC)
        for ct in range(nct):
            ps = K.psum()
            for kc in range(16):
                K.mm(ps[:, :], wr[:, kc, ct * 128:(ct + 1) * 128], hb[:, kc, :], kc == 0, kc == 15, [wr, hb], [ps])
            K.copy(ub[ct][:, 1:513], ps[:, :], [ps], [ub[ct]], eng="act")
            d = tmp[0]
            K.tt(d[:], ub[ct][:, 0:512], ub[ct][:, 1:513], ALU.subtract, [ub[ct]], [d])
            K.stt(uf[ct][:], d[:], sc(PV_MU + ct), ub[ct][:, 1:513], ALU.mult, ALU.add, [d, pv, ub[ct]], [uf[ct]])
            K.copy(ub[ct][:, 0:1], ub[ct][:, 512:513], [ub[ct]], [ub[ct]])
        r, k, v = uf[0], uf[1], uf[2]
        K.act(uf[3][0:96, :], uf[3][0:96, :], AF.Tanh, [uf[3]], [uf[3]])
        ps = K.psum()
        K.mm(ps[:, :], lora[0:96, 0, :], uf[3][0:96, :], True, True, [lora, uf[3]], [ps])
        dec = tmp[1]
        K.act(dec[:], ps[:, :], AF.Sigmoid, [ps, pv], [dec], bias=sc(PV_W0))
        K.act(dec[:], dec[:], AF.Exp, [dec], [dec], scale=-float(np.exp(-0.5)))
        K.dma("sp", R["fm_w"].ap()[:, t0:t0 + 512], dec[:], reads=[dec], writes=["fm_w"])
        ps = K.psum()
        K.mm(ps[:, :], lora[0:96, 1, :], uf[4][0:96, :], True, True, [lora, uf[4]], [ps])
        a = tmp[2]
        K.act(a[:], ps[:, :], AF.Sigmoid, [ps, pv], [a], bias=sc(PV_A0))
        K.act(uf[5][:], uf[5][:], AF.Sigmoid, [uf[5]], [uf[5]])
        K.act(uf[6][:], uf[6][:], AF.Sigmoid, [uf[6]], [uf[6]])
        ps = K.psum()
        K.mm(ps[:, :], lora[:, 2, :], uf[5][:], True, False, [lora, uf[5]], [ps])
        K.mm(ps[:, :], lora[:, 3, :], uf[6][:], False, True, [lora, uf[6]], [ps])
        g = tmp[3]
        K.copy(g[:], ps[:, :], [ps], [g], eng="act")
        K.dma("sp", R["fm_g"].ap()[:, t0:t0 + 512], g[:], reads=[g], writes=["fm_g"])
        if l == 0:
            K.dma("sp", R["fm_vfirst"].ap()[:, t0:t0 + 512], v[:], reads=[v], writes=["fm_vfirst"])
        else:
            ps = K.psum()
            K.mm(ps[:, :], lora[0:64, 4, :], uf[7][0:64, :], True, True, [lora, uf[7]], [ps])
            vr = tmp[4]
            K.act(vr[:], ps[:, :], AF.Sigmoid, [ps, pv], [vr], bias=sc(PV_V0))
            vf = tmp[5]
            K.dma("sp", vf[:], R["fm_vfirst"].ap()[:, t0:t0 + 512], reads=["fm_vfirst"], writes=[vf])
            K.tt(vf[:], vf[:], v[:], ALU.subtract, [vf, v], [vf])
            K.tt(vf[:], vf[:], vr[:], ALU.mult, [vf, vr], [vf])
            K.tt(v[:], v[:], vf[:], ALU.add, [v, vf], [v])
        kk = tmp[4]
        K.ts(kk[:], k[:], sc(PV_KK), None, ALU.mult, None, [k, pv], [kk])
        sq = tmp[5]
        K.tt(sq[:], kk[:], kk[:], ALU.mult, [kk], [sq])
        ps = K.psum()
        K.mm(ps[:, :], blk[:, :], sq[:], True, True, [blk, sq], [ps])
        rn = tmp[5]
        K.ts(rn[:], ps[:, :], 1e-24, None, ALU.max, None, [ps], [rn])
        K.act(rn[:], rn[:], AF.Sqrt, [rn], [rn])
        K.op("dve", lambda e: e.reciprocal(out=rn[:], in_=rn[:]), [rn], [rn])
        K.tt(kk[:], kk[:], rn[:], ALU.mult, [kk, rn], [kk])
        nkk = tmp[5]
        K.ts(nkk[:], kk[:], -1.0, None, ALU.mult, None, [kk], [nkk])
        K.dma("sp", R["fm_nkk"].ap()[:, t0:t0 + 512], nkk[:], reads=[nkk], writes=["fm_nkk"])
        t1 = tmp[6]
        K.ts(t1[:], a[:], sc(PV_KA), sc(PV_C1), ALU.mult, ALU.add, [a, pv], [t1])
        kp = tmp[7]
        K.tt(kp[:], k[:], t1[:], ALU.mult, [k, t1], [kp])
        K.dma("sp", R["fm_kp"].ap()[:, t0:t0 + 512], kp[:], reads=[kp], writes=["fm_kp"])
        kka = tmp[6]
        K.tt(kka[:], kk[:], a[:], ALU.mult, [kk, a], [kka])
        for which, (src, dst) in enumerate(((kka, "tm_kka"), (v, "tm_v"))):
            ps = K.psum()
            for i in range(4):
                K.tr(ps[:, i * 128:(i + 1) * 128], src[:, i * 128:(i + 1) * 128], C["identf"][:], [src, C["identf"]], [ps])
            K.copy(tmo[which][:], ps[:, :].rearrange("p (a b) -> p a b", a=4), [ps], [tmo[which]], eng="act")
            K.dma("sp", R[dst].ap()[t0:t0 + 512, :].rearrange("(a p) c -> p a c", p=128), tmo[which][:], reads=[tmo[which]], writes=[dst])
        t2 = tmp[0]
        K.stt(t2[:], r[:], sc(PV_RK), kp[:], ALU.mult, ALU.mult, [r, pv, kp], [t2])
        ps = K.psum()
        K.mm(ps[:, :], blk[:, :], t2[:], True, True, [blk, t2], [ps])
        bon = tmp[0]
        K.tt(bon[:], ps[:, :], v[:], ALU.mult, [ps, v], [bon])
        K.dma("sp", R["fm_bonus"].ap()[:, t0:t0 + 512], bon[:], reads=[bon], writes=["fm_bonus"])
        K.dma("sp", R["fm_r"].ap()[:, t0:t0 + 512], r[:], reads=[r], writes=["fm_r"])
    barrier(K)
    K.pop()

    K.push()
    NH = 2
    Sring = [[K.sb(f"sc_S{h}_{i}", [64, 64]) for i in range(4)] for h in range(NH)]
    kkaB = [[K.sb(f"sc_kkaB{h}_{i}", [64, CS, 64]) for i in range(2)] for h in range(NH)]
    vB = [[K.sb(f"sc_vB{h}_{i}", [64, CS, 64]) for i in range(2)] for h in range(NH)]
    Abuf = [[K.sb(f"sc_A{h}_{i}", [64, CS, 64]) for i in range(2)] for h in range(NH)]
    Dg = [K.sb(f"sc_Dg{h}", [64, CS, 64]) for h in range(NH)]
    fmb = {n: [[K.sb(f"sc_{n}{h}_{i}", [64, 512]) for i in range(2)] for h in range(NH)] for n in ("w", "nkk", "kp", "r", "g", "bonus")}
    yT = [K.sb(f"sc_yT{h}", [64, 512]) for h in range(NH)]
    pt = [K.sb(f"sc_pt{h}_{i}", [64, 512]) for h in range(NH) for i in range(3)]
    pvh = [K.sb(f"sc_pv{h}", [64, NV]) for h in range(NH)]
    ones64 = K.sb("sc_ones64", [64, 64])
    K.memset(ones64[:], 1.0 / 64, [ones64])
    for h in range(NH):
        K.dma("sp", pvh[h][:], K.inputs["pvec"].ap()[l][h * 64:(h + 1) * 64, :], writes=[pvh[h]])
        K.memset(Sring[h][0][:], 0.0, [Sring[h][0]])
    psS = [K.ps[0], K.ps[1]]
    psY = [K.ps[2], K.ps[3]]
    ident64 = C["identf"][0:64, 0:64]
    nchunk = 512 // CS
    pend_y = []
    for tb in range(NB):
        t0 = tb * 512
        for h in range(NH):
            for n in fmb:
                K.dma("sp", fmb[n][h][tb % 2][:], R["fm_" + n].ap()[h * 64:(h + 1) * 64, t0:t0 + 512], reads=["fm_" + n], writes=[fmb[n][h][tb % 2]])
        for ci in range(nchunk):
            c0 = ci * CS
            gi = tb * nchunk + ci
            for h in range(NH):
                kb, vb, A = kkaB[h][gi % 2], vB[h][gi % 2], Abuf[h][gi % 2]
                K.dma("sp", kb[:], R["tm_kka"].ap()[t0 + c0:t0 + c0 + CS, h * 64:(h + 1) * 64].partition_broadcast(64), reads=["tm_kka"], writes=[kb])
                K.dma("sp", vb[:], R["tm_v"].ap()[t0 + c0:t0 + c0 + CS, h * 64:(h + 1) * 64].partition_broadcast(64), reads=["tm_v"], writes=[vb])
                nk = fmb["nkk"][h][tb % 2]
                w = fmb["w"][h][tb % 2]
                K.tt(A[:], kb[:], nk[:, c0:c0 + CS].unsqueeze(2).to_broadcast([64, CS, 64]), ALU.mult, [kb, nk], [A], eng="pool")
                K.tt(Dg[h][:], ident64.unsqueeze(1).to_broadcast([64, CS, 64]), w[:, c0:c0 + CS].unsqueeze(2).to_broadcast([64, CS, 64]),
                     ALU.mult, [C["identf"], w], [Dg[h]], eng="pool")
                K.tt(A[:], A[:], Dg[h][:], ALU.add, [A, Dg[h]], [A])
            for s in range(CS):
                tg = gi * CS + s
                for h in range(NH):
                    A = Abuf[h][gi % 2]
                    Sp = Sring[h][tg % 4]
                    slot = tg % 8
                    pk = f"psS{h}_{slot}"
                    pso = psS[h][0:64, slot * 64:(slot + 1) * 64]
                    K.mm(pso, A[:, s, :], Sp[:], True, True, [A, Sp], [pk])
                for fn_ in pend_y:
                    fn_()
                pend_y.clear()
                for h in range(NH):
                    vb = vB[h][gi % 2]
                    Sn = Sring[h][(tg + 1) % 4]
                    slot = tg % 8
                    pk = f"psS{h}_{slot}"
                    pso = psS[h][0:64, slot * 64:(slot + 1) * 64]
                    kp = fmb["kp"][h][tb % 2]
                    K.stt(Sn[:], vb[:, s, :], kp[:, c0 + s:c0 + s + 1], pso, ALU.mult, ALU.add, [vb, kp, pk], [Sn])
                    rr = fmb["r"][h][tb % 2]

                    def ymm(h=h, Sn=Sn, rr=rr, col=c0 + s):
                        K.mm(psY[h][0:64, col:col + 1], Sn[:], rr[:, col:col + 1], True, True, [Sn, rr], [psY[h]])
                    pend_y.append(ymm)
        for fn_ in pend_y:
            fn_()
        pend_y.clear()
        for h in range(NH):
            y, p0, p1, p2 = yT[h], pt[h * 3], pt[h * 3 + 1], pt[h * 3 + 2]
            K.copy(y[:], psY[h][0:64, :], [psY[h]], [y], eng="act")
            ps = K.psum4()
            K.mm(ps[0:64, :], ones64[:], y[:], True, True, [ones64, y], [ps])
            K.tt(p0[:], y[:], ps[0:64, :], ALU.subtract, [y, ps], [p0])
            K.tt(p1[:], p0[:], p0[:], ALU.mult, [p0], [p1])
            ps = K.psum4()
            K.mm(ps[0:64, :], ones64[:], p1[:], True, True, [ones64, p1], [ps])
            K.ts(p1[:], ps[0:64, :], GN_EPS, None, ALU.add, None, [ps], [p1])
            K.act(p1[:], p1[:], AF.Sqrt, [p1], [p1])
            K.op("dve", lambda e: e.reciprocal(out=p1[:], in_=p1[:]), [p1], [p1])
            K.tt(p0[:], p0[:], p1[:], ALU.mult, [p0, p1], [p0])
            K.ts(p0[:], p0[:], pvh[h][:, PV_LNW:PV_LNW + 1], pvh[h][:, PV_LNB:PV_LNB + 1], ALU.mult, ALU.add, [p0, pvh[h]], [p0])
            bo, g = fmb["bonus"][h][tb % 2], fmb["g"][h][tb % 2]
            K.tt(p0[:], p0[:], bo[:], ALU.add, [p0, bo], [p0])
            K.tt(p2[:], p0[:], g[:], ALU.mult, [p0, g], [p2])
            ysrc_write(K, R["yB"], slice(h * 64, (h + 1) * 64), t0, 512, p2, p2)
    barrier(K)
    K.pop()


def select_chunk(K, dst, Y, C):
    oh = C["oh"]
    cand, acc = C["sel_cand"], C["sel_acc"]
    for kc in range(4):
        for j in range(4):
            K.dma("sp", cand[:, j, :], Y["all"][j].ap()[kc * 128:(kc + 1) * 128, :], reads=[Y["key"] + "_all"], writes=[cand])
        K.ts(acc[:], cand[:, 0, :], oh[:, 0:1], None, ALU.mult, None, [cand, oh], [acc])
        for j in range(1, 4):
            K.stt(acc[:], cand[:, j, :], oh[:, j:j + 1], acc[:], ALU.mult, ALU.add, [cand, oh, acc], [acc])
        K.copy(dst[:, kc, :], acc[:], [acc], [dst])


def merge_phase(K, l, C, S, xres):
    TC, NT = K.TC, K.NT
    hTx = C["hTx"]
    w_in = K.inputs["w_in"].ap()[l]
    K.push()
    wg = [K.sb(f"mg_wg{i}", [128, 16, 512], BF16) for i in range(2)]
    wb = [K.sb(f"mg_wb{i}", [128, 4, 512], BF16) for i in range(2)]
    psc = K.sb("mg_psc", [128, 512])
    macc = K.sb("mg_macc", [128, NT, 512])
    merged = K.sb("mg_merged", [128, NT, D], BF16)
    gt = [K.sb(f"mg_gt{i}", [128, 512]) for i in range(2)]
    tm = [K.sb(f"mg_tm{i}", [128, 512]) for i in range(2)]
    ys = [S["y_rwkvT"], S["y_nsaT"], S["y_convT"], S["y_memT"]]
    n = 0
    for nb in range(4):
        K.dma("sp", psc[:], K.inputs["pool_scale"].ap()[l:l + 1, nb * 512:(nb + 1) * 512].partition_broadcast(128), writes=[psc])
        for i in range(5):
            g_, b_ = wg[n % 2], wb[n % 2]
            n += 1
            load_w(K, g_, w_in, O_GATE + i * D + nb * 512, 512)
            if i < 4:
                K.dma("pool", b_[:], K.inputs["w_branch"].ap()[l, i].rearrange("(kc p) n -> p kc n", p=128)[:, :, nb * 512:(nb + 1) * 512], writes=[b_])
            else:
                K.dma("pool", b_[:, 0, :], K.inputs["pool_w"].ap()[l, nb], writes=[b_])
            for tt in range(NT):
                tsl = slice(tt * 128, (tt + 1) * 128)
                psg = K.psum()
                for kc in range(16):
                    K.mm(psg[:, :], hTx[:, kc, HALO + tt * 128: HALO + (tt + 1) * 128], g_[:, kc, :], kc == 0, kc == 15, [hTx, g_], [psg])
                G = gt[tt % 2]
                K.act(G[:], psg[:, :], AF.Sigmoid, [psg], [G])
                psb = K.psum()
                if i < 4:
                    for kc in range(4):
                        K.mm(psb[:, :], ys[i][:, kc, tsl], b_[:, kc, :], kc == 0, kc == 3, [ys[i], b_], [psb])
                else:
                    K.mm(psb[:, :], S["pooledT"][:, nb, tsl], b_[:, 0, :], True, True, [S["pooledT"], b_], [psb])
                    K.tt(G[:], G[:], psc[:], ALU.mult, [G, psc], [G])
                if i == 0:
                    K.tt(macc[:, tt, :], G[:], psb[:, :], ALU.mult, [G, psb], [macc])
                else:
                    t_ = tm[tt % 2]
                    K.tt(t_[:], G[:], psb[:, :], ALU.mult, [G, psb], [t_])
                    K.tt(macc[:, tt, :], macc[:, tt, :], t_[:], ALU.add, [macc, t_], [macc])
        K.copy(merged[:, :, nb * 512:(nb + 1) * 512], macc[:], [macc], [merged], eng="act")
    mT = hTx
    for tt in range(NT):
        for grp in range(2):
            ps = K.psum()
            psb_ = ps[:].bitcast(BF16)
            for i in range(8):
                kc = grp * 8 + i
                K.tr(psb_[:, i * 128:(i + 1) * 128], merged[:, tt, kc * 128:(kc + 1) * 128], C["identb"][:], [merged, C["identb"]], [ps])
            K.copy(mT[:, grp * 8:(grp + 1) * 8, HALO + tt * 128: HALO + (tt + 1) * 128], psb_.rearrange("p (a b) -> p a b", a=8), [ps], [mT],
                   eng="act" if grp == 0 else "dve")
    xt = [K.sb(f"mg_xt{i}", [128, 512]) for i in range(2)]
    for nb in range(4):
        wo = wg[nb % 2]
        load_w(K, wo, K.inputs["w_out"].ap()[l], nb * 512, 512)
        for tt in range(NT):
            ps = K.psum()
            for kc in range(16):
                K.mm(ps[:, :], mT[:, kc, HALO + tt * 128: HALO + (tt + 1) * 128], wo[:, kc, :], kc == 0, kc == 15, [mT, wo], [ps])
            x_ = xt[tt % 2]
            K.dma("sp", x_[:], xres.ap()[tt * 128:(tt + 1) * 128, nb * 512:(nb + 1) * 512], reads=["xres"], writes=[x_])
            K.tt(x_[:], x_[:], ps[:, :], ALU.add, [x_, ps], [x_])
            K.dma("sp", xres.ap()[tt * 128:(tt + 1) * 128, nb * 512:(nb + 1) * 512], x_[:], reads=[x_], writes=["xres"])
    barrier(K)
    K.pop()


def ffn_phase(K, l, C, xres, xout):
    TC = K.TC
    BL = min(512, TC)
    NBL = TC // BL
    TPB = BL // 128
    moe = (l % 2 == 1)
    K.push()
    h2T = C["hTx"]
    K.push()
    rn_alloc(K, C)
    K.dma("sp", C["gbc"][:], K.inputs["norm_ffn"].ap()[l:l + 1, :].partition_broadcast(128), writes=[C["gbc"]])
    rmsnorm_T(K, xres.ap(), C["gbc"], h2T, HALO, K.NT, C, "ffn")
    barrier(K)
    K.pop()
    NFE = 22
    NT = K.NT
    uT = K.sb("ff_uT", [128, NFE, TC], BF16)
    w1g = [K.sb(f"ff_w1_{i}", [128, 16, 256], BF16) for i in range(2)]
    w3g = [K.sb(f"ff_w3_{i}", [128, 16, 256], BF16) for i in range(2)]
    w2g = [K.sb(f"ff_w2_{i}", [128, 11, 512], BF16) for i in range(2)]
    sa = [K.sb(f"ff_sa{i}", [128, 512]) for i in range(2)]
    xacc = K.sb("ff_xacc", [128, NT, D])
    if moe:
        rt = K.sb("ff_rt", [128, 16, 8], BF16)
        K.dma("pool", rt[:], K.inputs["moe_router"].ap()[0].rearrange("(kc p) e -> p kc e", p=128), writes=[rt])
        comb = K.sb("ff_comb", [128, K.NT, 8])
        lg, l2, m1, m2, mk1, mk2 = (K.sb("ff_" + n, [128, 8]) for n in ("lg", "l2", "m1", "m2", "mk1", "mk2"))
        for tt in range(K.NT):
            ps = K.psum()
            for kc in range(16):
                K.mm(ps[:, 0:8], h2T[:, kc, HALO + tt * 128: HALO + (tt + 1) * 128], rt[:, kc, :], kc == 0, kc == 15, [h2T, rt], [ps])
            K.copy(lg[:], ps[:, 0:8], [ps], [lg])
            K.op("dve", lambda e: e.reduce_max(out=m1[:, 0:1], in_=lg[:], axis=AX.X), [lg], [m1])
            K.ts(mk1[:], lg[:], m1[:, 0:1], None, ALU.is_ge, None, [lg, m1], [mk1])
            K.stt(l2[:], mk1[:], -1e30, lg[:], ALU.mult, ALU.add, [mk1, lg], [l2])
            K.op("dve", lambda e: e.reduce_max(out=m2[:, 0:1], in_=l2[:], axis=AX.X), [l2], [m2])
            K.ts(mk2[:], l2[:], m2[:, 0:1], None, ALU.is_ge, None, [l2, m2], [mk2])
            K.tt(m1[:, 1:2], m1[:, 0:1], m2[:, 0:1], ALU.subtract, [m1, m2], [m1])
            K.act(m1[:, 2:3], m1[:, 1:2], AF.Sigmoid, [m1], [m1])
            K.ts(m1[:, 3:4], m1[:, 2:3], -1.0, 1.0, ALU.mult, ALU.add, [m1], [m1])
            K.ts(mk1[:], mk1[:], m1[:, 2:3], None, ALU.mult, None, [mk1, m1], [mk1])
            K.stt(comb[:, tt, :], mk2[:], m1[:, 3:4], mk1[:], ALU.mult, ALU.add, [mk2, m1, mk1], [comb])
    for tt in range(NT):
        K.dma("sp", xacc[:, tt, :], xres.ap()[tt * 128:(tt + 1) * 128, :], reads=["xres"], writes=[xacc])
    if moe:
        pexp = [tuple(K.inputs[n].ap()[0, e] for n in ("moe_w1", "moe_w3", "moe_w2")) + (0, e) for e in range(N_EXP)]
    else:
        pexp = [tuple(K.inputs[n].ap()[0] for n in ("ffn_w1", "ffn_w3", "ffn_w2")) + (fo, None) for fo in (0, NFE)]
    nld = [0]
    for (W1, W3, W2, fo, e) in pexp:
        for c0 in range(0, NFE * 128, 256):
            a_, b_ = w1g[nld[0] % 2], w3g[nld[0] % 2]
            nld[0] += 1
            load_w(K, a_, W1, fo * 128 + c0, 256)
            load_w(K, b_, W3, fo * 128 + c0, 256)
            for fi in range(2):
                f = c0 // 128 + fi
                for bl in range(NBL):
                    cb = HALO + bl * BL
                    pa, pb = K.psum_from(0, 4), K.psum_from(0, 4)
                    for kc in range(16):
                        K.mm(pa[:, 0:BL], a_[:, kc, fi * 128:(fi + 1) * 128], h2T[:, kc, cb:cb + BL], kc == 0, kc == 15, [a_, h2T], [pa])
                    for kc in range(16):
                        K.mm(pb[:, 0:BL], b_[:, kc, fi * 128:(fi + 1) * 128], h2T[:, kc, cb:cb + BL], kc == 0, kc == 15, [b_, h2T], [pb])
                    s_ = sa[(f * NBL + bl) % 2]
                    K.act(s_[:, 0:BL], pa[:, 0:BL], AF.Silu, [pa], [s_])
                    K.tt(uT[:, f, bl * BL:(bl + 1) * BL], s_[:, 0:BL], pb[:, 0:BL], ALU.mult, [s_, pb], [uT])
        for nb in range(4):
            for fh in range(2):
                w2_ = w2g[nld[0] % 2]
                nld[0] += 1
                r0 = (fo + fh * 11) * 128
                K.dma("pool", w2_[:, :, :], W2[r0:r0 + 11 * 128, nb * 512:(nb + 1) * 512].rearrange("(f p) n -> p f n", p=128), writes=[w2_])
                for tt in range(NT):
                    ps = K.psum_from(4, 4)
                    for f in range(11):
                        K.mm(ps[:, :], uT[:, fh * 11 + f, tt * 128:(tt + 1) * 128], w2_[:, f, :], f == 0, f == 10, [uT, w2_], [ps])
                    xs = xacc[:, tt, nb * 512:(nb + 1) * 512]
                    if moe:
                        K.stt(xs, ps[:, :], comb[:, tt, e:e + 1], xs, ALU.mult, ALU.add, [ps, comb, xacc], [xacc])
                    else:
                        K.tt(xs, xs, ps[:, :], ALU.add, [xacc, ps], [xacc])
    for tt in range(NT):
        K.dma("sp", xout.ap()[tt * 128:(tt + 1) * 128, :], xacc[:, tt, :], reads=[xacc], writes=[xout.name])
    barrier(K)
    K.pop()


WN_TM = 704
NWN2 = WN_TM + 140
SEL_N = 16
import os
NSA_STOP = int(os.environ.get('NSA_STOP', '0'))
NSA_SUB = int(os.environ.get('NSA_SUB', '0'))


def nsa_phase(K, l, C, R):
    T, TC = K.T, K.TC
    NB, NTT = T // 512, T // 128
    NS = T // 64
    NCMP = (T - 32) // 16 + 1
    CT = [(c0, min(128, NCMP - c0)) for c0 in range(0, NCMP, 128)]
    VW = 64 + NS + 1
    pv = C["pv"][l]
    sc = lambda i: pv[:, i:i + 1]
    K.push()
    qT = [K.sb(f"ns_q{i}T", [128, T], BF16) for i in range(2)]
    ksT, kwT = K.sb("ns_ksT", [128, T], BF16), K.sb("ns_kwT", [128, T], BF16)
    kcT, vcT = K.sb("ns_kcT", [64, T], BF16), K.sb("ns_vcT", [64, T], BF16)
    Vs, Vw = K.sb("ns_Vs", [128, NTT, 66], BF16), K.sb("ns_Vw", [128, NTT, 66], BF16)
    gts = K.sb("ns_g", [128, NTT, 12])
    Oacc = K.sb("ns_O", [128, NTT, 128])
    blk = K.sb("ns_blk", [128, 128])
    K.dma("sp", blk[:], K.inputs["blk64"].ap(), writes=[blk])
    K.memset(Vs[:, :, 64:65], 1.0, [Vs])
    K.memset(Vw[:, :, 64:65], 1.0, [Vw])
    imp = K.sb("ns_imp", [128, NTT, NS])
    selT = K.sb("ns_selT", [64, T], BF16)
    ebuf = [K.sb(f"ns_e{i}", [128, 512], BF16) for i in range(4)]
    rv = [K.sb(f"ns_rv{i}", [128, 2]) for i in range(2)]
    kcmpT = K.sb("ns_kcmpT", [128, 256], BF16)
    Vc = K.sb("ns_Vc", [128, len(CT), VW + 1], BF16)
    K.push()
    wn = K.sb("ns_wn", [128, 16, NWN2], BF16)
    wsrc = K.inputs["wn"].ap()[l].rearrange("(kc p) n -> p kc n", p=128)
    K.dma("pool", wn[:, :, 0:512], wsrc[:, :, 0:512], writes=[wn])
    K.dma("pool", wn[:, :, 512:NWN2], wsrc[:, :, 512:NWN2], writes=[wn])
    B1 = 256
    hTb = [K.sb("ns_hT0", [128, 16, B1], BF16)] * 2
    pf = K.sb("ns_pf", [128, B1])
    for tb in range(T // B1):
        t0 = tb * B1
        hb = hTb[tb % 2]
        load_hall_block(K, hb, C, t0, TC, B1)
        specs = [(0, 128, qT[0], PV_NSAG + 0), (128, 128, qT[1], PV_NSAG + 0), (384, 128, ksT, PV_NSAG + 2), (512, 128, kwT, PV_NSAG + 3),
                 (256, 64, kcT, None), (640, 64, vcT, None)]
        if NSA_SUB == 1:
            continue
        for (c0, nc_, dst, gi) in specs:
            ps = K.psum()
            for kc in range(16):
                K.mm(ps[0:nc_, 0:B1], wn[:, kc, c0:c0 + nc_], hb[:, kc, :], kc == 0, kc == 15, [wn, hb], [ps])
            if gi is None:
                K.copy(dst[:, t0:t0 + B1], ps[0:nc_, 0:B1], [ps], [dst], eng="act")
            else:
                K.copy(pf[:], ps[:, 0:B1], [ps], [pf], eng="act")
                sq = C["fm_sq"]
                K.tt(sq[:, 0:B1], pf[:], pf[:], ALU.mult, [pf], [sq])
                ps2 = K.psum()
                K.mm(ps2[:, 0:B1], blk[:, :], sq[:, 0:B1], True, True, [blk, sq], [ps2])
                rs = C["fm_rs"]
                K.ts(rs[:, 0:B1], ps2[:, 0:B1], 1.0 / 64, EPS, ALU.mult, ALU.add, [ps2], [rs])
                K.act(rs[:, 0:B1], rs[:, 0:B1], AF.Sqrt, [rs], [rs])
                K.op("dve", lambda e: e.reciprocal(out=rs[:, 0:B1], in_=rs[:, 0:B1]), [rs], [rs])
                K.stt(dst[:, t0:t0 + B1], pf[:], sc(gi), rs[:, 0:B1], ALU.mult, ALU.mult, [pf, pv, rs], [dst])
        if NSA_SUB == 2:
            continue
        for ti in range(B1 // 128):
            gt_ = tb * (B1 // 128) + ti
            ps = K.psum()
            for kc in range(16):
                K.mm(ps[:, 0:140], hb[:, kc, ti * 128:(ti + 1) * 128], wn[:, kc, WN_TM:WN_TM + 140], kc == 0, kc == 15, [wn, hb], [ps])
            K.copy(Vs[:, gt_, 0:64], ps[:, 0:64], [ps], [Vs], eng="act")
            K.copy(Vw[:, gt_, 0:64], ps[:, 64:128], [ps], [Vw])
            K.act(gts[:, gt_, :], ps[:, 128:140], AF.Sigmoid, [ps], [gts])
    barrier(K)
    K.pop()
    K.push()
    W1 = K.sb("ns_W1", [64, 32, 128], BF16)
    w2d = K.sb("ns_w2", [128, 128], BF16)
    posT = K.sb("ns_posT", [64, 32], BF16)
    hidT = K.sb("ns_hidT", [128, 256], BF16)
    hx = [K.sb(f"ns_hx{i}", [128, 256]) for i in range(3)]
    cb = K.sb("ns_cb", [128, 2])
    kg = K.sb("ns_kg", [128, 64])
    K.dma("sp", kg[:], K.inputs["nsa_qk_gain"].ap()[l, 1:2, :].partition_broadcast(128), writes=[kg])
    ktm = K.sb("ns_ktm", [128, 128])
    kss = K.sb("ns_kss", [128, 2])
    K.memset(Vc[:, :, VW - 1:VW], 1.0, [Vc])
    for ci, (c0, ncc) in enumerate(CT):
        K.dma("pool", Vc[0:ncc, ci, 64:64 + NS], K.inputs["ovl"].ap()[c0:c0 + ncc, 0:NS], writes=[Vc])
    for i in range(2):
        src = kcT if i == 0 else vcT
        K.dma("pool", W1[:], K.inputs["nsa_cmp_w1"].ap()[l, i].rearrange("(l d) j -> d l j", d=64), writes=[W1])
        K.dma("pool", posT[:], K.inputs["cmp_posT"].ap()[l, i], writes=[posT])
        K.dma("pool", w2d[:, 0:64], K.inputs["nsa_cmp_w2"].ap()[l, i], writes=[w2d])
        K.dma("pool", w2d[:, 64:128], K.inputs["nsa_cmp_w2"].ap()[l, i], writes=[w2d])
        ps = K.psum()
        for ll in range(32):
            K.mm(ps[:, 0:1], W1[:, ll, :], posT[:, ll:ll + 1], ll == 0, ll == 31, [W1, posT], [ps])
        K.tt(cb[:, i:i + 1], ps[:, 0:1], sc(PV_CB1 + i), ALU.add, [ps, pv], [cb])
        ps = K.psum()
        for ll in range(32):
            K.mm(ps[:, 0:NCMP], W1[:, ll, :], src[:, ll: ll + 16 * (NCMP - 1) + 1: 16], ll == 0, ll == 31, [W1, src], [ps])
        x_, x2, x3 = hx
        n_ = NCMP
        K.ts(x_[:, 0:n_], ps[:, 0:n_], cb[:, i:i + 1], None, ALU.add, None, [ps, cb], [x_])
        K.tt(x2[:, 0:n_], x_[:, 0:n_], x_[:, 0:n_], ALU.mult, [x_], [x2])
        K.ts(x2[:, 0:n_], x2[:, 0:n_], 0.044715, 1.0, ALU.mult, ALU.add, [x2], [x2])
        K.tt(x2[:, 0:n_], x2[:, 0:n_], x_[:, 0:n_], ALU.mult, [x2, x_], [x2])
        K.act(x3[:, 0:n_], x2[:, 0:n_], AF.Sigmoid, [x2], [x3], scale=1.5957691216057308)
        K.tt(hidT[:, 0:n_], x_[:, 0:n_], x3[:, 0:n_], ALU.mult, [x_, x3], [hidT])
        for ci, (c0, ncc) in enumerate(CT):
            ps = K.psum()
            K.mm(ps[0:ncc, 0:128], hidT[:, c0:c0 + ncc], w2d[:, :], True, True, [hidT, w2d], [ps])
            if i == 1:
                K.copy(Vc[0:ncc, ci, 0:64], ps[0:ncc, 0:64], [ps], [Vc], eng="act")
            else:
                K.copy(ktm[0:ncc, :], ps[0:ncc, 0:128], [ps], [ktm], eng="act")
                sq = C["fm_sq"]
                K.tt(sq[0:ncc, 0:64], ktm[0:ncc, 0:64], ktm[0:ncc, 0:64], ALU.mult, [ktm], [sq])
                K.op("dve", lambda e: e.reduce_sum(out=kss[0:ncc, 0:1], in_=sq[0:ncc, 0:64], axis=AX.X), [sq], [kss])
                K.ts(kss[0:ncc, 1:2], kss[0:ncc, 0:1], 1.0 / 64, EPS, ALU.mult, ALU.add, [kss], [kss])
                K.act(kss[0:ncc, 1:2], kss[0:ncc, 1:2], AF.Sqrt, [kss], [kss])
                K.op("dve", lambda e: e.reciprocal(out=kss[0:ncc, 1:2], in_=kss[0:ncc, 1:2]), [kss], [kss])
                for hf in range(2):
                    K.stt(ktm[0:ncc, hf * 64:(hf + 1) * 64], ktm[0:ncc, hf * 64:(hf + 1) * 64], kss[0:ncc, 1:2], kg[0:ncc, :], ALU.mult, ALU.mult,
                          [ktm, kss, kg], [ktm])
                ps2 = K.psum()
                K.tr(ps2[:, 0:ncc], ktm[0:ncc, :], C["identf"][0:ncc, 0:ncc], [ktm, C["identf"]], [ps2])
                K.copy(kcmpT[:, c0:c0 + ncc], ps2[:, 0:ncc], [ps2], [kcmpT])
    barrier(K)
    K.pop()
    K.push()
    maskc = K.sb("ns_maskc", [128, len(CT), T], BF16)
    for ci, (c0, ncc) in enumerate(CT):
        K.dma("pool", maskc[0:ncc, ci, :], K.inputs["maskc"].ap()[c0:c0 + ncc, 0:T], writes=[maskc])
    nrv = [0]

    def finish_acc(acc_ap, acckey, gtile, gate_idx):
        r_ = rv[nrv[0] % 2]
        nrv[0] += 1
        K.ts(r_[:, 0:1], acc_ap[:, 64:65], 1e-30, None, ALU.max, None, [acckey], [r_])
        K.op("dve", lambda e: e.reciprocal(out=r_[:, 0:1], in_=r_[:, 0:1]), [r_], [r_])
        K.tt(r_[:, 1:2], r_[:, 0:1], gts[:, gtile, gate_idx:gate_idx + 1], ALU.mult, [r_, gts], [r_])
        return r_

    first_o = {}
    for hd in range(4):
        qt, half = qT[hd // 2], slice((hd % 2) * 64, (hd % 2) * 64 + 64)
        for tb in range(NB):
            t0 = tb * 512
            for ci, (c0, ncc) in enumerate(CT):
                ps = K.psum_from(0, 7)
                K.mm(ps[0:ncc, :], kcmpT[half, c0:c0 + ncc], qt[half, t0:t0 + 512], True, True, [kcmpT, qt], [ps])
                e_ = ebuf[ci]
                K.act(e_[0:ncc, :], ps[0:ncc, :], AF.Exp, [ps], [e_], scale=0.125)
                K.tt(e_[0:ncc, :], e_[0:ncc, :], maskc[0:ncc, ci, t0:t0 + 512], ALU.mult, [e_, maskc], [e_])
            for ti in range(4):
                gt_ = tb * 4 + ti
                ps = K.psum_from(0, 7)
                for ci, (c0, ncc) in enumerate(CT):
                    K.mm(ps[:, 0:VW], ebuf[ci][0:ncc, ti * 128:(ti + 1) * 128], Vc[0:ncc, ci, 0:VW], ci == 0, ci == len(CT) - 1, [ebuf[ci], Vc], [ps])
                r_ = rv[nrv[0] % 2]
                nrv[0] += 1
                K.ts(r_[:, 0:1], ps[:, VW - 1:VW], 1e-30, None, ALU.max, None, [ps], [r_])
                K.op("dve", lambda e: e.reciprocal(out=r_[:, 0:1], in_=r_[:, 0:1]), [r_], [r_])
                if hd == 0:
                    K.ts(imp[:, gt_, :], ps[:, 64:64 + NS], r_[:, 0:1], None, ALU.mult, None, [ps, r_], [imp])
                else:
                    K.stt(imp[:, gt_, :], ps[:, 64:64 + NS], r_[:, 0:1], imp[:, gt_, :], ALU.mult, ALU.add, [ps, r_, imp], [imp])
                if hd < 2:
                    K.tt(r_[:, 1:2], r_[:, 0:1], gts[:, gt_, hd * 3:hd * 3 + 1], ALU.mult, [r_, gts], [r_])
                    K.ts(Oacc[:, gt_, hd * 64:(hd + 1) * 64], ps[:, 0:64], r_[:, 1:2], None, ALU.mult, None, [ps, r_], [Oacc])
    barrier(K)
    K.pop()
    K.push()
    keep, cadd = K.sb("ns_keep", [128, NS]), K.sb("ns_cadd", [128, NS])
    cmp3 = K.sb("ns_cmp3", [128, NS, NS])
    cnt, sel, ok = K.sb("ns_cnt", [128, NS]), K.sb("ns_sel", [128, NS]), K.sb("ns_ok", [128, NS])
    for gt_ in range(NTT):
        K.dma("sp", keep[:], K.inputs["tk_keep"].ap()[gt_ * 128:(gt_ + 1) * 128, 0:NS], writes=[keep])
        K.dma("sp", cadd[:], K.inputs["tk_cadd"].ap()[gt_ * 128:(gt_ + 1) * 128, 0:NS], writes=[cadd])
        im = imp[:, gt_, :]
        K.tt(im, im, keep[:], ALU.mult, [imp, keep], [imp])
        K.tt(im, im, cadd[:], ALU.add, [imp, cadd], [imp])
        K.tt(cmp3[:], im.unsqueeze(1).to_broadcast([128, NS, NS]), im.unsqueeze(2).to_broadcast([128, NS, NS]), ALU.is_gt, [imp], [cmp3])
        K.op("dve", lambda e: e.reduce_sum(out=cnt[:], in_=cmp3[:], axis=AX.X), [cmp3], [cnt])
        K.ts(sel[:], cnt[:], float(SEL_N) - 0.5, None, ALU.is_lt, None, [cnt], [sel])
        K.ts(ok[:], im, -1e8, None, ALU.is_gt, None, [imp], [ok])
        K.tt(sel[:], sel[:], ok[:], ALU.mult, [sel, ok], [sel])
        ps = K.psum_from(0, 7)
        K.tr(ps[0:NS, 0:128], sel[:], C["identf"][:], [sel, C["identf"]], [ps])
        K.copy(selT[0:NS, gt_ * 128:(gt_ + 1) * 128], ps[0:NS, 0:128], [ps], [selT], eng="act")
    barrier(K)
    K.pop()
    K.push()
    E2 = K.sb("ns_E2", [64, NTT, 128], BF16)
    K.dma("pool", E2[0:NS], K.inputs["e2"].ap()[0:NS, 0:NTT, :], writes=[E2])
    dmask = K.sb("ns_dmask", [128, 5, 512], BF16)
    K.dma("pool", dmask[:], K.inputs["dmask"].ap().rearrange("a p t -> p a t"), writes=[dmask])
    accb = K.ps[7]
    for hd in range(2):
        half = slice(hd * 64, hd * 64 + 64)
        for tb in range(NB):
            t0 = tb * 512
            njt = 4 * tb + 4
            for jt in range(njt):
                ps = K.psum_from(0, 4)
                K.mm(ps[:, :], ksT[half, jt * 128:(jt + 1) * 128], qT[0][half, t0:t0 + 512], True, True, [ksT, qT[0]], [ps])
                e_ = ebuf[jt % 2]
                K.act(e_[:, :], ps[:, :], AF.Exp, [ps], [e_], scale=0.125)
                pm = K.psum_from(0, 4)
                K.mm(pm[:, :], E2[0:NS, jt, :], selT[0:NS, t0:t0 + 512], True, True, [E2, selT], [pm])
                em = ebuf[2 + jt % 2]
                K.tt(em[:, :], e_[:, :], pm[:, :], ALU.mult, [e_, pm], [em])
                dd = jt - 4 * tb
                if dd >= 0:
                    K.tt(em[:, :], em[:, :], dmask[:, dd, :], ALU.mult, [em, dmask], [em])
                for ti in range(max(dd, 0), 4):
                    K.mm(K.ps[4 + ti][:, 0:65], em[:, ti * 128:(ti + 1) * 128], Vs[:, jt, 0:65], jt == 0, jt == 4 * tb + ti, [em, Vs], [K.ps[4 + ti]])
            for ti in range(4):
                gt_ = tb * 4 + ti
                a_ = K.ps[4 + ti][:, 0:65]
                r_ = finish_acc(a_, K.ps[4 + ti].name, gt_, hd * 3 + 1)
                K.stt(Oacc[:, gt_, hd * 64:(hd + 1) * 64], a_[:, 0:64], r_[:, 1:2], Oacc[:, gt_, hd * 64:(hd + 1) * 64], ALU.mult, ALU.add,
                      [K.ps[4 + ti], r_, Oacc], [Oacc])
    for hd in range(2):
        half = slice(hd * 64, hd * 64 + 64)
        for gt_ in range(NTT):
            jts = list(range(max(0, gt_ - 4), gt_ + 1))
            for jt in jts:
                ps = K.psum_from(0, 7)
                K.mm(ps[:, 0:128], kwT[half, jt * 128:(jt + 1) * 128], qT[0][half, gt_ * 128:(gt_ + 1) * 128], True, True, [kwT, qT[0]], [ps])
                e_ = ebuf[jt % 4]
                K.act(e_[:, 0:128], ps[:, 0:128], AF.Exp, [ps], [e_], scale=0.125)
                if jt == gt_:
                    K.tt(e_[:, 0:128], e_[:, 0:128], dmask[:, 0, 0:128], ALU.mult, [e_, dmask], [e_])
                elif jt == gt_ - 4:
                    K.tt(e_[:, 0:128], e_[:, 0:128], dmask[:, 4, 0:128], ALU.mult, [e_, dmask], [e_])
                K.mm(accb[:, 0:65], e_[:, 0:128], Vw[:, jt, 0:65], jt == jts[0], jt == jts[-1], [e_, Vw], ["ns_accb"])
            a_ = accb[:, 0:65]
            r_ = finish_acc(a_, "ns_accb", gt_, hd * 3 + 2)
            K.stt(Oacc[:, gt_, hd * 64:(hd + 1) * 64], a_[:, 0:64], r_[:, 1:2], Oacc[:, gt_, hd * 64:(hd + 1) * 64], ALU.mult, ALU.add,
                  ["ns_accb", r_, Oacc], [Oacc])
    ot = [K.sb(f"ns_ot{i}", [128, 512]) for i in range(2)]
    for tb in range(NB):
        ps = K.psum_from(0, 7)
        for ti in range(4):
            K.tr(ps[:, ti * 128:(ti + 1) * 128], Oacc[:, tb * 4 + ti, :], C["identf"][:], [Oacc, C["identf"]], [ps])
        o_ = ot[tb % 2]
        K.copy(o_[:], ps[:, :], [ps], [o_], eng="act")
        ysrc_write(K, R["yN"], slice(0, 128), tb * 512, 512, o_, o_)
    barrier(K)
    K.pop()
    K.pop()


INPUT_SHAPES = lambda T: {
    "x": [T // 4, D], "mem": [MEM_LEN, D], "w_in": [2, D, N_IN], "norm_mix": [2, D], "norm_ffn": [2, D], "norm_mem": [2, D],
    "mem_wkv": [2, D, 1024], "pool_w": [2, 4, 128, 512], "pool_scale": [2, D], "w_branch": [2, 4, 512, D], "w_out": [2, D, D],
    "ffn_w1": [1, D, D_FF], "ffn_w3": [1, D, D_FF], "ffn_w2": [1, D_FF, D], "moe_router": [1, D, 8],
    "moe_w1": [1, 8, D, E_FF], "moe_w3": [1, 8, D, E_FF], "moe_w2": [1, 8, E_FF, D],
    "nsa_cmp_w1": [2, 2, 2048, 128], "nsa_cmp_w2": [2, 2, 128, 64], "cmp_posT": [2, 2, 64, 32], "nsa_qk_gain": [2, 4, 64],
    "ident": [128, 128], "blk64": [128, 128], "ovl": [256, 64], "maskc": [256, T], "tk_keep": [T, 64], "tk_cadd": [T, 64],
    "e2": [64, 32, 128], "dmask": [5, 128, 512], "onehot": [128, 8], "invcnt": [4, T // 4], "pvec": [2, 128, NV],
    "wr": [2, D, 1024], "wn": [2, D, NWN], "lora": [2, 128, 5, 128],
}


def build(T, dbg=False, nlayers=DEPTH):
    K = KB(T)
    TC = K.TC
    out = K.dout("out", [TC, D])
    C = setup_common(K)
    xres = K.dscr("xres", [TC, D])
    K.dma("sp", xres.ap(), K.inputs["x"].ap(), writes=["xres"])
    C["hTx"] = K.sb("hTx", [128, 16, HALO + TC], BF16)
    alloc_gather(K, C)
    R = alloc_scratch(K)
    dbg_outs = []
    barrier(K)
    for l in range(nlayers):
        K.push()
        S = {n: K.sb(n, [128, 4, TC], BF16) for n in ("y_convT", "pooledT", "y_memT")}
        K.push()
        C["halo_cand"] = K.sb("halo_cand", [128, 4, 16, HALO], BF16)
        phase_norm_gather(K, l, C, xres)
        barrier(K)
        K.pop()
        local_mixers(K, l, C, S)
        rwkv_phase(K, l, C, R)
        ygather(K, R["yB"])
        nsa_phase(K, l, C, R)
        ygather(K, R["yN"])
        S["y_rwkvT"] = K.sb("y_rwkvT", [128, 4, TC], BF16)
        S["y_nsaT"] = K.sb("y_nsaT", [128, 4, TC], BF16)
        K.push()
        C["sel_cand"] = K.sb("sel_cand", [128, 4, TC])
        C["sel_acc"] = K.sb("sel_acc", [128, TC])
        select_chunk(K, S["y_rwkvT"], R["yB"], C)
        select_chunk(K, S["y_nsaT"], R["yN"], C)
        barrier(K)
        K.pop()
        if dbg and l == 0:
            for nm, Y in (("d_yN", R["yN"]), ("d_yB", R["yB"])):
                o = K.dout(nm, [512, T])
                for j in range(4):
                    K.dma("sp", o.ap()[:, j * TC:(j + 1) * TC], Y["all"][j].ap(), reads=[Y["key"] + "_all"], writes=[nm])
                dbg_outs.append(nm)
        merge_phase(K, l, C, S, xres)
        barrier(K)
        K.pop()
        if dbg and l == 0:
            o = K.dout("d_xmix", [TC, D])
            K.dma("sp", o.ap(), xres.ap(), reads=["xres"], writes=["d_xmix"])
            dbg_outs.append("d_xmix")
        last = (l == nlayers - 1)
        ffn_phase(K, l, C, xres, out if last else xres)
        if dbg and l == 0 and not last:
            o = K.dout("d_xout0", [TC, D])
            K.dma("sp", o.ap(), xres.ap(), reads=["xres"], writes=["d_xout0"])
            dbg_outs.append("d_xout0")
    K.fw.finish(["out"] + dbg_outs)
    return K


_CACHE = {}


def kernel(**inputs):
    T = int(np.asarray(inputs["x"]).shape[1])
    if T not in _CACHE:
        _CACHE[T] = build(T)
    K = _CACHE[T]
    maps = host_prep(inputs, T)
    maps = [{k: m[k] for k in K.inputs} for m in maps]
    res = run_bass_kernel_spmd(K.nc, maps, core_ids=list(range(NCORES)))
    TC = T // 4
    outp = np.zeros((2, T, D), np.float32)
    for c in range(NCORES):
        outp[c // 4, (c % 4) * TC:(c % 4 + 1) * TC] = res.results[c]["out"]
    return outp
```

```python
import numpy as np
import concourse.bass as bass
import concourse.mybir as mybir
from concourse.bass_utils import run_bass_kernel_spmd

F32 = mybir.dt.float32
BF16 = mybir.dt.bfloat16
ALU = mybir.AluOpType
AF = mybir.ActivationFunctionType
AX = mybir.AxisListType

D = 2048
NCORES = 8
MEM_LEN = 256
DEPTH = 2
D_FF = 5632
E_FF = 2816
N_EXP = 8
EPS = 1e-6
O_RWKV, O_NSA, O_CONV, O_POOL, O_MEM, O_GATE = 0, 1984, 3288, 4824, 5336, 5848
N_IN = 16088
HALO = 16


class FW:
    def __init__(self, nc, n_dma_sems=20):
        self.nc = nc
        self.eng = {"pe": nc.tensor, "act": nc.scalar, "dve": nc.vector, "pool": nc.gpsimd, "sp": nc.sync}
        self.sem = {}
        self.cnt = {}
        for e in self.eng:
            self.sem[e] = nc.semaphore("s_" + e).__enter__()
            self.cnt[e] = 0
        self.dsem = {}
        for q in ("sp", "pool", "act"):
            lst = [[nc.semaphore(f"d_{q}{i}").__enter__(), 0] for i in range(n_dma_sems)]
            self.dsem[q] = [lst, 0]
        self.ccsem = nc.semaphore("ccsem").__enter__()
        self.cccnt = 0
        self.seen = {e: {} for e in self.eng}
        self.lastw = {}
        self.readers = {}
        self.ninstr = 0
        self.excl = {"ns_accb"}

    @staticmethod
    def _k(x):
        return x if isinstance(x, str) else x.name

    def _wait(self, e, tok):
        if tok is None:
            return
        sem, val, src = tok
        if src == "pe" and e == "pe":
            return
        k = id(sem)
        if self.seen[e].get(k, 0) >= val:
            return
        self.eng[e].wait_ge(sem, val)
        self.seen[e][k] = val

    def _deps(self, e, reads, writes):
        for k in reads:
            self._wait(e, self.lastw.get(k))
        for k in writes:
            self._wait(e, self.lastw.get(k))
            for t in self.readers.get(k, ()):
                self._wait(e, t)

    def _record(self, tok, reads, writes):
        for k in reads:
            self.readers.setdefault(k, []).append(tok)
        for k in writes:
            self.lastw[k] = tok
            self.readers[k] = []
        self.ninstr += 1

    def op(self, e, fn, reads=(), writes=()):
        reads = [self._k(x) for x in reads]
        writes = [self._k(x) for x in writes]
        for k in reads:
            if (k.startswith("psb") or k in self.excl) and k not in writes:
                writes.append(k)
        self._deps(e, reads, writes)
        ins = fn(self.eng[e])
        self.cnt[e] += 1
        ins.then_inc(self.sem[e], 1)
        tok = (self.sem[e], self.cnt[e], e)
        self._record(tok, reads, writes)
        return tok

    def dma(self, q, out, in_, reads=(), writes=(), **kw):
        reads = [self._k(x) for x in reads]
        writes = [self._k(x) for x in writes]
        lst, idx = self.dsem[q]
        ent = lst[idx % len(lst)]
        self.dsem[q][1] += 1
        sem, tgt = ent
        if tgt > 0:
            self._wait(q, (sem, tgt, "dma"))
        self._deps(q, reads, writes)
        self.eng[q].dma_start(out=out, in_=in_, **kw).then_inc(sem, 16)
        ent[1] = tgt + 16
        tok = (sem, tgt + 16, "dma")
        self._record(tok, reads, writes)
        return tok

    def allgather(self, src, dst, groups, reads=(), writes=()):
        reads = [self._k(x) for x in reads]
        writes = [self._k(x) for x in writes]
        self._deps("pool", reads, writes)
        self.nc.gpsimd.collective_compute("AllGather", ALU.bypass, replica_groups=groups,
                                          ins=[src.ap().opt()], outs=[dst.ap().opt()]).then_inc(self.ccsem)
        self.cccnt += 1
        tok = (self.ccsem, self.cccnt, "cc")
        self._record(tok, reads, writes)
        return tok

    def finish(self, keys):
        for k in keys:
            self._wait("sp", self.lastw.get(k))


class LazyInputs(dict):
    def __init__(self, kb):
        super().__init__()
        self.kb = kb

    def __missing__(self, name):
        shp = INPUT_SHAPES(self.kb.T)[name]
        t = self.kb.nc.dram_tensor(name, list(shp), F32, kind="ExternalInput")
        self[name] = t
        return t


class KB:
    def __init__(self, T):
        self.T = T
        self.TC = T // 4
        self.NT = self.TC // 128
        self.nc = bass.Bass("TRN2", target_bir_lowering=False)
        self.fw = FW(self.nc)
        self.inputs = LazyInputs(self)
        self.outputs = {}
        self._uid = 0
        self._psn = 0
        self.ps = [self.nc.psum_tensor(f"psb{i}", [128, 512], F32).__enter__() for i in range(8)]
        self.scopes = []

    def din(self, name, shape, dtype=F32):
        t = self.nc.dram_tensor(name, list(shape), dtype, kind="ExternalInput")
        self.inputs[name] = t
        return t

    def dout(self, name, shape, dtype=F32):
        t = self.nc.dram_tensor(name, list(shape), dtype, kind="ExternalOutput")
        self.outputs[name] = t
        return t

    def dscr(self, name, shape, dtype=F32):
        return self.nc.dram_tensor(name, list(shape), dtype)

    def sb(self, name, shape, dtype=F32):
        self._uid += 1
        g = self.nc.sbuf_tensor(f"{name}_u{self._uid}", list(shape), dtype)
        t = g.__enter__()
        if self.scopes:
            self.scopes[-1].append(g)
        return t

    def push(self):
        self.scopes.append([])

    def pop(self):
        for g in reversed(self.scopes.pop()):
            g.__exit__(None, None, None)

    def psum(self):
        p = self.ps[self._psn % 8]
        self._psn += 1
        return p

    def psum_from(self, lo, n):
        p = self.ps[lo + self._psn % n]
        self._psn += 1
        return p

    def psum4(self):
        p = self.ps[4 + self._psn % 4]
        self._psn += 1
        return p

    def op(self, e, fn, reads=(), writes=()):
        return self.fw.op(e, fn, reads, writes)

    def dma(self, q, out, in_, reads=(), writes=(), **kw):
        return self.fw.dma(q, out, in_, reads, writes, **kw)

    def mm(self, out, lhsT, rhs, start, stop, reads, writes):
        return self.fw.op("pe", lambda e: e.matmul(out, lhsT, rhs, start=start, stop=stop), reads, writes)

    def tr(self, out, in_, ident, reads, writes):
        return self.fw.op("pe", lambda e: e.transpose(out, in_, ident), reads, writes)

    def act(self, out, in_, func, reads, writes, bias=None, scale=None, accum_out=None):
        kw = {}
        if bias is not None:
            kw["bias"] = bias
        if scale is not None:
            kw["scale"] = scale
        if accum_out is not None:
            kw["accum_out"] = accum_out
        return self.fw.op("act", lambda e: e.activation(out=out, in_=in_, func=func, **kw), reads, writes)

    def tt(self, out, in0, in1, op, reads, writes, eng="dve"):
        return self.fw.op(eng, lambda e: e.tensor_tensor(out=out, in0=in0, in1=in1, op=op), reads, writes)

    def ts(self, out, in0, s1, s2, op0, op1, reads, writes, eng="dve"):
        if s2 is None:
            s2, op1 = 0.0, ALU.add
        return self.fw.op(eng, lambda e: e.tensor_scalar(out=out, in0=in0, scalar1=s1, scalar2=s2, op0=op0, op1=op1), reads, writes)

    def stt(self, out, in0, scalar, in1, op0, op1, reads, writes, eng="dve"):
        return self.fw.op(eng, lambda e: e.scalar_tensor_tensor(out=out, in0=in0, scalar=scalar, in1=in1, op0=op0, op1=op1), reads, writes)

    def copy(self, out, in_, reads, writes, eng="dve"):
        if eng == "act":
            return self.fw.op("act", lambda e: e.copy(out=out, in_=in_), reads, writes)
        return self.fw.op(eng, lambda e: e.tensor_copy(out=out, in_=in_), reads, writes)

    def memset(self, ap, val, writes, eng="dve"):
        return self.fw.op(eng, lambda e: e.memset(ap, val), (), writes)


def bcast_rows(dram_ap_1d_row, nparts):
    return dram_ap_1d_row.partition_broadcast(nparts)


def barrier(K):
    fw = K.fw
    toks = [(fw.sem[e], fw.cnt[e], e) for e in fw.eng if fw.cnt[e] > 0]
    for q in fw.dsem:
        for sem, tgt in fw.dsem[q][0]:
            if tgt > 0:
                toks.append((sem, tgt, "dma"))
    if fw.cccnt:
        toks.append((fw.ccsem, fw.cccnt, "cc"))
    for e in fw.eng:
        for t in toks:
            if t[2] == e:
                continue
            fw._wait(e, t)


def rmsnorm_T(K, x_rows, gbc, hT, col0, ntiles, C, tag):
    for tt in range(ntiles):
        b = tt % 2
        xt, junk, ss, hb = C["xt"][b], C["junk"][b], C["ss"][b], C["hb"][b]
        K.dma("sp", xt[:], x_rows[tt * 128:(tt + 1) * 128, :], writes=[xt])
        K.memset(ss[:], 0.0, [ss])
        K.act(junk[:], xt[:], AF.Square, [xt], [junk, ss], accum_out=ss[:, 0:1])
        K.ts(ss[:, 1:2], ss[:, 0:1], 1.0 / D, EPS, ALU.mult, ALU.add, [ss], [ss])
        K.act(ss[:, 1:2], ss[:, 1:2], AF.Sqrt, [ss], [ss])
        K.op("dve", lambda e: e.reciprocal(out=ss[:, 1:2], in_=ss[:, 1:2]), [ss], [ss])
        K.stt(hb[:], xt[:], ss[:, 1:2], gbc[:], ALU.mult, ALU.mult, [xt, ss, gbc], [hb])
        for grp in range(2):
            ps = K.psum()
            psb = ps[:].bitcast(BF16)
            for i in range(8):
                kc = grp * 8 + i
                K.tr(psb[:, i * 128:(i + 1) * 128], hb[:, kc * 128:(kc + 1) * 128], C["identb"][:], [hb, C["identb"]], [ps])
            K.copy(hT[:, grp * 8:(grp + 1) * 8, col0 + tt * 128: col0 + (tt + 1) * 128],
                   psb.rearrange("p (a b) -> p a b", a=8), [ps], [hT], eng="act" if grp == 0 else "dve")


def load_w(K, wt, W, c0, ncols, kchunks=16, key=None):
    src = W.rearrange("(kc p) n -> p kc n", p=128)[:, :, c0:c0 + ncols]
    K.dma("pool", wt[:, 0:kchunks, 0:ncols], src, writes=[key or wt])


def fm_colsum_norm(K, out_bf, outkey, src_f32, tagkey, n, nparts, ones_f, gain_col, inv_n, C):
    sq = C["fm_sq"]
    K.tt(sq[0:nparts, 0:n], src_f32, src_f32, ALU.mult, [tagkey], [sq])
    ps = K.psum()
    K.mm(ps[0:nparts, 0:n], ones_f[0:nparts, 0:nparts], sq[0:nparts, 0:n], True, True, [sq, ones_f], [ps])
    rs = C["fm_rs"]
    K.ts(rs[0:nparts, 0:n], ps[0:nparts, 0:n], inv_n, EPS, ALU.mult, ALU.add, [ps], [rs])
    K.act(rs[0:nparts, 0:n], rs[0:nparts, 0:n], AF.Sqrt, [rs], [rs])
    K.op("dve", lambda e: e.reciprocal(out=rs[0:nparts, 0:n], in_=rs[0:nparts, 0:n]), [rs], [rs])
    K.stt(out_bf, src_f32, gain_col, rs[0:nparts, 0:n], ALU.mult, ALU.mult, [tagkey, rs], [outkey])


PV_CONV = 0
PV_MEMQG = 12
PV_MEMKG = 13
PV_MU = 14
PV_W0, PV_A0, PV_KK, PV_KA, PV_RK, PV_LNW, PV_LNB, PV_V0, PV_C1 = 22, 23, 24, 25, 26, 27, 28, 29, 30
PV_NSAG = 31
PV_CB1 = 35
NV = 40


def local_mixers(K, l, C, S):
    TC, W = K.TC, HALO + K.TC
    hTx, pv = C["hTx"], C["pv"][l]
    w_in = K.inputs["w_in"].ap()[l]
    tokblocks = [(0, HALO)] + [(HALO + i * 512, min(512, TC - i * 512)) for i in range((TC + 511) // 512)]
    locblocks = tokblocks[1:]
    K.push()
    wts = [K.sb(f"lm_wt{i}", [128, 16, 512], BF16) for i in range(2)]
    wi = [0]

    def nextw():
        w = wts[wi[0] % 2]
        wi[0] += 1
        return w

    def proj(wt, c_lo, ncol, dst, blocks, evac=None):
        for (c0, n) in blocks:
            ps = K.psum()
            for kc in range(16):
                K.mm(ps[0:ncol, 0:n], wt[:, kc, c_lo:c_lo + ncol], hTx[:, kc, c0:c0 + n], kc == 0, kc == 15, [wt, hTx], [ps])
            K.copy(dst[0:ncol, c0:c0 + n], ps[0:ncol, 0:n], [ps], [dst], eng="act")

    K.push()
    bt, ct, xt_ = (K.sb(n, [128, W]) for n in ("cv_b", "cv_c", "cv_x"))
    z, acc = K.sb("cv_z", [128, W]), K.sb("cv_acc", [128, TC])
    for j in range(4):
        wt = nextw()
        for i in range(3):
            src = w_in.rearrange("(kc p) n -> p kc n", p=128)[:, :, O_CONV + i * 512 + j * 128: O_CONV + i * 512 + (j + 1) * 128]
            K.dma("pool", wt[:, :, i * 128:(i + 1) * 128], src, writes=[wt])
        proj(wt, 0, 128, bt, locblocks)
        proj(wt, 128, 128, ct, tokblocks)
        proj(wt, 256, 128, xt_, tokblocks)
        K.tt(z[:], ct[:], xt_[:], ALU.mult, [ct, xt_], [z])
        cw = lambda i: pv[:, PV_CONV + j * 3 + i: PV_CONV + j * 3 + i + 1]
        K.ts(acc[:], z[:, HALO:W], cw(2), None, ALU.mult, None, [z, pv], [acc])
        K.stt(acc[:], z[:, HALO - 1:W - 1], cw(1), acc[:], ALU.mult, ALU.add, [z, pv, acc], [acc])
        K.stt(acc[:], z[:, HALO - 2:W - 2], cw(0), acc[:], ALU.mult, ALU.add, [z, pv, acc], [acc])
        K.tt(S["y_convT"][:, j, :], bt[:, HALO:W], acc[:], ALU.mult, [bt, acc], [S["y_convT"]])
    barrier(K)
    K.pop()
    K.push()
    acc = K.sb("pl_acc", [128, TC])
    wt = nextw()
    load_w(K, wt, w_in, O_POOL, 512)
    pa, pb, pu = K.sb("pl_a", [128, W]), K.sb("pl_b", [128, W]), K.sb("pl_u", [128, W])
    invc = K.sb("pl_invc", [128, TC])
    for g in range(4):
        proj(wt, g * 128, 128, pu, tokblocks)
        K.dma("sp", invc[:], K.inputs["invcnt"].ap()[g:g + 1, :].partition_broadcast(128), writes=[invc])
        cur, oth = pu, pa
        for si, sh in enumerate([1, 2, 4, 8][:g + 1]):
            K.tt(oth[:, sh:W], cur[:, sh:W], cur[:, 0:W - sh], ALU.add, [cur], [oth])
            cur, oth = oth, (pb if oth is pa else pa)
        K.tt(acc[:], cur[:, HALO:W], invc[:], ALU.mult, [cur, invc], [acc])
        K.tt(S["pooledT"][:, g, :], acc[:], pu[:, HALO:W], ALU.subtract, [acc, pu], [S["pooledT"]])
    barrier(K)
    K.pop()
    memT = K.sb("mm_memT", [128, 16, MEM_LEN], BF16)
    gm = K.sb("mm_g", [128, D])
    K.dma("sp", gm[:], K.inputs["norm_mem"].ap()[l:l + 1, :].partition_broadcast(128), writes=[gm])
    K.push()
    rn_alloc(K, C)
    rmsnorm_T(K, K.inputs["mem"].ap(), gm, memT, 0, 2, C, "mem")
    barrier(K)
    K.pop()
    wkv = K.inputs["mem_wkv"].ap()[l]
    kT = K.sb("mm_kT", [128, 4, MEM_LEN], BF16)
    vsb = K.sb("mm_v", [128, 2, 512], BF16)
    kf = K.sb("mm_kf", [128, 512])
    wt = nextw()
    load_w(K, wt, wkv, 0, 512)
    for h in range(4):
        ps = K.psum()
        for kc in range(16):
            K.mm(ps[:, 0:MEM_LEN], wt[:, kc, h * 128:(h + 1) * 128], memT[:, kc, :], kc == 0, kc == 15, [wt, memT], [ps])
        K.copy(kf[:, 0:MEM_LEN], ps[:, 0:MEM_LEN], [ps], [kf], eng="act")
        fm_colsum_norm(K, kT[:, h, :], kT, kf[:, 0:MEM_LEN], kf, MEM_LEN, 128, C["ones_f"], pv[:, PV_MEMKG:PV_MEMKG + 1], 1.0 / 128, C)
    wt = nextw()
    load_w(K, wt, wkv, 512, 512)
    for mt in range(2):
        ps = K.psum()
        for kc in range(16):
            K.mm(ps[:, :], memT[:, kc, mt * 128:(mt + 1) * 128], wt[:, kc, :], kc == 0, kc == 15, [wt, memT], [ps])
        K.copy(vsb[:, mt, :], ps[:, :], [ps], [vsb], eng="act")
    wt = nextw()
    load_w(K, wt, w_in, O_MEM, 512)
    qf, qT = K.sb("mm_qf", [128, W]), K.sb("mm_qT", [128, 512], BF16)
    es = [K.sb(f"mm_e{i}", [128, 512], BF16) for i in range(2)]
    rden = K.sb("mm_rden", [128, 512])
    for h in range(4):
        proj(wt, h * 128, 128, qf, locblocks)
        for (c0, n) in locblocks:
            fm_colsum_norm(K, qT[:, 0:n], qT, qf[:, c0:c0 + n], qf, n, 128, C["ones_f"], pv[:, PV_MEMQG:PV_MEMQG + 1], 1.0 / 128, C)
            for mt in range(2):
                ps = K.psum()
                K.mm(ps[:, 0:n], kT[:, h, mt * 128:(mt + 1) * 128], qT[:, 0:n], True, True, [kT, qT], [ps])
                K.act(es[mt][:, 0:n], ps[:, 0:n], AF.Exp, [ps], [es[mt]], scale=float(128 ** -0.5))
            po, pd = K.psum(), K.psum()
            for mt in range(2):
                K.mm(po[:, 0:n], vsb[:, mt, h * 128:(h + 1) * 128], es[mt][:, 0:n], mt == 0, mt == 1, [vsb, es[mt]], [po])
            for mt in range(2):
                K.mm(pd[:, 0:n], C["ones_b"][:, :], es[mt][:, 0:n], mt == 0, mt == 1, [C["ones_b"], es[mt]], [pd])
            K.op("dve", lambda e: e.reciprocal(out=rden[:, 0:n], in_=pd[:, 0:n]), [pd], [rden])
            K.tt(S["y_memT"][:, h, c0 - HALO:c0 - HALO + n], po[:, 0:n], rden[:, 0:n], ALU.mult, [po, rden], [S["y_memT"]])
    barrier(K)
    K.pop()


GROUPS = [[0, 1, 2, 3], [4, 5, 6, 7]]


def alloc_gather(K, C):
    TC = K.TC
    C["hT_src"] = [K.dscr(f"hT_src{q}", [256, TC], BF16) for q in range(8)]
    C["hT_all"] = [K.dscr(f"hT_all{q}", [4 * 256, TC], BF16) for q in range(8)]


def alloc_scratch(K):
    T, TC = K.T, K.TC
    R = {n: K.dscr(n, [128, T]) for n in ("fm_r", "fm_w", "fm_nkk", "fm_kp", "fm_g", "fm_bonus", "fm_vfirst")}
    R["tm_kka"] = K.dscr("tm_kka", [T, 128])
    R["tm_v"] = K.dscr("tm_v", [T, 128])
    for nm in ("yB", "yN"):
        R[nm] = {"key": nm, "src": [K.dscr(f"{nm}_src{j}", [128, TC]) for j in range(4)],
                 "all": [K.dscr(f"{nm}_all{j}", [512, TC]) for j in range(4)]}
    return R


def setup_common(K):
    C = {}
    C["identf"] = K.sb("identf", [128, 128])
    C["identb"] = K.sb("identb", [128, 128], BF16)
    C["ones_f"] = K.sb("ones_f", [128, 128])
    C["ones_b"] = K.sb("ones_b", [128, 128], BF16)
    K.dma("sp", C["identf"][:], K.inputs["ident"].ap(), writes=[C["identf"]])
    K.copy(C["identb"][:], C["identf"][:], [C["identf"]], [C["identb"]])
    K.memset(C["ones_f"][:], 1.0, [C["ones_f"]])
    K.memset(C["ones_b"][:], 1.0, [C["ones_b"]])
    C["fm_sq"] = K.sb("fm_sq", [128, 512])
    C["fm_rs"] = K.sb("fm_rs", [128, 512])
    C["oh"] = K.sb("oh", [128, 8])
    K.dma("sp", C["oh"][:], K.inputs["onehot"].ap(), writes=[C["oh"]])
    C["pv"] = []
    for l in range(DEPTH):
        t = K.sb(f"pv{l}", [128, NV])
        K.dma("sp", t[:], K.inputs["pvec"].ap()[l], writes=[t])
        K.ts(t[:, PV_C1:PV_C1 + 1], t[:, PV_KA:PV_KA + 1], -1.0, 1.0, ALU.mult, ALU.add, [t], [t])
        C["pv"].append(t)
    return C


def rn_alloc(K, C):
    C["xt"] = [K.sb(f"rn_xt{i}", [128, D]) for i in range(2)]
    C["junk"] = [K.sb(f"rn_junk{i}", [128, D], BF16) for i in range(2)]
    C["ss"] = [K.sb(f"rn_ss{i}", [128, 2]) for i in range(2)]
    C["hb"] = [K.sb(f"rn_hb{i}", [128, D], BF16) for i in range(2)]
    C["gbc"] = K.sb("gbc", [128, D])


def phase_norm_gather(K, l, C, xres):
    TC = K.TC
    hTx = C["hTx"]
    K.push()
    rn_alloc(K, C)
    K.dma("sp", C["gbc"][:], K.inputs["norm_mix"].ap()[l:l + 1, :].partition_broadcast(128), writes=[C["gbc"]])
    rmsnorm_T(K, xres.ap(), C["gbc"], hTx, HALO, K.NT, C, "mix")
    barrier(K)
    K.pop()
    for q in range(NHQ):
        K.dma("sp", C["hT_src"][q].ap().rearrange("(kc p) t -> p kc t", p=128), hTx[:, 2 * q:2 * q + 2, HALO:HALO + TC], reads=[hTx], writes=["hT_src"])
    for q in range(NHQ):
        K.fw.allgather(C["hT_src"][q], C["hT_all"][q], GROUPS, reads=["hT_src"], writes=["hT_all"])
    cand = C["halo_cand"]
    C["_hall_dst"] = cand
    for r in range(4):
        hall_read(K, cand[:, r], C, r, TC - HALO, HALO)
    oh = C["oh"]
    K.ts(hTx[:, :, 0:HALO], cand[:, 0], oh[:, 4:5], None, ALU.mult, None, [cand, oh], [hTx])
    for r in range(1, 4):
        K.stt(hTx[:, :, 0:HALO], cand[:, r], oh[:, 4 + r:5 + r], hTx[:, :, 0:HALO], ALU.mult, ALU.add, [cand, oh, hTx], [hTx])


def rwkv_cols(hp):
    cols = []
    pad = lambda a, n: list(a) + [-1] * (n - len(a))
    cols += list(range(128 * hp, 128 * hp + 128))
    cols += list(range(512 + 128 * hp, 512 + 128 * hp + 128))
    cols += list(range(1024 + 128 * hp, 1024 + 128 * hp + 128))
    cols += pad(range(1536, 1632), 128)
    cols += pad(range(1632, 1728), 128)
    cols += list(range(1728, 1984))
    cols += pad(range(1984, 2048), 128)
    return np.array(cols)


def nsa_cols(hp):
    hk = hp // 2
    mine = [2 * hp, 2 * hp + 1]
    oth = [h for h in range(4 * hk, 4 * hk + 4) if h not in mine]
    heads = mine + oth
    grp = lambda i: list(range(512 + 128 * i + 64 * hk, 512 + 128 * i + 64 * hk + 64))
    cols = []
    for h in heads:
        cols += list(range(64 * h, 64 * h + 64))
    cols += grp(0) + grp(0) + grp(2) + grp(2) + grp(4) + grp(4)
    cols += grp(1)
    cols += grp(3) + grp(5)
    for h in heads:
        cols += [512 + 768 + 3 * h + i for i in range(3)]
    return np.array(cols)


NWN = 844


def host_prep(inp, T):
    TC = T // 4
    f = lambda a: np.ascontiguousarray(np.asarray(a, dtype=np.float32))
    sh = {k: f(inp[k]) for k in ("w_in", "norm_mix", "norm_ffn", "norm_mem", "mem_wkv", "pool_w", "pool_scale", "w_branch",
                                 "w_out", "ffn_w1", "ffn_w3", "ffn_w2", "moe_router", "moe_w1", "moe_w3", "moe_w2",
                                 "nsa_cmp_w1", "nsa_cmp_w2", "nsa_cmp_pos")}
    sh["ident"] = np.eye(128, dtype=np.float32)
    sh["blk64"] = np.kron(np.eye(2), np.ones((64, 64))).astype(np.float32)
    sh["cmp_posT"] = np.ascontiguousarray(np.transpose(sh["nsa_cmp_pos"], (0, 1, 3, 2)))
    sh["nsa_qk_gain"] = f(inp["nsa_qk_gain"])
    NS, NTT = T // 64, T // 128
    cc = np.arange(256)[:, None] * 16
    ss_ = np.arange(64)[None, :] * 64
    sh["ovl"] = (np.clip(np.minimum(cc + 32, ss_ + 64) - np.maximum(cc, ss_), 0, None) / 32.0).astype(np.float32)
    tt_ = np.arange(T)
    sh["maskc"] = ((np.arange(256)[:, None] * 16 + 31) <= tt_[None, :]).astype(np.float32)
    cur = (tt_ // 64)[:, None]
    sid = np.arange(64)[None, :]
    valid = sid <= cur
    f0, f1, f2 = (sid == 0), (sid == cur), (sid == cur - 1)
    forced = f0 | f1 | f2
    sh["tk_keep"] = (valid & ~forced).astype(np.float32)
    cadd = np.where(valid, 0.0, -1e9)
    cadd = np.where(f2, 1e9, cadd)
    cadd = np.where(f1, 2e9, cadd)
    cadd = np.where(f0, 3e9, cadd)
    sh["tk_cadd"] = cadd.astype(np.float32)
    e2 = np.zeros((64, 32, 128), np.float32)
    for jt in range(32):
        for j in range(128):
            e2[2 * jt + j // 64, jt, j] = 1.0
    sh["e2"] = e2
    dm = np.zeros((5, 128, 512), np.float32)
    jj = np.arange(128)[:, None]
    t5 = np.arange(512)[None, :]
    for dd in range(4):
        dm[dd] = (t5 >= dd * 128 + jj)
    dm[4] = (jj > t5)
    sh["dmask"] = dm
    x = f(inp["x"])
    mem = f(inp["mem"])
    w_in = sh["w_in"]
    wfull = [np.concatenate([w_in[0][:, :1984], np.zeros((D, 64), np.float32)], 1),
             np.concatenate([w_in[1][:, :1984], f(inp["vres_in"])[0]], 1)]
    mufull = [np.concatenate([f(inp["rwkv_mu"])[0], np.zeros(64, np.float32)]),
              np.concatenate([f(inp["rwkv_mu"])[1], f(inp["vres_mu"])[0]])]
    maps = []
    for c in range(NCORES):
        b, j = c // 4, c % 4
        hp = j
        m = dict(sh)
        m["x"] = np.ascontiguousarray(x[b, j * TC:(j + 1) * TC])
        m["mem"] = np.ascontiguousarray(mem[b])
        oh = np.zeros((128, 8), np.float32)
        oh[:, j] = 1.0
        if j > 0:
            oh[:, 4 + j - 1] = 1.0
        m["onehot"] = oh
        tg = np.arange(j * TC, (j + 1) * TC, dtype=np.float32) + 1.0
        m["invcnt"] = np.stack([1.0 / np.minimum(tg, w) for w in (2, 4, 8, 16)]).astype(np.float32)
        rc = rwkv_cols(hp)
        ncl = nsa_cols(hp)
        wr = np.zeros((DEPTH, D, 1024), np.float32)
        wn = np.zeros((DEPTH, D, NWN), np.float32)
        pv = np.zeros((DEPTH, 128, NV), np.float32)
        lora = np.zeros((DEPTH, 128, 5, 128), np.float32)
        ch = slice(128 * hp, 128 * hp + 128)
        for l in range(DEPTH):
            ok = rc >= 0
            wr[l][:, ok] = wfull[l][:, rc[ok]]
            wn[l] = w_in[l][:, O_NSA + ncl]
            cw = f(inp["conv_w"])[l]
            for jj in range(4):
                for i in range(3):
                    pv[l, :, PV_CONV + jj * 3 + i] = cw[i, jj * 128:(jj + 1) * 128]
            pv[l, :, PV_MEMQG] = f(inp["mem_qk_gain"])[l, 0]
            pv[l, :, PV_MEMKG] = f(inp["mem_qk_gain"])[l, 1]
            mu = np.zeros(1024, np.float32)
            mu[ok] = mufull[l][rc[ok]]
            pv[l, :, PV_MU:PV_MU + 8] = mu.reshape(8, 128).T
            for idx, nm in ((PV_W0, "rwkv_w0"), (PV_A0, "rwkv_a0"), (PV_KK, "rwkv_kk"), (PV_KA, "rwkv_ka"), (PV_RK, "rwkv_rk"),
                            (PV_LNW, "rwkv_ln_w"), (PV_LNB, "rwkv_ln_b")):
                pv[l, :, idx] = f(inp[nm])[l, ch]
            if l == 1:
                pv[l, :, PV_V0] = f(inp["vres_v0"])[0, ch]
                lora[l, 0:64, 4] = f(inp["vres_up"])[0][:, ch]
            g = f(inp["nsa_qk_gain"])[l]
            for i in range(4):
                pv[l, :, PV_NSAG + i] = np.concatenate([g[i], g[i]])
            pv[l, :, PV_CB1:PV_CB1 + 2] = f(inp["nsa_cmp_b1"])[l].T
            lora[l, 0:96, 0] = f(inp["rwkv_w2"])[l][:, ch]
            lora[l, 0:96, 1] = f(inp["rwkv_a2"])[l][:, ch]
            lora[l, :, 2] = f(inp["rwkv_g2"])[l][0:128, ch]
            lora[l, :, 3] = f(inp["rwkv_g2"])[l][128:256, ch]
        m["wr"], m["wn"], m["pvec"], m["lora"] = wr, wn, pv, lora
        maps.append(m)
    return maps


NHQ = 8


def hall_read(K, dst3, C, r, col0, n):
    for q in range(NHQ):
        src = C["hT_all"][q].ap()[r * 256:(r + 1) * 256, col0:col0 + n].rearrange("(kc p) t -> p kc t", p=128)
        K.dma("sp", dst3[:, 2 * q:2 * q + 2, :], src, reads=["hT_all"], writes=[C["_hall_dst"]])


def ysrc_write(K, Y, rows, t0, n, src_tile, src_ap):
    TC = K.TC
    done = 0
    while done < n:
        j, off = (t0 + done) // TC, (t0 + done) % TC
        m = min(n - done, TC - off)
        K.dma("sp", Y["src"][j].ap()[rows, off:off + m], src_ap[:, done:done + m], reads=[src_tile], writes=[Y["key"] + "_src"])
        done += m


def ygather(K, Y):
    for j in range(4):
        K.fw.allgather(Y["src"][j], Y["all"][j], GROUPS, reads=[Y["key"] + "_src"], writes=[Y["key"] + "_all"])


def load_hall_block(K, hb, C, t0, TC, blk=512):
    sub = min(blk, TC)
    for si in range(blk // sub):
        tok = t0 + si * sub
        r_, off = tok // TC, tok % TC
        C["_hall_dst"] = hb
        hall_read(K, hb[:, :, si * sub:(si + 1) * sub], C, r_, off, sub)


CS = 16
GN_EPS = 64e-5


def rwkv_phase(K, l, C, R):
    T = K.T
    TC = K.TC
    NB = T // 512
    pv = C["pv"][l]
    K.push()
    wr = K.sb("rw_w", [128, 16, 1024], BF16)
    for i in range(2):
        K.dma("pool", wr[:, :, i * 512:(i + 1) * 512],
              K.inputs["wr"].ap()[l].rearrange("(kc p) n -> p kc n", p=128)[:, :, i * 512:(i + 1) * 512], writes=[wr])
    lora = K.sb("rw_lora", [128, 5, 128])
    K.dma("sp", lora[:], K.inputs["lora"].ap()[l], writes=[lora])
    blk = K.sb("rw_blk", [128, 128])
    K.dma("sp", blk[:], K.inputs["blk64"].ap(), writes=[blk])
    hTb = [K.sb(f"rw_hT{i}", [128, 16, 512], BF16) for i in range(2)]
    nct = 8 if l == 1 else 7
    ub = [K.sb(f"rw_ub{i}", [128, 513]) for i in range(nct)]
    uf = [K.sb(f"rw_uf{i}", [128, 512]) for i in range(nct)]
    tmp = [K.sb(f"rw_t{i}", [128, 512]) for i in range(8)]
    tmo = [K.sb(f"rw_tmo{i}", [128, 4, 128]) for i in range(2)]
    for ct in range(nct):
        K.memset(ub[ct][:, 0:1], 0.0, [ub[ct]])
    sc = lambda i: pv[:, i:i + 1]
    for tb in range(NB):
        t0 = tb * 512
        hb = hTb[tb % 2]
        load_hall_block(K, hb, C, t0, TC)
        for ct in range(nct):
            ps = K.psum()
            for kc in range(16):
                K.mm(ps[:, :], wr[:, kc, ct * 128:(ct + 1) * 128], hb[:, kc, :], kc == 0, kc == 15, [wr, hb], [ps])
            K.copy(ub[ct][:, 1:513], ps[:, :], [ps], [ub[ct]], eng="act")
            d = tmp[0]
            K.tt(d[:], ub[ct][:, 0:512], ub[ct][:, 1:513], ALU.subtract, [ub[ct]], [d])
            K.stt(uf[ct][:], d[:], sc(PV_MU + ct), ub[ct][:, 1:513], ALU.mult, ALU.add, [d, pv, ub[ct]], [uf[ct]])
            K.copy(ub[ct][:, 0:1], ub[ct][:, 512:513], [ub[ct]], [ub[ct]])
        r, k, v = uf[0], uf[1], uf[2]
        K.act(uf[3][0:96, :], uf[3][0:96, :], AF.Tanh, [uf[3]], [uf[3]])
        ps = K.psum()
        K.mm(ps[:, :], lora[0:96, 0, :], uf[3][0:96, :], True, True, [lora, uf[3]], [ps])
        dec = tmp[1]
        K.act(dec[:], ps[:, :], AF.Sigmoid, [ps, pv], [dec], bias=sc(PV_W0))
        K.act(dec[:], dec[:], AF.Exp, [dec], [dec], scale=-float(np.exp(-0.5)))
        K.dma("sp", R["fm_w"].ap()[:, t0:t0 + 512], dec[:], reads=[dec], writes=["fm_w"])
        ps = K.psum()
        K.mm(ps[:, :], lora[0:96, 1, :], uf[4][0:96, :], True, True, [lora, uf[4]], [ps])
        a = tmp[2]
        K.act(a[:], ps[:, :], AF.Sigmoid, [ps, pv], [a], bias=sc(PV_A0))
        K.act(uf[5][:], uf[5][:], AF.Sigmoid, [uf[5]], [uf[5]])
        K.act(uf[6][:], uf[6][:], AF.Sigmoid, [uf[6]], [uf[6]])
        ps = K.psum()
        K.mm(ps[:, :], lora[:, 2, :], uf[5][:], True, False, [lora, uf[5]], [ps])
        K.mm(ps[:, :], lora[:, 3, :], uf[6][:], False, True, [lora, uf[6]], [ps])
        g = tmp[3]
        K.copy(g[:], ps[:, :], [ps], [g], eng="act")
        K.dma("sp", R["fm_g"].ap()[:, t0:t0 + 512], g[:], reads=[g], writes=["fm_g"])
        if l == 0:
            K.dma("sp", R["fm_vfirst"].ap()[:, t0:t0 + 512], v[:], reads=[v], writes=["fm_vfirst"])
        else:
            ps = K.psum()
            K.mm(ps[:, :], lora[0:64, 4, :], uf[7][0:64, :], True, True, [lora, uf[7]], [ps])
            vr = tmp[4]
            K.act(vr[:], ps[:, :], AF.Sigmoid, [ps, pv], [vr], bias=sc(PV_V0))
            vf = tmp[5]
            K.dma("sp", vf[:], R["fm_vfirst"].ap()[:, t0:t0 + 512], reads=["fm_vfirst"], writes=[vf])
            K.tt(vf[:], vf[:], v[:], ALU.subtract, [vf, v], [vf])
            K.tt(vf[:], vf[:], vr[:], ALU.mult, [vf, vr], [vf])
            K.tt(v[:], v[:], vf[:], ALU.add, [v, vf], [v])
        kk = tmp[4]
        K.ts(kk[:], k[:], sc(PV_KK), None, ALU.mult, None, [k, pv], [kk])
        sq = tmp[5]
        K.tt(sq[:], kk[:], kk[:], ALU.mult, [kk], [sq])
        ps = K.psum()
        K.mm(ps[:, :], blk[:, :], sq[:], True, True, [blk, sq], [ps])
        rn = tmp[5]
        K.ts(rn[:], ps[:, :], 1e-24, None, ALU.max, None, [ps], [rn])
        K.act(rn[:], rn[:], AF.Sqrt, [rn], [rn])
        K.op("dve", lambda e: e.reciprocal(out=rn[:], in_=rn[:]), [rn], [rn])
        K.tt(kk[:], kk[:], rn[:], ALU.mult, [kk, rn], [kk])
        nkk = tmp[5]
        K.ts(nkk[:], kk[:], -1.0, None, ALU.mult, None, [kk], [nkk])
        K.dma("sp", R["fm_nkk"].ap()[:, t0:t0 + 512], nkk[:], reads=[nkk], writes=["fm_nkk"])
        t1 = tmp[6]
        K.ts(t1[:], a[:], sc(PV_KA), sc(PV_C1), ALU.mult, ALU.add, [a, pv], [t1])
        kp = tmp[7]
        K.tt(kp[:], k[:], t1[:], ALU.mult, [k, t1], [kp])
        K.dma("sp", R["fm_kp"].ap()[:, t0:t0 + 512], kp[:], reads=[kp], writes=["fm_kp"])
        kka = tmp[6]
        K.tt(kka[:], kk[:], a[:], ALU.mult, [kk, a], [kka])
        for which, (src, dst) in enumerate(((kka, "tm_kka"), (v, "tm_v"))):
            ps = K.psum()
            for i in range(4):
                K.tr(ps[:, i * 128:(i + 1) * 128], src[:, i * 128:(i + 1) * 128], C["identf"][:], [src, C["identf"]], [ps])
            K.copy(tmo[which][:], ps[:, :].rearrange("p (a b) -> p a b", a=4), [ps], [tmo[which]], eng="act")
            K.dma("sp", R[dst].ap()[t0:t0 + 512, :].rearrange("(a p) c -> p a c", p=128), tmo[which][:], reads=[tmo[which]], writes=[dst])
        t2 = tmp[0]
        K.stt(t2[:], r[:], sc(PV_RK), kp[:], ALU.mult, ALU.mult, [r, pv, kp], [t2])
        ps = K.psum()
        K.mm(ps[:, :], blk[:, :], t2[:], True, True, [blk, t2], [ps])
        bon = tmp[0]
        K.tt(bon[:], ps[:, :], v[:], ALU.mult, [ps, v], [bon])
        K.dma("sp", R["fm_bonus"].ap()[:, t0:t0 + 512], bon[:], reads=[bon], writes=["fm_bonus"])
        K.dma("sp", R["fm_r"].ap()[:, t0:t0 + 512], r[:], reads=[r], writes=["fm_r"])
    barrier(K)
    K.pop()

    K.push()
    NH = 2
    Sring = [[K.sb(f"sc_S{h}_{i}", [64, 64]) for i in range(4)] for h in range(NH)]
    kkaB = [[K.sb(f"sc_kkaB{h}_{i}", [64, CS, 64]) for i in range(2)] for h in range(NH)]
    vB = [[K.sb(f"sc_vB{h}_{i}", [64, CS, 64]) for i in range(2)] for h in range(NH)]
    Abuf = [[K.sb(f"sc_A{h}_{i}", [64, CS, 64]) for i in range(2)] for h in range(NH)]
    Dg = [K.sb(f"sc_Dg{h}", [64, CS, 64]) for h in range(NH)]
    fmb = {n: [[K.sb(f"sc_{n}{h}_{i}", [64, 512]) for i in range(2)] for h in range(NH)] for n in ("w", "nkk", "kp", "r", "g", "bonus")}
    yT = [K.sb(f"sc_yT{h}", [64, 512]) for h in range(NH)]
    pt = [K.sb(f"sc_pt{h}_{i}", [64, 512]) for h in range(NH) for i in range(3)]
    pvh = [K.sb(f"sc_pv{h}", [64, NV]) for h in range(NH)]
    ones64 = K.sb("sc_ones64", [64, 64])
    K.memset(ones64[:], 1.0 / 64, [ones64])
    for h in range(NH):
        K.dma("sp", pvh[h][:], K.inputs["pvec"].ap()[l][h * 64:(h + 1) * 64, :], writes=[pvh[h]])
        K.memset(Sring[h][0][:], 0.0, [Sring[h][0]])
    psS = [K.ps[0], K.ps[1]]
    psY = [K.ps[2], K.ps[3]]
    ident64 = C["identf"][0:64, 0:64]
    nchunk = 512 // CS
    pend_y = []
    for tb in range(NB):
        t0 = tb * 512
        for h in range(NH):
            for n in fmb:
                K.dma("sp", fmb[n][h][tb % 2][:], R["fm_" + n].ap()[h * 64:(h + 1) * 64, t0:t0 + 512], reads=["fm_" + n], writes=[fmb[n][h][tb % 2]])
        for ci in range(nchunk):
            c0 = ci * CS
            gi = tb * nchunk + ci
            for h in range(NH):
                kb, vb, A = kkaB[h][gi % 2], vB[h][gi % 2], Abuf[h][gi % 2]
                K.dma("sp", kb[:], R["tm_kka"].ap()[t0 + c0:t0 + c0 + CS, h * 64:(h + 1) * 64].partition_broadcast(64), reads=["tm_kka"], writes=[kb])
                K.dma("sp", vb[:], R["tm_v"].ap()[t0 + c0:t0 + c0 + CS, h * 64:(h + 1) * 64].partition_broadcast(64), reads=["tm_v"], writes=[vb])
                nk = fmb["nkk"][h][tb % 2]
                w = fmb["w"][h][tb % 2]
                K.tt(A[:], kb[:], nk[:, c0:c0 + CS].unsqueeze(2).to_broadcast([64, CS, 64]), ALU.mult, [kb, nk], [A], eng="pool")
                K.tt(Dg[h][:], ident64.unsqueeze(1).to_broadcast([64, CS, 64]), w[:, c0:c0 + CS].unsqueeze(2).to_broadcast([64, CS, 64]),
                     ALU.mult, [C["identf"], w], [Dg[h]], eng="pool")
                K.tt(A[:], A[:], Dg[h][:], ALU.add, [A, Dg[h]], [A], eng="pool" if h == 0 else "dve")
            for s in range(CS):
                tg = gi * CS + s
                for h in range(NH):
                    A = Abuf[h][gi % 2]
                    Sp = Sring[h][tg % 4]
                    slot = tg % 8
                    pk = f"psS{h}_{slot}"
                    pso = psS[h][0:64, slot * 64:(slot + 1) * 64]
                    K.mm(pso, A[:, s, :], Sp[:], True, True, [A, Sp], [pk])
                for fn_ in pend_y:
                    fn_()
                pend_y.clear()
                for h in range(NH):
                    vb = vB[h][gi % 2]
                    Sn = Sring[h][(tg + 1) % 4]
                    slot = tg % 8
                    pk = f"psS{h}_{slot}"
                    pso = psS[h][0:64, slot * 64:(slot + 1) * 64]
                    kp = fmb["kp"][h][tb % 2]
                    K.stt(Sn[:], vb[:, s, :], kp[:, c0 + s:c0 + s + 1], pso, ALU.mult, ALU.add, [vb, kp, pk], [Sn])
                    rr = fmb["r"][h][tb % 2]

                    def ymm(h=h, Sn=Sn, rr=rr, col=c0 + s):
                        K.mm(psY[h][0:64, col:col + 1], Sn[:], rr[:, col:col + 1], True, True, [Sn, rr], [psY[h]])
                    pend_y.append(ymm)
        for fn_ in pend_y:
            fn_()
        pend_y.clear()
        for h in range(NH):
            y, p0, p1, p2 = yT[h], pt[h * 3], pt[h * 3 + 1], pt[h * 3 + 2]
            K.copy(y[:], psY[h][0:64, :], [psY[h]], [y], eng="act")
            ps = K.psum4()
            K.mm(ps[0:64, :], ones64[:], y[:], True, True, [ones64, y], [ps])
            K.tt(p0[:], y[:], ps[0:64, :], ALU.subtract, [y, ps], [p0])
            K.tt(p1[:], p0[:], p0[:], ALU.mult, [p0], [p1])
            ps = K.psum4()
            K.mm(ps[0:64, :], ones64[:], p1[:], True, True, [ones64, p1], [ps])
            K.ts(p1[:], ps[0:64, :], GN_EPS, None, ALU.add, None, [ps], [p1])
            K.act(p1[:], p1[:], AF.Sqrt, [p1], [p1])
            K.op("dve", lambda e: e.reciprocal(out=p1[:], in_=p1[:]), [p1], [p1])
            K.tt(p0[:], p0[:], p1[:], ALU.mult, [p0, p1], [p0])
            K.ts(p0[:], p0[:], pvh[h][:, PV_LNW:PV_LNW + 1], pvh[h][:, PV_LNB:PV_LNB + 1], ALU.mult, ALU.add, [p0, pvh[h]], [p0])
            bo, g = fmb["bonus"][h][tb % 2], fmb["g"][h][tb % 2]
            K.tt(p0[:], p0[:], bo[:], ALU.add, [p0, bo], [p0])
            K.tt(p2[:], p0[:], g[:], ALU.mult, [p0, g], [p2])
            ysrc_write(K, R["yB"], slice(h * 64, (h + 1) * 64), t0, 512, p2, p2)
    barrier(K)
    K.pop()


def select_chunk(K, dst, Y, C):
    oh = C["oh"]
    cand, acc = C["sel_cand"], C["sel_acc"]
    for kc in range(4):
        for j in range(4):
            K.dma("sp", cand[:, j, :], Y["all"][j].ap()[kc * 128:(kc + 1) * 128, :], reads=[Y["key"] + "_all"], writes=[cand])
        K.ts(acc[:], cand[:, 0, :], oh[:, 0:1], None, ALU.mult, None, [cand, oh], [acc])
        for j in range(1, 4):
            K.stt(acc[:], cand[:, j, :], oh[:, j:j + 1], acc[:], ALU.mult, ALU.add, [cand, oh, acc], [acc])
        K.copy(dst[:, kc, :], acc[:], [acc], [dst])


def merge_phase(K, l, C, S, xres):
    TC, NT = K.TC, K.NT
    hTx = C["hTx"]
    w_in = K.inputs["w_in"].ap()[l]
    K.push()
    wg = [K.sb(f"mg_wg{i}", [128, 16, 512], BF16) for i in range(2)]
    wb = [K.sb(f"mg_wb{i}", [128, 4, 512], BF16) for i in range(2)]
    psc = K.sb("mg_psc", [128, 512])
    macc = K.sb("mg_macc", [128, NT, 512])
    merged = K.sb("mg_merged", [128, NT, D], BF16)
    gt = [K.sb(f"mg_gt{i}", [128, 512]) for i in range(2)]
    tm = [K.sb(f"mg_tm{i}", [128, 512]) for i in range(2)]
    ys = [S["y_rwkvT"], S["y_nsaT"], S["y_convT"], S["y_memT"]]
    n = 0
    for nb in range(4):
        K.dma("sp", psc[:], K.inputs["pool_scale"].ap()[l:l + 1, nb * 512:(nb + 1) * 512].partition_broadcast(128), writes=[psc])
        for i in range(5):
            g_, b_ = wg[n % 2], wb[n % 2]
            n += 1
            load_w(K, g_, w_in, O_GATE + i * D + nb * 512, 512)
            if i < 4:
                K.dma("pool", b_[:], K.inputs["w_branch"].ap()[l, i].rearrange("(kc p) n -> p kc n", p=128)[:, :, nb * 512:(nb + 1) * 512], writes=[b_])
            else:
                K.dma("pool", b_[:, 0, :], K.inputs["pool_w"].ap()[l, nb], writes=[b_])
            for tt in range(NT):
                tsl = slice(tt * 128, (tt + 1) * 128)
                psg = K.psum()
                for kc in range(16):
                    K.mm(psg[:, :], hTx[:, kc, HALO + tt * 128: HALO + (tt + 1) * 128], g_[:, kc, :], kc == 0, kc == 15, [hTx, g_], [psg])
                G = gt[tt % 2]
                K.act(G[:], psg[:, :], AF.Sigmoid, [psg], [G])
                psb = K.psum()
                if i < 4:
                    for kc in range(4):
                        K.mm(psb[:, :], ys[i][:, kc, tsl], b_[:, kc, :], kc == 0, kc == 3, [ys[i], b_], [psb])
                else:
                    K.mm(psb[:, :], S["pooledT"][:, nb, tsl], b_[:, 0, :], True, True, [S["pooledT"], b_], [psb])
                    K.tt(G[:], G[:], psc[:], ALU.mult, [G, psc], [G])
                if i == 0:
                    K.tt(macc[:, tt, :], G[:], psb[:, :], ALU.mult, [G, psb], [macc])
                else:
                    t_ = tm[tt % 2]
                    K.tt(t_[:], G[:], psb[:, :], ALU.mult, [G, psb], [t_])
                    K.tt(macc[:, tt, :], macc[:, tt, :], t_[:], ALU.add, [macc, t_], [macc])
        K.copy(merged[:, :, nb * 512:(nb + 1) * 512], macc[:], [macc], [merged], eng="act")
    mT = hTx
    for tt in range(NT):
        for grp in range(2):
            ps = K.psum()
            psb_ = ps[:].bitcast(BF16)
            for i in range(8):
                kc = grp * 8 + i
                K.tr(psb_[:, i * 128:(i + 1) * 128], merged[:, tt, kc * 128:(kc + 1) * 128], C["identb"][:], [merged, C["identb"]], [ps])
            K.copy(mT[:, grp * 8:(grp + 1) * 8, HALO + tt * 128: HALO + (tt + 1) * 128], psb_.rearrange("p (a b) -> p a b", a=8), [ps], [mT],
                   eng="act" if grp == 0 else "dve")
    xt = [K.sb(f"mg_xt{i}", [128, 512]) for i in range(2)]
    for nb in range(4):
        wo = wg[nb % 2]
        load_w(K, wo, K.inputs["w_out"].ap()[l], nb * 512, 512)
        for tt in range(NT):
            ps = K.psum()
            for kc in range(16):
                K.mm(ps[:, :], mT[:, kc, HALO + tt * 128: HALO + (tt + 1) * 128], wo[:, kc, :], kc == 0, kc == 15, [mT, wo], [ps])
            x_ = xt[tt % 2]
            K.dma("sp", x_[:], xres.ap()[tt * 128:(tt + 1) * 128, nb * 512:(nb + 1) * 512], reads=["xres"], writes=[x_])
            K.tt(x_[:], x_[:], ps[:, :], ALU.add, [x_, ps], [x_])
            K.dma("sp", xres.ap()[tt * 128:(tt + 1) * 128, nb * 512:(nb + 1) * 512], x_[:], reads=[x_], writes=["xres"])
    barrier(K)
    K.pop()


def ffn_phase(K, l, C, xres, xout):
    TC = K.TC
    BL = min(512, TC)
    NBL = TC // BL
    TPB = BL // 128
    moe = (l % 2 == 1)
    K.push()
    h2T = C["hTx"]
    K.push()
    rn_alloc(K, C)
    K.dma("sp", C["gbc"][:], K.inputs["norm_ffn"].ap()[l:l + 1, :].partition_broadcast(128), writes=[C["gbc"]])
    rmsnorm_T(K, xres.ap(), C["gbc"], h2T, HALO, K.NT, C, "ffn")
    barrier(K)
    K.pop()
    NFE = 22
    NT = K.NT
    uT = K.sb("ff_uT", [128, NFE, TC], BF16)
    w1g = [K.sb(f"ff_w1_{i}", [128, 16, 256], BF16) for i in range(2)]
    w3g = [K.sb(f"ff_w3_{i}", [128, 16, 256], BF16) for i in range(2)]
    w2g = [K.sb(f"ff_w2_{i}", [128, 11, 512], BF16) for i in range(2)]
    sa = [K.sb(f"ff_sa{i}", [128, 512]) for i in range(2)]
    xacc = K.sb("ff_xacc", [128, NT, D])
    if moe:
        rt = K.sb("ff_rt", [128, 16, 8], BF16)
        K.dma("pool", rt[:], K.inputs["moe_router"].ap()[0].rearrange("(kc p) e -> p kc e", p=128), writes=[rt])
        comb = K.sb("ff_comb", [128, K.NT, 8])
        lg, l2, m1, m2, mk1, mk2 = (K.sb("ff_" + n, [128, 8]) for n in ("lg", "l2", "m1", "m2", "mk1", "mk2"))
        for tt in range(K.NT):
            ps = K.psum()
            for kc in range(16):
                K.mm(ps[:, 0:8], h2T[:, kc, HALO + tt * 128: HALO + (tt + 1) * 128], rt[:, kc, :], kc == 0, kc == 15, [h2T, rt], [ps])
            K.copy(lg[:], ps[:, 0:8], [ps], [lg])
            K.op("dve", lambda e: e.reduce_max(out=m1[:, 0:1], in_=lg[:], axis=AX.X), [lg], [m1])
            K.ts(mk1[:], lg[:], m1[:, 0:1], None, ALU.is_ge, None, [lg, m1], [mk1])
            K.stt(l2[:], mk1[:], -1e30, lg[:], ALU.mult, ALU.add, [mk1, lg], [l2])
            K.op("dve", lambda e: e.reduce_max(out=m2[:, 0:1], in_=l2[:], axis=AX.X), [l2], [m2])
            K.ts(mk2[:], l2[:], m2[:, 0:1], None, ALU.is_ge, None, [l2, m2], [mk2])
            K.tt(m1[:, 1:2], m1[:, 0:1], m2[:, 0:1], ALU.subtract, [m1, m2], [m1])
            K.act(m1[:, 2:3], m1[:, 1:2], AF.Sigmoid, [m1], [m1])
            K.ts(m1[:, 3:4], m1[:, 2:3], -1.0, 1.0, ALU.mult, ALU.add, [m1], [m1])
            K.ts(mk1[:], mk1[:], m1[:, 2:3], None, ALU.mult, None, [mk1, m1], [mk1])
            K.stt(comb[:, tt, :], mk2[:], m1[:, 3:4], mk1[:], ALU.mult, ALU.add, [mk2, m1, mk1], [comb])
    for tt in range(NT):
        K.dma("sp", xacc[:, tt, :], xres.ap()[tt * 128:(tt + 1) * 128, :], reads=["xres"], writes=[xacc])
    if moe:
        pexp = [tuple(K.inputs[n].ap()[0, e] for n in ("moe_w1", "moe_w3", "moe_w2")) + (0, e) for e in range(N_EXP)]
    else:
        pexp = [tuple(K.inputs[n].ap()[0] for n in ("ffn_w1", "ffn_w3", "ffn_w2")) + (fo, None) for fo in (0, NFE)]
    nld = [0]
    for (W1, W3, W2, fo, e) in pexp:
        for c0 in range(0, NFE * 128, 256):
            a_, b_ = w1g[nld[0] % 2], w3g[nld[0] % 2]
            nld[0] += 1
            load_w(K, a_, W1, fo * 128 + c0, 256)
            load_w(K, b_, W3, fo * 128 + c0, 256)
            for fi in range(2):
                f = c0 // 128 + fi
                for bl in range(NBL):
                    cb = HALO + bl * BL
                    pa, pb = K.psum_from(0, 4), K.psum_from(0, 4)
                    for kc in range(16):
                        K.mm(pa[:, 0:BL], a_[:, kc, fi * 128:(fi + 1) * 128], h2T[:, kc, cb:cb + BL], kc == 0, kc == 15, [a_, h2T], [pa])
                    for kc in range(16):
                        K.mm(pb[:, 0:BL], b_[:, kc, fi * 128:(fi + 1) * 128], h2T[:, kc, cb:cb + BL], kc == 0, kc == 15, [b_, h2T], [pb])
                    s_ = sa[(f * NBL + bl) % 2]
                    K.act(s_[:, 0:BL], pa[:, 0:BL], AF.Silu, [pa], [s_])
                    K.tt(uT[:, f, bl * BL:(bl + 1) * BL], s_[:, 0:BL], pb[:, 0:BL], ALU.mult, [s_, pb], [uT])
        for nb in range(4):
            for fh in range(2):
                w2_ = w2g[nld[0] % 2]
                nld[0] += 1
                r0 = (fo + fh * 11) * 128
                K.dma("pool", w2_[:, :, :], W2[r0:r0 + 11 * 128, nb * 512:(nb + 1) * 512].rearrange("(f p) n -> p f n", p=128), writes=[w2_])
                for tt in range(NT):
                    ps = K.psum_from(4, 4)
                    for f in range(11):
                        K.mm(ps[:, :], uT[:, fh * 11 + f, tt * 128:(tt + 1) * 128], w2_[:, f, :], f == 0, f == 10, [uT, w2_], [ps])
                    xs = xacc[:, tt, nb * 512:(nb + 1) * 512]
                    if moe:
                        K.stt(xs, ps[:, :], comb[:, tt, e:e + 1], xs, ALU.mult, ALU.add, [ps, comb, xacc], [xacc])
                    else:
                        K.tt(xs, xs, ps[:, :], ALU.add, [xacc, ps], [xacc])
    for tt in range(NT):
        K.dma("sp", xout.ap()[tt * 128:(tt + 1) * 128, :], xacc[:, tt, :], reads=[xacc], writes=[xout.name])
    barrier(K)
    K.pop()


WN_TM = 704
NWN2 = WN_TM + 140
SEL_N = 16
import os
NSA_STOP = int(os.environ.get('NSA_STOP', '0'))
NSA_SUB = int(os.environ.get('NSA_SUB', '0'))


def nsa_phase(K, l, C, R):
    T, TC = K.T, K.TC
    NB, NTT = T // 512, T // 128
    NS = T // 64
    NCMP = (T - 32) // 16 + 1
    CT = [(c0, min(128, NCMP - c0)) for c0 in range(0, NCMP, 128)]
    VW = 64 + NS + 1
    pv = C["pv"][l]
    sc = lambda i: pv[:, i:i + 1]
    K.push()
    qT = [K.sb(f"ns_q{i}T", [128, T], BF16) for i in range(2)]
    ksT, kwT = K.sb("ns_ksT", [128, T], BF16), K.sb("ns_kwT", [128, T], BF16)
    kcT, vcT = K.sb("ns_kcT", [64, T], BF16), K.sb("ns_vcT", [64, T], BF16)
    Vs, Vw = K.sb("ns_Vs", [128, NTT, 66], BF16), K.sb("ns_Vw", [128, NTT, 66], BF16)
    gts = K.sb("ns_g", [128, NTT, 12])
    Oacc = K.sb("ns_O", [128, NTT, 128])
    blk = K.sb("ns_blk", [128, 128])
    K.dma("sp", blk[:], K.inputs["blk64"].ap(), writes=[blk])
    K.memset(Vs[:, :, 64:65], 1.0, [Vs])
    K.memset(Vw[:, :, 64:65], 1.0, [Vw])
    imp = K.sb("ns_imp", [128, NTT, NS])
    selT = K.sb("ns_selT", [64, T], BF16)
    ebuf = [K.sb(f"ns_e{i}", [128, 512], BF16) for i in range(4)]
    rv = [K.sb(f"ns_rv{i}", [128, 2]) for i in range(2)]
    kcmpT = K.sb("ns_kcmpT", [128, 256], BF16)
    Vc = K.sb("ns_Vc", [128, len(CT), VW + 1], BF16)
    K.push()
    wn = K.sb("ns_wn", [128, 16, NWN2], BF16)
    wsrc = K.inputs["wn"].ap()[l].rearrange("(kc p) n -> p kc n", p=128)
    K.dma("pool", wn[:, :, 0:512], wsrc[:, :, 0:512], writes=[wn])
    K.dma("pool", wn[:, :, 512:NWN2], wsrc[:, :, 512:NWN2], writes=[wn])
    B1 = 256
    hTb = [K.sb("ns_hT0", [128, 16, B1], BF16)] * 2
    pf = K.sb("ns_pf", [128, B1])
    for tb in range(T // B1):
        t0 = tb * B1
        hb = hTb[tb % 2]
        load_hall_block(K, hb, C, t0, TC, B1)
        specs = [(0, 128, qT[0], PV_NSAG + 0), (128, 128, qT[1], PV_NSAG + 0), (384, 128, ksT, PV_NSAG + 2), (512, 128, kwT, PV_NSAG + 3),
                 (256, 64, kcT, None), (640, 64, vcT, None)]
        if NSA_SUB == 1:
            continue
        for (c0, nc_, dst, gi) in specs:
            ps = K.psum()
            for kc in range(16):
                K.mm(ps[0:nc_, 0:B1], wn[:, kc, c0:c0 + nc_], hb[:, kc, :], kc == 0, kc == 15, [wn, hb], [ps])
            if gi is None:
                K.copy(dst[:, t0:t0 + B1], ps[0:nc_, 0:B1], [ps], [dst], eng="act")
            else:
                K.copy(pf[:], ps[:, 0:B1], [ps], [pf], eng="act")
                sq = C["fm_sq"]
                K.tt(sq[:, 0:B1], pf[:], pf[:], ALU.mult, [pf], [sq])
                ps2 = K.psum()
                K.mm(ps2[:, 0:B1], blk[:, :], sq[:, 0:B1], True, True, [blk, sq], [ps2])
                rs = C["fm_rs"]
                K.ts(rs[:, 0:B1], ps2[:, 0:B1], 1.0 / 64, EPS, ALU.mult, ALU.add, [ps2], [rs])
                K.act(rs[:, 0:B1], rs[:, 0:B1], AF.Sqrt, [rs], [rs])
                K.op("dve", lambda e: e.reciprocal(out=rs[:, 0:B1], in_=rs[:, 0:B1]), [rs], [rs])
                K.stt(dst[:, t0:t0 + B1], pf[:], sc(gi), rs[:, 0:B1], ALU.mult, ALU.mult, [pf, pv, rs], [dst])
        if NSA_SUB == 2:
            continue
        for ti in range(B1 // 128):
            gt_ = tb * (B1 // 128) + ti
            ps = K.psum()
            for kc in range(16):
                K.mm(ps[:, 0:140], hb[:, kc, ti * 128:(ti + 1) * 128], wn[:, kc, WN_TM:WN_TM + 140], kc == 0, kc == 15, [wn, hb], [ps])
            K.copy(Vs[:, gt_, 0:64], ps[:, 0:64], [ps], [Vs], eng="act")
            K.copy(Vw[:, gt_, 0:64], ps[:, 64:128], [ps], [Vw])
            K.act(gts[:, gt_, :], ps[:, 128:140], AF.Sigmoid, [ps], [gts])
    barrier(K)
    K.pop()
    K.push()
    W1 = K.sb("ns_W1", [64, 32, 128], BF16)
    w2d = K.sb("ns_w2", [128, 128], BF16)
    posT = K.sb("ns_posT", [64, 32], BF16)
    hidT = K.sb("ns_hidT", [128, 256], BF16)
    hx = [K.sb(f"ns_hx{i}", [128, 256]) for i in range(3)]
    cb = K.sb("ns_cb", [128, 2])
    kg = K.sb("ns_kg", [128, 64])
    K.dma("sp", kg[:], K.inputs["nsa_qk_gain"].ap()[l, 1:2, :].partition_broadcast(128), writes=[kg])
    ktm = K.sb("ns_ktm", [128, 128])
    kss = K.sb("ns_kss", [128, 2])
    K.memset(Vc[:, :, VW - 1:VW], 1.0, [Vc])
    for ci, (c0, ncc) in enumerate(CT):
        K.dma("pool", Vc[0:ncc, ci, 64:64 + NS], K.inputs["ovl"].ap()[c0:c0 + ncc, 0:NS], writes=[Vc])
    for i in range(2):
        src = kcT if i == 0 else vcT
        K.dma("pool", W1[:], K.inputs["nsa_cmp_w1"].ap()[l, i].rearrange("(l d) j -> d l j", d=64), writes=[W1])
        K.dma("pool", posT[:], K.inputs["cmp_posT"].ap()[l, i], writes=[posT])
        K.dma("pool", w2d[:, 0:64], K.inputs["nsa_cmp_w2"].ap()[l, i], writes=[w2d])
        K.dma("pool", w2d[:, 64:128], K.inputs["nsa_cmp_w2"].ap()[l, i], writes=[w2d])
        ps = K.psum()
        for ll in range(32):
            K.mm(ps[:, 0:1], W1[:, ll, :], posT[:, ll:ll + 1], ll == 0, ll == 31, [W1, posT], [ps])
        K.tt(cb[:, i:i + 1], ps[:, 0:1], sc(PV_CB1 + i), ALU.add, [ps, pv], [cb])
        ps = K.psum()
        for ll in range(32):
            K.mm(ps[:, 0:NCMP], W1[:, ll, :], src[:, ll: ll + 16 * (NCMP - 1) + 1: 16], ll == 0, ll == 31, [W1, src], [ps])
        x_, x2, x3 = hx
        n_ = NCMP
        K.ts(x_[:, 0:n_], ps[:, 0:n_], cb[:, i:i + 1], None, ALU.add, None, [ps, cb], [x_])
        K.tt(x2[:, 0:n_], x_[:, 0:n_], x_[:, 0:n_], ALU.mult, [x_], [x2])
        K.ts(x2[:, 0:n_], x2[:, 0:n_], 0.044715, 1.0, ALU.mult, ALU.add, [x2], [x2])
        K.tt(x2[:, 0:n_], x2[:, 0:n_], x_[:, 0:n_], ALU.mult, [x2, x_], [x2])
        K.act(x3[:, 0:n_], x2[:, 0:n_], AF.Sigmoid, [x2], [x3], scale=1.5957691216057308)
        K.tt(hidT[:, 0:n_], x_[:, 0:n_], x3[:, 0:n_], ALU.mult, [x_, x3], [hidT])
        for ci, (c0, ncc) in enumerate(CT):
            ps = K.psum()
            K.mm(ps[0:ncc, 0:128], hidT[:, c0:c0 + ncc], w2d[:, :], True, True, [hidT, w2d], [ps])
            if i == 1:
                K.copy(Vc[0:ncc, ci, 0:64], ps[0:ncc, 0:64], [ps], [Vc], eng="act")
            else:
                K.copy(ktm[0:ncc, :], ps[0:ncc, 0:128], [ps], [ktm], eng="act")
                sq = C["fm_sq"]
                K.tt(sq[0:ncc, 0:64], ktm[0:ncc, 0:64], ktm[0:ncc, 0:64], ALU.mult, [ktm], [sq])
                K.op("dve", lambda e: e.reduce_sum(out=kss[0:ncc, 0:1], in_=sq[0:ncc, 0:64], axis=AX.X), [sq], [kss])
                K.ts(kss[0:ncc, 1:2], kss[0:ncc, 0:1], 1.0 / 64, EPS, ALU.mult, ALU.add, [kss], [kss])
                K.act(kss[0:ncc, 1:2], kss[0:ncc, 1:2], AF.Sqrt, [kss], [kss])
                K.op("dve", lambda e: e.reciprocal(out=kss[0:ncc, 1:2], in_=kss[0:ncc, 1:2]), [kss], [kss])
                for hf in range(2):
                    K.stt(ktm[0:ncc, hf * 64:(hf + 1) * 64], ktm[0:ncc, hf * 64:(hf + 1) * 64], kss[0:ncc, 1:2], kg[0:ncc, :], ALU.mult, ALU.mult,
                          [ktm, kss, kg], [ktm])
                ps2 = K.psum()
                K.tr(ps2[:, 0:ncc], ktm[0:ncc, :], C["identf"][0:ncc, 0:ncc], [ktm, C["identf"]], [ps2])
                K.copy(kcmpT[:, c0:c0 + ncc], ps2[:, 0:ncc], [ps2], [kcmpT])
    barrier(K)
    K.pop()
    K.push()
    maskc = K.sb("ns_maskc", [128, len(CT), T], BF16)
    for ci, (c0, ncc) in enumerate(CT):
        K.dma("pool", maskc[0:ncc, ci, :], K.inputs["maskc"].ap()[c0:c0 + ncc, 0:T], writes=[maskc])
    nrv = [0]

    def finish_acc(acc_ap, acckey, gtile, gate_idx):
        r_ = rv[nrv[0] % 2]
        nrv[0] += 1
        K.ts(r_[:, 0:1], acc_ap[:, 64:65], 1e-30, None, ALU.max, None, [acckey], [r_])
        K.op("dve", lambda e: e.reciprocal(out=r_[:, 0:1], in_=r_[:, 0:1]), [r_], [r_])
        K.tt(r_[:, 1:2], r_[:, 0:1], gts[:, gtile, gate_idx:gate_idx + 1], ALU.mult, [r_, gts], [r_])
        return r_

    first_o = {}
    for hd in range(4):
        qt, half = qT[hd // 2], slice((hd % 2) * 64, (hd % 2) * 64 + 64)
        for tb in range(NB):
            t0 = tb * 512
            for ci, (c0, ncc) in enumerate(CT):
                ps = K.psum_from(0, 7)
                K.mm(ps[0:ncc, :], kcmpT[half, c0:c0 + ncc], qt[half, t0:t0 + 512], True, True, [kcmpT, qt], [ps])
                e_ = ebuf[ci]
                K.act(e_[0:ncc, :], ps[0:ncc, :], AF.Exp, [ps], [e_], scale=0.125)
                K.tt(e_[0:ncc, :], e_[0:ncc, :], maskc[0:ncc, ci, t0:t0 + 512], ALU.mult, [e_, maskc], [e_])
            for ti in range(4):
                gt_ = tb * 4 + ti
                ps = K.psum_from(0, 7)
                for ci, (c0, ncc) in enumerate(CT):
                    K.mm(ps[:, 0:VW], ebuf[ci][0:ncc, ti * 128:(ti + 1) * 128], Vc[0:ncc, ci, 0:VW], ci == 0, ci == len(CT) - 1, [ebuf[ci], Vc], [ps])
                r_ = rv[nrv[0] % 2]
                nrv[0] += 1
                K.ts(r_[:, 0:1], ps[:, VW - 1:VW], 1e-30, None, ALU.max, None, [ps], [r_])
                K.op("dve", lambda e: e.reciprocal(out=r_[:, 0:1], in_=r_[:, 0:1]), [r_], [r_])
                if hd == 0:
                    K.ts(imp[:, gt_, :], ps[:, 64:64 + NS], r_[:, 0:1], None, ALU.mult, None, [ps, r_], [imp])
                else:
                    K.stt(imp[:, gt_, :], ps[:, 64:64 + NS], r_[:, 0:1], imp[:, gt_, :], ALU.mult, ALU.add, [ps, r_, imp], [imp])
                if hd < 2:
                    K.tt(r_[:, 1:2], r_[:, 0:1], gts[:, gt_, hd * 3:hd * 3 + 1], ALU.mult, [r_, gts], [r_])
                    K.ts(Oacc[:, gt_, hd * 64:(hd + 1) * 64], ps[:, 0:64], r_[:, 1:2], None, ALU.mult, None, [ps, r_], [Oacc])
    barrier(K)
    K.pop()
    K.push()
    keep, cadd = K.sb("ns_keep", [128, NS]), K.sb("ns_cadd", [128, NS])
    cmp3 = K.sb("ns_cmp3", [128, NS, NS])
    cnt, sel, ok = K.sb("ns_cnt", [128, NS]), K.sb("ns_sel", [128, NS]), K.sb("ns_ok", [128, NS])
    for gt_ in range(NTT):
        K.dma("sp", keep[:], K.inputs["tk_keep"].ap()[gt_ * 128:(gt_ + 1) * 128, 0:NS], writes=[keep])
        K.dma("sp", cadd[:], K.inputs["tk_cadd"].ap()[gt_ * 128:(gt_ + 1) * 128, 0:NS], writes=[cadd])
        im = imp[:, gt_, :]
        K.tt(im, im, keep[:], ALU.mult, [imp, keep], [imp])
        K.tt(im, im, cadd[:], ALU.add, [imp, cadd], [imp])
        K.tt(cmp3[:], im.unsqueeze(1).to_broadcast([128, NS, NS]), im.unsqueeze(2).to_broadcast([128, NS, NS]), ALU.is_gt, [imp], [cmp3])
        K.op("dve", lambda e: e.reduce_sum(out=cnt[:], in_=cmp3[:], axis=AX.X), [cmp3], [cnt])
        K.ts(sel[:], cnt[:], float(SEL_N) - 0.5, None, ALU.is_lt, None, [cnt], [sel])
        K.ts(ok[:], im, -1e8, None, ALU.is_gt, None, [imp], [ok])
        K.tt(sel[:], sel[:], ok[:], ALU.mult, [sel, ok], [sel])
        ps = K.psum_from(0, 7)
        K.tr(ps[0:NS, 0:128], sel[:], C["identf"][:], [sel, C["identf"]], [ps])
        K.copy(selT[0:NS, gt_ * 128:(gt_ + 1) * 128], ps[0:NS, 0:128], [ps], [selT], eng="act")
    barrier(K)
    K.pop()
    K.push()
    E2 = K.sb("ns_E2", [64, NTT, 128], BF16)
    K.dma("pool", E2[0:NS], K.inputs["e2"].ap()[0:NS, 0:NTT, :], writes=[E2])
    dmask = K.sb("ns_dmask", [128, 5, 512], BF16)
    K.dma("pool", dmask[:], K.inputs["dmask"].ap().rearrange("a p t -> p a t"), writes=[dmask])
    accb = K.ps[7]
    for hd in range(2):
        half = slice(hd * 64, hd * 64 + 64)
        for tb in range(NB):
            t0 = tb * 512
            njt = 4 * tb + 4
            for jt in range(njt):
                ps = K.psum_from(0, 4)
                K.mm(ps[:, :], ksT[half, jt * 128:(jt + 1) * 128], qT[0][half, t0:t0 + 512], True, True, [ksT, qT[0]], [ps])
                e_ = ebuf[jt % 2]
                K.act(e_[:, :], ps[:, :], AF.Exp, [ps], [e_], scale=0.125)
                pm = K.psum_from(0, 4)
                K.mm(pm[:, :], E2[0:NS, jt, :], selT[0:NS, t0:t0 + 512], True, True, [E2, selT], [pm])
                em = ebuf[2 + jt % 2]
                K.tt(em[:, :], e_[:, :], pm[:, :], ALU.mult, [e_, pm], [em])
                dd = jt - 4 * tb
                if dd >= 0:
                    K.tt(em[:, :], em[:, :], dmask[:, dd, :], ALU.mult, [em, dmask], [em])
                for ti in range(max(dd, 0), 4):
                    K.mm(K.ps[4 + ti][:, 0:65], em[:, ti * 128:(ti + 1) * 128], Vs[:, jt, 0:65], jt == 0, jt == 4 * tb + ti, [em, Vs], [K.ps[4 + ti]])
            for ti in range(4):
                gt_ = tb * 4 + ti
                a_ = K.ps[4 + ti][:, 0:65]
                r_ = finish_acc(a_, K.ps[4 + ti].name, gt_, hd * 3 + 1)
                K.stt(Oacc[:, gt_, hd * 64:(hd + 1) * 64], a_[:, 0:64], r_[:, 1:2], Oacc[:, gt_, hd * 64:(hd + 1) * 64], ALU.mult, ALU.add,
                      [K.ps[4 + ti], r_, Oacc], [Oacc])
    for hd in range(2):
        half = slice(hd * 64, hd * 64 + 64)
        for gt_ in range(NTT):
            jts = list(range(max(0, gt_ - 4), gt_ + 1))
            for jt in jts:
                ps = K.psum_from(0, 7)
                K.mm(ps[:, 0:128], kwT[half, jt * 128:(jt + 1) * 128], qT[0][half, gt_ * 128:(gt_ + 1) * 128], True, True, [kwT, qT[0]], [ps])
                e_ = ebuf[jt % 4]
                K.act(e_[:, 0:128], ps[:, 0:128], AF.Exp, [ps], [e_], scale=0.125)
                if jt == gt_:
                    K.tt(e_[:, 0:128], e_[:, 0:128], dmask[:, 0, 0:128], ALU.mult, [e_, dmask], [e_])
                elif jt == gt_ - 4:
                    K.tt(e_[:, 0:128], e_[:, 0:128], dmask[:, 4, 0:128], ALU.mult, [e_, dmask], [e_])
                K.mm(accb[:, 0:65], e_[:, 0:128], Vw[:, jt, 0:65], jt == jts[0], jt == jts[-1], [e_, Vw], ["ns_accb"])
            a_ = accb[:, 0:65]
            r_ = finish_acc(a_, "ns_accb", gt_, hd * 3 + 2)
            K.stt(Oacc[:, gt_, hd * 64:(hd + 1) * 64], a_[:, 0:64], r_[:, 1:2], Oacc[:, gt_, hd * 64:(hd + 1) * 64], ALU.mult, ALU.add,
                  ["ns_accb", r_, Oacc], [Oacc])
    ot = [K.sb(f"ns_ot{i}", [128, 512]) for i in range(2)]
    for tb in range(NB):
        ps = K.psum_from(0, 7)
        for ti in range(4):
            K.tr(ps[:, ti * 128:(ti + 1) * 128], Oacc[:, tb * 4 + ti, :], C["identf"][:], [Oacc, C["identf"]], [ps])
        o_ = ot[tb % 2]
        K.copy(o_[:], ps[:, :], [ps], [o_], eng="act")
        ysrc_write(K, R["yN"], slice(0, 128), tb * 512, 512, o_, o_)
    barrier(K)
    K.pop()
    K.pop()


INPUT_SHAPES = lambda T: {
    "x": [T // 4, D], "mem": [MEM_LEN, D], "w_in": [2, D, N_IN], "norm_mix": [2, D], "norm_ffn": [2, D], "norm_mem": [2, D],
    "mem_wkv": [2, D, 1024], "pool_w": [2, 4, 128, 512], "pool_scale": [2, D], "w_branch": [2, 4, 512, D], "w_out": [2, D, D],
    "ffn_w1": [1, D, D_FF], "ffn_w3": [1, D, D_FF], "ffn_w2": [1, D_FF, D], "moe_router": [1, D, 8],
    "moe_w1": [1, 8, D, E_FF], "moe_w3": [1, 8, D, E_FF], "moe_w2": [1, 8, E_FF, D],
    "nsa_cmp_w1": [2, 2, 2048, 128], "nsa_cmp_w2": [2, 2, 128, 64], "cmp_posT": [2, 2, 64, 32], "nsa_qk_gain": [2, 4, 64],
    "ident": [128, 128], "blk64": [128, 128], "ovl": [256, 64], "maskc": [256, T], "tk_keep": [T, 64], "tk_cadd": [T, 64],
    "e2": [64, 32, 128], "dmask": [5, 128, 512], "onehot": [128, 8], "invcnt": [4, T // 4], "pvec": [2, 128, NV],
    "wr": [2, D, 1024], "wn": [2, D, NWN], "lora": [2, 128, 5, 128],
}


def build(T, dbg=False, nlayers=DEPTH):
    K = KB(T)
    TC = K.TC
    out = K.dout("out", [TC, D])
    C = setup_common(K)
    xres = K.dscr("xres", [TC, D])
    K.dma("sp", xres.ap(), K.inputs["x"].ap(), writes=["xres"])
    C["hTx"] = K.sb("hTx", [128, 16, HALO + TC], BF16)
    alloc_gather(K, C)
    R = alloc_scratch(K)
    dbg_outs = []
    barrier(K)
    for l in range(nlayers):
        K.push()
        S = {n: K.sb(n, [128, 4, TC], BF16) for n in ("y_convT", "pooledT", "y_memT")}
        K.push()
        C["halo_cand"] = K.sb("halo_cand", [128, 4, 16, HALO], BF16)
        phase_norm_gather(K, l, C, xres)
        barrier(K)
        K.pop()
        local_mixers(K, l, C, S)
        rwkv_phase(K, l, C, R)
        ygather(K, R["yB"])
        nsa_phase(K, l, C, R)
        ygather(K, R["yN"])
        S["y_rwkvT"] = K.sb("y_rwkvT", [128, 4, TC], BF16)
        S["y_nsaT"] = K.sb("y_nsaT", [128, 4, TC], BF16)
        K.push()
        C["sel_cand"] = K.sb("sel_cand", [128, 4, TC])
        C["sel_acc"] = K.sb("sel_acc", [128, TC])
        select_chunk(K, S["y_rwkvT"], R["yB"], C)
        select_chunk(K, S["y_nsaT"], R["yN"], C)
        barrier(K)
        K.pop()
        if dbg and l == 0:
            for nm, Y in (("d_yN", R["yN"]), ("d_yB", R["yB"])):
                o = K.dout(nm, [512, T])
                for j in range(4):
                    K.dma("sp", o.ap()[:, j * TC:(j + 1) * TC], Y["all"][j].ap(), reads=[Y["key"] + "_all"], writes=[nm])
                dbg_outs.append(nm)
        merge_phase(K, l, C, S, xres)
        barrier(K)
        K.pop()
        if dbg and l == 0:
            o = K.dout("d_xmix", [TC, D])
            K.dma("sp", o.ap(), xres.ap(), reads=["xres"], writes=["d_xmix"])
            dbg_outs.append("d_xmix")
        last = (l == nlayers - 1)
        ffn_phase(K, l, C, xres, out if last else xres)
        if dbg and l == 0 and not last:
            o = K.dout("d_xout0", [TC, D])
            K.dma("sp", o.ap(), xres.ap(), reads=["xres"], writes=["d_xout0"])
            dbg_outs.append("d_xout0")
    K.fw.finish(["out"] + dbg_outs)
    return K


_CACHE = {}


def kernel(**inputs):
    T = int(np.asarray(inputs["x"]).shape[1])
    if T not in _CACHE:
        _CACHE[T] = build(T)
    K = _CACHE[T]
    maps = host_prep(inputs, T)
    maps = [{k: m[k] for k in K.inputs} for m in maps]
    res = run_bass_kernel_spmd(K.nc, maps, core_ids=list(range(NCORES)))
    TC = T // 4
    outp = np.zeros((2, T, D), np.float32)
    for c in range(NCORES):
        outp[c // 4, (c % 4) * TC:(c % 4 + 1) * TC] = res.results[c]["out"]
    return outp
```

```python
import numpy as np
import concourse.bass as bass
import concourse.mybir as mybir
from concourse.bass_utils import run_bass_kernel_spmd

F32 = mybir.dt.float32
BF16 = mybir.dt.bfloat16
ALU = mybir.AluOpType
AF = mybir.ActivationFunctionType
AX = mybir.AxisListType

D = 2048
NCORES = 8
MEM_LEN = 256
DEPTH = 2
D_FF = 5632
E_FF = 2816
N_EXP = 8
EPS = 1e-6
O_RWKV, O_NSA, O_CONV, O_POOL, O_MEM, O_GATE = 0, 1984, 3288, 4824, 5336, 5848
N_IN = 16088
HALO = 16


class FW:
    def __init__(self, nc, n_dma_sems=20):
        self.nc = nc
        self.eng = {"pe": nc.tensor, "act": nc.scalar, "dve": nc.vector, "pool": nc.gpsimd, "sp": nc.sync}
        self.sem = {}
        self.cnt = {}
        for e in self.eng:
            self.sem[e] = nc.semaphore("s_" + e).__enter__()
            self.cnt[e] = 0
        self.dsem = {}
        for q in ("sp", "pool", "act"):
            lst = [[nc.semaphore(f"d_{q}{i}").__enter__(), 0] for i in range(n_dma_sems)]
            self.dsem[q] = [lst, 0]
        self.ccsem = nc.semaphore("ccsem").__enter__()
        self.cccnt = 0
        self.seen = {e: {} for e in self.eng}
        self.lastw = {}
        self.readers = {}
        self.ninstr = 0
        self.excl = {"ns_accb"}

    @staticmethod
    def _k(x):
        return x if isinstance(x, str) else x.name

    def _wait(self, e, tok):
        if tok is None:
            return
        sem, val, src = tok
        if src == "pe" and e == "pe":
            return
        k = id(sem)
        if self.seen[e].get(k, 0) >= val:
            return
        self.eng[e].wait_ge(sem, val)
        self.seen[e][k] = val

    def _deps(self, e, reads, writes):
        for k in reads:
            self._wait(e, self.lastw.get(k))
        for k in writes:
            self._wait(e, self.lastw.get(k))
            for t in self.readers.get(k, ()):
                self._wait(e, t)

    def _record(self, tok, reads, writes):
        for k in reads:
            self.readers.setdefault(k, []).append(tok)
        for k in writes:
            self.lastw[k] = tok
            self.readers[k] = []
        self.ninstr += 1

    def op(self, e, fn, reads=(), writes=()):
        reads = [self._k(x) for x in reads]
        writes = [self._k(x) for x in writes]
        for k in reads:
            if (k.startswith("psb") or k in self.excl) and k not in writes:
                writes.append(k)
        self._deps(e, reads, writes)
        ins = fn(self.eng[e])
        self.cnt[e] += 1
        ins.then_inc(self.sem[e], 1)
        tok = (self.sem[e], self.cnt[e], e)
        self._record(tok, reads, writes)
        return tok

    def dma(self, q, out, in_, reads=(), writes=(), **kw):
        reads = [self._k(x) for x in reads]
        writes = [self._k(x) for x in writes]
        lst, idx = self.dsem[q]
        ent = lst[idx % len(lst)]
        self.dsem[q][1] += 1
        sem, tgt = ent
        if tgt > 0:
            self._wait(q, (sem, tgt, "dma"))
        self._deps(q, reads, writes)
        self.eng[q].dma_start(out=out, in_=in_, **kw).then_inc(sem, 16)
        ent[1] = tgt + 16
        tok = (sem, tgt + 16, "dma")
        self._record(tok, reads, writes)
        return tok

    def allgather(self, src, dst, groups, reads=(), writes=()):
        reads = [self._k(x) for x in reads]
        writes = [self._k(x) for x in writes]
        self._deps("pool", reads, writes)
        self.nc.gpsimd.collective_compute("AllGather", ALU.bypass, replica_groups=groups,
                                          ins=[src.ap().opt()], outs=[dst.ap().opt()]).then_inc(self.ccsem)
        self.cccnt += 1
        tok = (self.ccsem, self.cccnt, "cc")
        self._record(tok, reads, writes)
        return tok

    def finish(self, keys):
        for k in keys:
            self._wait("sp", self.lastw.get(k))


class LazyInputs(dict):
    def __init__(self, kb):
        super().__init__()
        self.kb = kb

    def __missing__(self, name):
        shp = INPUT_SHAPES(self.kb.T)[name]
        t = self.kb.nc.dram_tensor(name, list(shp), F32, kind="ExternalInput")
        self[name] = t
        return t


class KB:
    def __init__(self, T):
        self.T = T
        self.TC = T // 4
        self.NT = self.TC // 128
        self.nc = bass.Bass("TRN2", target_bir_lowering=False)
        self.fw = FW(self.nc)
        self.inputs = LazyInputs(self)
        self.outputs = {}
        self._uid = 0
        self._psn = 0
        self.ps = [self.nc.psum_tensor(f"psb{i}", [128, 512], F32).__enter__() for i in range(8)]
        self.scopes = []

    def din(self, name, shape, dtype=F32):
        t = self.nc.dram_tensor(name, list(shape), dtype, kind="ExternalInput")
        self.inputs[name] = t
        return t

    def dout(self, name, shape, dtype=F32):
        t = self.nc.dram_tensor(name, list(shape), dtype, kind="ExternalOutput")
        self.outputs[name] = t
        return t

    def dscr(self, name, shape, dtype=F32):
        return self.nc.dram_tensor(name, list(shape), dtype)

    def sb(self, name, shape, dtype=F32):
        self._uid += 1
        g = self.nc.sbuf_tensor(f"{name}_u{self._uid}", list(shape), dtype)
        t = g.__enter__()
        if self.scopes:
            self.scopes[-1].append(g)
        return t

    def push(self):
        self.scopes.append([])

    def pop(self):
        for g in reversed(self.scopes.pop()):
            g.__exit__(None, None, None)

    def psum(self):
        p = self.ps[self._psn % 8]
        self._psn += 1
        return p

    def psum_from(self, lo, n):
        p = self.ps[lo + self._psn % n]
        self._psn += 1
        return p

    def psum4(self):
        p = self.ps[4 + self._psn % 4]
        self._psn += 1
        return p

    def op(self, e, fn, reads=(), writes=()):
        return self.fw.op(e, fn, reads, writes)

    def dma(self, q, out, in_, reads=(), writes=(), **kw):
        return self.fw.dma(q, out, in_, reads, writes, **kw)

    def mm(self, out, lhsT, rhs, start, stop, reads, writes):
        return self.fw.op("pe", lambda e: e.matmul(out, lhsT, rhs, start=start, stop=stop), reads, writes)

    def tr(self, out, in_, ident, reads, writes):
        return self.fw.op("pe", lambda e: e.transpose(out, in_, ident), reads, writes)

    def act(self, out, in_, func, reads, writes, bias=None, scale=None, accum_out=None):
        kw = {}
        if bias is not None:
            kw["bias"] = bias
        if scale is not None:
            kw["scale"] = scale
        if accum_out is not None:
            kw["accum_out"] = accum_out
        return self.fw.op("act", lambda e: e.activation(out=out, in_=in_, func=func, **kw), reads, writes)

    def tt(self, out, in0, in1, op, reads, writes, eng="dve"):
        return self.fw.op(eng, lambda e: e.tensor_tensor(out=out, in0=in0, in1=in1, op=op), reads, writes)

    def ts(self, out, in0, s1, s2, op0, op1, reads, writes, eng="dve"):
        if s2 is None:
            s2, op1 = 0.0, ALU.add
        return self.fw.op(eng, lambda e: e.tensor_scalar(out=out, in0=in0, scalar1=s1, scalar2=s2, op0=op0, op1=op1), reads, writes)

    def stt(self, out, in0, scalar, in1, op0, op1, reads, writes, eng="dve"):
        return self.fw.op(eng, lambda e: e.scalar_tensor_tensor(out=out, in0=in0, scalar=scalar, in1=in1, op0=op0, op1=op1), reads, writes)

    def copy(self, out, in_, reads, writes, eng="dve"):
        if eng == "act":
            return self.fw.op("act", lambda e: e.copy(out=out, in_=in_), reads, writes)
        return self.fw.op(eng, lambda e: e.tensor_copy(out=out, in_=in_), reads, writes)

    def memset(self, ap, val, writes, eng="dve"):
        return self.fw.op(eng, lambda e: e.memset(ap, val), (), writes)


def bcast_rows(dram_ap_1d_row, nparts):
    return dram_ap_1d_row.partition_broadcast(nparts)


def barrier(K):
    fw = K.fw
    toks = [(fw.sem[e], fw.cnt[e], e) for e in fw.eng if fw.cnt[e] > 0]
    for q in fw.dsem:
        for sem, tgt in fw.dsem[q][0]:
            if tgt > 0:
                toks.append((sem, tgt, "dma"))
    if fw.cccnt:
        toks.append((fw.ccsem, fw.cccnt, "cc"))
    for e in fw.eng:
        for t in toks:
            if t[2] == e:
                continue
            fw._wait(e, t)


def rmsnorm_T(K, x_rows, gbc, hT, col0, ntiles, C, tag):
    for tt in range(ntiles):
        b = tt % 2
        xt, junk, ss, hb = C["xt"][b], C["junk"][b], C["ss"][b], C["hb"][b]
        K.dma("sp", xt[:], x_rows[tt * 128:(tt + 1) * 128, :], writes=[xt])
        K.memset(ss[:], 0.0, [ss])
        K.act(junk[:], xt[:], AF.Square, [xt], [junk, ss], accum_out=ss[:, 0:1])
        K.ts(ss[:, 1:2], ss[:, 0:1], 1.0 / D, EPS, ALU.mult, ALU.add, [ss], [ss])
        K.act(ss[:, 1:2], ss[:, 1:2], AF.Sqrt, [ss], [ss])
        K.op("dve", lambda e: e.reciprocal(out=ss[:, 1:2], in_=ss[:, 1:2]), [ss], [ss])
        K.stt(hb[:], xt[:], ss[:, 1:2], gbc[:], ALU.mult, ALU.mult, [xt, ss, gbc], [hb])
        for grp in range(2):
            ps = K.psum()
            psb = ps[:].bitcast(BF16)
            for i in range(8):
                kc = grp * 8 + i
                K.tr(psb[:, i * 128:(i + 1) * 128], hb[:, kc * 128:(kc + 1) * 128], C["identb"][:], [hb, C["identb"]], [ps])
            K.copy(hT[:, grp * 8:(grp + 1) * 8, col0 + tt * 128: col0 + (tt + 1) * 128],
                   psb.rearrange("p (a b) -> p a b", a=8), [ps], [hT], eng="act" if grp == 0 else "dve")


def load_w(K, wt, W, c0, ncols, kchunks=16, key=None):
    src = W.rearrange("(kc p) n -> p kc n", p=128)[:, :, c0:c0 + ncols]
    K.dma("pool", wt[:, 0:kchunks, 0:ncols], src, writes=[key or wt])


def fm_colsum_norm(K, out_bf, outkey, src_f32, tagkey, n, nparts, ones_f, gain_col, inv_n, C):
    sq = C["fm_sq"]
    K.tt(sq[0:nparts, 0:n], src_f32, src_f32, ALU.mult, [tagkey], [sq])
    ps = K.psum()
    K.mm(ps[0:nparts, 0:n], ones_f[0:nparts, 0:nparts], sq[0:nparts, 0:n], True, True, [sq, ones_f], [ps])
    rs = C["fm_rs"]
    K.ts(rs[0:nparts, 0:n], ps[0:nparts, 0:n], inv_n, EPS, ALU.mult, ALU.add, [ps], [rs])
    K.act(rs[0:nparts, 0:n], rs[0:nparts, 0:n], AF.Sqrt, [rs], [rs])
    K.op("dve", lambda e: e.reciprocal(out=rs[0:nparts, 0:n], in_=rs[0:nparts, 0:n]), [rs], [rs])
    K.stt(out_bf, src_f32, gain_col, rs[0:nparts, 0:n], ALU.mult, ALU.mult, [tagkey, rs], [outkey])


PV_CONV = 0
PV_MEMQG = 12
PV_MEMKG = 13
PV_MU = 14
PV_W0, PV_A0, PV_KK, PV_KA, PV_RK, PV_LNW, PV_LNB, PV_V0, PV_C1 = 22, 23, 24, 25, 26, 27, 28, 29, 30
PV_NSAG = 31
PV_CB1 = 35
NV = 40


def local_mixers(K, l, C, S):
    TC, W = K.TC, HALO + K.TC
    hTx, pv = C["hTx"], C["pv"][l]
    w_in = K.inputs["w_in"].ap()[l]
    tokblocks = [(0, HALO)] + [(HALO + i * 512, min(512, TC - i * 512)) for i in range((TC + 511) // 512)]
    locblocks = tokblocks[1:]
    K.push()
    wts = [K.sb(f"lm_wt{i}", [128, 16, 512], BF16) for i in range(2)]
    wi = [0]

    def nextw():
        w = wts[wi[0] % 2]
        wi[0] += 1
        return w

    def proj(wt, c_lo, ncol, dst, blocks, evac=None):
        for (c0, n) in blocks:
            ps = K.psum()
            for kc in range(16):
                K.mm(ps[0:ncol, 0:n], wt[:, kc, c_lo:c_lo + ncol], hTx[:, kc, c0:c0 + n], kc == 0, kc == 15, [wt, hTx], [ps])
            K.copy(dst[0:ncol, c0:c0 + n], ps[0:ncol, 0:n], [ps], [dst], eng="act")

    K.push()
    bt, ct, xt_ = (K.sb(n, [128, W]) for n in ("cv_b", "cv_c", "cv_x"))
    z, acc = K.sb("cv_z", [128, W]), K.sb("cv_acc", [128, TC])
    for j in range(4):
        wt = nextw()
        for i in range(3):
            src = w_in.rearrange("(kc p) n -> p kc n", p=128)[:, :, O_CONV + i * 512 + j * 128: O_CONV + i * 512 + (j + 1) * 128]
            K.dma("pool", wt[:, :, i * 128:(i + 1) * 128], src, writes=[wt])
        proj(wt, 0, 128, bt, locblocks)
        proj(wt, 128, 128, ct, tokblocks)
        proj(wt, 256, 128, xt_, tokblocks)
        K.tt(z[:], ct[:], xt_[:], ALU.mult, [ct, xt_], [z])
        cw = lambda i: pv[:, PV_CONV + j * 3 + i: PV_CONV + j * 3 + i + 1]
        K.ts(acc[:], z[:, HALO:W], cw(2), None, ALU.mult, None, [z, pv], [acc])
        K.stt(acc[:], z[:, HALO - 1:W - 1], cw(1), acc[:], ALU.mult, ALU.add, [z, pv, acc], [acc])
        K.stt(acc[:], z[:, HALO - 2:W - 2], cw(0), acc[:], ALU.mult, ALU.add, [z, pv, acc], [acc])
        K.tt(S["y_convT"][:, j, :], bt[:, HALO:W], acc[:], ALU.mult, [bt, acc], [S["y_convT"]])
    barrier(K)
    K.pop()
    K.push()
    acc = K.sb("pl_acc", [128, TC])
    wt = nextw()
    load_w(K, wt, w_in, O_POOL, 512)
    pa, pb, pu = K.sb("pl_a", [128, W]), K.sb("pl_b", [128, W]), K.sb("pl_u", [128, W])
    invc = K.sb("pl_invc", [128, TC])
    for g in range(4):
        proj(wt, g * 128, 128, pu, tokblocks)
        K.dma("sp", invc[:], K.inputs["invcnt"].ap()[g:g + 1, :].partition_broadcast(128), writes=[invc])
        cur, oth = pu, pa
        for si, sh in enumerate([1, 2, 4, 8][:g + 1]):
            K.tt(oth[:, sh:W], cur[:, sh:W], cur[:, 0:W - sh], ALU.add, [cur], [oth])
            cur, oth = oth, (pb if oth is pa else pa)
        K.tt(acc[:], cur[:, HALO:W], invc[:], ALU.mult, [cur, invc], [acc])
        K.tt(S["pooledT"][:, g, :], acc[:], pu[:, HALO:W], ALU.subtract, [acc, pu], [S["pooledT"]])
    barrier(K)
    K.pop()
    memT = K.sb("mm_memT", [128, 16, MEM_LEN], BF16)
    gm = K.sb("mm_g", [128, D])
    K.dma("sp", gm[:], K.inputs["norm_mem"].ap()[l:l + 1, :].partition_broadcast(128), writes=[gm])
    K.push()
    rn_alloc(K, C)
    rmsnorm_T(K, K.inputs["mem"].ap(), gm, memT, 0, 2, C, "mem")
    barrier(K)
    K.pop()
    wkv = K.inputs["mem_wkv"].ap()[l]
    kT = K.sb("mm_kT", [128, 4, MEM_LEN], BF16)
    vsb = K.sb("mm_v", [128, 2, 512], BF16)
    kf = K.sb("mm_kf", [128, 512])
    wt = nextw()
    load_w(K, wt, wkv, 0, 512)
    for h in range(4):
        ps = K.psum()
        for kc in range(16):
            K.mm(ps[:, 0:MEM_LEN], wt[:, kc, h * 128:(h + 1) * 128], memT[:, kc, :], kc == 0, kc == 15, [wt, memT], [ps])
        K.copy(kf[:, 0:MEM_LEN], ps[:, 0:MEM_LEN], [ps], [kf], eng="act")
        fm_colsum_norm(K, kT[:, h, :], kT, kf[:, 0:MEM_LEN], kf, MEM_LEN, 128, C["ones_f"], pv[:, PV_MEMKG:PV_MEMKG + 1], 1.0 / 128, C)
    wt = nextw()
    load_w(K, wt, wkv, 512, 512)
    for mt in range(2):
        ps = K.psum()
        for kc in range(16):
            K.mm(ps[:, :], memT[:, kc, mt * 128:(mt + 1) * 128], wt[:, kc, :], kc == 0, kc == 15, [wt, memT], [ps])
        K.copy(vsb[:, mt, :], ps[:, :], [ps], [vsb], eng="act")
    wt = nextw()
    load_w(K, wt, w_in, O_MEM, 512)
    qf, qT = K.sb("mm_qf", [128, W]), K.sb("mm_qT", [128, 512], BF16)
    es = [K.sb(f"mm_e{i}", [128, 512], BF16) for i in range(2)]
    rden = K.sb("mm_rden", [128, 512])
    for h in range(4):
        proj(wt, h * 128, 128, qf, locblocks)
        for (c0, n) in locblocks:
            fm_colsum_norm(K, qT[:, 0:n], qT, qf[:, c0:c0 + n], qf, n, 128, C["ones_f"], pv[:, PV_MEMQG:PV_MEMQG + 1], 1.0 / 128, C)
            for mt in range(2):
                ps = K.psum()
                K.mm(ps[:, 0:n], kT[:, h, mt * 128:(mt + 1) * 128], qT[:, 0:n], True, True, [kT, qT], [ps])
                K.act(es[mt][:, 0:n], ps[:, 0:n], AF.Exp, [ps], [es[mt]], scale=float(128 ** -0.5))
            po, pd = K.psum(), K.psum()
            for mt in range(2):
                K.mm(po[:, 0:n], vsb[:, mt, h * 128:(h + 1) * 128], es[mt][:, 0:n], mt == 0, mt == 1, [vsb, es[mt]], [po])
            for mt in range(2):
                K.mm(pd[:, 0:n], C["ones_b"][:, :], es[mt][:, 0:n], mt == 0, mt == 1, [C["ones_b"], es[mt]], [pd])
            K.op("dve", lambda e: e.reciprocal(out=rden[:, 0:n], in_=pd[:, 0:n]), [pd], [rden])
            K.tt(S["y_memT"][:, h, c0 - HALO:c0 - HALO + n], po[:, 0:n], rden[:, 0:n], ALU.mult, [po, rden], [S["y_memT"]])
    barrier(K)
    K.pop()


GROUPS = [[0, 1, 2, 3], [4, 5, 6, 7]]


def alloc_gather(K, C):
    TC = K.TC
    C["hT_src"] = [K.dscr(f"hT_src{q}", [256, TC], BF16) for q in range(8)]
    C["hT_all"] = [K.dscr(f"hT_all{q}", [4 * 256, TC], BF16) for q in range(8)]


def alloc_scratch(K):
    T, TC = K.T, K.TC
    R = {n: K.dscr(n, [128, T]) for n in ("fm_r", "fm_w", "fm_nkk", "fm_kp", "fm_g", "fm_bonus", "fm_vfirst")}
    R["tm_kka"] = K.dscr("tm_kka", [T, 128])
    R["tm_v"] = K.dscr("tm_v", [T, 128])
    for nm in ("yB", "yN"):
        R[nm] = {"key": nm, "src": [K.dscr(f"{nm}_src{j}", [128, TC]) for j in range(4)],
                 "all": [K.dscr(f"{nm}_all{j}", [512, TC]) for j in range(4)]}
    return R


def setup_common(K):
    C = {}
    C["identf"] = K.sb("identf", [128, 128])
    C["identb"] = K.sb("identb", [128, 128], BF16)
    C["ones_f"] = K.sb("ones_f", [128, 128])
    C["ones_b"] = K.sb("ones_b", [128, 128], BF16)
    K.dma("sp", C["identf"][:], K.inputs["ident"].ap(), writes=[C["identf"]])
    K.copy(C["identb"][:], C["identf"][:], [C["identf"]], [C["identb"]])
    K.memset(C["ones_f"][:], 1.0, [C["ones_f"]])
    K.memset(C["ones_b"][:], 1.0, [C["ones_b"]])
    C["fm_sq"] = K.sb("fm_sq", [128, 512])
    C["fm_rs"] = K.sb("fm_rs", [128, 512])
    C["oh"] = K.sb("oh", [128, 8])
    K.dma("sp", C["oh"][:], K.inputs["onehot"].ap(), writes=[C["oh"]])
    C["pv"] = []
    for l in range(DEPTH):
        t = K.sb(f"pv{l}", [128, NV])
        K.dma("sp", t[:], K.inputs["pvec"].ap()[l], writes=[t])
        K.ts(t[:, PV_C1:PV_C1 + 1], t[:, PV_KA:PV_KA + 1], -1.0, 1.0, ALU.mult, ALU.add, [t], [t])
        C["pv"].append(t)
    return C


def rn_alloc(K, C):
    C["xt"] = [K.sb(f"rn_xt{i}", [128, D]) for i in range(2)]
    C["junk"] = [K.sb(f"rn_junk{i}", [128, D], BF16) for i in range(2)]
    C["ss"] = [K.sb(f"rn_ss{i}", [128, 2]) for i in range(2)]
    C["hb"] = [K.sb(f"rn_hb{i}", [128, D], BF16) for i in range(2)]
    C["gbc"] = K.sb("gbc", [128, D])


def phase_norm_gather(K, l, C, xres):
    TC = K.TC
    hTx = C["hTx"]
    K.push()
    rn_alloc(K, C)
    K.dma("sp", C["gbc"][:], K.inputs["norm_mix"].ap()[l:l + 1, :].partition_broadcast(128), writes=[C["gbc"]])
    rmsnorm_T(K, xres.ap(), C["gbc"], hTx, HALO, K.NT, C, "mix")
    barrier(K)
    K.pop()
    for q in range(NHQ):
        K.dma("sp", C["hT_src"][q].ap().rearrange("(kc p) t -> p kc t", p=128), hTx[:, 2 * q:2 * q + 2, HALO:HALO + TC], reads=[hTx], writes=["hT_src"])
    for q in range(NHQ):
        K.fw.allgather(C["hT_src"][q], C["hT_all"][q], GROUPS, reads=["hT_src"], writes=["hT_all"])
    cand = C["halo_cand"]
    C["_hall_dst"] = cand
    for r in range(4):
        hall_read(K, cand[:, r], C, r, TC - HALO, HALO)
    oh = C["oh"]
    K.ts(hTx[:, :, 0:HALO], cand[:, 0], oh[:, 4:5], None, ALU.mult, None, [cand, oh], [hTx])
    for r in range(1, 4):
        K.stt(hTx[:, :, 0:HALO], cand[:, r], oh[:, 4 + r:5 + r], hTx[:, :, 0:HALO], ALU.mult, ALU.add, [cand, oh, hTx], [hTx])


def rwkv_cols(hp):
    cols = []
    pad = lambda a, n: list(a) + [-1] * (n - len(a))
    cols += list(range(128 * hp, 128 * hp + 128))
    cols += list(range(512 + 128 * hp, 512 + 128 * hp + 128))
    cols += list(range(1024 + 128 * hp, 1024 + 128 * hp + 128))
    cols += pad(range(1536, 1632), 128)
    cols += pad(range(1632, 1728), 128)
    cols += list(range(1728, 1984))
    cols += pad(range(1984, 2048), 128)
    return np.array(cols)


def nsa_cols(hp):
    hk = hp // 2
    mine = [2 * hp, 2 * hp + 1]
    oth = [h for h in range(4 * hk, 4 * hk + 4) if h not in mine]
    heads = mine + oth
    grp = lambda i: list(range(512 + 128 * i + 64 * hk, 512 + 128 * i + 64 * hk + 64))
    cols = []
    for h in heads:
        cols += list(range(64 * h, 64 * h + 64))
    cols += grp(0) + grp(0) + grp(2) + grp(2) + grp(4) + grp(4)
    cols += grp(1)
    cols += grp(3) + grp(5)
    for h in heads:
        cols += [512 + 768 + 3 * h + i for i in range(3)]
    return np.array(cols)


NWN = 844


def host_prep(inp, T):
    TC = T // 4
    f = lambda a: np.ascontiguousarray(np.asarray(a, dtype=np.float32))
    sh = {k: f(inp[k]) for k in ("w_in", "norm_mix", "norm_ffn", "norm_mem", "mem_wkv", "pool_w", "pool_scale", "w_branch",
                                 "w_out", "ffn_w1", "ffn_w3", "ffn_w2", "moe_router", "moe_w1", "moe_w3", "moe_w2",
                                 "nsa_cmp_w1", "nsa_cmp_w2", "nsa_cmp_pos")}
    sh["ident"] = np.eye(128, dtype=np.float32)
    sh["blk64"] = np.kron(np.eye(2), np.ones((64, 64))).astype(np.float32)
    sh["cmp_posT"] = np.ascontiguousarray(np.transpose(sh["nsa_cmp_pos"], (0, 1, 3, 2)))
    sh["nsa_qk_gain"] = f(inp["nsa_qk_gain"])
    NS, NTT = T // 64, T // 128
    cc = np.arange(256)[:, None] * 16
    ss_ = np.arange(64)[None, :] * 64
    sh["ovl"] = (np.clip(np.minimum(cc + 32, ss_ + 64) - np.maximum(cc, ss_), 0, None) / 32.0).astype(np.float32)
    tt_ = np.arange(T)
    sh["maskc"] = ((np.arange(256)[:, None] * 16 + 31) <= tt_[None, :]).astype(np.float32)
    cur = (tt_ // 64)[:, None]
    sid = np.arange(64)[None, :]
    valid = sid <= cur
    f0, f1, f2 = (sid == 0), (sid == cur), (sid == cur - 1)
    forced = f0 | f1 | f2
    sh["tk_keep"] = (valid & ~forced).astype(np.float32)
    cadd = np.where(valid, 0.0, -1e9)
    cadd = np.where(f2, 1e9, cadd)
    cadd = np.where(f1, 2e9, cadd)
    cadd = np.where(f0, 3e9, cadd)
    sh["tk_cadd"] = cadd.astype(np.float32)
    e2 = np.zeros((64, 32, 128), np.float32)
    for jt in range(32):
        for j in range(128):
            e2[2 * jt + j // 64, jt, j] = 1.0
    sh["e2"] = e2
    dm = np.zeros((5, 128, 512), np.float32)
    jj = np.arange(128)[:, None]
    t5 = np.arange(512)[None, :]
    for dd in range(4):
        dm[dd] = (t5 >= dd * 128 + jj)
    dm[4] = (jj > t5)
    sh["dmask"] = dm
    x = f(inp["x"])
    mem = f(inp["mem"])
    w_in = sh["w_in"]
    wfull = [np.concatenate([w_in[0][:, :1984], np.zeros((D, 64), np.float32)], 1),
             np.concatenate([w_in[1][:, :1984], f(inp["vres_in"])[0]], 1)]
    mufull = [np.concatenate([f(inp["rwkv_mu"])[0], np.zeros(64, np.float32)]),
              np.concatenate([f(inp["rwkv_mu"])[1], f(inp["vres_mu"])[0]])]
    maps = []
    for c in range(NCORES):
        b, j = c // 4, c % 4
        hp = j
        m = dict(sh)
        m["x"] = np.ascontiguousarray(x[b, j * TC:(j + 1) * TC])
        m["mem"] = np.ascontiguousarray(mem[b])
        oh = np.zeros((128, 8), np.float32)
        oh[:, j] = 1.0
        if j > 0:
            oh[:, 4 + j - 1] = 1.0
        m["onehot"] = oh
        tg = np.arange(j * TC, (j + 1) * TC, dtype=np.float32) + 1.0
        m["invcnt"] = np.stack([1.0 / np.minimum(tg, w) for w in (2, 4, 8, 16)]).astype(np.float32)
        rc = rwkv_cols(hp)
        ncl = nsa_cols(hp)
        wr = np.zeros((DEPTH, D, 1024), np.float32)
        wn = np.zeros((DEPTH, D, NWN), np.float32)
        pv = np.zeros((DEPTH, 128, NV), np.float32)
        lora = np.zeros((DEPTH, 128, 5, 128), np.float32)
        ch = slice(128 * hp, 128 * hp + 128)
        for l in range(DEPTH):
            ok = rc >= 0
            wr[l][:, ok] = wfull[l][:, rc[ok]]
            wn[l] = w_in[l][:, O_NSA + ncl]
            cw = f(inp["conv_w"])[l]
            for jj in range(4):
                for i in range(3):
                    pv[l, :, PV_CONV + jj * 3 + i] = cw[i, jj * 128:(jj + 1) * 128]
            pv[l, :, PV_MEMQG] = f(inp["mem_qk_gain"])[l, 0]
            pv[l, :, PV_MEMKG] = f(inp["mem_qk_gain"])[l, 1]
            mu = np.zeros(1024, np.float32)
            mu[ok] = mufull[l][rc[ok]]
            pv[l, :, PV_MU:PV_MU + 8] = mu.reshape(8, 128).T
            for idx, nm in ((PV_W0, "rwkv_w0"), (PV_A0, "rwkv_a0"), (PV_KK, "rwkv_kk"), (PV_KA, "rwkv_ka"), (PV_RK, "rwkv_rk"),
                            (PV_LNW, "rwkv_ln_w"), (PV_LNB, "rwkv_ln_b")):
                pv[l, :, idx] = f(inp[nm])[l, ch]
            if l == 1:
                pv[l, :, PV_V0] = f(inp["vres_v0"])[0, ch]
                lora[l, 0:64, 4] = f(inp["vres_up"])[0][:, ch]
            g = f(inp["nsa_qk_gain"])[l]
            for i in range(4):
                pv[l, :, PV_NSAG + i] = np.concatenate([g[i], g[i]])
            pv[l, :, PV_CB1:PV_CB1 + 2] = f(inp["nsa_cmp_b1"])[l].T
            lora[l, 0:96, 0] = f(inp["rwkv_w2"])[l][:, ch]
            lora[l, 0:96, 1] = f(inp["rwkv_a2"])[l][:, ch]
            lora[l, :, 2] = f(inp["rwkv_g2"])[l][0:128, ch]
            lora[l, :, 3] = f(inp["rwkv_g2"])[l][128:256, ch]
        m["wr"], m["wn"], m["pvec"], m["lora"] = wr, wn, pv, lora
        maps.append(m)
    return maps


NHQ = 8


def hall_read(K, dst3, C, r, col0, n):
    for q in range(NHQ):
        src = C["hT_all"][q].ap()[r * 256:(r + 1) * 256, col0:col0 + n].rearrange("(kc p) t -> p kc t", p=128)
        K.dma("sp", dst3[:, 2 * q:2 * q + 2, :], src, reads=["hT_all"], writes=[C["_hall_dst"]])


def ysrc_write(K, Y, rows, t0, n, src_tile, src_ap):
    TC = K.TC
    done = 0
    while done < n:
        j, off = (t0 + done) // TC, (t0 + done) % TC
        m = min(n - done, TC - off)
        K.dma("sp", Y["src"][j].ap()[rows, off:off + m], src_ap[:, done:done + m], reads=[src_tile], writes=[Y["key"] + "_src"])
        done += m


def ygather(K, Y):
    for j in range(4):
        K.fw.allgather(Y["src"][j], Y["all"][j], GROUPS, reads=[Y["key"] + "_src"], writes=[Y["key"] + "_all"])


def load_hall_block(K, hb, C, t0, TC, blk=512):
    sub = min(blk, TC)
    for si in range(blk // sub):
        tok = t0 + si * sub
        r_, off = tok // TC, tok % TC
        C["_hall_dst"] = hb
        hall_read(K, hb[:, :, si * sub:(si + 1) * sub], C, r_, off, sub)


CS = 16
GN_EPS = 64e-5


def rwkv_phase(K, l, C, R):
    T = K.T
    TC = K.TC
    NB = T // 512
    pv = C["pv"][l]
    K.push()
    wr = K.sb("rw_w", [128, 16, 1024], BF16)
    for i in range(2):
        K.dma("pool", wr[:, :, i * 512:(i + 1) * 512],
              K.inputs["wr"].ap()[l].rearrange("(kc p) n -> p kc n", p=128)[:, :, i * 512:(i + 1) * 512], writes=[wr])
    lora = K.sb("rw_lora", [128, 5, 128])
    K.dma("sp", lora[:], K.inputs["lora"].ap()[l], writes=[lora])
    blk = K.sb("rw_blk", [128, 128])
    K.dma("sp", blk[:], K.inputs["blk64"].ap(), writes=[blk])
    hTb = [K.sb(f"rw_hT{i}", [128, 16, 512], BF16) for i in range(2)]
    nct = 8 if l == 1 else 7
    ub = [K.sb(f"rw_ub{i}", [128, 513]) for i in range(nct)]
    uf = [K.sb(f"rw_uf{i}", [128, 512]) for i in range(nct)]
    tmp = [K.sb(f"rw_t{i}", [128, 512]) for i in range(8)]
    tmo = [K.sb(f"rw_tmo{i}", [128, 4, 128]) for i in range(2)]
    for ct in range(nct):
        K.memset(ub[ct][:, 0:1], 0.0, [ub[ct]])
    sc = lambda i: pv[:, i:i + 1]
    for tb in range(NB):
        t0 = tb * 512
        hb = hTb[tb % 2]
        load_hall_block(K, hb, C, t0, TC)
        for ct in range(nct):
            ps = K.psum()
            for kc in range(16):
                K.mm(ps[:, :], wr[:, kc, ct * 128:(ct + 1) * 128], hb[:, kc, :], kc == 0, kc == 15, [wr, hb], [ps])
            K.copy(ub[ct][:, 1:513], ps[:, :], [ps], [ub[ct]], eng="act")
            d = tmp[0]
            K.tt(d[:], ub[ct][:, 0:512], ub[ct][:, 1:513], ALU.subtract, [ub[ct]], [d])
            K.stt(uf[ct][:], d[:], sc(PV_MU + ct), ub[ct][:, 1:513], ALU.mult, ALU.add, [d, pv, ub[ct]], [uf[ct]])
            K.copy(ub[ct][:, 0:1], ub[ct][:, 512:513], [ub[ct]], [ub[ct]])
        r, k, v = uf[0], uf[1], uf[2]
        K.act(uf[3][0:96, :], uf[3][0:96, :], AF.Tanh, [uf[3]], [uf[3]])
        ps = K.psum()
        K.mm(ps[:, :], lora[0:96, 0, :], uf[3][0:96, :], True, True, [lora, uf[3]], [ps])
        dec = tmp[1]
        K.act(dec[:], ps[:, :], AF.Sigmoid, [ps, pv], [dec], bias=sc(PV_W0))
        K.act(dec[:], dec[:], AF.Exp, [dec], [dec], scale=-float(np.exp(-0.5)))
        K.dma("sp", R["fm_w"].ap()[:, t0:t0 + 512], dec[:], reads=[dec], writes=["fm_w"])
        ps = K.psum()
        K.mm(ps[:, :], lora[0:96, 1, :], uf[4][0:96, :], True, True, [lora, uf[4]], [ps])
        a = tmp[2]
        K.act(a[:], ps[:, :], AF.Sigmoid, [ps, pv], [a], bias=sc(PV_A0))
        K.act(uf[5][:], uf[5][:], AF.Sigmoid, [uf[5]], [uf[5]])
        K.act(uf[6][:], uf[6][:], AF.Sigmoid, [uf[6]], [uf[6]])
        ps = K.psum()
        K.mm(ps[:, :], lora[:, 2, :], uf[5][:], True, False, [lora, uf[5]], [ps])
        K.mm(ps[:, :], lora[:, 3, :], uf[6][:], False, True, [lora, uf[6]], [ps])
        g = tmp[3]
        K.copy(g[:], ps[:, :], [ps], [g], eng="act")
        K.dma("sp", R["fm_g"].ap()[:, t0:t0 + 512], g[:], reads=[g], writes=["fm_g"])
        if l == 0:
            K.dma("sp", R["fm_vfirst"].ap()[:, t0:t0 + 512], v[:], reads=[v], writes=["fm_vfirst"])
        else:
            ps = K.psum()
            K.mm(ps[:, :], lora[0:64, 4, :], uf[7][0:64, :], True, True, [lora, uf[7]], [ps])
            vr = tmp[4]
            K.act(vr[:], ps[:, :], AF.Sigmoid, [ps, pv], [vr], bias=sc(PV_V0))
            vf = tmp[5]
            K.dma("sp", vf[:], R["fm_vfirst"].ap()[:, t0:t0 + 512], reads=["fm_vfirst"], writes=[vf])
            K.tt(vf[:], vf[:], v[:], ALU.subtract, [vf, v], [vf])
            K.tt(vf[:], vf[:], vr[:], ALU.mult, [vf, vr], [vf])
            K.tt(v[:], v[:], vf[:], ALU.add, [v, vf], [v])
        kk = tmp[4]
        K.ts(kk[:], k[:], sc(PV_KK), None, ALU.mult, None, [k, pv], [kk])
        sq = tmp[5]
        K.tt(sq[:], kk[:], kk[:], ALU.mult, [kk], [sq])
        ps = K.psum()
        K.mm(ps[:, :], blk[:, :], sq[:], True, True, [blk, sq], [ps])
        rn = tmp[5]
        K.ts(rn[:], ps[:, :], 1e-24, None, ALU.max, None, [ps], [rn])
        K.act(rn[:], rn[:], AF.Sqrt, [rn], [rn])
        K.op("dve", lambda e: e.reciprocal(out=rn[:], in_=rn[:]), [rn], [rn])
        K.tt(kk[:], kk[:], rn[:], ALU.mult, [kk, rn], [kk])
        nkk = tmp[5]
        K.ts(nkk[:], kk[:], -1.0, None, ALU.mult, None, [kk], [nkk])
        K.dma("sp", R["fm_nkk"].ap()[:, t0:t0 + 512], nkk[:], reads=[nkk], writes=["fm_nkk"])
        t1 = tmp[6]
        K.ts(t1[:], a[:], sc(PV_KA), sc(PV_C1), ALU.mult, ALU.add, [a, pv], [t1])
        kp = tmp[7]
        K.tt(kp[:], k[:], t1[:], ALU.mult, [k, t1], [kp])
        K.dma("sp", R["fm_kp"].ap()[:, t0:t0 + 512], kp[:], reads=[kp], writes=["fm_kp"])
        kka = tmp[6]
        K.tt(kka[:], kk[:], a[:], ALU.mult, [kk, a], [kka])
        for which, (src, dst) in enumerate(((kka, "tm_kka"), (v, "tm_v"))):
            ps = K.psum()
            for i in range(4):
                K.tr(ps[:, i * 128:(i + 1) * 128], src[:, i * 128:(i + 1) * 128], C["identf"][:], [src, C["identf"]], [ps])
            K.copy(tmo[which][:], ps[:, :].rearrange("p (a b) -> p a b", a=4), [ps], [tmo[which]], eng="act")
            K.dma("sp", R[dst].ap()[t0:t0 + 512, :].rearrange("(a p) c -> p a c", p=128), tmo[which][:], reads=[tmo[which]], writes=[dst])
        t2 = tmp[0]
        K.stt(t2[:], r[:], sc(PV_RK), kp[:], ALU.mult, ALU.mult, [r, pv, kp], [t2])
        ps = K.psum()
        K.mm(ps[:, :], blk[:, :], t2[:], True, True, [blk, t2], [ps])
        bon = tmp[0]
        K.tt(bon[:], ps[:, :], v[:], ALU.mult, [ps, v], [bon])
        K.dma("sp", R["fm_bonus"].ap()[:, t0:t0 + 512], bon[:], reads=[bon], writes=["fm_bonus"])
        K.dma("sp", R["fm_r"].ap()[:, t0:t0 + 512], r[:], reads=[r], writes=["fm_r"])
    barrier(K)
    K.pop()

    K.push()
    NH = 2
    Sring = [[K.sb(f"sc_S{h}_{i}", [64, 64]) for i in range(4)] for h in range(NH)]
    kkaB = [[K.sb(f"sc_kkaB{h}_{i}", [64, CS, 64]) for i in range(2)] for h in range(NH)]
    vB = [[K.sb(f"sc_vB{h}_{i}", [64, CS, 64]) for i in range(2)] for h in range(NH)]
    Abuf = [[K.sb(f"sc_A{h}_{i}", [64, CS, 64]) for i in range(2)] for h in range(NH)]
    Dg = [K.sb(f"sc_Dg{h}", [64, CS, 64]) for h in range(NH)]
    fmb = {n: [[K.sb(f"sc_{n}{h}_{i}", [64, 512]) for i in range(2)] for h in range(NH)] for n in ("w", "nkk", "kp", "r", "g", "bonus")}
    yT = [K.sb(f"sc_yT{h}", [64, 512]) for h in range(NH)]
    pt = [K.sb(f"sc_pt{h}_{i}", [64, 512]) for h in range(NH) for i in range(3)]
    pvh = [K.sb(f"sc_pv{h}", [64, NV]) for h in range(NH)]
    ones64 = K.sb("sc_ones64", [64, 64])
    K.memset(ones64[:], 1.0 / 64, [ones64])
    for h in range(NH):
        K.dma("sp", pvh[h][:], K.inputs["pvec"].ap()[l][h * 64:(h + 1) * 64, :], writes=[pvh[h]])
        K.memset(Sring[h][0][:], 0.0, [Sring[h][0]])
    psS = [K.ps[0], K.ps[1]]
    psY = [K.ps[2], K.ps[3]]
    ident64 = C["identf"][0:64, 0:64]
    nchunk = 512 // CS
    pend_y = []
    for tb in range(NB):
        t0 = tb * 512
        for h in range(NH):
            for n in fmb:
                K.dma("sp", fmb[n][h][tb % 2][:], R["fm_" + n].ap()[h * 64:(h + 1) * 64, t0:t0 + 512], reads=["fm_" + n], writes=[fmb[n][h][tb % 2]])
        for ci in range(nchunk):
            c0 = ci * CS
            gi = tb * nchunk + ci
            for h in range(NH):
                kb, vb, A = kkaB[h][gi % 2], vB[h][gi % 2], Abuf[h][gi % 2]
                K.dma("sp", kb[:], R["tm_kka"].ap()[t0 + c0:t0 + c0 + CS, h * 64:(h + 1) * 64].partition_broadcast(64), reads=["tm_kka"], writes=[kb])
                K.dma("sp", vb[:], R["tm_v"].ap()[t0 + c0:t0 + c0 + CS, h * 64:(h + 1) * 64].partition_broadcast(64), reads=["tm_v"], writes=[vb])
                nk = fmb["nkk"][h][tb % 2]
                w = fmb["w"][h][tb % 2]
                K.tt(A[:], kb[:], nk[:, c0:c0 + CS].unsqueeze(2).to_broadcast([64, CS, 64]), ALU.mult, [kb, nk], [A], eng="pool")
                K.tt(Dg[h][:], ident64.unsqueeze(1).to_broadcast([64, CS, 64]), w[:, c0:c0 + CS].unsqueeze(2).to_broadcast([64, CS, 64]),
                     ALU.mult, [C["identf"], w], [Dg[h]], eng="pool")
                K.tt(A[:], A[:], Dg[h][:], ALU.add, [A, Dg[h]], [A], eng="pool")
            for s in range(CS):
                tg = gi * CS + s
                for h in range(NH):
                    A = Abuf[h][gi % 2]
                    Sp = Sring[h][tg % 4]
                    slot = tg % 8
                    pk = f"psS{h}_{slot}"
                    pso = psS[h][0:64, slot * 64:(slot + 1) * 64]
                    K.mm(pso, A[:, s, :], Sp[:], True, True, [A, Sp], [pk])
                for fn_ in pend_y:
                    fn_()
                pend_y.clear()
                for h in range(NH):
                    vb = vB[h][gi % 2]
                    Sn = Sring[h][(tg + 1) % 4]
                    slot = tg % 8
                    pk = f"psS{h}_{slot}"
                    pso = psS[h][0:64, slot * 64:(slot + 1) * 64]
                    kp = fmb["kp"][h][tb % 2]
                    K.stt(Sn[:], vb[:, s, :], kp[:, c0 + s:c0 + s + 1], pso, ALU.mult, ALU.add, [vb, kp, pk], [Sn])
                    rr = fmb["r"][h][tb % 2]

                    def ymm(h=h, Sn=Sn, rr=rr, col=c0 + s):
                        K.mm(psY[h][0:64, col:col + 1], Sn[:], rr[:, col:col + 1], True, True, [Sn, rr], [psY[h]])
                    pend_y.append(ymm)
        for fn_ in pend_y:
            fn_()
        pend_y.clear()
        for h in range(NH):
            y, p0, p1, p2 = yT[h], pt[h * 3], pt[h * 3 + 1], pt[h * 3 + 2]
            K.copy(y[:], psY[h][0:64, :], [psY[h]], [y], eng="act")
            ps = K.psum4()
            K.mm(ps[0:64, :], ones64[:], y[:], True, True, [ones64, y], [ps])
            K.tt(p0[:], y[:], ps[0:64, :], ALU.subtract, [y, ps], [p0])
            K.tt(p1[:], p0[:], p0[:], ALU.mult, [p0], [p1])
            ps = K.psum4()
            K.mm(ps[0:64, :], ones64[:], p1[:], True, True, [ones64, p1], [ps])
            K.ts(p1[:], ps[0:64, :], GN_EPS, None, ALU.add, None, [ps], [p1])
            K.act(p1[:], p1[:], AF.Sqrt, [p1], [p1])
            K.op("dve", lambda e: e.reciprocal(out=p1[:], in_=p1[:]), [p1], [p1])
            K.tt(p0[:], p0[:], p1[:], ALU.mult, [p0, p1], [p0])
            K.ts(p0[:], p0[:], pvh[h][:, PV_LNW:PV_LNW + 1], pvh[h][:, PV_LNB:PV_LNB + 1], ALU.mult, ALU.add, [p0, pvh[h]], [p0])
            bo, g = fmb["bonus"][h][tb % 2], fmb["g"][h][tb % 2]
            K.tt(p0[:], p0[:], bo[:], ALU.add, [p0, bo], [p0])
            K.tt(p2[:], p0[:], g[:], ALU.mult, [p0, g], [p2])
            ysrc_write(K, R["yB"], slice(h * 64, (h + 1) * 64), t0, 512, p2, p2)
    barrier(K)
    K.pop()


def select_chunk(K, dst, Y, C):
    oh = C["oh"]
    cand, acc = C["sel_cand"], C["sel_acc"]
    for kc in range(4):
        for j in range(4):
            K.dma("sp", cand[:, j, :], Y["all"][j].ap()[kc * 128:(kc + 1) * 128, :], reads=[Y["key"] + "_all"], writes=[cand])
        K.ts(acc[:], cand[:, 0, :], oh[:, 0:1], None, ALU.mult, None, [cand, oh], [acc])
        for j in range(1, 4):
            K.stt(acc[:], cand[:, j, :], oh[:, j:j + 1], acc[:], ALU.mult, ALU.add, [cand, oh, acc], [acc])
        K.copy(dst[:, kc, :], acc[:], [acc], [dst])


def merge_phase(K, l, C, S, xres):
    TC, NT = K.TC, K.NT
    hTx = C["hTx"]
    w_in = K.inputs["w_in"].ap()[l]
    K.push()
    wg = [K.sb(f"mg_wg{i}", [128, 16, 512], BF16) for i in range(2)]
    wb = [K.sb(f"mg_wb{i}", [128, 4, 512], BF16) for i in range(2)]
    psc = K.sb("mg_psc", [128, 512])
    macc = K.sb("mg_macc", [128, NT, 512])
    merged = K.sb("mg_merged", [128, NT, D], BF16)
    gt = [K.sb(f"mg_gt{i}", [128, 512]) for i in range(2)]
    tm = [K.sb(f"mg_tm{i}", [128, 512]) for i in range(2)]
    ys = [S["y_rwkvT"], S["y_nsaT"], S["y_convT"], S["y_memT"]]
    n = 0
    for nb in range(4):
        K.dma("sp", psc[:], K.inputs["pool_scale"].ap()[l:l + 1, nb * 512:(nb + 1) * 512].partition_broadcast(128), writes=[psc])
        for i in range(5):
            g_, b_ = wg[n % 2], wb[n % 2]
            n += 1
            load_w(K, g_, w_in, O_GATE + i * D + nb * 512, 512)
            if i < 4:
                K.dma("pool", b_[:], K.inputs["w_branch"].ap()[l, i].rearrange("(kc p) n -> p kc n", p=128)[:, :, nb * 512:(nb + 1) * 512], writes=[b_])
            else:
                K.dma("pool", b_[:, 0, :], K.inputs["pool_w"].ap()[l, nb], writes=[b_])
            for tt in range(NT):
                tsl = slice(tt * 128, (tt + 1) * 128)
                psg = K.psum()
                for kc in range(16):
                    K.mm(psg[:, :], hTx[:, kc, HALO + tt * 128: HALO + (tt + 1) * 128], g_[:, kc, :], kc == 0, kc == 15, [hTx, g_], [psg])
                G = gt[tt % 2]
                K.act(G[:], psg[:, :], AF.Sigmoid, [psg], [G])
                psb = K.psum()
                if i < 4:
                    for kc in range(4):
                        K.mm(psb[:, :], ys[i][:, kc, tsl], b_[:, kc, :], kc == 0, kc == 3, [ys[i], b_], [psb])
                else:
                    K.mm(psb[:, :], S["pooledT"][:, nb, tsl], b_[:, 0, :], True, True, [S["pooledT"], b_], [psb])
                    K.tt(G[:], G[:], psc[:], ALU.mult, [G, psc], [G])
                if i == 0:
                    K.tt(macc[:, tt, :], G[:], psb[:, :], ALU.mult, [G, psb], [macc])
                else:
                    t_ = tm[tt % 2]
                    K.tt(t_[:], G[:], psb[:, :], ALU.mult, [G, psb], [t_])
                    K.tt(macc[:, tt, :], macc[:, tt, :], t_[:], ALU.add, [macc, t_], [macc])
        K.copy(merged[:, :, nb * 512:(nb + 1) * 512], macc[:], [macc], [merged], eng="act")
    mT = hTx
    for tt in range(NT):
        for grp in range(2):
            ps = K.psum()
            psb_ = ps[:].bitcast(BF16)
            for i in range(8):
                kc = grp * 8 + i
                K.tr(psb_[:, i * 128:(i + 1) * 128], merged[:, tt, kc * 128:(kc + 1) * 128], C["identb"][:], [merged, C["identb"]], [ps])
            K.copy(mT[:, grp * 8:(grp + 1) * 8, HALO + tt * 128: HALO + (tt + 1) * 128], psb_.rearrange("p (a b) -> p a b", a=8), [ps], [mT],
                   eng="act" if grp == 0 else "dve")
    xt = [K.sb(f"mg_xt{i}", [128, 512]) for i in range(2)]
    for nb in range(4):
        wo = wg[nb % 2]
        load_w(K, wo, K.inputs["w_out"].ap()[l], nb * 512, 512)
        for tt in range(NT):
            ps = K.psum()
            for kc in range(16):
                K.mm(ps[:, :], mT[:, kc, HALO + tt * 128: HALO + (tt + 1) * 128], wo[:, kc, :], kc == 0, kc == 15, [mT, wo], [ps])
            x_ = xt[tt % 2]
            K.dma("sp", x_[:], xres.ap()[tt * 128:(tt + 1) * 128, nb * 512:(nb + 1) * 512], reads=["xres"], writes=[x_])
            K.tt(x_[:], x_[:], ps[:, :], ALU.add, [x_, ps], [x_])
            K.dma("sp", xres.ap()[tt * 128:(tt + 1) * 128, nb * 512:(nb + 1) * 512], x_[:], reads=[x_], writes=["xres"])
    barrier(K)
    K.pop()


def ffn_phase(K, l, C, xres, xout):
    TC = K.TC
    BL = min(512, TC)
    NBL = TC // BL
    TPB = BL // 128
    moe = (l % 2 == 1)
    K.push()
    h2T = C["hTx"]
    K.push()
    rn_alloc(K, C)
    K.dma("sp", C["gbc"][:], K.inputs["norm_ffn"].ap()[l:l + 1, :].partition_broadcast(128), writes=[C["gbc"]])
    rmsnorm_T(K, xres.ap(), C["gbc"], h2T, HALO, K.NT, C, "ffn")
    barrier(K)
    K.pop()
    NFE = 22
    NT = K.NT
    uT = K.sb("ff_uT", [128, NFE, TC], BF16)
    w1g = [K.sb(f"ff_w1_{i}", [128, 16, 256], BF16) for i in range(2)]
    w3g = [K.sb(f"ff_w3_{i}", [128, 16, 256], BF16) for i in range(2)]
    w2g = [K.sb(f"ff_w2_{i}", [128, 11, 512], BF16) for i in range(2)]
    sa = [K.sb(f"ff_sa{i}", [128, 512]) for i in range(2)]
    xacc = K.sb("ff_xacc", [128, NT, D])
    if moe:
        rt = K.sb("ff_rt", [128, 16, 8], BF16)
        K.dma("pool", rt[:], K.inputs["moe_router"].ap()[0].rearrange("(kc p) e -> p kc e", p=128), writes=[rt])
        comb = K.sb("ff_comb", [128, K.NT, 8])
        lg, l2, m1, m2, mk1, mk2 = (K.sb("ff_" + n, [128, 8]) for n in ("lg", "l2", "m1", "m2", "mk1", "mk2"))
        for tt in range(K.NT):
            ps = K.psum()
            for kc in range(16):
                K.mm(ps[:, 0:8], h2T[:, kc, HALO + tt * 128: HALO + (tt + 1) * 128], rt[:, kc, :], kc == 0, kc == 15, [h2T, rt], [ps])
            K.copy(lg[:], ps[:, 0:8], [ps], [lg])
            K.op("dve", lambda e: e.reduce_max(out=m1[:, 0:1], in_=lg[:], axis=AX.X), [lg], [m1])
            K.ts(mk1[:], lg[:], m1[:, 0:1], None, ALU.is_ge, None, [lg, m1], [mk1])
            K.stt(l2[:], mk1[:], -1e30, lg[:], ALU.mult, ALU.add, [mk1, lg], [l2])
            K.op("dve", lambda e: e.reduce_max(out=m2[:, 0:1], in_=l2[:], axis=AX.X), [l2], [m2])
            K.ts(mk2[:], l2[:], m2[:, 0:1], None, ALU.is_ge, None, [l2, m2], [mk2])
            K.tt(m1[:, 1:2], m1[:, 0:1], m2[:, 0:1], ALU.subtract, [m1, m2], [m1])
            K.act(m1[:, 2:3], m1[:, 1:2], AF.Sigmoid, [m1], [m1])
            K.ts(m1[:, 3:4], m1[:, 2:3], -1.0, 1.0, ALU.mult, ALU.add, [m1], [m1])
            K.ts(mk1[:], mk1[:], m1[:, 2:3], None, ALU.mult, None, [mk1, m1], [mk1])
            K.stt(comb[:, tt, :], mk2[:], m1[:, 3:4], mk1[:], ALU.mult, ALU.add, [mk2, m1, mk1], [comb])
    for tt in range(NT):
        K.dma("sp", xacc[:, tt, :], xres.ap()[tt * 128:(tt + 1) * 128, :], reads=["xres"], writes=[xacc])
    if moe:
        pexp = [tuple(K.inputs[n].ap()[0, e] for n in ("moe_w1", "moe_w3", "moe_w2")) + (0, e) for e in range(N_EXP)]
    else:
        pexp = [tuple(K.inputs[n].ap()[0] for n in ("ffn_w1", "ffn_w3", "ffn_w2")) + (fo, None) for fo in (0, NFE)]
    nld = [0]
    for (W1, W3, W2, fo, e) in pexp:
        for c0 in range(0, NFE * 128, 256):
            a_, b_ = w1g[nld[0] % 2], w3g[nld[0] % 2]
            nld[0] += 1
            load_w(K, a_, W1, fo * 128 + c0, 256)
            load_w(K, b_, W3, fo * 128 + c0, 256)
            for fi in range(2):
                f = c0 // 128 + fi
                for bl in range(NBL):
                    cb = HALO + bl * BL
                    pa, pb = K.psum_from(0, 4), K.psum_from(0, 4)
                    for kc in range(16):
                        K.mm(pa[:, 0:BL], a_[:, kc, fi * 128:(fi + 1) * 128], h2T[:, kc, cb:cb + BL], kc == 0, kc == 15, [a_, h2T], [pa])
                    for kc in range(16):
                        K.mm(pb[:, 0:BL], b_[:, kc, fi * 128:(fi + 1) * 128], h2T[:, kc, cb:cb + BL], kc == 0, kc == 15, [b_, h2T], [pb])
                    s_ = sa[(f * NBL + bl) % 2]
                    K.act(s_[:, 0:BL], pa[:, 0:BL], AF.Silu, [pa], [s_])
                    K.tt(uT[:, f, bl * BL:(bl + 1) * BL], s_[:, 0:BL], pb[:, 0:BL], ALU.mult, [s_, pb], [uT])
        for nb in range(4):
            for fh in range(2):
                w2_ = w2g[nld[0] % 2]
                nld[0] += 1
                r0 = (fo + fh * 11) * 128
                K.dma("pool", w2_[:, :, :], W2[r0:r0 + 11 * 128, nb * 512:(nb + 1) * 512].rearrange("(f p) n -> p f n", p=128), writes=[w2_])
                for tt in range(NT):
                    ps = K.psum_from(4, 4)
                    for f in range(11):
                        K.mm(ps[:, :], uT[:, fh * 11 + f, tt * 128:(tt + 1) * 128], w2_[:, f, :], f == 0, f == 10, [uT, w2_], [ps])
                    xs = xacc[:, tt, nb * 512:(nb + 1) * 512]
                    if moe:
                        K.stt(xs, ps[:, :], comb[:, tt, e:e + 1], xs, ALU.mult, ALU.add, [ps, comb, xacc], [xacc])
                    else:
                        K.tt(xs, xs, ps[:, :], ALU.add, [xacc, ps], [xacc])
    for tt in range(NT):
        K.dma("sp", xout.ap()[tt * 128:(tt + 1) * 128, :], xacc[:, tt, :], reads=[xacc], writes=[xout.name])
    barrier(K)
    K.pop()


WN_TM = 704
NWN2 = WN_TM + 140
SEL_N = 16
import os
NSA_STOP = int(os.environ.get('NSA_STOP', '0'))
NSA_SUB = int(os.environ.get('NSA_SUB', '0'))


def nsa_phase(K, l, C, R):
    T, TC = K.T, K.TC
    NB, NTT = T // 512, T // 128
    NS = T // 64
    NCMP = (T - 32) // 16 + 1
    CT = [(c0, min(128, NCMP - c0)) for c0 in range(0, NCMP, 128)]
    VW = 64 + NS + 1
    pv = C["pv"][l]
    sc = lambda i: pv[:, i:i + 1]
    K.push()
    qT = [K.sb(f"ns_q{i}T", [128, T], BF16) for i in range(2)]
    ksT, kwT = K.sb("ns_ksT", [128, T], BF16), K.sb("ns_kwT", [128, T], BF16)
    kcT, vcT = K.sb("ns_kcT", [64, T], BF16), K.sb("ns_vcT", [64, T], BF16)
    Vs, Vw = K.sb("ns_Vs", [128, NTT, 66], BF16), K.sb("ns_Vw", [128, NTT, 66], BF16)
    gts = K.sb("ns_g", [128, NTT, 12])
    Oacc = K.sb("ns_O", [128, NTT, 128])
    blk = K.sb("ns_blk", [128, 128])
    K.dma("sp", blk[:], K.inputs["blk64"].ap(), writes=[blk])
    K.memset(Vs[:, :, 64:65], 1.0, [Vs])
    K.memset(Vw[:, :, 64:65], 1.0, [Vw])
    imp = K.sb("ns_imp", [128, NTT, NS])
    selT = K.sb("ns_selT", [64, T], BF16)
    ebuf = [K.sb(f"ns_e{i}", [128, 512], BF16) for i in range(4)]
    rv = [K.sb(f"ns_rv{i}", [128, 2]) for i in range(2)]
    kcmpT = K.sb("ns_kcmpT", [128, 256], BF16)
    Vc = K.sb("ns_Vc", [128, len(CT), VW + 1], BF16)
    K.push()
    wn = K.sb("ns_wn", [128, 16, NWN2], BF16)
    wsrc = K.inputs["wn"].ap()[l].rearrange("(kc p) n -> p kc n", p=128)
    K.dma("pool", wn[:, :, 0:512], wsrc[:, :, 0:512], writes=[wn])
    K.dma("pool", wn[:, :, 512:NWN2], wsrc[:, :, 512:NWN2], writes=[wn])
    B1 = 256
    hTb = [K.sb("ns_hT0", [128, 16, B1], BF16)] * 2
    pf = K.sb("ns_pf", [128, B1])
    for tb in range(T // B1):
        t0 = tb * B1
        hb = hTb[tb % 2]
        load_hall_block(K, hb, C, t0, TC, B1)
        specs = [(0, 128, qT[0], PV_NSAG + 0), (128, 128, qT[1], PV_NSAG + 0), (384, 128, ksT, PV_NSAG + 2), (512, 128, kwT, PV_NSAG + 3),
                 (256, 64, kcT, None), (640, 64, vcT, None)]
        if NSA_SUB == 1:
            continue
        for (c0, nc_, dst, gi) in specs:
            ps = K.psum()
            for kc in range(16):
                K.mm(ps[0:nc_, 0:B1], wn[:, kc, c0:c0 + nc_], hb[:, kc, :], kc == 0, kc == 15, [wn, hb], [ps])
            if gi is None:
                K.copy(dst[:, t0:t0 + B1], ps[0:nc_, 0:B1], [ps], [dst], eng="act")
            else:
                K.copy(pf[:], ps[:, 0:B1], [ps], [pf], eng="act")
                sq = C["fm_sq"]
                K.tt(sq[:, 0:B1], pf[:], pf[:], ALU.mult, [pf], [sq])
                ps2 = K.psum()
                K.mm(ps2[:, 0:B1], blk[:, :], sq[:, 0:B1], True, True, [blk, sq], [ps2])
                rs = C["fm_rs"]
                K.ts(rs[:, 0:B1], ps2[:, 0:B1], 1.0 / 64, EPS, ALU.mult, ALU.add, [ps2], [rs])
                K.act(rs[:, 0:B1], rs[:, 0:B1], AF.Sqrt, [rs], [rs])
                K.op("dve", lambda e: e.reciprocal(out=rs[:, 0:B1], in_=rs[:, 0:B1]), [rs], [rs])
                K.stt(dst[:, t0:t0 + B1], pf[:], sc(gi), rs[:, 0:B1], ALU.mult, ALU.mult, [pf, pv, rs], [dst])
        if NSA_SUB == 2:
            continue
        for ti in range(B1 // 128):
            gt_ = tb * (B1 // 128) + ti
            ps = K.psum()
            for kc in range(16):
                K.mm(ps[:, 0:140], hb[:, kc, ti * 128:(ti + 1) * 128], wn[:, kc, WN_TM:WN_TM + 140], kc == 0, kc == 15, [wn, hb], [ps])
            K.copy(Vs[:, gt_, 0:64], ps[:, 0:64], [ps], [Vs], eng="act")
            K.copy(Vw[:, gt_, 0:64], ps[:, 64:128], [ps], [Vw])
            K.act(gts[:, gt_, :], ps[:, 128:140], AF.Sigmoid, [ps], [gts])
    barrier(K)
    K.pop()
    K.push()
    W1 = K.sb("ns_W1", [64, 32, 128], BF16)
    w2d = K.sb("ns_w2", [128, 128], BF16)
    posT = K.sb("ns_posT", [64, 32], BF16)
    hidT = K.sb("ns_hidT", [128, 256], BF16)
    hx = [K.sb(f"ns_hx{i}", [128, 256]) for i in range(3)]
    cb = K.sb("ns_cb", [128, 2])
    kg = K.sb("ns_kg", [128, 64])
    K.dma("sp", kg[:], K.inputs["nsa_qk_gain"].ap()[l, 1:2, :].partition_broadcast(128), writes=[kg])
    ktm = K.sb("ns_ktm", [128, 128])
    kss = K.sb("ns_kss", [128, 2])
    K.memset(Vc[:, :, VW - 1:VW], 1.0, [Vc])
    for ci, (c0, ncc) in enumerate(CT):
        K.dma("pool", Vc[0:ncc, ci, 64:64 + NS], K.inputs["ovl"].ap()[c0:c0 + ncc, 0:NS], writes=[Vc])
    for i in range(2):
        src = kcT if i == 0 else vcT
        K.dma("pool", W1[:], K.inputs["nsa_cmp_w1"].ap()[l, i].rearrange("(l d) j -> d l j", d=64), writes=[W1])
        K.dma("pool", posT[:], K.inputs["cmp_posT"].ap()[l, i], writes=[posT])
        K.dma("pool", w2d[:, 0:64], K.inputs["nsa_cmp_w2"].ap()[l, i], writes=[w2d])
        K.dma("pool", w2d[:, 64:128], K.inputs["nsa_cmp_w2"].ap()[l, i], writes=[w2d])
        ps = K.psum()
        for ll in range(32):
            K.mm(ps[:, 0:1], W1[:, ll, :], posT[:, ll:ll + 1], ll == 0, ll == 31, [W1, posT], [ps])
        K.tt(cb[:, i:i + 1], ps[:, 0:1], sc(PV_CB1 + i), ALU.add, [ps, pv], [cb])
        ps = K.psum()
        for ll in range(32):
            K.mm(ps[:, 0:NCMP], W1[:, ll, :], src[:, ll: ll + 16 * (NCMP - 1) + 1: 16], ll == 0, ll == 31, [W1, src], [ps])
        x_, x2, x3 = hx
        n_ = NCMP
        K.ts(x_[:, 0:n_], ps[:, 0:n_], cb[:, i:i + 1], None, ALU.add, None, [ps, cb], [x_])
        K.tt(x2[:, 0:n_], x_[:, 0:n_], x_[:, 0:n_], ALU.mult, [x_], [x2])
        K.ts(x2[:, 0:n_], x2[:, 0:n_], 0.044715, 1.0, ALU.mult, ALU.add, [x2], [x2])
        K.tt(x2[:, 0:n_], x2[:, 0:n_], x_[:, 0:n_], ALU.mult, [x2, x_], [x2])
        K.act(x3[:, 0:n_], x2[:, 0:n_], AF.Sigmoid, [x2], [x3], scale=1.5957691216057308)
        K.tt(hidT[:, 0:n_], x_[:, 0:n_], x3[:, 0:n_], ALU.mult, [x_, x3], [hidT])
        for ci, (c0, ncc) in enumerate(CT):
            ps = K.psum()
            K.mm(ps[0:ncc, 0:128], hidT[:, c0:c0 + ncc], w2d[:, :], True, True, [hidT, w2d], [ps])
            if i == 1:
                K.copy(Vc[0:ncc, ci, 0:64], ps[0:ncc, 0:64], [ps], [Vc], eng="act")
            else:
                K.copy(ktm[0:ncc, :], ps[0:ncc, 0:128], [ps], [ktm], eng="act")
                sq = C["fm_sq"]
                K.tt(sq[0:ncc, 0:64], ktm[0:ncc, 0:64], ktm[0:ncc, 0:64], ALU.mult, [ktm], [sq])
                K.op("dve", lambda e: e.reduce_sum(out=kss[0:ncc, 0:1], in_=sq[0:ncc, 0:64], axis=AX.X), [sq], [kss])
                K.ts(kss[0:ncc, 1:2], kss[0:ncc, 0:1], 1.0 / 64, EPS, ALU.mult, ALU.add, [kss], [kss])
                K.act(kss[0:ncc, 1:2], kss[0:ncc, 1:2], AF.Sqrt, [kss], [kss])
                K.op("dve", lambda e: e.reciprocal(out=kss[0:ncc, 1:2], in_=kss[0:ncc, 1:2]), [kss], [kss])
                for hf in range(2):
                    K.stt(ktm[0:ncc, hf * 64:(hf + 1) * 64], ktm[0:ncc, hf * 64:(hf + 1) * 64], kss[0:ncc, 1:2], kg[0:ncc, :], ALU.mult, ALU.mult,
                          [ktm, kss, kg], [ktm])
                ps2 = K.psum()
                K.tr(ps2[:, 0:ncc], ktm[0:ncc, :], C["identf"][0:ncc, 0:ncc], [ktm, C["identf"]], [ps2])
                K.copy(kcmpT[:, c0:c0 + ncc], ps2[:, 0:ncc], [ps2], [kcmpT])
    barrier(K)
    K.pop()
    K.push()
    maskc = K.sb("ns_maskc", [128, len(CT), T], BF16)
    for ci, (c0, ncc) in enumerate(CT):
        K.dma("pool", maskc[0:ncc, ci, :], K.inputs["maskc"].ap()[c0:c0 + ncc, 0:T], writes=[maskc])
    nrv = [0]

    def finish_acc(acc_ap, acckey, gtile, gate_idx):
        r_ = rv[nrv[0] % 2]
        nrv[0] += 1
        K.ts(r_[:, 0:1], acc_ap[:, 64:65], 1e-30, None, ALU.max, None, [acckey], [r_])
        K.op("dve", lambda e: e.reciprocal(out=r_[:, 0:1], in_=r_[:, 0:1]), [r_], [r_])
        K.tt(r_[:, 1:2], r_[:, 0:1], gts[:, gtile, gate_idx:gate_idx + 1], ALU.mult, [r_, gts], [r_])
        return r_

    first_o = {}
    for hd in range(4):
        qt, half = qT[hd // 2], slice((hd % 2) * 64, (hd % 2) * 64 + 64)
        for tb in range(NB):
            t0 = tb * 512
            for ci, (c0, ncc) in enumerate(CT):
                ps = K.psum_from(0, 7)
                K.mm(ps[0:ncc, :], kcmpT[half, c0:c0 + ncc], qt[half, t0:t0 + 512], True, True, [kcmpT, qt], [ps])
                e_ = ebuf[ci]
                K.act(e_[0:ncc, :], ps[0:ncc, :], AF.Exp, [ps], [e_], scale=0.125)
                K.tt(e_[0:ncc, :], e_[0:ncc, :], maskc[0:ncc, ci, t0:t0 + 512], ALU.mult, [e_, maskc], [e_])
            for ti in range(4):
                gt_ = tb * 4 + ti
                ps = K.psum_from(0, 7)
                for ci, (c0, ncc) in enumerate(CT):
                    K.mm(ps[:, 0:VW], ebuf[ci][0:ncc, ti * 128:(ti + 1) * 128], Vc[0:ncc, ci, 0:VW], ci == 0, ci == len(CT) - 1, [ebuf[ci], Vc], [ps])
                r_ = rv[nrv[0] % 2]
                nrv[0] += 1
                K.ts(r_[:, 0:1], ps[:, VW - 1:VW], 1e-30, None, ALU.max, None, [ps], [r_])
                K.op("dve", lambda e: e.reciprocal(out=r_[:, 0:1], in_=r_[:, 0:1]), [r_], [r_])
                if hd == 0:
                    K.ts(imp[:, gt_, :], ps[:, 64:64 + NS], r_[:, 0:1], None, ALU.mult, None, [ps, r_], [imp])
                else:
                    K.stt(imp[:, gt_, :], ps[:, 64:64 + NS], r_[:, 0:1], imp[:, gt_, :], ALU.mult, ALU.add, [ps, r_, imp], [imp])
                if hd < 2:
                    K.tt(r_[:, 1:2], r_[:, 0:1], gts[:, gt_, hd * 3:hd * 3 + 1], ALU.mult, [r_, gts], [r_])
                    K.ts(Oacc[:, gt_, hd * 64:(hd + 1) * 64], ps[:, 0:64], r_[:, 1:2], None, ALU.mult, None, [ps, r_], [Oacc])
    barrier(K)
    K.pop()
    K.push()
    keep, cadd = K.sb("ns_keep", [128, NS]), K.sb("ns_cadd", [128, NS])
    cmp3 = K.sb("ns_cmp3", [128, NS, NS])
    cnt, sel, ok = K.sb("ns_cnt", [128, NS]), K.sb("ns_sel", [128, NS]), K.sb("ns_ok", [128, NS])
    for gt_ in range(NTT):
        K.dma("sp", keep[:], K.inputs["tk_keep"].ap()[gt_ * 128:(gt_ + 1) * 128, 0:NS], writes=[keep])
        K.dma("sp", cadd[:], K.inputs["tk_cadd"].ap()[gt_ * 128:(gt_ + 1) * 128, 0:NS], writes=[cadd])
        im = imp[:, gt_, :]
        K.tt(im, im, keep[:], ALU.mult, [imp, keep], [imp])
        K.tt(im, im, cadd[:], ALU.add, [imp, cadd], [imp])
        K.tt(cmp3[:], im.unsqueeze(1).to_broadcast([128, NS, NS]), im.unsqueeze(2).to_broadcast([128, NS, NS]), ALU.is_gt, [imp], [cmp3])
        K.op("dve", lambda e: e.reduce_sum(out=cnt[:], in_=cmp3[:], axis=AX.X), [cmp3], [cnt])
        K.ts(sel[:], cnt[:], float(SEL_N) - 0.5, None, ALU.is_lt, None, [cnt], [sel])
        K.ts(ok[:], im, -1e8, None, ALU.is_gt, None, [imp], [ok])
        K.tt(sel[:], sel[:], ok[:], ALU.mult, [sel, ok], [sel])
        ps = K.psum_from(0, 7)
        K.tr(ps[0:NS, 0:128], sel[:], C["identf"][:], [sel, C["identf"]], [ps])
        K.copy(selT[0:NS, gt_ * 128:(gt_ + 1) * 128], ps[0:NS, 0:128], [ps], [selT], eng="act")
    barrier(K)
    K.pop()
    K.push()
    E2 = K.sb("ns_E2", [64, NTT, 128], BF16)
    K.dma("pool", E2[0:NS], K.inputs["e2"].ap()[0:NS, 0:NTT, :], writes=[E2])
    dmask = K.sb("ns_dmask", [128, 5, 512], BF16)
    K.dma("pool", dmask[:], K.inputs["dmask"].ap().rearrange("a p t -> p a t"), writes=[dmask])
    accb = K.ps[7]
    for hd in range(2):
        half = slice(hd * 64, hd * 64 + 64)
        for tb in range(NB):
            t0 = tb * 512
            njt = 4 * tb + 4
            for jt in range(njt):
                ps = K.psum_from(0, 4)
                K.mm(ps[:, :], ksT[half, jt * 128:(jt + 1) * 128], qT[0][half, t0:t0 + 512], True, True, [ksT, qT[0]], [ps])
                e_ = ebuf[jt % 2]
                K.act(e_[:, :], ps[:, :], AF.Exp, [ps], [e_], scale=0.125)
                pm = K.psum_from(0, 4)
                K.mm(pm[:, :], E2[0:NS, jt, :], selT[0:NS, t0:t0 + 512], True, True, [E2, selT], [pm])
                em = ebuf[2 + jt % 2]
                K.tt(em[:, :], e_[:, :], pm[:, :], ALU.mult, [e_, pm], [em])
                dd = jt - 4 * tb
                if dd >= 0:
                    K.tt(em[:, :], em[:, :], dmask[:, dd, :], ALU.mult, [em, dmask], [em])
                for ti in range(max(dd, 0), 4):
                    K.mm(K.ps[4 + ti][:, 0:65], em[:, ti * 128:(ti + 1) * 128], Vs[:, jt, 0:65], jt == 0, jt == 4 * tb + ti, [em, Vs], [K.ps[4 + ti]])
            for ti in range(4):
                gt_ = tb * 4 + ti
                a_ = K.ps[4 + ti][:, 0:65]
                r_ = finish_acc(a_, K.ps[4 + ti].name, gt_, hd * 3 + 1)
                K.stt(Oacc[:, gt_, hd * 64:(hd + 1) * 64], a_[:, 0:64], r_[:, 1:2], Oacc[:, gt_, hd * 64:(hd + 1) * 64], ALU.mult, ALU.add,
                      [K.ps[4 + ti], r_, Oacc], [Oacc])
    for hd in range(2):
        half = slice(hd * 64, hd * 64 + 64)
        for gt_ in range(NTT):
            jts = list(range(max(0, gt_ - 4), gt_ + 1))
            for jt in jts:
                ps = K.psum_from(0, 7)
                K.mm(ps[:, 0:128], kwT[half, jt * 128:(jt + 1) * 128], qT[0][half, gt_ * 128:(gt_ + 1) * 128], True, True, [kwT, qT[0]], [ps])
                e_ = ebuf[jt % 4]
                K.act(e_[:, 0:128], ps[:, 0:128], AF.Exp, [ps], [e_], scale=0.125)
                if jt == gt_:
                    K.tt(e_[:, 0:128], e_[:, 0:128], dmask[:, 0, 0:128], ALU.mult, [e_, dmask], [e_])
                elif jt == gt_ - 4:
                    K.tt(e_[:, 0:128], e_[:, 0:128], dmask[:, 4, 0:128], ALU.mult, [e_, dmask], [e_])
                K.mm(accb[:, 0:65], e_[:, 0:128], Vw[:, jt, 0:65], jt == jts[0], jt == jts[-1], [e_, Vw], ["ns_accb"])
            a_ = accb[:, 0:65]
            r_ = finish_acc(a_, "ns_accb", gt_, hd * 3 + 2)
            K.stt(Oacc[:, gt_, hd * 64:(hd + 1) * 64], a_[:, 0:64], r_[:, 1:2], Oacc[:, gt_, hd * 64:(hd + 1) * 64], ALU.mult, ALU.add,
                  ["ns_accb", r_, Oacc], [Oacc])
    ot = [K.sb(f"ns_ot{i}", [128, 512]) for i in range(2)]
    for tb in range(NB):
        ps = K.psum_from(0, 7)
        for ti in range(4):
            K.tr(ps[:, ti * 128:(ti + 1) * 128], Oacc[:, tb * 4 + ti, :], C["identf"][:], [Oacc, C["identf"]], [ps])
        o_ = ot[tb % 2]
        K.copy(o_[:], ps[:, :], [ps], [o_], eng="act")
        ysrc_write(K, R["yN"], slice(0, 128), tb * 512, 512, o_, o_)
    barrier(K)
    K.pop()
    K.pop()


INPUT_SHAPES = lambda T: {
    "x": [T // 4, D], "mem": [MEM_LEN, D], "w_in": [2, D, N_IN], "norm_mix": [2, D], "norm_ffn": [2, D], "norm_mem": [2, D],
    "mem_wkv": [2, D, 1024], "pool_w": [2, 4, 128, 512], "pool_scale": [2, D], "w_branch": [2, 4, 512, D], "w_out": [2, D, D],
    "ffn_w1": [1, D, D_FF], "ffn_w3": [1, D, D_FF], "ffn_w2": [1, D_FF, D], "moe_router": [1, D, 8],
    "moe_w1": [1, 8, D, E_FF], "moe_w3": [1, 8, D, E_FF], "moe_w2": [1, 8, E_FF, D],
    "nsa_cmp_w1": [2, 2, 2048, 128], "nsa_cmp_w2": [2, 2, 128, 64], "cmp_posT": [2, 2, 64, 32], "nsa_qk_gain": [2, 4, 64],
    "ident": [128, 128], "blk64": [128, 128], "ovl": [256, 64], "maskc": [256, T], "tk_keep": [T, 64], "tk_cadd": [T, 64],
    "e2": [64, 32, 128], "dmask": [5, 128, 512], "onehot": [128, 8], "invcnt": [4, T // 4], "pvec": [2, 128, NV],
    "wr": [2, D, 1024], "wn": [2, D, NWN], "lora": [2, 128, 5, 128],
}


def build(T, dbg=False, nlayers=DEPTH):
    K = KB(T)
    TC = K.TC
    out = K.dout("out", [TC, D])
    C = setup_common(K)
    xres = K.dscr("xres", [TC, D])
    K.dma("sp", xres.ap(), K.inputs["x"].ap(), writes=["xres"])
    C["hTx"] = K.sb("hTx", [128, 16, HALO + TC], BF16)
    alloc_gather(K, C)
    R = alloc_scratch(K)
    dbg_outs = []
    barrier(K)
    for l in range(nlayers):
        K.push()
        S = {n: K.sb(n, [128, 4, TC], BF16) for n in ("y_convT", "pooledT", "y_memT")}
        K.push()
        C["halo_cand"] = K.sb("halo_cand", [128, 4, 16, HALO], BF16)
        phase_norm_gather(K, l, C, xres)
        barrier(K)
        K.pop()
        local_mixers(K, l, C, S)
        rwkv_phase(K, l, C, R)
        ygather(K, R["yB"])
        nsa_phase(K, l, C, R)
        ygather(K, R["yN"])
        S["y_rwkvT"] = K.sb("y_rwkvT", [128, 4, TC], BF16)
        S["y_nsaT"] = K.sb("y_nsaT", [128, 4, TC], BF16)
        K.push()
        C["sel_cand"] = K.sb("sel_cand", [128, 4, TC])
        C["sel_acc"] = K.sb("sel_acc", [128, TC])
        select_chunk(K, S["y_rwkvT"], R["yB"], C)
        select_chunk(K, S["y_nsaT"], R["yN"], C)
        barrier(K)
        K.pop()
        if dbg and l == 0:
            for nm, Y in (("d_yN", R["yN"]), ("d_yB", R["yB"])):
                o = K.dout(nm, [512, T])
                for j in range(4):
                    K.dma("sp", o.ap()[:, j * TC:(j + 1) * TC], Y["all"][j].ap(), reads=[Y["key"] + "_all"], writes=[nm])
                dbg_outs.append(nm)
        merge_phase(K, l, C, S, xres)
        barrier(K)
        K.pop()
        if dbg and l == 0:
            o = K.dout("d_xmix", [TC, D])
            K.dma("sp", o.ap(), xres.ap(), reads=["xres"], writes=["d_xmix"])
            dbg_outs.append("d_xmix")
        last = (l == nlayers - 1)
        ffn_phase(K, l, C, xres, out if last else xres)
        if dbg and l == 0 and not last:
            o = K.dout("d_xout0", [TC, D])
            K.dma("sp", o.ap(), xres.ap(), reads=["xres"], writes=["d_xout0"])
            dbg_outs.append("d_xout0")
    K.fw.finish(["out"] + dbg_outs)
    return K


_CACHE = {}


def kernel(**inputs):
    T = int(np.asarray(inputs["x"]).shape[1])
    if T not in _CACHE:
        _CACHE[T] = build(T)
    K = _CACHE[T]
    maps = host_prep(inputs, T)
    maps = [{k: m[k] for k in K.inputs} for m in maps]
    res = run_bass_kernel_spmd(K.nc, maps, core_ids=list(range(NCORES)))
    TC = T // 4
    outp = np.zeros((2, T, D), np.float32)
    for c in range(NCORES):
        outp[c // 4, (c % 4) * TC:(c % 4 + 1) * TC] = res.results[c]["out"]
    return outp
```
